# Optimizing a Trainium2 kernel written in Bass

```python
import math
import jax
import jax.numpy as jnp
from jax import lax
import numpy as np

D_MODEL = 1024
BATCH = 8
SEQ = 4096
DEPTH = 1

D_MIX = D_MODEL
CONV_WIDTH = D_MIX // 2
SG_WIDTH = D_MIX - CONV_WIDTH
N_CONV_HEADS = 8
N_SG_HEADS = 8
CONV_HEAD_DIM = CONV_WIDTH // N_CONV_HEADS
SG_HEAD_DIM = SG_WIDTH // N_SG_HEADS
CONV_KERNEL = 31
SG_CHUNK = 128
PEER_HEADS = 8
PEER_TOPK = 16
N_KEYS = 128
N_EXPERTS = N_KEYS * N_KEYS
D_KEY = 256
D_HALF = D_KEY // 2
PEER_TOKEN_BLOCK = 128
PLE_DIM = 256
ALPHA = (2.0 * DEPTH) ** 0.25
BETA = (8.0 * DEPTH) ** -0.25
LN_EPS = 1e-5
D_IN = 2 * CONV_WIDTH + 2 * SG_WIDTH

kernel_name = "hymba_conformer_sgu_peer_deepnorm"


def layer_norm(x, g, b):
    xf = x.astype(jnp.float32)
    mu = jnp.mean(xf, axis=-1, keepdims=True)
    var = jnp.mean(jnp.square(xf - mu), axis=-1, keepdims=True)
    return ((xf - mu) * lax.rsqrt(var + LN_EPS)).astype(x.dtype) * g + b


def group_norm(x, g, b, groups):
    c = x.shape[-1]
    xs = x.reshape(*x.shape[:-1], groups, c // groups)
    y = layer_norm(xs, g.reshape(groups, c // groups), b.reshape(groups, c // groups))
    return y.reshape(x.shape)


def conformer_conv(a, gate, conv_w, conv_b, gn_g, gn_b):
    c = a * jax.nn.sigmoid(gate)
    rhs = conv_w[:, None, :].astype(c.dtype)
    y = lax.conv_general_dilated(
        c, rhs, window_strides=(1,), padding=[(CONV_KERNEL - 1, 0)],
        dimension_numbers=("NWC", "WIO", "NWC"),
        feature_group_count=CONV_WIDTH) + conv_b
    y = group_norm(y, gn_g, gn_b, N_CONV_HEADS)
    return jax.nn.silu(y)


def spatial_gating(u, v, ln_g, ln_b, sg_w, sg_b):
    bsz, s, _ = u.shape
    u = jax.nn.gelu(u, approximate=False)
    v = group_norm(jax.nn.gelu(v, approximate=False), ln_g, ln_b, N_SG_HEADS)
    nc = s // SG_CHUNK
    vc = v.reshape(bsz, nc, SG_CHUNK, N_SG_HEADS, SG_HEAD_DIM)
    uc = u.reshape(bsz, nc, SG_CHUNK, N_SG_HEADS, SG_HEAD_DIM)
    mask = jnp.tril(jnp.ones((SG_CHUNK, SG_CHUNK), dtype=sg_w.dtype))
    wm = sg_w * mask[None]
    f = jnp.einsum("hts,bcshd->bcthd", wm, vc) + sg_b.T[None, None, :, :, None]
    return (uc * f).reshape(bsz, s, SG_WIDTH)


def peer(h, wq, keys, u_tab, v_tab):
    bsz, s, d = h.shape
    xt = h.reshape(bsz * s // PEER_TOKEN_BLOCK, PEER_TOKEN_BLOCK, d)

    def block(xc):
        c = xc.shape[0]
        q = (xc @ wq).reshape(c, PEER_HEADS, 2, D_HALF)
        s1 = jnp.einsum("chd,hkd->chk", q[:, :, 0], keys[:, 0])
        s2 = jnp.einsum("chd,hkd->chk", q[:, :, 1], keys[:, 1])
        v1, i1 = lax.top_k(s1, PEER_TOPK)
        v2, i2 = lax.top_k(s2, PEER_TOPK)
        comb = (v1[..., :, None] + v2[..., None, :]).reshape(c, PEER_HEADS, PEER_TOPK * PEER_TOPK)
        sc, pos = lax.top_k(comb, PEER_TOPK)
        a_sel = jnp.take_along_axis(i1, pos // PEER_TOPK, axis=-1)
        b_sel = jnp.take_along_axis(i2, pos % PEER_TOPK, axis=-1)
        idx = a_sel * N_KEYS + b_sel
        g = jax.nn.softmax(sc.astype(jnp.float32), axis=-1).astype(xc.dtype)
        ug = jnp.take(u_tab, idx, axis=0)
        act = jax.nn.gelu(jnp.einsum("cd,chkd->chk", xc, ug), approximate=False)
        vg = jnp.take(v_tab, idx, axis=0)
        return jnp.einsum("chk,chkd->cd", g * act, vg)

    y = lax.map(block, xt)
    return y.reshape(bsz, s, d)


def setup_inputs(seed: int = 0) -> dict:
    key = jax.random.key(seed)
    ks = jax.random.split(key, 32)
    L = DEPTH
    f32 = jnp.float32

    def nrm(k, shape, scale):
        return jax.random.normal(k, shape, f32) * scale

    return {
        "x": nrm(ks[0], (BATCH, SEQ, D_MODEL), 1.0),
        "p": nrm(ks[1], (DEPTH, BATCH, SEQ, PLE_DIM), 1.0),
        "ln0_g": 1.0 + nrm(ks[2], (D_MODEL,), 0.02),
        "ln0_b": nrm(ks[3], (D_MODEL,), 0.02),
        "w_in": nrm(ks[4], (L, D_MODEL, D_IN), D_MODEL ** -0.5),
        "b_in": nrm(ks[5], (L, D_IN), 0.02),
        "conv_w": nrm(ks[6], (L, CONV_KERNEL, CONV_WIDTH), CONV_KERNEL ** -0.5),
        "conv_b": nrm(ks[7], (L, CONV_WIDTH), 0.02),
        "gn_g": 1.0 + nrm(ks[8], (L, CONV_WIDTH), 0.02),
        "gn_b": nrm(ks[9], (L, CONV_WIDTH), 0.02),
        "sg_ln_g": 1.0 + nrm(ks[10], (L, SG_WIDTH), 0.02),
        "sg_ln_b": nrm(ks[11], (L, SG_WIDTH), 0.02),
        "sg_w": nrm(ks[12], (L, N_SG_HEADS, SG_CHUNK, SG_CHUNK), SG_CHUNK ** -0.5),
        "sg_b": 1.0 + nrm(ks[13], (L, N_SG_HEADS, SG_CHUNK), 0.02),
        "w_o": nrm(ks[14], (L, D_MIX, D_MODEL), BETA * D_MIX ** -0.5),
        "b_o": nrm(ks[15], (L, D_MODEL), 0.02),
        "ln1_g": 1.0 + nrm(ks[16], (L, D_MODEL), 0.02),
        "ln1_b": nrm(ks[17], (L, D_MODEL), 0.02),
        "peer_wq": nrm(ks[18], (L, D_MODEL, PEER_HEADS * D_KEY), D_MODEL ** -0.5),
        "peer_keys": nrm(ks[19], (L, PEER_HEADS, 2, N_KEYS, D_HALF), D_HALF ** -0.5),
        "peer_u": nrm(ks[20], (L, N_EXPERTS, D_MODEL), D_MODEL ** -0.5),
        "peer_v": nrm(ks[21], (L, N_EXPERTS, D_MODEL), BETA * 0.5),
        "ple_wp": nrm(ks[22], (L, PLE_DIM, D_MODEL), BETA * PLE_DIM ** -0.5),
        "ple_wg": nrm(ks[23], (L, D_MODEL, D_MODEL), D_MODEL ** -0.5),
        "ple_bg": nrm(ks[24], (L, D_MODEL), 0.02),
        "ln2_g": 1.0 + nrm(ks[25], (L, D_MODEL), 0.02),
        "ln2_b": nrm(ks[26], (L, D_MODEL), 0.02),
    }


def reference(x, p, ln0_g, ln0_b, w_in, b_in, conv_w, conv_b, gn_g, gn_b,
              sg_ln_g, sg_ln_b, sg_w, sg_b, w_o, b_o, ln1_g, ln1_b,
              peer_wq, peer_keys, peer_u, peer_v, ple_wp, ple_wg, ple_bg,
              ln2_g, ln2_b):
    h = layer_norm(x, ln0_g, ln0_b)
    for i in range(DEPTH):
        z = h @ w_in[i] + b_in[i]
        ca, cg, su, sv = jnp.split(
            z, [CONV_WIDTH, 2 * CONV_WIDTH, 2 * CONV_WIDTH + SG_WIDTH], axis=-1)
        conv_out = conformer_conv(ca, cg, conv_w[i], conv_b[i], gn_g[i], gn_b[i])
        sg_out = spatial_gating(su, sv, sg_ln_g[i], sg_ln_b[i], sg_w[i], sg_b[i])
        mix = jnp.concatenate([conv_out, sg_out], axis=-1) @ w_o[i] + b_o[i]
        h = layer_norm(ALPHA * h + mix, ln1_g[i], ln1_b[i])
        ff = peer(h, peer_wq[i], peer_keys[i], peer_u[i], peer_v[i])
        ple = jax.nn.sigmoid(h @ ple_wg[i] + ple_bg[i]) * (p[i] @ ple_wp[i])
        h = layer_norm(ALPHA * h + ff + ple, ln2_g[i], ln2_b[i])
    return h
```

```python
import numpy as np
from contextlib import ExitStack
import concourse.bass as bass
import concourse.mybir as mybir
from concourse.bass_utils import run_bass_kernel_spmd

F32 = mybir.dt.float32
BF16 = mybir.dt.bfloat16
U32 = mybir.dt.uint32
AF = mybir.ActivationFunctionType
ALU = mybir.AluOpType
AX = mybir.AxisListType

D = 1024
SEQ = 4096
ALPHA = float(2.0 ** 0.25)
EPS = 1e-5
NEG = -1.0e30
ENGS = ["sp", "pool", "act", "dve", "pe"]
NRING = 8
GRP = 8
NDIAG = 16


class Buf:
    __slots__ = ("name", "w", "r", "dsem", "dcnt")

    def __init__(self, name):
        self.name = name
        self.w = None
        self.r = []
        self.dsem = None
        self.dcnt = 0


class Prog:
    def __init__(self, nc, es):
        self.nc = nc
        self.es = es
        self.streams = {e: [] for e in ENGS}
        self.esem = {e: es.enter_context(nc.semaphore("es_" + e)) for e in ENGS}
        self.ecnt = {e: 0 for e in ENGS}
        self.waited = {e: {} for e in ENGS}
        self.dma_toks = []
        self.nd = 0

    def _wait(self, e, tok):
        sem, val = tok
        if self.waited[e].get(sem, 0) >= val:
            return
        self.waited[e][sem] = val
        self.streams[e].append(("w", sem, val))

    def _deps(self, e, who, reads, writes):
        for b in reads:
            if b.w is not None:
                self._wait(e, b.w[1])
        for b in writes:
            if b.w is not None and not (who == "pe" and b.w[0] == "pe"):
                self._wait(e, b.w[1])
            for (re_, tok) in b.r:
                self._wait(e, tok)

    def op(self, e, fns, reads=(), writes=()):
        if callable(fns):
            fns = [fns]
        self._deps(e, e, reads, writes)
        self.ecnt[e] += 1
        tok = (self.esem[e], self.ecnt[e])
        self.streams[e].append(("i", fns, self.esem[e], 1))
        for b in reads:
            b.r.append((e, tok))
        for b in writes:
            b.w = (e, tok)
            b.r = []
        return tok

    def dma(self, e, fn, sbuf, reads=(), writes=()):
        if sbuf.dsem is None:
            self.nd += 1
            sbuf.dsem = self.es.enter_context(self.nc.semaphore("ds%d" % self.nd))
        wr = list(writes)
        if sbuf not in wr:
            wr.append(sbuf)
        rd = [b for b in reads if b is not sbuf]
        self._deps(e, "dma", rd, wr)
        sbuf.dcnt += 16
        tok = (sbuf.dsem, sbuf.dcnt)
        self.streams[e].append(("i", [fn], sbuf.dsem, 16))
        for b in rd:
            b.r.append(("dma", tok))
        for b in writes:
            b.w = ("dma", tok)
            b.r = []
        if sbuf not in writes:
            sbuf.r.append(("dma", tok))
        self.dma_toks.append(tok)
        return tok

    def barrier(self):
        toks = [(self.esem[e], self.ecnt[e]) for e in ENGS if self.ecnt[e] > 0] + self.dma_toks
        for e in ENGS:
            for t in toks:
                self._wait(e, t)
        self.dma_toks = []

    def emit(self, block):
        def mk(en):
            items = self.streams[en]

            def f(eng):
                for it in items:
                    if it[0] == "w":
                        eng.wait_ge(it[1], it[2])
                    else:
                        ins = None
                        for fn in it[1]:
                            ins = fn(eng)
                        ins.then_inc(it[2], it[3])
            return f
        block.sync(mk("sp"))
        block.gpsimd(mk("pool"))
        block.scalar(mk("act"))
        block.vector(mk("dve"))
        block.tensor(mk("pe"))
        self.streams = {e: [] for e in ENGS}


def build_program(nblk=32, debug_h1=False):
    nc = bass.Bass("TRN2", target_bir_lowering=False)
    ntok = nblk * 128

    def din(name, shape):
        return nc.dram_tensor(name, list(shape), F32, kind="ExternalInput").ap()

    x_d = din("x", [ntok, D])
    p_d = din("p", [ntok, 256])
    ln0_g = din("ln0_g", [D]); ln0_b = din("ln0_b", [D])
    w_in = din("w_in", [D, 2048]); b_in = din("b_in", [2048])
    conv_w = din("conv_w", [31, 512]); conv_b = din("conv_b", [512])
    gn_g = din("gn_g", [512]); gn_b = din("gn_b", [512])
    sg_ln_g = din("sg_ln_g", [512]); sg_ln_b = din("sg_ln_b", [512])
    sg_w = din("sg_w", [8, 128, 128]); sg_b = din("sg_b", [8, 128])
    w_o = din("w_o", [D, D]); b_o = din("b_o", [D])
    ln1_g = din("ln1_g", [D]); ln1_b = din("ln1_b", [D])
    peer_wq = din("peer_wq", [D, 2048])
    peer_keys = din("peer_keys", [16, 128, 128])
    peer_u = din("peer_u", [16384, D]); peer_v = din("peer_v", [16384, D])
    ple_wp = din("ple_wp", [256, D]); ple_wg = din("ple_wg", [D, D]); ple_bg = din("ple_bg", [D])
    ln2_g = din("ln2_g", [D]); ln2_b = din("ln2_b", [D])
    out_d = nc.dram_tensor("out", [ntok, D], F32, kind="ExternalOutput").ap()
    h1_d = nc.dram_tensor("h1s", [ntok, D], F32, kind="Internal").ap()

    with ExitStack() as outer:
        P = Prog(nc, outer)
        h1d_bufs = [Buf("h1d%d" % b) for b in range(nblk)]

        with ExitStack() as s1:
            def sbt(name, shape, dt):
                return s1.enter_context(nc.sbuf_tensor(name, list(shape), dt))

            def pst(name, shape, dt):
                return s1.enter_context(nc.psum_tensor(name, list(shape), dt))

            w_in_bf = sbt("w_in_bf", [128, 8, 2048], BF16); B_w_in = Buf("w_in")
            w_o_bf = sbt("w_o_bf", [128, 8, 1024], BF16); B_w_o = Buf("w_o")
            cdiag = sbt("cdiag", [128, 124, 128], BF16); B_cdiag = Buf("cdiag")
            g0 = sbt("g0", [128, D], F32); b0 = sbt("b0", [128, D], F32)
            g1 = sbt("g1", [128, D], F32); b1 = sbt("b1", [128, D], F32)
            sglg = sbt("sglg", [128, 512], F32); sglb = sbt("sglb", [128, 512], F32)
            B_bc = Buf("bc")
            identf = sbt("identf", [128, 128], F32); ident = sbt("ident", [128, 128], BF16)
            B_ident = Buf("ident")
            Gm = sbt("Gm", [128, 128], F32); B_G = Buf("G")
            rows = sbt("rows", [28, 128], F32); B_rows = Buf("rows")
            cwrow = sbt("cwrow", [31, 512], F32); B_cwrow = Buf("cwrow")
            cols = sbt("cols", [128, 28], F32); B_cols = Buf("cols")
            cw = sbt("cw", [128, 4, 31], F32); B_cw = Buf("cw")
            brow = sbt("brow", [1, 2048], BF16); B_brow = Buf("brow")
            ones_row = sbt("ones_row", [1, 128], BF16); B_ones = Buf("ones")
            eps_t = sbt("eps_t", [128, 1], F32); B_eps = Buf("eps")
            sgw_f = sbt("sgw_f", [128, 8, 128], F32); B_sgwf = Buf("sgwf")
            sgw_m = sbt("sgw_m", [128, 8, 128], BF16); B_sgwm = Buf("sgwm")
            wmT = sbt("wmT", [128, 8, 128], BF16); B_wmT = Buf("wmT")

            ptb = pst("ptb", [128, 8, 128], BF16); B_ptb = Buf("ptb")
            pbs = [pst("pb%d" % i, [128, 4, 128], F32) for i in range(7)]
            B_pb = [Buf("pb%d" % i) for i in range(7)]

            def flat(t):
                return t[:].rearrange("p c t -> p (c t)")

            for k2 in range(2):
                for kk in range(8):
                    P.dma("pool", (lambda g, kk=kk, k2=k2: g.dma_start(
                        out=w_in_bf[:, kk, k2 * 1024:(k2 + 1) * 1024],
                        in_=w_in[kk * 128:(kk + 1) * 128, k2 * 1024:(k2 + 1) * 1024])), B_w_in, writes=[B_w_in])
            for kk in range(8):
                P.dma("pool", (lambda g, kk=kk: g.dma_start(
                    out=w_o_bf[:, kk, :], in_=w_o[kk * 128:(kk + 1) * 128, :])), B_w_o, writes=[B_w_o])
            P.dma("pool", lambda g: g.dma_start(out=brow[:, 0:1024], in_=b_in[1024:2048].unsqueeze(0)), B_brow, writes=[B_brow])
            P.dma("pool", lambda g: g.dma_start(out=brow[:, 1024:2048], in_=b_o.unsqueeze(0)), B_brow, writes=[B_brow])
            for (t_, v_) in ((g0, ln0_g), (b0, ln0_b), (g1, ln1_g), (b1, ln1_b), (sglg, sg_ln_g), (sglb, sg_ln_b)):
                P.dma("sp", (lambda q, t_=t_, v_=v_: q.dma_start(out=t_[:], in_=v_.partition_broadcast(128))), B_bc, writes=[B_bc])
            P.dma("sp", lambda q: q.dma_start(out=rows[0:8, :], in_=b_in[0:1024].rearrange("(c p) -> c p", p=128)), B_rows, writes=[B_rows])
            P.dma("sp", lambda q: q.dma_start(out=rows[8:12, :], in_=conv_b.rearrange("(c p) -> c p", p=128)), B_rows, writes=[B_rows])
            P.dma("sp", lambda q: q.dma_start(out=rows[12:16, :], in_=gn_g.rearrange("(c p) -> c p", p=128)), B_rows, writes=[B_rows])
            P.dma("sp", lambda q: q.dma_start(out=rows[16:20, :], in_=gn_b.rearrange("(c p) -> c p", p=128)), B_rows, writes=[B_rows])
            P.dma("sp", lambda q: q.dma_start(out=rows[20:28, :], in_=sg_b), B_rows, writes=[B_rows])
            P.dma("sp", lambda q: q.dma_start(out=cwrow[:], in_=conv_w), B_cwrow, writes=[B_cwrow])
            P.dma("sp", lambda q: q.dma_start(out=sgw_f[:], in_=sg_w.rearrange("h t s -> t h s")), B_sgwf, writes=[B_sgwf])

            P.op("pool", lambda g: g.memset(identf[:], 0.0), writes=[B_ident])
            P.op("pool", lambda g: g.affine_select(out=identf[:], in_=identf[:], pattern=[[-1, 128]],
                                                   compare_op=ALU.not_equal, fill=1.0, base=0, channel_multiplier=1),
                 reads=[B_ident], writes=[B_ident])
            P.op("dve", lambda v: v.tensor_copy(out=ident[:], in_=identf[:]), reads=[B_ident], writes=[B_ident])
            P.op("dve", lambda v: v.memset(Gm[:], 0.0), writes=[B_G])
            P.op("dve", lambda v: v.memset(Gm[0:64, 0:64], 1.0 / 64.0), writes=[B_G])
            P.op("dve", lambda v: v.memset(Gm[64:128, 64:128], 1.0 / 64.0), writes=[B_G])
            P.op("dve", lambda v: v.memset(ones_row[:], 1.0), writes=[B_ones])
            P.op("dve", lambda v: v.memset(eps_t[:], EPS), writes=[B_eps])
            P.op("pe", lambda t: t.transpose(out=pbs[0][:, 0, 0:28], in_=rows[:, :], identity=identf[0:28, 0:28]),
                 reads=[B_rows, B_ident], writes=[B_pb[0]])
            P.op("dve", lambda v: v.tensor_copy(out=cols[:], in_=pbs[0][:, 0, 0:28]), reads=[B_pb[0]], writes=[B_cols])
            P.op("pe", [(lambda t, c=c: t.transpose(out=pbs[1][:, c, 0:31], in_=cwrow[:, c * 128:(c + 1) * 128],
                                                      identity=identf[0:31, 0:31])) for c in range(4)],
                 reads=[B_cwrow, B_ident], writes=[B_pb[1]])
            P.op("dve", lambda v: v.tensor_copy(out=cw[:], in_=pbs[1][:, :, 0:31]), reads=[B_pb[1]], writes=[B_cw])
            for c in range(4):
                P.op("dve", (lambda v, c=c: v.tensor_tensor(
                    out=cdiag[:, c * 31:(c + 1) * 31, :],
                    in0=identf[:].unsqueeze(1).to_broadcast([128, 31, 128]),
                    in1=cw[:, c, :].unsqueeze(2).to_broadcast([128, 31, 128]), op=ALU.mult)),
                    reads=[B_ident, B_cw], writes=[B_cdiag])
            P.op("pool", lambda g: g.affine_select(out=sgw_m[:], in_=sgw_f[:], pattern=[[0, 8], [-1, 128]],
                                                   compare_op=ALU.is_ge, fill=0.0, base=0, channel_multiplier=1),
                 reads=[B_sgwf], writes=[B_sgwm])
            P.op("pe", [(lambda t, h=h: t.transpose(out=ptb[:, h, :], in_=sgw_m[:, h, :], identity=ident[:])) for h in range(8)],
                 reads=[B_sgwm, B_ident], writes=[B_ptb])
            P.op("dve", lambda v: v.tensor_copy(out=wmT[:], in_=ptb[:]), reads=[B_ptb], writes=[B_wmT])

            def dbl(name, shape, dt):
                return [sbt("%s%d" % (name, i), shape, dt) for i in range(2)], [Buf("%s%d" % (name, i)) for i in range(2)]
            xb, B_xb = dbl("xb", [128, D], F32)
            h0bf, B_h0bf = dbl("h0bf", [128, D], BF16)
            h0T, B_h0T = dbl("h0T", [128, 8, 128], BF16)
            sig, B_sig = dbl("sig", [128, 4, 128], F32)
            cT, B_cT = dbl("cT", [128, 4, 158], BF16)
            yT, B_yT = dbl("yT", [128, 4, 128], F32)
            dd, B_dd = dbl("dd", [128, 4, 128], F32)
            sq, B_sq = dbl("sq", [128, 4, 128], F32)
            coT, B_coT = dbl("coT", [128, 4, 128], BF16)
            uu, B_uu = dbl("uu", [128, 512], F32)
            gv, B_gv = dbl("gv", [128, 512], F32)
            sq2, B_sq2 = dbl("sq2", [128, 512], F32)
            vbf, B_vbf = dbl("vbf", [128, 512], BF16)
            sgo, B_sgo = dbl("sgo", [128, 512], BF16)
            sgoT, B_sgoT = dbl("sgoT", [128, 4, 128], BF16)
            r1, B_r1 = dbl("r1", [128, D], F32)
            st, B_st = dbl("st", [128, 64], F32)

            P.op("dve", lambda v: v.memset(cT[0][:, :, 0:30], 0.0), writes=[B_cT[0]])

            def layer_norm(src, Bsrc, dst, Bdst, gt, bt, stt, Bstt, base):
                s_stats = stt[:, base:base + 12]
                s_mv = stt[:, base + 12:base + 14]
                s_sd = stt[:, base + 14:base + 15]
                s_rs = stt[:, base + 15:base + 16]
                s_nm = stt[:, base + 16:base + 17]
                P.op("dve", [lambda v: v.bn_stats(out=stt[:, base:base + 6], in_=src[:, 0:512]),
                             lambda v: v.bn_stats(out=stt[:, base + 6:base + 12], in_=src[:, 512:1024])],
                     reads=[Bsrc], writes=[Bstt])
                P.op("dve", lambda v: v.bn_aggr(out=s_mv, in_=s_stats), reads=[Bstt], writes=[Bstt])
                P.op("act", lambda a: a.activation(out=s_sd, in_=stt[:, base + 13:base + 14], func=AF.Sqrt,
                                                   bias=eps_t[:, 0:1], scale=1.0), reads=[Bstt, B_eps], writes=[Bstt])
                P.op("dve", lambda v: v.reciprocal(out=s_rs, in_=s_sd), reads=[Bstt], writes=[Bstt])
                P.op("dve", lambda v: v.tensor_scalar(out=s_nm, in0=stt[:, base + 12:base + 13], scalar1=s_rs, scalar2=-1.0,
                                                      op0=ALU.mult, op1=ALU.mult), reads=[Bstt], writes=[Bstt])
                P.op("act", lambda a: a.activation(out=dst[:], in_=src[:], func=AF.Identity, bias=s_nm, scale=s_rs),
                     reads=[Bsrc, Bstt], writes=[Bdst])

            for b in range(nblk):
                i = b % 2
                j = (b + 1) % 2
                r0 = b * 128
                X = xb[i]; BX = B_xb[i]
                P.dma("sp", (lambda q, X=X, r0=r0: q.dma_start(out=X[:], in_=x_d[r0:r0 + 128, :])), BX, writes=[BX])
                layer_norm(X, BX, X, BX, g0, b0, st[i], B_st[i], 0)
                P.op("pool", lambda g, X=X: g.tensor_tensor(out=X[:], in0=X[:], in1=g0[:], op=ALU.mult), reads=[BX, B_bc], writes=[BX])
                P.op("pool", lambda g, X=X: g.tensor_tensor(out=X[:], in0=X[:], in1=b0[:], op=ALU.add), reads=[BX, B_bc], writes=[BX])
                P.op("act", lambda a, X=X, i=i: a.activation(out=h0bf[i][:], in_=X[:], func=AF.Copy), reads=[BX], writes=[B_h0bf[i]])
                P.op("pe", [(lambda t, k=k, i=i: t.transpose(out=ptb[:, k, :], in_=h0bf[i][:, k * 128:(k + 1) * 128], identity=ident[:]))
                            for k in range(8)], reads=[B_h0bf[i], B_ident], writes=[B_ptb])
                P.op("act", lambda a, i=i: a.activation(out=h0T[i][:], in_=ptb[:], func=AF.Copy), reads=[B_ptb], writes=[B_h0T[i]])
                for (bank, off) in ((0, 0), (1, 512)):
                    P.op("pe", [(lambda t, c=c, k=k, bank=bank, off=off, i=i: t.matmul(
                        pbs[bank][:, c, :], lhsT=w_in_bf[:, k, off + c * 128:off + (c + 1) * 128], rhs=h0T[i][:, k, :],
                        start=(k == 0), stop=(k == 7))) for c in range(4) for k in range(8)],
                        reads=[B_w_in, B_h0T[i]], writes=[B_pb[bank]])
                for (bank, off) in ((2, 1024), (3, 1536)):
                    fl = [(lambda t, bank=bank, off=off: t.matmul(flat(pbs[bank]), lhsT=ones_row[:, :], rhs=brow[:, off - 1024:off - 512],
                                                                   start=True, stop=False))]
                    fl += [(lambda t, k=k, bank=bank, off=off, i=i: t.matmul(flat(pbs[bank]), lhsT=h0T[i][:, k, :],
                                                                              rhs=w_in_bf[:, k, off:off + 512], start=False, stop=(k == 7)))
                           for k in range(8)]
                    P.op("pe", fl, reads=[B_w_in, B_h0T[i], B_ones, B_brow], writes=[B_pb[bank]])
                P.op("act", [(lambda a, c=c, i=i: a.activation(out=sig[i][:, c, :], in_=pbs[1][:, c, :], func=AF.Sigmoid,
                                                               bias=cols[:, 4 + c:5 + c], scale=1.0)) for c in range(4)],
                     reads=[B_pb[1], B_cols], writes=[B_sig[i]])
                P.op("dve", [(lambda v, c=c, i=i: v.scalar_tensor_tensor(out=cT[i][:, c, 30:158], in0=pbs[0][:, c, :],
                                                                          scalar=cols[:, c:c + 1], in1=sig[i][:, c, :],
                                                                          op0=ALU.add, op1=ALU.mult)) for c in range(4)],
                     reads=[B_pb[0], B_sig[i], B_cols], writes=[B_cT[i]])
                if b + 1 < nblk:
                    P.op("pool", lambda g, i=i, j=j: g.tensor_copy(out=cT[j][:, :, 0:30], in_=cT[i][:, :, 128:158]),
                         reads=[B_cT[i]], writes=[B_cT[j]])
                P.op("act", lambda a, i=i: a.activation(out=uu[i][:], in_=flat(pbs[2]), func=AF.Gelu), reads=[B_pb[2]], writes=[B_uu[i]])
                P.op("act", lambda a, i=i: a.activation(out=gv[i][:], in_=flat(pbs[3]), func=AF.Gelu), reads=[B_pb[3]], writes=[B_gv[i]])
                P.op("pe", [(lambda t, c=c, k=k, i=i: t.matmul(pbs[0][:, c, :], lhsT=cdiag[:, c * 31 + k, :], rhs=cT[i][:, c, k:k + 128],
                                                               start=(k == 0), stop=(k == 30))) for c in range(4) for k in range(31)],
                     reads=[B_cdiag, B_cT[i]], writes=[B_pb[0]])
                P.op("act", [(lambda a, c=c, i=i: a.activation(out=yT[i][:, c, :], in_=pbs[0][:, c, :], func=AF.Identity,
                                                               bias=cols[:, 8 + c:9 + c], scale=1.0)) for c in range(4)],
                     reads=[B_pb[0], B_cols], writes=[B_yT[i]])
                P.op("pe", lambda t, i=i: t.matmul(flat(pbs[1]), lhsT=Gm[:], rhs=flat(yT[i]), start=True, stop=True),
                     reads=[B_G, B_yT[i]], writes=[B_pb[1]])
                P.op("dve", lambda v, i=i: v.tensor_tensor(out=flat(dd[i]), in0=flat(yT[i]), in1=flat(pbs[1]), op=ALU.subtract),
                     reads=[B_yT[i], B_pb[1]], writes=[B_dd[i]])
                P.op("act", lambda a, i=i: a.activation(out=flat(sq[i]), in_=flat(dd[i]), func=AF.Square), reads=[B_dd[i]], writes=[B_sq[i]])
                P.op("pe", lambda t, i=i: t.matmul(flat(pbs[5]), lhsT=Gm[:], rhs=flat(sq[i]), start=True, stop=True),
                     reads=[B_G, B_sq[i]], writes=[B_pb[5]])
                P.op("act", lambda a, i=i: a.activation(out=flat(sq[i]), in_=flat(pbs[5]), func=AF.Sqrt, bias=eps_t[:, 0:1], scale=1.0),
                     reads=[B_pb[5], B_eps], writes=[B_sq[i]])
                P.op("dve", lambda v, i=i: v.reciprocal(out=flat(sq[i]), in_=flat(sq[i])), reads=[B_sq[i]], writes=[B_sq[i]])
                P.op("dve", lambda v, i=i: v.tensor_tensor(out=flat(dd[i]), in0=flat(dd[i]), in1=flat(sq[i]), op=ALU.mult),
                     reads=[B_dd[i], B_sq[i]], writes=[B_dd[i]])
                P.op("act", [(lambda a, c=c, i=i: a.activation(out=coT[i][:, c, :], in_=dd[i][:, c, :], func=AF.Silu,
                                                               bias=cols[:, 16 + c:17 + c], scale=cols[:, 12 + c:13 + c])) for c in range(4)],
                     reads=[B_dd[i], B_cols], writes=[B_coT[i]])
                gv3 = gv[i][:].rearrange("p (g d) -> p g d", g=8)
                sq3 = sq2[i][:].rearrange("p (g d) -> p g d", g=8)
                S = st[i]; BS = B_st[i]
                P.op("dve", lambda v, gv3=gv3, S=S: v.tensor_reduce(out=S[:, 24:32], in_=gv3, axis=AX.X, op=ALU.add), reads=[B_gv[i]], writes=[BS])
                P.op("dve", lambda v, S=S: v.tensor_scalar(out=S[:, 24:32], in0=S[:, 24:32], scalar1=1.0 / 64.0, scalar2=None, op0=ALU.mult),
                     reads=[BS], writes=[BS])
                P.op("dve", lambda v, gv3=gv3, S=S: v.tensor_tensor(out=gv3, in0=gv3, in1=S[:, 24:32].unsqueeze(2).to_broadcast([128, 8, 64]),
                                                                     op=ALU.subtract), reads=[B_gv[i], BS], writes=[B_gv[i]])
                P.op("act", lambda a, i=i: a.activation(out=sq2[i][:], in_=gv[i][:], func=AF.Square), reads=[B_gv[i]], writes=[B_sq2[i]])
                P.op("dve", lambda v, sq3=sq3, S=S: v.tensor_reduce(out=S[:, 32:40], in_=sq3, axis=AX.X, op=ALU.add), reads=[B_sq2[i]], writes=[BS])
                P.op("act", lambda a, S=S: a.activation(out=S[:, 40:48], in_=S[:, 32:40], func=AF.Sqrt, bias=eps_t[:, 0:1], scale=1.0 / 64.0),
                     reads=[BS, B_eps], writes=[BS])
                P.op("dve", lambda v, S=S: v.reciprocal(out=S[:, 48:56], in_=S[:, 40:48]), reads=[BS], writes=[BS])
                P.op("dve", lambda v, gv3=gv3, S=S: v.tensor_tensor(out=gv3, in0=gv3, in1=S[:, 48:56].unsqueeze(2).to_broadcast([128, 8, 64]),
                                                                     op=ALU.mult), reads=[B_gv[i], BS], writes=[B_gv[i]])
                P.op("pool", lambda g, i=i: g.tensor_tensor(out=gv[i][:], in0=gv[i][:], in1=sglg[:], op=ALU.mult), reads=[B_gv[i], B_bc], writes=[B_gv[i]])
                P.op("pool", lambda g, i=i: g.tensor_tensor(out=vbf[i][:], in0=gv[i][:], in1=sglb[:], op=ALU.add), reads=[B_gv[i], B_bc], writes=[B_vbf[i]])
                P.op("pe", [(lambda t, h=h, i=i: t.matmul(flat(pbs[4])[:, h * 64:(h + 1) * 64], lhsT=wmT[:, h, :], rhs=vbf[i][:, h * 64:(h + 1) * 64],
                                                          start=True, stop=True)) for h in range(8)],
                     reads=[B_wmT, B_vbf[i]], writes=[B_pb[4]])
                f3 = flat(pbs[4]).rearrange("p (g d) -> p g d", g=8)
                P.op("dve", lambda v, f3=f3, i=i: v.tensor_tensor(out=sq2[i][:].rearrange("p (g d) -> p g d", g=8), in0=f3,
                                                                  in1=cols[:, 20:28].unsqueeze(2).to_broadcast([128, 8, 64]), op=ALU.add),
                     reads=[B_pb[4], B_cols], writes=[B_sq2[i]])
                P.op("dve", lambda v, i=i: v.tensor_tensor(out=sgo[i][:], in0=sq2[i][:], in1=uu[i][:], op=ALU.mult),
                     reads=[B_sq2[i], B_uu[i]], writes=[B_sgo[i]])
                P.op("pe", [(lambda t, c=c, i=i: t.transpose(out=ptb[:, c, :], in_=sgo[i][:, c * 128:(c + 1) * 128], identity=ident[:]))
                            for c in range(4)], reads=[B_sgo[i], B_ident], writes=[B_ptb])
                P.op("act", lambda a, i=i: a.activation(out=sgoT[i][:], in_=ptb[:, 0:4, :], func=AF.Copy), reads=[B_ptb], writes=[B_sgoT[i]])
                for n in range(2):
                    fl = [(lambda t, n=n: t.matmul(flat(pbs[2 + n]), lhsT=ones_row[:, :], rhs=brow[:, 1024 + n * 512:1024 + (n + 1) * 512],
                                                   start=True, stop=False))]
                    fl += [(lambda t, k=k, n=n, i=i: t.matmul(flat(pbs[2 + n]), lhsT=(coT[i][:, k, :] if k < 4 else sgoT[i][:, k - 4, :]),
                                                              rhs=w_o_bf[:, k, n * 512:(n + 1) * 512], start=False, stop=(k == 7)))
                           for k in range(8)]
                    P.op("pe", fl, reads=[B_w_o, B_coT[i], B_sgoT[i], B_ones, B_brow], writes=[B_pb[2 + n]])
                P.op("dve", [(lambda v, n=n, i=i, X=X: v.scalar_tensor_tensor(out=r1[i][:, n * 512:(n + 1) * 512], in0=X[:, n * 512:(n + 1) * 512],
                                                                               scalar=ALPHA, in1=flat(pbs[2 + n]), op0=ALU.mult, op1=ALU.add))
                             for n in range(2)], reads=[BX, B_pb[2], B_pb[3]], writes=[B_r1[i]])
                R = r1[i]; BR = B_r1[i]
                layer_norm(R, BR, R, BR, g1, b1, st[i], B_st[i], 0)
                P.op("pool", lambda g, R=R: g.tensor_tensor(out=R[:], in0=R[:], in1=g1[:], op=ALU.mult), reads=[BR, B_bc], writes=[BR])
                P.op("pool", lambda g, R=R: g.tensor_tensor(out=R[:], in0=R[:], in1=b1[:], op=ALU.add), reads=[BR, B_bc], writes=[BR])
                dst = out_d if debug_h1 else h1_d
                P.dma("sp", (lambda q, R=R, r0=r0, dst=dst: q.dma_start(out=dst[r0:r0 + 128, :], in_=R[:])), BR, reads=[BR], writes=[h1d_bufs[b]])

            P.barrier()
            with nc.Block() as blk:
                P.emit(blk)

        if debug_h1:
            return nc

        with ExitStack() as s2:
            def sbt(name, shape, dt):
                return s2.enter_context(nc.sbuf_tensor(name, list(shape), dt))

            def pst(name, shape, dt):
                return s2.enter_context(nc.psum_tensor(name, list(shape), dt))

            def flat(t):
                return t[:].rearrange("p c t -> p (c t)")

            wq_bf = sbt("wq_bf", [128, 8, 2048], BF16); B_wq = Buf("wq")
            wg_bf = sbt("wg_bf", [128, 8, 1024], BF16); B_wg = Buf("wg")
            wp_bf = sbt("wp_bf", [128, 2, 1024], BF16); B_wp = Buf("wp")
            keys_f = sbt("keys_f", [128, 16, 128], BF16); B_keysf = Buf("keysf")
            keysT = sbt("keysT", [128, 16, 128], BF16); B_keysT = Buf("keysT")
            g2 = sbt("g2", [128, D], F32); b2 = sbt("b2", [128, D], F32); B_bc = Buf("bc2")
            identf = sbt("identf2", [128, 128], F32); ident = sbt("ident2", [128, 128], BF16); B_ident = Buf("ident2")
            brow = sbt("brow2", [1, 1024], BF16); B_brow = Buf("brow2")
            ones_row = sbt("ones_row2", [1, 128], BF16); B_ones = Buf("ones2")
            eps_t = sbt("eps_t2", [128, 1], F32); B_eps = Buf("eps2")
            iota16 = sbt("iota16", [128, 16], F32); B_iota = Buf("iota")

            ptb = pst("ptb2", [128, 8, 128], BF16); B_ptb = Buf("ptb2")
            pq = [pst("pq%d" % i, [128, 4, 128], F32) for i in range(2)]; B_pq = [Buf("pq%d" % i) for i in range(2)]
            psc = [pst("psc%d" % i, [128, 4, 128], F32) for i in range(2)]; B_psc = [Buf("psc%d" % i) for i in range(2)]
            ppl = pst("ppl", [128, 4, 128], F32); B_ppl = Buf("ppl")
            py = [pst("py%d" % i, [128, 4, 128], F32) for i in range(2)]; B_py = Buf("py")

            for k2 in range(2):
                for kk in range(8):
                    P.dma("pool", (lambda g, kk=kk, k2=k2: g.dma_start(
                        out=wq_bf[:, kk, k2 * 1024:(k2 + 1) * 1024],
                        in_=peer_wq[kk * 128:(kk + 1) * 128, k2 * 1024:(k2 + 1) * 1024])), B_wq, writes=[B_wq])
            for kk in range(8):
                P.dma("pool", (lambda g, kk=kk: g.dma_start(out=wg_bf[:, kk, :], in_=ple_wg[kk * 128:(kk + 1) * 128, :])), B_wg, writes=[B_wg])
            for kk in range(2):
                P.dma("pool", (lambda g, kk=kk: g.dma_start(out=wp_bf[:, kk, :], in_=ple_wp[kk * 128:(kk + 1) * 128, :])), B_wp, writes=[B_wp])
            P.dma("pool", lambda g: g.dma_start(out=keys_f[:], in_=peer_keys.rearrange("h k d -> k h d")), B_keysf, writes=[B_keysf])
            P.dma("pool", lambda g: g.dma_start(out=brow[:, :], in_=ple_bg.unsqueeze(0)), B_brow, writes=[B_brow])
            for (t_, v_) in ((g2, ln2_g), (b2, ln2_b)):
                P.dma("sp", (lambda q, t_=t_, v_=v_: q.dma_start(out=t_[:], in_=v_.partition_broadcast(128))), B_bc, writes=[B_bc])
            P.op("pool", lambda g: g.memset(identf[:], 0.0), writes=[B_ident])
            P.op("pool", lambda g: g.affine_select(out=identf[:], in_=identf[:], pattern=[[-1, 128]],
                                                   compare_op=ALU.not_equal, fill=1.0, base=0, channel_multiplier=1),
                 reads=[B_ident], writes=[B_ident])
            P.op("pool", lambda g: g.iota(iota16[:], pattern=[[1, 16]], base=0, channel_multiplier=0, allow_small_or_imprecise_dtypes=True),
                 writes=[B_iota])
            P.op("dve", lambda v: v.tensor_copy(out=ident[:], in_=identf[:]), reads=[B_ident], writes=[B_ident])
            P.op("dve", lambda v: v.memset(ones_row[:], 1.0), writes=[B_ones])
            P.op("dve", lambda v: v.memset(eps_t[:], EPS), writes=[B_eps])
            for half in range(2):
                P.op("pe", [(lambda t, q=q, half=half: t.transpose(out=ptb[:, q, :], in_=keys_f[:, half * 8 + q, :], identity=ident[:]))
                            for q in range(8)], reads=[B_keysf, B_ident], writes=[B_ptb])
                P.op("dve", lambda v, half=half: v.tensor_copy(out=keysT[:, half * 8:(half + 1) * 8, :], in_=ptb[:]),
                     reads=[B_ptb], writes=[B_keysT])

            Uring = [sbt("U%d" % s, [128, D], BF16) for s in range(NRING)]; B_U = [Buf("U%d" % s) for s in range(NRING)]
            Vring = [sbt("V%d" % s, [128, D], BF16) for s in range(NRING)]; B_V = [Buf("V%d" % s) for s in range(NRING)]
            dring = [sbt("dg%d" % s, [128, 128], BF16) for s in range(NDIAG)]; B_dg = [Buf("dg%d" % s) for s in range(NDIAG)]

            def dbl(name, shape, dt):
                return [sbt("%s%d" % (name, i), shape, dt) for i in range(2)], [Buf("%s%d" % (name, i)) for i in range(2)]
            h1, B_h1 = dbl("h1_", [128, D], F32)
            rp, B_rp = dbl("rp", [128, D], F32)
            idx, B_idx = dbl("idx", [128, 128], U32)
            gsm, B_gsm = dbl("gsm", [128, 128], F32)
            pld, B_pld = dbl("pld", [128, 256], F32)
            h1bf = sbt("h1bf", [128, D], BF16); B_h1bf = Buf("h1bf")
            h1T = sbt("h1T", [128, 8, 128], BF16); B_h1T = Buf("h1T")
            pbf = sbt("pbf", [128, 256], BF16); B_pbf = Buf("pbf")
            pT = sbt("pT", [128, 2, 128], BF16); B_pT = Buf("pT")
            qT = sbt("qT", [128, 16, 128], BF16); B_qT = [Buf("qT%d" % i) for i in range(4)]
            scs = sbt("scs", [128, 16, 128], F32); B_scs = [Buf("scs%d" % i) for i in range(4)]
            wk = sbt("wk", [128, 16, 128], F32); B_wk = Buf("wk")
            m8 = sbt("m8", [128, 16, 16], F32); B_m8 = Buf("m8")
            i8 = sbt("i8", [128, 16, 16], U32); B_i8 = Buf("i8")
            i8f = sbt("i8f", [128, 16, 16], F32); B_i8f = Buf("i8f")
            comb = sbt("comb", [128, 4, 256], F32); B_comb = Buf("comb")
            wk2 = sbt("wk2", [128, 4, 256], F32); B_wk2 = Buf("wk2")
            c16 = sbt("c16", [128, 8, 16], F32); B_c16 = Buf("c16")
            pos = sbt("pos", [128, 8, 16], U32); B_pos = Buf("pos")
            apos = sbt("apos", [128, 8, 16], U32); bpos = sbt("bpos", [128, 8, 16], U32)
            aposf = sbt("aposf", [128, 8, 16], F32); bposf = sbt("bposf", [128, 8, 16], F32); B_ab = Buf("ab")
            oh = sbt("oh", [128, 4, 16, 16], F32); B_oh = Buf("oh")
            asel = sbt("asel", [128, 8, 16], F32); bsel = sbt("bsel", [128, 8, 16], F32); B_sel = Buf("sel")
            idxf = sbt("idxf", [128, 128], F32); B_idxf = Buf("idxf")
            sm = sbt("sm", [128, 32], F32); B_sm = Buf("sm")
            gate = sbt("gate", [128, 512], F32); B_gate = Buf("gate")
            junk = sbt("junk", [128, D], BF16); B_junk = Buf("junk")
            actt = sbt("actt", [128, 128], F32); B_actg = [Buf("actg%d" % g) for g in range(128 // GRP)]
            gel = sbt("gel", [128, 128], F32); B_gelg = [Buf("gelg%d" % g) for g in range(128 // GRP)]
            wgt = sbt("wgt", [128, 128], F32); B_wgtg = [Buf("wgtg%d" % g) for g in range(128 // GRP)]
            r2 = sbt("r2", [128, D], F32); B_r2 = Buf("r2")
            st2 = sbt("st2", [128, 32], F32); B_st2 = Buf("st2")

            NG = 128 // GRP
            ring_ctr = [0]

            def front(b):
                i = b % 2
                r0 = b * 128
                H = h1[i]; BH = B_h1[i]
                P.dma("sp", (lambda q, H=H, r0=r0: q.dma_start(out=H[:], in_=h1_d[r0:r0 + 128, :])), BH, reads=[h1d_bufs[b]], writes=[BH])
                P.dma("sp", (lambda q, i=i, r0=r0: q.dma_start(out=pld[i][:], in_=p_d[r0:r0 + 128, :])), B_pld[i], writes=[B_pld[i]])
                P.op("act", lambda a, H=H: a.activation(out=h1bf[:], in_=H[:], func=AF.Copy), reads=[BH], writes=[B_h1bf])
                P.op("act", lambda a, i=i: a.activation(out=pbf[:], in_=pld[i][:], func=AF.Copy), reads=[B_pld[i]], writes=[B_pbf])
                P.op("pe", [(lambda t, k=k: t.transpose(out=ptb[:, k, :], in_=h1bf[:, k * 128:(k + 1) * 128], identity=ident[:]))
                            for k in range(8)], reads=[B_h1bf, B_ident], writes=[B_ptb])
                P.op("act", lambda a: a.activation(out=h1T[:], in_=ptb[:], func=AF.Copy), reads=[B_ptb], writes=[B_h1T])
                P.op("pe", [(lambda t, k=k: t.transpose(out=ptb[:, k, :], in_=pbf[:, k * 128:(k + 1) * 128], identity=ident[:]))
                            for k in range(2)], reads=[B_pbf, B_ident], writes=[B_ptb])
                P.op("act", lambda a: a.activation(out=pT[:], in_=ptb[:, 0:2, :], func=AF.Copy), reads=[B_ptb], writes=[B_pT])
                for qd in range(4):
                    pb_ = pq[qd % 2]; Bp = B_pq[qd % 2]
                    P.op("pe", [(lambda t, c=c, k=k, qd=qd, pb_=pb_: t.matmul(
                        pb_[:, c, :], lhsT=wq_bf[:, k, (qd * 4 + c) * 128:(qd * 4 + c + 1) * 128], rhs=h1T[:, k, :],
                        start=(k == 0), stop=(k == 7))) for c in range(4) for k in range(8)],
                        reads=[B_wq, B_h1T], writes=[Bp])
                    P.op("act", lambda a, qd=qd, pb_=pb_: a.activation(out=qT[:, qd * 4:(qd + 1) * 4, :], in_=pb_[:], func=AF.Copy),
                         reads=[Bp], writes=[B_qT[qd]])
                    ps_ = psc[qd % 2]; Bs = B_psc[qd % 2]
                    P.op("pe", [(lambda t, c=c, qd=qd, ps_=ps_: t.matmul(ps_[:, c, :], lhsT=qT[:, qd * 4 + c, :], rhs=keysT[:, qd * 4 + c, :],
                                                                          start=True, stop=True)) for c in range(4)],
                         reads=[B_qT[qd], B_keysT], writes=[Bs])
                    P.op("act", lambda a, qd=qd, ps_=ps_: a.activation(out=scs[:, qd * 4:(qd + 1) * 4, :], in_=ps_[:], func=AF.Copy),
                         reads=[Bs], writes=[B_scs[qd]])
                for n in range(2):
                    fl = [(lambda t, n=n: t.matmul(flat(ppl), lhsT=ones_row[:, :], rhs=brow[:, n * 512:(n + 1) * 512], start=True, stop=False))]
                    fl += [(lambda t, k=k, n=n: t.matmul(flat(ppl), lhsT=h1T[:, k, :], rhs=wg_bf[:, k, n * 512:(n + 1) * 512],
                                                          start=False, stop=(k == 7))) for k in range(8)]
                    P.op("pe", fl, reads=[B_wg, B_h1T, B_ones, B_brow], writes=[B_ppl])
                    P.op("act", lambda a: a.activation(out=gate[:], in_=flat(ppl), func=AF.Sigmoid), reads=[B_ppl], writes=[B_gate])
                    P.op("pe", [(lambda t, k=k, n=n: t.matmul(flat(ppl), lhsT=pT[:, k, :], rhs=wp_bf[:, k, n * 512:(n + 1) * 512],
                                                               start=(k == 0), stop=(k == 1))) for k in range(2)],
                         reads=[B_wp, B_pT], writes=[B_ppl])
                    P.op("dve", lambda v: v.tensor_tensor(out=gate[:], in0=gate[:], in1=flat(ppl), op=ALU.mult),
                         reads=[B_gate, B_ppl], writes=[B_gate])
                    P.op("dve", lambda v, n=n, i=i, H=H: v.scalar_tensor_tensor(out=rp[i][:, n * 512:(n + 1) * 512], in0=H[:, n * 512:(n + 1) * 512],
                                                                               scalar=ALPHA, in1=gate[:], op0=ALU.mult, op1=ALU.add),
                         reads=[BH, B_gate], writes=[B_rp[i]])
                P.op("dve", [(lambda v, hh=hh: v.max(out=m8[:, hh, 0:8], in_=scs[:, hh, :])) for hh in range(16)], reads=B_scs, writes=[B_m8])
                P.op("dve", [(lambda v, hh=hh: v.match_replace(out=wk[:, hh, :], in_to_replace=m8[:, hh, 0:8], in_values=scs[:, hh, :], imm_value=NEG))
                             for hh in range(16)], reads=B_scs + [B_m8], writes=[B_wk])
                P.op("dve", [(lambda v, hh=hh: v.max(out=m8[:, hh, 8:16], in_=wk[:, hh, :])) for hh in range(16)], reads=[B_wk], writes=[B_m8])
                P.op("dve", [(lambda v, hh=hh, o=o: v.max_index(out=i8[:, hh, o:o + 8], in_max=m8[:, hh, o:o + 8], in_values=scs[:, hh, :]))
                             for hh in range(16) for o in (0, 8)], reads=B_scs + [B_m8], writes=[B_i8])
                P.op("dve", lambda v: v.tensor_copy(out=i8f[:], in_=i8[:]), reads=[B_i8], writes=[B_i8f])
                m84 = m8[:].rearrange("p (h t) k -> p h t k", t=2)
                i84 = i8f[:].rearrange("p (h t) k -> p h t k", t=2)
                for hf in range(2):
                    hs = slice(hf * 4, hf * 4 + 4)
                    comb4 = comb[:].rearrange("p h (a c) -> p h a c", a=16)
                    P.op("dve", lambda v, hs=hs, comb4=comb4: v.tensor_tensor(
                        out=comb4, in0=m84[:, hs, 0, :].unsqueeze(3).to_broadcast([128, 4, 16, 16]),
                        in1=m84[:, hs, 1, :].unsqueeze(2).to_broadcast([128, 4, 16, 16]), op=ALU.add),
                        reads=[B_m8], writes=[B_comb])
                    P.op("dve", [(lambda v, h=h, hf=hf: v.max(out=c16[:, hf * 4 + h, 0:8], in_=comb[:, h, :])) for h in range(4)],
                         reads=[B_comb], writes=[B_c16])
                    P.op("dve", [(lambda v, h=h, hf=hf: v.match_replace(out=wk2[:, h, :], in_to_replace=c16[:, hf * 4 + h, 0:8],
                                                                         in_values=comb[:, h, :], imm_value=NEG)) for h in range(4)],
                         reads=[B_comb, B_c16], writes=[B_wk2])
                    P.op("dve", [(lambda v, h=h, hf=hf: v.max(out=c16[:, hf * 4 + h, 8:16], in_=wk2[:, h, :])) for h in range(4)],
                         reads=[B_wk2], writes=[B_c16])
                    P.op("dve", [(lambda v, h=h, hf=hf, o=o: v.max_index(out=pos[:, hf * 4 + h, o:o + 8], in_max=c16[:, hf * 4 + h, o:o + 8],
                                                                          in_values=comb[:, h, :])) for h in range(4) for o in (0, 8)],
                         reads=[B_comb, B_c16], writes=[B_pos])
                P.op("dve", [lambda v: v.tensor_single_scalar(out=apos[:], in_=pos[:], scalar=4, op=ALU.logical_shift_right),
                             lambda v: v.tensor_single_scalar(out=bpos[:], in_=pos[:], scalar=15, op=ALU.bitwise_and)],
                     reads=[B_pos], writes=[B_ab])
                P.op("dve", [lambda v: v.tensor_copy(out=aposf[:], in_=apos[:]),
                             lambda v: v.tensor_copy(out=bposf[:], in_=bpos[:])], reads=[B_ab], writes=[B_ab])
                io4 = iota16[:].unsqueeze(1).unsqueeze(1).to_broadcast([128, 4, 16, 16])
                for (pf, tsel, sel) in ((aposf, 0, asel), (bposf, 1, bsel)):
                    for hf in range(2):
                        hs = slice(hf * 4, hf * 4 + 4)
                        P.op("dve", lambda v, pf=pf, hs=hs: v.tensor_tensor(out=oh[:], in0=pf[:, hs, :].unsqueeze(3).to_broadcast([128, 4, 16, 16]),
                                                                            in1=io4, op=ALU.is_equal), reads=[B_ab, B_iota], writes=[B_oh])
                        P.op("dve", lambda v, tsel=tsel, hs=hs: v.tensor_tensor(out=oh[:], in0=oh[:],
                                                                                in1=i84[:, hs, tsel, :].unsqueeze(2).to_broadcast([128, 4, 16, 16]),
                                                                                op=ALU.mult), reads=[B_oh, B_i8f], writes=[B_oh])
                        P.op("dve", lambda v, sel=sel, hs=hs: v.tensor_reduce(out=sel[:, hs, :], in_=oh[:], axis=AX.X, op=ALU.add),
                             reads=[B_oh], writes=[B_sel])
                P.op("dve", lambda v: v.scalar_tensor_tensor(out=idxf[:], in0=asel[:].rearrange("p h k -> p (h k)"), scalar=128.0,
                                                             in1=bsel[:].rearrange("p h k -> p (h k)"), op0=ALU.mult, op1=ALU.add),
                     reads=[B_sel], writes=[B_idxf])
                P.op("dve", lambda v, i=i: v.tensor_copy(out=idx[i][:], in_=idxf[:]), reads=[B_idxf], writes=[B_idx[i]])
                P.op("dve", lambda v: v.tensor_scalar(out=sm[:, 0:8], in0=c16[:, :, 0], scalar1=-1.0, scalar2=None, op0=ALU.mult),
                     reads=[B_c16], writes=[B_sm])
                G3 = gsm[i][:].rearrange("p (h k) -> p h k", h=8)
                P.op("act", [(lambda a, h=h, G3=G3: a.activation(out=G3[:, h, :], in_=c16[:, h, :], func=AF.Exp, bias=sm[:, h:h + 1], scale=1.0,
                                                                  accum_out=sm[:, 8 + h:9 + h])) for h in range(8)],
                     reads=[B_c16, B_sm], writes=[B_gsm[i], B_sm])
                P.op("dve", lambda v: v.reciprocal(out=sm[:, 16:24], in_=sm[:, 8:16]), reads=[B_sm], writes=[B_sm])
                P.op("dve", lambda v, G3=G3: v.tensor_tensor(out=G3, in0=G3, in1=sm[:, 16:24].unsqueeze(2).to_broadcast([128, 8, 16]), op=ALU.mult),
                     reads=[B_gsm[i], B_sm], writes=[B_gsm[i]])

            nuse = nblk * 128

            def issue_gather(which, n):
                if n >= nuse:
                    return
                b_, jj = divmod(n, 128)
                i_ = b_ % 2
                s_ = n % NRING
                ring, Br, tab = (Uring, B_U, peer_u) if which == 0 else (Vring, B_V, peer_v)
                P.dma("pool", (lambda q, ring=ring, s_=s_, tab=tab, jj=jj, i_=i_: q.indirect_dma_start(
                    out=ring[s_][:], out_offset=None, in_=tab,
                    in_offset=bass.IndirectOffsetOnAxis(ap=idx[i_][:, jj:jj + 1], axis=0))),
                    Br[s_], reads=[B_idx[i_]], writes=[Br[s_]])

            def wmul(b, g):
                i = b % 2
                gs = slice(g * GRP, (g + 1) * GRP)
                P.op("dve", lambda v, gs=gs, i=i: v.tensor_tensor(out=wgt[:, gs], in0=gel[:, gs], in1=gsm[i][:, gs], op=ALU.mult),
                     reads=[B_gelg[g], B_gsm[i]], writes=[B_wgtg[g]])

            def vside(b, g):
                for e in range(GRP):
                    jj = g * GRP + e
                    s = (b * 128 + jj) % NRING
                    ds = (b * 128 + jj) % NDIAG
                    P.op("act", lambda a, ds=ds, jj=jj: a.activation(out=dring[ds][:], in_=identf[:], func=AF.Copy, scale=wgt[:, jj:jj + 1]),
                         reads=[B_wgtg[g], B_ident], writes=[B_dg[ds]])
                    P.op("pe", [(lambda t, n=n, ds=ds, s=s, jj=jj: t.matmul(flat(py[n]), lhsT=dring[ds][:], rhs=Vring[s][:, n * 512:(n + 1) * 512],
                                                                             start=(jj == 0), stop=(jj == 127))) for n in range(2)],
                         reads=[B_dg[ds], B_V[s]], writes=[B_py])
                    issue_gather(1, b * 128 + jj + NRING)

            def back(b):
                i = b % 2
                r0 = b * 128
                H = h1[i]; BH = B_h1[i]
                for g in range(NG):
                    gs = slice(g * GRP, (g + 1) * GRP)
                    for e in range(GRP):
                        jj = g * GRP + e
                        s = (b * 128 + jj) % NRING
                        P.op("dve", lambda v, s=s, jj=jj, H=H: v.scalar_tensor_tensor(out=junk[:], in0=Uring[s][:], scalar=1.0, in1=H[:],
                                                                                     op0=ALU.mult, op1=ALU.mult, accum_out=actt[:, jj:jj + 1]),
                             reads=[B_U[s], BH], writes=[B_junk, B_actg[g]])
                        issue_gather(0, b * 128 + jj + NRING)
                    P.op("act", lambda a, gs=gs: a.activation(out=gel[:, gs], in_=actt[:, gs], func=AF.Gelu), reads=[B_actg[g]], writes=[B_gelg[g]])
                    if g >= 1:
                        wmul(b, g - 1)
                        vside(b, g - 1)
                wmul(b, NG - 1)
                vside(b, NG - 1)
                P.op("dve", [(lambda v, n=n, i=i: v.tensor_tensor(out=r2[:, n * 512:(n + 1) * 512], in0=rp[i][:, n * 512:(n + 1) * 512],
                                                                  in1=flat(py[n]), op=ALU.add)) for n in range(2)],
                     reads=[B_rp[i], B_py], writes=[B_r2])
                S = st2
                P.op("dve", [lambda v: v.bn_stats(out=S[:, 0:6], in_=r2[:, 0:512]),
                             lambda v: v.bn_stats(out=S[:, 6:12], in_=r2[:, 512:1024])], reads=[B_r2], writes=[B_st2])
                P.op("dve", lambda v: v.bn_aggr(out=S[:, 12:14], in_=S[:, 0:12]), reads=[B_st2], writes=[B_st2])
                P.op("act", lambda a: a.activation(out=S[:, 14:15], in_=S[:, 13:14], func=AF.Sqrt, bias=eps_t[:, 0:1], scale=1.0),
                     reads=[B_st2, B_eps], writes=[B_st2])
                P.op("dve", lambda v: v.reciprocal(out=S[:, 15:16], in_=S[:, 14:15]), reads=[B_st2], writes=[B_st2])
                P.op("dve", lambda v: v.tensor_scalar(out=S[:, 16:17], in0=S[:, 12:13], scalar1=S[:, 15:16], scalar2=-1.0, op0=ALU.mult, op1=ALU.mult),
                     reads=[B_st2], writes=[B_st2])
                P.op("act", lambda a: a.activation(out=r2[:], in_=r2[:], func=AF.Identity, bias=S[:, 16:17], scale=S[:, 15:16]),
                     reads=[B_r2, B_st2], writes=[B_r2])
                P.op("dve", lambda v: v.tensor_tensor(out=r2[:], in0=r2[:], in1=g2[:], op=ALU.mult), reads=[B_r2, B_bc], writes=[B_r2])
                P.op("dve", lambda v: v.tensor_tensor(out=r2[:], in0=r2[:], in1=b2[:], op=ALU.add), reads=[B_r2, B_bc], writes=[B_r2])
                P.dma("sp", (lambda q, r0=r0: q.dma_start(out=out_d[r0:r0 + 128, :], in_=r2[:])), B_r2, reads=[B_r2])

            front(0)
            for n_ in range(NRING):
                issue_gather(0, n_)
            for n_ in range(NRING):
                issue_gather(1, n_)
            for b in range(nblk):
                if b + 1 < nblk:
                    front(b + 1)
                back(b)

            P.barrier()
            with nc.Block() as blk:
                P.emit(blk)
    return nc


_W_NAMES = ["ln0_g", "ln0_b", "w_in", "b_in", "conv_w", "conv_b", "gn_g", "gn_b", "sg_ln_g", "sg_ln_b", "sg_w", "sg_b",
            "w_o", "b_o", "ln1_g", "ln1_b", "peer_wq", "peer_keys", "peer_u", "peer_v", "ple_wp", "ple_wg", "ple_bg",
            "ln2_g", "ln2_b"]


def _prep_weights(inp):
    w = {}
    for k in _W_NAMES:
        a = np.asarray(inp[k], dtype=np.float32)
        if k in ("ln0_g", "ln0_b"):
            w[k] = np.ascontiguousarray(a.reshape(1024))
        elif k == "peer_keys":
            w[k] = np.ascontiguousarray(a.reshape(16, 128, 128))
        else:
            w[k] = np.ascontiguousarray(a[0])
    return w


def kernel(**inputs):
    n = 8
    x = np.asarray(inputs["x"], dtype=np.float32)
    p = np.asarray(inputs["p"], dtype=np.float32)
    w = _prep_weights(inputs)
    nc = build_program(32)
    in_maps = []
    for c in range(n):
        m = {"x": np.ascontiguousarray(x[c]), "p": np.ascontiguousarray(p[0, c])}
        m.update(w)
        in_maps.append(m)
    res = run_bass_kernel_spmd(nc, in_maps, core_ids=list(range(n)))
    return np.stack([np.asarray(r["out"], dtype=np.float32) for r in res.results], axis=0)
```

```python
import numpy as np
from contextlib import ExitStack
import concourse.bass as bass
import concourse.mybir as mybir
from concourse.bass_utils import run_bass_kernel_spmd

F32 = mybir.dt.float32
BF16 = mybir.dt.bfloat16
U32 = mybir.dt.uint32
AF = mybir.ActivationFunctionType
ALU = mybir.AluOpType
AX = mybir.AxisListType

D = 1024
SEQ = 4096
ALPHA = float(2.0 ** 0.25)
EPS = 1e-5
NEG = -1.0e30
ENGS = ["sp", "pool", "act", "dve", "pe"]
NRING = 16
GRP = 8
NDIAG = 16


class Buf:
    __slots__ = ("name", "w", "r", "dsem", "dcnt")

    def __init__(self, name):
        self.name = name
        self.w = None
        self.r = []
        self.dsem = None
        self.dcnt = 0


class Prog:
    def __init__(self, nc, es):
        self.nc = nc
        self.es = es
        self.streams = {e: [] for e in ENGS}
        self.esem = {e: es.enter_context(nc.semaphore("es_" + e)) for e in ENGS}
        self.ecnt = {e: 0 for e in ENGS}
        self.waited = {e: {} for e in ENGS}
        self.dma_toks = []
        self.nd = 0

    def _wait(self, e, tok):
        sem, val = tok
        if self.waited[e].get(sem, 0) >= val:
            return
        self.waited[e][sem] = val
        self.streams[e].append(("w", sem, val))

    def _deps(self, e, who, reads, writes):
        for b in reads:
            if b.w is not None:
                self._wait(e, b.w[1])
        for b in writes:
            if b.w is not None and not (who == "pe" and b.w[0] == "pe"):
                self._wait(e, b.w[1])
            for (re_, tok) in b.r:
                self._wait(e, tok)

    def op(self, e, fns, reads=(), writes=()):
        if callable(fns):
            fns = [fns]
        self._deps(e, e, reads, writes)
        self.ecnt[e] += 1
        tok = (self.esem[e], self.ecnt[e])
        self.streams[e].append(("i", fns, self.esem[e], 1))
        for b in reads:
            b.r.append((e, tok))
        for b in writes:
            b.w = (e, tok)
            b.r = []
        return tok

    def dma(self, e, fn, sbuf, reads=(), writes=()):
        if sbuf.dsem is None:
            self.nd += 1
            sbuf.dsem = self.es.enter_context(self.nc.semaphore("ds%d" % self.nd))
        wr = list(writes)
        if sbuf not in wr:
            wr.append(sbuf)
        rd = [b for b in reads if b is not sbuf]
        self._deps(e, "dma", rd, wr)
        sbuf.dcnt += 16
        tok = (sbuf.dsem, sbuf.dcnt)
        self.streams[e].append(("i", [fn], sbuf.dsem, 16))
        for b in rd:
            b.r.append(("dma", tok))
        for b in writes:
            b.w = ("dma", tok)
            b.r = []
        if sbuf not in writes:
            sbuf.r.append(("dma", tok))
        self.dma_toks.append(tok)
        return tok

    def dma_nowait(self, e, fn, buf):
        if buf.dsem is None:
            self.nd += 1
            buf.dsem = self.es.enter_context(self.nc.semaphore("ds%d" % self.nd))
        buf.dcnt += 16
        tok = (buf.dsem, buf.dcnt)
        self.streams[e].append(("i", [fn], buf.dsem, 16))
        self.dma_toks.append(tok)
        return tok

    def barrier(self):
        toks = [(self.esem[e], self.ecnt[e]) for e in ENGS if self.ecnt[e] > 0] + self.dma_toks
        mx = {}
        for (sem, val) in toks:
            if mx.get(sem, 0) < val:
                mx[sem] = val
        for e in ENGS:
            for sem, val in mx.items():
                self._wait(e, (sem, val))
        self.dma_toks = []

    def emit(self, block):
        def mk(en):
            items = self.streams[en]

            def f(eng):
                for it in items:
                    if it[0] == "w":
                        eng.wait_ge(it[1], it[2])
                    else:
                        ins = None
                        for fn in it[1]:
                            ins = fn(eng)
                        ins.then_inc(it[2], it[3])
            return f
        block.sync(mk("sp"))
        block.gpsimd(mk("pool"))
        block.scalar(mk("act"))
        block.vector(mk("dve"))
        block.tensor(mk("pe"))
        self.streams = {e: [] for e in ENGS}


def build_program(nblk=32, debug_h1=False):
    nc = bass.Bass("TRN2", target_bir_lowering=False)
    ntok = nblk * 128

    def din(name, shape):
        return nc.dram_tensor(name, list(shape), F32, kind="ExternalInput").ap()

    x_d = din("x", [ntok, D])
    p_d = din("p", [ntok, 256])
    ln0_g = din("ln0_g", [D]); ln0_b = din("ln0_b", [D])
    w_in = din("w_in", [D, 2048]); b_in = din("b_in", [2048])
    conv_w = din("conv_w", [31, 512]); conv_b = din("conv_b", [512])
    gn_g = din("gn_g", [512]); gn_b = din("gn_b", [512])
    sg_ln_g = din("sg_ln_g", [512]); sg_ln_b = din("sg_ln_b", [512])
    sg_w = din("sg_w", [8, 128, 128]); sg_b = din("sg_b", [8, 128])
    w_o = din("w_o", [D, D]); b_o = din("b_o", [D])
    ln1_g = din("ln1_g", [D]); ln1_b = din("ln1_b", [D])
    peer_wq = din("peer_wq", [D, 2048])
    peer_keys = din("peer_keys", [16, 128, 128])
    peer_u = din("peer_u", [16384, D]); peer_v = din("peer_v", [16384, D])
    ple_wp = din("ple_wp", [256, D]); ple_wg = din("ple_wg", [D, D]); ple_bg = din("ple_bg", [D])
    ln2_g = din("ln2_g", [D]); ln2_b = din("ln2_b", [D])
    out_d = nc.dram_tensor("out", [ntok, D], F32, kind="ExternalOutput").ap()
    h1_d = nc.dram_tensor("h1s", [ntok, D], F32, kind="Internal").ap()
    uv_d = nc.dram_tensor("uvbf", [16384, 2 * D], BF16, kind="Internal").ap()

    with ExitStack() as outer:
        P = Prog(nc, outer)
        h1d_bufs = [Buf("h1d%d" % b) for b in range(nblk)]

        with ExitStack() as s1:
            def sbt(name, shape, dt):
                return s1.enter_context(nc.sbuf_tensor(name, list(shape), dt))

            def pst(name, shape, dt):
                return s1.enter_context(nc.psum_tensor(name, list(shape), dt))

            w_in_bf = sbt("w_in_bf", [128, 8, 2048], BF16); B_w_in = Buf("w_in")
            w_o_bf = sbt("w_o_bf", [128, 8, 1024], BF16); B_w_o = Buf("w_o")
            cdiag = sbt("cdiag", [128, 124, 128], BF16); B_cdiag = Buf("cdiag")
            g0 = sbt("g0", [128, D], F32); b0 = sbt("b0", [128, D], F32)
            g1 = sbt("g1", [128, D], F32); b1 = sbt("b1", [128, D], F32)
            sglg = sbt("sglg", [128, 512], F32); sglb = sbt("sglb", [128, 512], F32)
            B_bc = Buf("bc")
            identf = sbt("identf", [128, 128], F32); ident = sbt("ident", [128, 128], BF16)
            B_ident = Buf("ident")
            Gm = sbt("Gm", [128, 128], F32); B_G = Buf("G")
            rows = sbt("rows", [28, 128], F32); B_rows = Buf("rows")
            cwrow = sbt("cwrow", [31, 512], F32); B_cwrow = Buf("cwrow")
            cols = sbt("cols", [128, 28], F32); B_cols = Buf("cols")
            cw = sbt("cw", [128, 4, 31], F32); B_cw = Buf("cw")
            brow = sbt("brow", [1, 2048], BF16); B_brow = Buf("brow")
            ones_row = sbt("ones_row", [1, 128], BF16); B_ones = Buf("ones")
            eps_t = sbt("eps_t", [128, 1], F32); B_eps = Buf("eps")
            sgw_f = sbt("sgw_f", [128, 8, 128], F32); B_sgwf = Buf("sgwf")
            sgw_m = sbt("sgw_m", [128, 8, 128], BF16); B_sgwm = Buf("sgwm")
            wmT = sbt("wmT", [128, 8, 128], BF16); B_wmT = Buf("wmT")

            ptb = pst("ptb", [128, 8, 128], BF16); B_ptb = Buf("ptb")
            pbs = [pst("pb%d" % i, [128, 4, 128], F32) for i in range(7)]
            B_pb = [Buf("pb%d" % i) for i in range(7)]

            def flat(t):
                return t[:].rearrange("p c t -> p (c t)")

            for k2 in range(2):
                for kk in range(8):
                    P.dma("pool", (lambda g, kk=kk, k2=k2: g.dma_start(
                        out=w_in_bf[:, kk, k2 * 1024:(k2 + 1) * 1024],
                        in_=w_in[kk * 128:(kk + 1) * 128, k2 * 1024:(k2 + 1) * 1024])), B_w_in, writes=[B_w_in])
            for kk in range(8):
                P.dma("pool", (lambda g, kk=kk: g.dma_start(
                    out=w_o_bf[:, kk, :], in_=w_o[kk * 128:(kk + 1) * 128, :])), B_w_o, writes=[B_w_o])
            P.dma("pool", lambda g: g.dma_start(out=brow[:, 0:1024], in_=b_in[1024:2048].unsqueeze(0)), B_brow, writes=[B_brow])
            P.dma("pool", lambda g: g.dma_start(out=brow[:, 1024:2048], in_=b_o.unsqueeze(0)), B_brow, writes=[B_brow])
            for (t_, v_) in ((g0, ln0_g), (b0, ln0_b), (g1, ln1_g), (b1, ln1_b), (sglg, sg_ln_g), (sglb, sg_ln_b)):
                P.dma("sp", (lambda q, t_=t_, v_=v_: q.dma_start(out=t_[:], in_=v_.partition_broadcast(128))), B_bc, writes=[B_bc])
            P.dma("sp", lambda q: q.dma_start(out=rows[0:8, :], in_=b_in[0:1024].rearrange("(c p) -> c p", p=128)), B_rows, writes=[B_rows])
            P.dma("sp", lambda q: q.dma_start(out=rows[8:12, :], in_=conv_b.rearrange("(c p) -> c p", p=128)), B_rows, writes=[B_rows])
            P.dma("sp", lambda q: q.dma_start(out=rows[12:16, :], in_=gn_g.rearrange("(c p) -> c p", p=128)), B_rows, writes=[B_rows])
            P.dma("sp", lambda q: q.dma_start(out=rows[16:20, :], in_=gn_b.rearrange("(c p) -> c p", p=128)), B_rows, writes=[B_rows])
            P.dma("sp", lambda q: q.dma_start(out=rows[20:28, :], in_=sg_b), B_rows, writes=[B_rows])
            P.dma("sp", lambda q: q.dma_start(out=cwrow[:], in_=conv_w), B_cwrow, writes=[B_cwrow])
            P.dma("sp", lambda q: q.dma_start(out=sgw_f[:], in_=sg_w.rearrange("h t s -> t h s")), B_sgwf, writes=[B_sgwf])

            P.op("pool", lambda g: g.memset(identf[:], 0.0), writes=[B_ident])
            P.op("pool", lambda g: g.affine_select(out=identf[:], in_=identf[:], pattern=[[-1, 128]],
                                                   compare_op=ALU.not_equal, fill=1.0, base=0, channel_multiplier=1),
                 reads=[B_ident], writes=[B_ident])
            P.op("dve", lambda v: v.tensor_copy(out=ident[:], in_=identf[:]), reads=[B_ident], writes=[B_ident])
            P.op("dve", lambda v: v.memset(Gm[:], 0.0), writes=[B_G])
            P.op("dve", lambda v: v.memset(Gm[0:64, 0:64], 1.0 / 64.0), writes=[B_G])
            P.op("dve", lambda v: v.memset(Gm[64:128, 64:128], 1.0 / 64.0), writes=[B_G])
            P.op("dve", lambda v: v.memset(ones_row[:], 1.0), writes=[B_ones])
            P.op("dve", lambda v: v.memset(eps_t[:], EPS), writes=[B_eps])
            P.op("pe", lambda t: t.transpose(out=pbs[0][:, 0, 0:28], in_=rows[:, :], identity=identf[0:28, 0:28]),
                 reads=[B_rows, B_ident], writes=[B_pb[0]])
            P.op("dve", lambda v: v.tensor_copy(out=cols[:], in_=pbs[0][:, 0, 0:28]), reads=[B_pb[0]], writes=[B_cols])
            P.op("pe", [(lambda t, c=c: t.transpose(out=pbs[1][:, c, 0:31], in_=cwrow[:, c * 128:(c + 1) * 128],
                                                      identity=identf[0:31, 0:31])) for c in range(4)],
                 reads=[B_cwrow, B_ident], writes=[B_pb[1]])
            P.op("dve", lambda v: v.tensor_copy(out=cw[:], in_=pbs[1][:, :, 0:31]), reads=[B_pb[1]], writes=[B_cw])
            for c in range(4):
                P.op("dve", (lambda v, c=c: v.tensor_tensor(
                    out=cdiag[:, c * 31:(c + 1) * 31, :],
                    in0=identf[:].unsqueeze(1).to_broadcast([128, 31, 128]),
                    in1=cw[:, c, :].unsqueeze(2).to_broadcast([128, 31, 128]), op=ALU.mult)),
                    reads=[B_ident, B_cw], writes=[B_cdiag])
            P.op("pool", lambda g: g.affine_select(out=sgw_m[:], in_=sgw_f[:], pattern=[[0, 8], [-1, 128]],
                                                   compare_op=ALU.is_ge, fill=0.0, base=0, channel_multiplier=1),
                 reads=[B_sgwf], writes=[B_sgwm])
            P.op("pe", [(lambda t, h=h: t.transpose(out=ptb[:, h, :], in_=sgw_m[:, h, :], identity=ident[:])) for h in range(8)],
                 reads=[B_sgwm, B_ident], writes=[B_ptb])
            P.op("dve", lambda v: v.tensor_copy(out=wmT[:], in_=ptb[:]), reads=[B_ptb], writes=[B_wmT])

            def dbl(name, shape, dt):
                return [sbt("%s%d" % (name, i), shape, dt) for i in range(2)], [Buf("%s%d" % (name, i)) for i in range(2)]
            xb, B_xb = dbl("xb", [128, D], F32)
            h0bf, B_h0bf = dbl("h0bf", [128, D], BF16)
            h0T, B_h0T = dbl("h0T", [128, 8, 128], BF16)
            sig, B_sig = dbl("sig", [128, 4, 128], F32)
            cT, B_cT = dbl("cT", [128, 4, 158], BF16)
            yT, B_yT = dbl("yT", [128, 4, 128], F32)
            dd, B_dd = dbl("dd", [128, 4, 128], F32)
            sq, B_sq = dbl("sq", [128, 4, 128], F32)
            coT, B_coT = dbl("coT", [128, 4, 128], BF16)
            uu, B_uu = dbl("uu", [128, 512], F32)
            gv, B_gv = dbl("gv", [128, 512], F32)
            sq2, B_sq2 = dbl("sq2", [128, 512], F32)
            vbf, B_vbf = dbl("vbf", [128, 512], BF16)
            sgo, B_sgo = dbl("sgo", [128, 512], BF16)
            sgoT, B_sgoT = dbl("sgoT", [128, 4, 128], BF16)
            r1, B_r1 = dbl("r1", [128, D], F32)
            st, B_st = dbl("st", [128, 64], F32)

            P.op("dve", lambda v: v.memset(cT[0][:, :, 0:30], 0.0), writes=[B_cT[0]])

            def layer_norm(src, Bsrc, dst, Bdst, gt, bt, stt, Bstt, base):
                s_stats = stt[:, base:base + 12]
                s_mv = stt[:, base + 12:base + 14]
                s_sd = stt[:, base + 14:base + 15]
                s_rs = stt[:, base + 15:base + 16]
                s_nm = stt[:, base + 16:base + 17]
                P.op("dve", [lambda v: v.bn_stats(out=stt[:, base:base + 6], in_=src[:, 0:512]),
                             lambda v: v.bn_stats(out=stt[:, base + 6:base + 12], in_=src[:, 512:1024])],
                     reads=[Bsrc], writes=[Bstt])
                P.op("dve", lambda v: v.bn_aggr(out=s_mv, in_=s_stats), reads=[Bstt], writes=[Bstt])
                P.op("act", lambda a: a.activation(out=s_sd, in_=stt[:, base + 13:base + 14], func=AF.Sqrt,
                                                   bias=eps_t[:, 0:1], scale=1.0), reads=[Bstt, B_eps], writes=[Bstt])
                P.op("dve", lambda v: v.reciprocal(out=s_rs, in_=s_sd), reads=[Bstt], writes=[Bstt])
                P.op("dve", lambda v: v.tensor_scalar(out=s_nm, in0=stt[:, base + 12:base + 13], scalar1=s_rs, scalar2=-1.0,
                                                      op0=ALU.mult, op1=ALU.mult), reads=[Bstt], writes=[Bstt])
                P.op("act", lambda a: a.activation(out=dst[:], in_=src[:], func=AF.Identity, bias=s_nm, scale=s_rs),
                     reads=[Bsrc, Bstt], writes=[Bdst])

            B_uvtab = Buf("uvtab")
            conv_jobs = [(t_, c_) for c_ in range(32) for t_ in range(2)]

            def issue_conv(k):
                for (t_, c_) in conv_jobs[k::nblk] if nblk < 32 else conv_jobs[2 * k:2 * k + 2]:
                    tab = peer_u if t_ == 0 else peer_v
                    P.dma_nowait("pool", (lambda q, tab=tab, t_=t_, c_=c_: q.dma_start(
                        out=uv_d[c_ * 512:(c_ + 1) * 512, t_ * D:(t_ + 1) * D], in_=tab[c_ * 512:(c_ + 1) * 512, :])), B_uvtab)

            for b in range(nblk):
                i = b % 2
                j = (b + 1) % 2
                r0 = b * 128
                X = xb[i]; BX = B_xb[i]
                if not debug_h1:
                    issue_conv(b)
                P.dma("sp", (lambda q, X=X, r0=r0: q.dma_start(out=X[:], in_=x_d[r0:r0 + 128, :])), BX, writes=[BX])
                layer_norm(X, BX, X, BX, g0, b0, st[i], B_st[i], 0)
                P.op("pool", lambda g, X=X: g.tensor_tensor(out=X[:], in0=X[:], in1=g0[:], op=ALU.mult), reads=[BX, B_bc], writes=[BX])
                P.op("pool", lambda g, X=X: g.tensor_tensor(out=X[:], in0=X[:], in1=b0[:], op=ALU.add), reads=[BX, B_bc], writes=[BX])
                P.op("act", lambda a, X=X, i=i: a.activation(out=h0bf[i][:], in_=X[:], func=AF.Copy), reads=[BX], writes=[B_h0bf[i]])
                P.op("pe", [(lambda t, k=k, i=i: t.transpose(out=ptb[:, k, :], in_=h0bf[i][:, k * 128:(k + 1) * 128], identity=ident[:]))
                            for k in range(8)], reads=[B_h0bf[i], B_ident], writes=[B_ptb])
                P.op("act", lambda a, i=i: a.activation(out=h0T[i][:], in_=ptb[:], func=AF.Copy), reads=[B_ptb], writes=[B_h0T[i]])
                for (bank, off) in ((0, 0), (1, 512)):
                    P.op("pe", [(lambda t, c=c, k=k, bank=bank, off=off, i=i: t.matmul(
                        pbs[bank][:, c, :], lhsT=w_in_bf[:, k, off + c * 128:off + (c + 1) * 128], rhs=h0T[i][:, k, :],
                        start=(k == 0), stop=(k == 7))) for c in range(4) for k in range(8)],
                        reads=[B_w_in, B_h0T[i]], writes=[B_pb[bank]])
                for (bank, off) in ((2, 1024), (3, 1536)):
                    fl = [(lambda t, bank=bank, off=off: t.matmul(flat(pbs[bank]), lhsT=ones_row[:, :], rhs=brow[:, off - 1024:off - 512],
                                                                   start=True, stop=False))]
                    fl += [(lambda t, k=k, bank=bank, off=off, i=i: t.matmul(flat(pbs[bank]), lhsT=h0T[i][:, k, :],
                                                                              rhs=w_in_bf[:, k, off:off + 512], start=False, stop=(k == 7)))
                           for k in range(8)]
                    P.op("pe", fl, reads=[B_w_in, B_h0T[i], B_ones, B_brow], writes=[B_pb[bank]])
                P.op("act", [(lambda a, c=c, i=i: a.activation(out=sig[i][:, c, :], in_=pbs[1][:, c, :], func=AF.Sigmoid,
                                                               bias=cols[:, 4 + c:5 + c], scale=1.0)) for c in range(4)],
                     reads=[B_pb[1], B_cols], writes=[B_sig[i]])
                P.op("dve", [(lambda v, c=c, i=i: v.scalar_tensor_tensor(out=cT[i][:, c, 30:158], in0=pbs[0][:, c, :],
                                                                          scalar=cols[:, c:c + 1], in1=sig[i][:, c, :],
                                                                          op0=ALU.add, op1=ALU.mult)) for c in range(4)],
                     reads=[B_pb[0], B_sig[i], B_cols], writes=[B_cT[i]])
                if b + 1 < nblk:
                    P.op("pool", lambda g, i=i, j=j: g.tensor_copy(out=cT[j][:, :, 0:30], in_=cT[i][:, :, 128:158]),
                         reads=[B_cT[i]], writes=[B_cT[j]])
                P.op("act", lambda a, i=i: a.activation(out=uu[i][:], in_=flat(pbs[2]), func=AF.Gelu), reads=[B_pb[2]], writes=[B_uu[i]])
                P.op("act", lambda a, i=i: a.activation(out=gv[i][:], in_=flat(pbs[3]), func=AF.Gelu), reads=[B_pb[3]], writes=[B_gv[i]])
                P.op("pe", [(lambda t, c=c, k=k, i=i: t.matmul(pbs[0][:, c, :], lhsT=cdiag[:, c * 31 + k, :], rhs=cT[i][:, c, k:k + 128],
                                                               start=(k == 0), stop=(k == 30))) for c in range(4) for k in range(31)],
                     reads=[B_cdiag, B_cT[i]], writes=[B_pb[0]])
                P.op("act", [(lambda a, c=c, i=i: a.activation(out=yT[i][:, c, :], in_=pbs[0][:, c, :], func=AF.Identity,
                                                               bias=cols[:, 8 + c:9 + c], scale=1.0)) for c in range(4)],
                     reads=[B_pb[0], B_cols], writes=[B_yT[i]])
                P.op("pe", lambda t, i=i: t.matmul(flat(pbs[1]), lhsT=Gm[:], rhs=flat(yT[i]), start=True, stop=True),
                     reads=[B_G, B_yT[i]], writes=[B_pb[1]])
                P.op("dve", lambda v, i=i: v.tensor_tensor(out=flat(dd[i]), in0=flat(yT[i]), in1=flat(pbs[1]), op=ALU.subtract),
                     reads=[B_yT[i], B_pb[1]], writes=[B_dd[i]])
                P.op("act", lambda a, i=i: a.activation(out=flat(sq[i]), in_=flat(dd[i]), func=AF.Square), reads=[B_dd[i]], writes=[B_sq[i]])
                P.op("pe", lambda t, i=i: t.matmul(flat(pbs[5]), lhsT=Gm[:], rhs=flat(sq[i]), start=True, stop=True),
                     reads=[B_G, B_sq[i]], writes=[B_pb[5]])
                P.op("act", lambda a, i=i: a.activation(out=flat(sq[i]), in_=flat(pbs[5]), func=AF.Sqrt, bias=eps_t[:, 0:1], scale=1.0),
                     reads=[B_pb[5], B_eps], writes=[B_sq[i]])
                P.op("dve", lambda v, i=i: v.reciprocal(out=flat(sq[i]), in_=flat(sq[i])), reads=[B_sq[i]], writes=[B_sq[i]])
                P.op("dve", lambda v, i=i: v.tensor_tensor(out=flat(dd[i]), in0=flat(dd[i]), in1=flat(sq[i]), op=ALU.mult),
                     reads=[B_dd[i], B_sq[i]], writes=[B_dd[i]])
                P.op("act", [(lambda a, c=c, i=i: a.activation(out=coT[i][:, c, :], in_=dd[i][:, c, :], func=AF.Silu,
                                                               bias=cols[:, 16 + c:17 + c], scale=cols[:, 12 + c:13 + c])) for c in range(4)],
                     reads=[B_dd[i], B_cols], writes=[B_coT[i]])
                gv3 = gv[i][:].rearrange("p (g d) -> p g d", g=8)
                sq3 = sq2[i][:].rearrange("p (g d) -> p g d", g=8)
                S = st[i]; BS = B_st[i]
                P.op("dve", lambda v, gv3=gv3, S=S: v.tensor_reduce(out=S[:, 24:32], in_=gv3, axis=AX.X, op=ALU.add), reads=[B_gv[i]], writes=[BS])
                P.op("dve", lambda v, S=S: v.tensor_scalar(out=S[:, 24:32], in0=S[:, 24:32], scalar1=1.0 / 64.0, scalar2=None, op0=ALU.mult),
                     reads=[BS], writes=[BS])
                P.op("dve", lambda v, gv3=gv3, S=S: v.tensor_tensor(out=gv3, in0=gv3, in1=S[:, 24:32].unsqueeze(2).to_broadcast([128, 8, 64]),
                                                                     op=ALU.subtract), reads=[B_gv[i], BS], writes=[B_gv[i]])
                P.op("act", lambda a, i=i: a.activation(out=sq2[i][:], in_=gv[i][:], func=AF.Square), reads=[B_gv[i]], writes=[B_sq2[i]])
                P.op("dve", lambda v, sq3=sq3, S=S: v.tensor_reduce(out=S[:, 32:40], in_=sq3, axis=AX.X, op=ALU.add), reads=[B_sq2[i]], writes=[BS])
                P.op("act", lambda a, S=S: a.activation(out=S[:, 40:48], in_=S[:, 32:40], func=AF.Sqrt, bias=eps_t[:, 0:1], scale=1.0 / 64.0),
                     reads=[BS, B_eps], writes=[BS])
                P.op("dve", lambda v, S=S: v.reciprocal(out=S[:, 48:56], in_=S[:, 40:48]), reads=[BS], writes=[BS])
                P.op("dve", lambda v, gv3=gv3, S=S: v.tensor_tensor(out=gv3, in0=gv3, in1=S[:, 48:56].unsqueeze(2).to_broadcast([128, 8, 64]),
                                                                     op=ALU.mult), reads=[B_gv[i], BS], writes=[B_gv[i]])
                P.op("pool", lambda g, i=i: g.tensor_tensor(out=gv[i][:], in0=gv[i][:], in1=sglg[:], op=ALU.mult), reads=[B_gv[i], B_bc], writes=[B_gv[i]])
                P.op("pool", lambda g, i=i: g.tensor_tensor(out=vbf[i][:], in0=gv[i][:], in1=sglb[:], op=ALU.add), reads=[B_gv[i], B_bc], writes=[B_vbf[i]])
                P.op("pe", [(lambda t, h=h, i=i: t.matmul(flat(pbs[4])[:, h * 64:(h + 1) * 64], lhsT=wmT[:, h, :], rhs=vbf[i][:, h * 64:(h + 1) * 64],
                                                          start=True, stop=True)) for h in range(8)],
                     reads=[B_wmT, B_vbf[i]], writes=[B_pb[4]])
                f3 = flat(pbs[4]).rearrange("p (g d) -> p g d", g=8)
                P.op("dve", lambda v, f3=f3, i=i: v.tensor_tensor(out=sq2[i][:].rearrange("p (g d) -> p g d", g=8), in0=f3,
                                                                  in1=cols[:, 20:28].unsqueeze(2).to_broadcast([128, 8, 64]), op=ALU.add),
                     reads=[B_pb[4], B_cols], writes=[B_sq2[i]])
                P.op("dve", lambda v, i=i: v.tensor_tensor(out=sgo[i][:], in0=sq2[i][:], in1=uu[i][:], op=ALU.mult),
                     reads=[B_sq2[i], B_uu[i]], writes=[B_sgo[i]])
                P.op("pe", [(lambda t, c=c, i=i: t.transpose(out=ptb[:, c, :], in_=sgo[i][:, c * 128:(c + 1) * 128], identity=ident[:]))
                            for c in range(4)], reads=[B_sgo[i], B_ident], writes=[B_ptb])
                P.op("act", lambda a, i=i: a.activation(out=sgoT[i][:], in_=ptb[:, 0:4, :], func=AF.Copy), reads=[B_ptb], writes=[B_sgoT[i]])
                for n in range(2):
                    fl = [(lambda t, n=n: t.matmul(flat(pbs[2 + n]), lhsT=ones_row[:, :], rhs=brow[:, 1024 + n * 512:1024 + (n + 1) * 512],
                                                   start=True, stop=False))]
                    fl += [(lambda t, k=k, n=n, i=i: t.matmul(flat(pbs[2 + n]), lhsT=(coT[i][:, k, :] if k < 4 else sgoT[i][:, k - 4, :]),
                                                              rhs=w_o_bf[:, k, n * 512:(n + 1) * 512], start=False, stop=(k == 7)))
                           for k in range(8)]
                    P.op("pe", fl, reads=[B_w_o, B_coT[i], B_sgoT[i], B_ones, B_brow], writes=[B_pb[2 + n]])
                P.op("dve", [(lambda v, n=n, i=i, X=X: v.scalar_tensor_tensor(out=r1[i][:, n * 512:(n + 1) * 512], in0=X[:, n * 512:(n + 1) * 512],
                                                                               scalar=ALPHA, in1=flat(pbs[2 + n]), op0=ALU.mult, op1=ALU.add))
                             for n in range(2)], reads=[BX, B_pb[2], B_pb[3]], writes=[B_r1[i]])
                R = r1[i]; BR = B_r1[i]
                layer_norm(R, BR, R, BR, g1, b1, st[i], B_st[i], 0)
                P.op("pool", lambda g, R=R: g.tensor_tensor(out=R[:], in0=R[:], in1=g1[:], op=ALU.mult), reads=[BR, B_bc], writes=[BR])
                P.op("pool", lambda g, R=R: g.tensor_tensor(out=R[:], in0=R[:], in1=b1[:], op=ALU.add), reads=[BR, B_bc], writes=[BR])
                dst = out_d if debug_h1 else h1_d
                P.dma("sp", (lambda q, R=R, r0=r0, dst=dst: q.dma_start(out=dst[r0:r0 + 128, :], in_=R[:])), BR, reads=[BR], writes=[h1d_bufs[b]])

            P.barrier()
            with nc.Block() as blk:
                P.emit(blk)

        if debug_h1:
            return nc

        with ExitStack() as s2:
            def sbt(name, shape, dt):
                return s2.enter_context(nc.sbuf_tensor(name, list(shape), dt))

            def pst(name, shape, dt):
                return s2.enter_context(nc.psum_tensor(name, list(shape), dt))

            def flat(t):
                return t[:].rearrange("p c t -> p (c t)")

            wq_bf = sbt("wq_bf", [128, 8, 2048], BF16); B_wq = Buf("wq")
            wg_bf = sbt("wg_bf", [128, 8, 1024], BF16); B_wg = Buf("wg")
            wp_bf = sbt("wp_bf", [128, 2, 1024], BF16); B_wp = Buf("wp")
            keys_f = sbt("keys_f", [128, 16, 128], BF16); B_keysf = Buf("keysf")
            keysT = sbt("keysT", [128, 16, 128], BF16); B_keysT = Buf("keysT")
            g2 = sbt("g2", [128, D], F32); b2 = sbt("b2", [128, D], F32); B_bc = Buf("bc2")
            identf = sbt("identf2", [128, 128], F32); ident = sbt("ident2", [128, 128], BF16); B_ident = Buf("ident2")
            brow = sbt("brow2", [1, 1024], BF16); B_brow = Buf("brow2")
            ones_row = sbt("ones_row2", [1, 128], BF16); B_ones = Buf("ones2")
            eps_t = sbt("eps_t2", [128, 1], F32); B_eps = Buf("eps2")
            iota16 = sbt("iota16", [128, 16], F32); B_iota = Buf("iota")

            ptb = pst("ptb2", [128, 8, 128], BF16); B_ptb = Buf("ptb2")
            pq = [pst("pq%d" % i, [128, 4, 128], F32) for i in range(2)]; B_pq = [Buf("pq%d" % i) for i in range(2)]
            psc = [pst("psc%d" % i, [128, 4, 128], F32) for i in range(2)]; B_psc = [Buf("psc%d" % i) for i in range(2)]
            ppl = pst("ppl", [128, 4, 128], F32); B_ppl = Buf("ppl")
            py = [pst("py%d" % i, [128, 4, 128], F32) for i in range(2)]; B_py = Buf("py")

            for k2 in range(2):
                for kk in range(8):
                    P.dma("pool", (lambda g, kk=kk, k2=k2: g.dma_start(
                        out=wq_bf[:, kk, k2 * 1024:(k2 + 1) * 1024],
                        in_=peer_wq[kk * 128:(kk + 1) * 128, k2 * 1024:(k2 + 1) * 1024])), B_wq, writes=[B_wq])
            for kk in range(8):
                P.dma("pool", (lambda g, kk=kk: g.dma_start(out=wg_bf[:, kk, :], in_=ple_wg[kk * 128:(kk + 1) * 128, :])), B_wg, writes=[B_wg])
            for kk in range(2):
                P.dma("pool", (lambda g, kk=kk: g.dma_start(out=wp_bf[:, kk, :], in_=ple_wp[kk * 128:(kk + 1) * 128, :])), B_wp, writes=[B_wp])
            P.dma("pool", lambda g: g.dma_start(out=keys_f[:], in_=peer_keys.rearrange("h k d -> k h d")), B_keysf, writes=[B_keysf])
            P.dma("pool", lambda g: g.dma_start(out=brow[:, :], in_=ple_bg.unsqueeze(0)), B_brow, writes=[B_brow])
            for (t_, v_) in ((g2, ln2_g), (b2, ln2_b)):
                P.dma("sp", (lambda q, t_=t_, v_=v_: q.dma_start(out=t_[:], in_=v_.partition_broadcast(128))), B_bc, writes=[B_bc])
            P.op("pool", lambda g: g.memset(identf[:], 0.0), writes=[B_ident])
            P.op("pool", lambda g: g.affine_select(out=identf[:], in_=identf[:], pattern=[[-1, 128]],
                                                   compare_op=ALU.not_equal, fill=1.0, base=0, channel_multiplier=1),
                 reads=[B_ident], writes=[B_ident])
            P.op("pool", lambda g: g.iota(iota16[:], pattern=[[1, 16]], base=0, channel_multiplier=0, allow_small_or_imprecise_dtypes=True),
                 writes=[B_iota])
            P.op("dve", lambda v: v.tensor_copy(out=ident[:], in_=identf[:]), reads=[B_ident], writes=[B_ident])
            P.op("dve", lambda v: v.memset(ones_row[:], 1.0), writes=[B_ones])
            P.op("dve", lambda v: v.memset(eps_t[:], EPS), writes=[B_eps])
            for half in range(2):
                P.op("pe", [(lambda t, q=q, half=half: t.transpose(out=ptb[:, q, :], in_=keys_f[:, half * 8 + q, :], identity=ident[:]))
                            for q in range(8)], reads=[B_keysf, B_ident], writes=[B_ptb])
                P.op("dve", lambda v, half=half: v.tensor_copy(out=keysT[:, half * 8:(half + 1) * 8, :], in_=ptb[:]),
                     reads=[B_ptb], writes=[B_keysT])

            UV = [sbt("UV%d" % s, [128, 2 * D], BF16) for s in range(NRING)]; B_UV = [Buf("UV%d" % s) for s in range(NRING)]
            dring = [sbt("dg%d" % s, [128, 128], BF16) for s in range(NDIAG)]; B_dg = [Buf("dg%d" % s) for s in range(NDIAG)]

            def dbl(name, shape, dt):
                return [sbt("%s%d" % (name, i), shape, dt) for i in range(2)], [Buf("%s%d" % (name, i)) for i in range(2)]
            h1, B_h1 = dbl("h1_", [128, D], F32)
            rp, B_rp = dbl("rp", [128, D], F32)
            idx, B_idx = dbl("idx", [128, 128], U32)
            gsm, B_gsm = dbl("gsm", [128, 128], F32)
            pld, B_pld = dbl("pld", [128, 256], F32)
            h1bf = sbt("h1bf", [128, D], BF16); B_h1bf = Buf("h1bf")
            h1T = sbt("h1T", [128, 8, 128], BF16); B_h1T = Buf("h1T")
            pbf = sbt("pbf", [128, 256], BF16); B_pbf = Buf("pbf")
            pT = sbt("pT", [128, 2, 128], BF16); B_pT = Buf("pT")
            qT = sbt("qT", [128, 16, 128], BF16); B_qT = [Buf("qT%d" % i) for i in range(4)]
            scs = sbt("scs", [128, 16, 128], F32); B_scs = [Buf("scs%d" % i) for i in range(4)]
            wk = sbt("wk", [128, 8, 128], F32); B_wk = Buf("wk")
            m8 = sbt("m8", [128, 16, 16], F32); B_m8 = Buf("m8")
            i8 = sbt("i8", [128, 16, 16], U32); B_i8 = Buf("i8")
            i8f = sbt("i8f", [128, 16, 16], F32); B_i8f = Buf("i8f")
            comb = sbt("comb", [128, 4, 256], F32); B_comb = Buf("comb")
            wk2 = sbt("wk2", [128, 4, 256], F32); B_wk2 = Buf("wk2")
            c16 = sbt("c16", [128, 8, 16], F32); B_c16 = Buf("c16")
            pos = sbt("pos", [128, 8, 16], U32); B_pos = Buf("pos")
            apos = sbt("apos", [128, 8, 16], U32); bpos = sbt("bpos", [128, 8, 16], U32)
            aposf = sbt("aposf", [128, 8, 16], F32); bposf = sbt("bposf", [128, 8, 16], F32); B_ab = Buf("ab")
            oh = sbt("oh", [128, 4, 16, 16], F32); B_oh = Buf("oh")
            asel = sbt("asel", [128, 8, 16], F32); bsel = sbt("bsel", [128, 8, 16], F32); B_sel = Buf("sel")
            idxf = sbt("idxf", [128, 128], F32); B_idxf = Buf("idxf")
            sm = sbt("sm", [128, 32], F32); B_sm = Buf("sm")
            gate = sbt("gate", [128, 512], F32); B_gate = Buf("gate")
            junk = sbt("junk", [128, D], BF16); B_junk = Buf("junk")
            actt = sbt("actt", [128, 128], F32); B_actg = [Buf("actg%d" % g) for g in range(128 // GRP)]
            gel = sbt("gel", [128, 128], F32); B_gelg = [Buf("gelg%d" % g) for g in range(128 // GRP)]
            wgt = sbt("wgt", [128, 128], F32); B_wgtg = [Buf("wgtg%d" % g) for g in range(128 // GRP)]
            st2 = sbt("st2", [128, 32], F32); B_st2 = Buf("st2")

            NG = 128 // GRP
            ring_ctr = [0]

            def front(b):
                i = b % 2
                r0 = b * 128
                H = h1[i]; BH = B_h1[i]
                P.dma("sp", (lambda q, H=H, r0=r0: q.dma_start(out=H[:], in_=h1_d[r0:r0 + 128, :])), BH, reads=[h1d_bufs[b]], writes=[BH])
                P.dma("sp", (lambda q, i=i, r0=r0: q.dma_start(out=pld[i][:], in_=p_d[r0:r0 + 128, :])), B_pld[i], writes=[B_pld[i]])
                P.op("act", lambda a, H=H: a.activation(out=h1bf[:], in_=H[:], func=AF.Copy), reads=[BH], writes=[B_h1bf])
                P.op("act", lambda a, i=i: a.activation(out=pbf[:], in_=pld[i][:], func=AF.Copy), reads=[B_pld[i]], writes=[B_pbf])
                P.op("pe", [(lambda t, k=k: t.transpose(out=ptb[:, k, :], in_=h1bf[:, k * 128:(k + 1) * 128], identity=ident[:]))
                            for k in range(8)], reads=[B_h1bf, B_ident], writes=[B_ptb])
                P.op("act", lambda a: a.activation(out=h1T[:], in_=ptb[:], func=AF.Copy), reads=[B_ptb], writes=[B_h1T])
                P.op("pe", [(lambda t, k=k: t.transpose(out=ptb[:, k, :], in_=pbf[:, k * 128:(k + 1) * 128], identity=ident[:]))
                            for k in range(2)], reads=[B_pbf, B_ident], writes=[B_ptb])
                P.op("act", lambda a: a.activation(out=pT[:], in_=ptb[:, 0:2, :], func=AF.Copy), reads=[B_ptb], writes=[B_pT])
                yield
                for qd in range(4):
                    pb_ = pq[qd % 2]; Bp = B_pq[qd % 2]
                    P.op("pe", [(lambda t, c=c, k=k, qd=qd, pb_=pb_: t.matmul(
                        pb_[:, c, :], lhsT=wq_bf[:, k, (qd * 4 + c) * 128:(qd * 4 + c + 1) * 128], rhs=h1T[:, k, :],
                        start=(k == 0), stop=(k == 7))) for c in range(4) for k in range(8)],
                        reads=[B_wq, B_h1T], writes=[Bp])
                    P.op("act", lambda a, qd=qd, pb_=pb_: a.activation(out=qT[:, qd * 4:(qd + 1) * 4, :], in_=pb_[:], func=AF.Copy),
                         reads=[Bp], writes=[B_qT[qd]])
                    ps_ = psc[qd % 2]; Bs = B_psc[qd % 2]
                    P.op("pe", [(lambda t, c=c, qd=qd, ps_=ps_: t.matmul(ps_[:, c, :], lhsT=qT[:, qd * 4 + c, :], rhs=keysT[:, qd * 4 + c, :],
                                                                          start=True, stop=True)) for c in range(4)],
                         reads=[B_qT[qd], B_keysT], writes=[Bs])
                    P.op("act", lambda a, qd=qd, ps_=ps_: a.activation(out=scs[:, qd * 4:(qd + 1) * 4, :], in_=ps_[:], func=AF.Copy),
                         reads=[Bs], writes=[B_scs[qd]])
                    yield
                for n in range(2):
                    fl = [(lambda t, n=n: t.matmul(flat(ppl), lhsT=ones_row[:, :], rhs=brow[:, n * 512:(n + 1) * 512], start=True, stop=False))]
                    fl += [(lambda t, k=k, n=n: t.matmul(flat(ppl), lhsT=h1T[:, k, :], rhs=wg_bf[:, k, n * 512:(n + 1) * 512],
                                                          start=False, stop=(k == 7))) for k in range(8)]
                    P.op("pe", fl, reads=[B_wg, B_h1T, B_ones, B_brow], writes=[B_ppl])
                    P.op("act", lambda a: a.activation(out=gate[:], in_=flat(ppl), func=AF.Sigmoid), reads=[B_ppl], writes=[B_gate])
                    P.op("pe", [(lambda t, k=k, n=n: t.matmul(flat(ppl), lhsT=pT[:, k, :], rhs=wp_bf[:, k, n * 512:(n + 1) * 512],
                                                               start=(k == 0), stop=(k == 1))) for k in range(2)],
                         reads=[B_wp, B_pT], writes=[B_ppl])
                    P.op("dve", lambda v: v.tensor_tensor(out=gate[:], in0=gate[:], in1=flat(ppl), op=ALU.mult),
                         reads=[B_gate, B_ppl], writes=[B_gate])
                    P.op("dve", lambda v, n=n, i=i, H=H: v.scalar_tensor_tensor(out=rp[i][:, n * 512:(n + 1) * 512], in0=H[:, n * 512:(n + 1) * 512],
                                                                               scalar=ALPHA, in1=gate[:], op0=ALU.mult, op1=ALU.add),
                         reads=[BH, B_gate], writes=[B_rp[i]])
                    yield
                P.op("dve", [(lambda v, hh=hh: v.max(out=m8[:, hh, 0:8], in_=scs[:, hh, :])) for hh in range(16)], reads=B_scs, writes=[B_m8])
                for hv in range(2):
                    P.op("dve", [(lambda v, hh=hh, hv=hv: v.match_replace(out=wk[:, hh - 8 * hv, :], in_to_replace=m8[:, hh, 0:8],
                                                                          in_values=scs[:, hh, :], imm_value=NEG))
                                 for hh in range(8 * hv, 8 * hv + 8)], reads=B_scs + [B_m8], writes=[B_wk])
                    P.op("dve", [(lambda v, hh=hh, hv=hv: v.max(out=m8[:, hh, 8:16], in_=wk[:, hh - 8 * hv, :]))
                                 for hh in range(8 * hv, 8 * hv + 8)], reads=[B_wk], writes=[B_m8])
                yield
                P.op("dve", [(lambda v, hh=hh, o=o: v.max_index(out=i8[:, hh, o:o + 8], in_max=m8[:, hh, o:o + 8], in_values=scs[:, hh, :]))
                             for hh in range(16) for o in (0, 8)], reads=B_scs + [B_m8], writes=[B_i8])
                P.op("dve", lambda v: v.tensor_copy(out=i8f[:], in_=i8[:]), reads=[B_i8], writes=[B_i8f])
                yield
                m84 = m8[:].rearrange("p (h t) k -> p h t k", t=2)
                i84 = i8f[:].rearrange("p (h t) k -> p h t k", t=2)
                for hf in range(2):
                    hs = slice(hf * 4, hf * 4 + 4)
                    comb4 = comb[:].rearrange("p h (a c) -> p h a c", a=16)
                    P.op("dve", lambda v, hs=hs, comb4=comb4: v.tensor_tensor(
                        out=comb4, in0=m84[:, hs, 0, :].unsqueeze(3).to_broadcast([128, 4, 16, 16]),
                        in1=m84[:, hs, 1, :].unsqueeze(2).to_broadcast([128, 4, 16, 16]), op=ALU.add),
                        reads=[B_m8], writes=[B_comb])
                    P.op("dve", [(lambda v, h=h, hf=hf: v.max(out=c16[:, hf * 4 + h, 0:8], in_=comb[:, h, :])) for h in range(4)],
                         reads=[B_comb], writes=[B_c16])
                    P.op("dve", [(lambda v, h=h, hf=hf: v.match_replace(out=wk2[:, h, :], in_to_replace=c16[:, hf * 4 + h, 0:8],
                                                                         in_values=comb[:, h, :], imm_value=NEG)) for h in range(4)],
                         reads=[B_comb, B_c16], writes=[B_wk2])
                    P.op("dve", [(lambda v, h=h, hf=hf: v.max(out=c16[:, hf * 4 + h, 8:16], in_=wk2[:, h, :])) for h in range(4)],
                         reads=[B_wk2], writes=[B_c16])
                    P.op("dve", [(lambda v, h=h, hf=hf, o=o: v.max_index(out=pos[:, hf * 4 + h, o:o + 8], in_max=c16[:, hf * 4 + h, o:o + 8],
                                                                          in_values=comb[:, h, :])) for h in range(4) for o in (0, 8)],
                         reads=[B_comb, B_c16], writes=[B_pos])
                    yield
                P.op("dve", [lambda v: v.tensor_single_scalar(out=apos[:], in_=pos[:], scalar=4, op=ALU.logical_shift_right),
                             lambda v: v.tensor_single_scalar(out=bpos[:], in_=pos[:], scalar=15, op=ALU.bitwise_and)],
                     reads=[B_pos], writes=[B_ab])
                P.op("dve", [lambda v: v.tensor_copy(out=aposf[:], in_=apos[:]),
                             lambda v: v.tensor_copy(out=bposf[:], in_=bpos[:])], reads=[B_ab], writes=[B_ab])
                io4 = iota16[:].unsqueeze(1).unsqueeze(1).to_broadcast([128, 4, 16, 16])
                for (pf, tsel, sel) in ((aposf, 0, asel), (bposf, 1, bsel)):
                    for hf in range(2):
                        hs = slice(hf * 4, hf * 4 + 4)
                        P.op("dve", lambda v, pf=pf, hs=hs: v.tensor_tensor(out=oh[:], in0=pf[:, hs, :].unsqueeze(3).to_broadcast([128, 4, 16, 16]),
                                                                            in1=io4, op=ALU.is_equal), reads=[B_ab, B_iota], writes=[B_oh])
                        P.op("dve", lambda v, tsel=tsel, hs=hs: v.tensor_tensor(out=oh[:], in0=oh[:],
                                                                                in1=i84[:, hs, tsel, :].unsqueeze(2).to_broadcast([128, 4, 16, 16]),
                                                                                op=ALU.mult), reads=[B_oh, B_i8f], writes=[B_oh])
                        P.op("dve", lambda v, sel=sel, hs=hs: v.tensor_reduce(out=sel[:, hs, :], in_=oh[:], axis=AX.X, op=ALU.add),
                             reads=[B_oh], writes=[B_sel])
                        yield
                P.op("dve", lambda v: v.scalar_tensor_tensor(out=idxf[:], in0=asel[:].rearrange("p h k -> p (h k)"), scalar=128.0,
                                                             in1=bsel[:].rearrange("p h k -> p (h k)"), op0=ALU.mult, op1=ALU.add),
                     reads=[B_sel], writes=[B_idxf])
                P.op("dve", lambda v, i=i: v.tensor_copy(out=idx[i][:], in_=idxf[:]), reads=[B_idxf], writes=[B_idx[i]])
                P.op("dve", lambda v: v.tensor_scalar(out=sm[:, 0:8], in0=c16[:, :, 0], scalar1=-1.0, scalar2=None, op0=ALU.mult),
                     reads=[B_c16], writes=[B_sm])
                G3 = gsm[i][:].rearrange("p (h k) -> p h k", h=8)
                P.op("act", [(lambda a, h=h, G3=G3: a.activation(out=G3[:, h, :], in_=c16[:, h, :], func=AF.Exp, bias=sm[:, h:h + 1], scale=1.0,
                                                                  accum_out=sm[:, 8 + h:9 + h])) for h in range(8)],
                     reads=[B_c16, B_sm], writes=[B_gsm[i], B_sm])
                P.op("dve", lambda v: v.reciprocal(out=sm[:, 16:24], in_=sm[:, 8:16]), reads=[B_sm], writes=[B_sm])
                P.op("dve", lambda v, G3=G3: v.tensor_tensor(out=G3, in0=G3, in1=sm[:, 16:24].unsqueeze(2).to_broadcast([128, 8, 16]), op=ALU.mult),
                     reads=[B_gsm[i], B_sm], writes=[B_gsm[i]])

            nuse = nblk * 128

            def issue_gather(n):
                if n >= nuse:
                    return
                b_, jj = divmod(n, 128)
                i_ = b_ % 2
                s_ = n % NRING
                P.dma("pool", (lambda q, s_=s_, jj=jj, i_=i_: q.indirect_dma_start(
                    out=UV[s_][:], out_offset=None, in_=uv_d,
                    in_offset=bass.IndirectOffsetOnAxis(ap=idx[i_][:, jj:jj + 1], axis=0))),
                    B_UV[s_], reads=[B_idx[i_]], writes=[B_UV[s_]])

            def wmul(b, g):
                i = b % 2
                gs = slice(g * GRP, (g + 1) * GRP)
                P.op("dve", lambda v, gs=gs, i=i: v.tensor_tensor(out=wgt[:, gs], in0=gel[:, gs], in1=gsm[i][:, gs], op=ALU.mult),
                     reads=[B_gelg[g], B_gsm[i]], writes=[B_wgtg[g]])

            def vside(b, g):
                for e in range(GRP):
                    jj = g * GRP + e
                    s = (b * 128 + jj) % NRING
                    ds = (b * 128 + jj) % NDIAG
                    P.op("act", lambda a, ds=ds, jj=jj: a.activation(out=dring[ds][:], in_=identf[:], func=AF.Copy, scale=wgt[:, jj:jj + 1]),
                         reads=[B_wgtg[g], B_ident], writes=[B_dg[ds]])
                    P.op("pe", [(lambda t, n=n, ds=ds, s=s, jj=jj: t.matmul(flat(py[n]), lhsT=dring[ds][:], rhs=UV[s][:, D + n * 512:D + (n + 1) * 512],
                                                                             start=(jj == 0), stop=(jj == 127))) for n in range(2)],
                         reads=[B_dg[ds], B_UV[s]], writes=[B_py])
                    issue_gather(b * 128 + jj + NRING)

            def back(b, fg):
                i = b % 2
                r0 = b * 128
                H = h1[i]; BH = B_h1[i]
                for g in range(NG):
                    gs = slice(g * GRP, (g + 1) * GRP)
                    if g == NG - 1 and fg is not None:
                        for _ in fg:
                            pass
                    for e in range(GRP):
                        jj = g * GRP + e
                        s = (b * 128 + jj) % NRING
                        P.op("dve", lambda v, s=s, jj=jj, H=H: v.scalar_tensor_tensor(out=junk[:], in0=UV[s][:, 0:D], scalar=1.0, in1=H[:],
                                                                                     op0=ALU.mult, op1=ALU.mult, accum_out=actt[:, jj:jj + 1]),
                             reads=[B_UV[s], BH], writes=[B_junk, B_actg[g]])
                        if e == 0 and g >= 1:
                            wmul(b, g - 1)
                            vside(b, g - 1)
                    P.op("act", lambda a, gs=gs: a.activation(out=gel[:, gs], in_=actt[:, gs], func=AF.Gelu), reads=[B_actg[g]], writes=[B_gelg[g]])
                    if fg is not None and g < NG - 1:
                        next(fg, None)
                wmul(b, NG - 1)
                vside(b, NG - 1)
                r2 = rp[i]; B_r2 = B_rp[i]
                P.op("dve", [(lambda v, n=n, i=i, r2=r2: v.tensor_tensor(out=r2[:, n * 512:(n + 1) * 512], in0=rp[i][:, n * 512:(n + 1) * 512],
                                                                  in1=flat(py[n]), op=ALU.add)) for n in range(2)],
                     reads=[B_r2, B_py], writes=[B_r2])
                S = st2
                P.op("dve", [lambda v, r2=r2: v.bn_stats(out=S[:, 0:6], in_=r2[:, 0:512]),
                             lambda v, r2=r2: v.bn_stats(out=S[:, 6:12], in_=r2[:, 512:1024])], reads=[B_r2], writes=[B_st2])
                P.op("dve", lambda v: v.bn_aggr(out=S[:, 12:14], in_=S[:, 0:12]), reads=[B_st2], writes=[B_st2])
                P.op("act", lambda a: a.activation(out=S[:, 14:15], in_=S[:, 13:14], func=AF.Sqrt, bias=eps_t[:, 0:1], scale=1.0),
                     reads=[B_st2, B_eps], writes=[B_st2])
                P.op("dve", lambda v: v.reciprocal(out=S[:, 15:16], in_=S[:, 14:15]), reads=[B_st2], writes=[B_st2])
                P.op("dve", lambda v: v.tensor_scalar(out=S[:, 16:17], in0=S[:, 12:13], scalar1=S[:, 15:16], scalar2=-1.0, op0=ALU.mult, op1=ALU.mult),
                     reads=[B_st2], writes=[B_st2])
                P.op("act", lambda a, r2=r2: a.activation(out=r2[:], in_=r2[:], func=AF.Identity, bias=S[:, 16:17], scale=S[:, 15:16]),
                     reads=[B_r2, B_st2], writes=[B_r2])
                P.op("dve", lambda v, r2=r2: v.tensor_tensor(out=r2[:], in0=r2[:], in1=g2[:], op=ALU.mult), reads=[B_r2, B_bc], writes=[B_r2])
                P.op("dve", lambda v, r2=r2: v.tensor_tensor(out=r2[:], in0=r2[:], in1=b2[:], op=ALU.add), reads=[B_r2, B_bc], writes=[B_r2])
                P.dma("sp", (lambda q, r0=r0, r2=r2: q.dma_start(out=out_d[r0:r0 + 128, :], in_=r2[:])), B_r2, reads=[B_r2])

            for _ in front(0):
                pass
            for n_ in range(NRING):
                issue_gather(n_)
            for b in range(nblk):
                back(b, front(b + 1) if b + 1 < nblk else None)

            P.barrier()
            with nc.Block() as blk:
                P.emit(blk)
    return nc


_W_NAMES = ["ln0_g", "ln0_b", "w_in", "b_in", "conv_w", "conv_b", "gn_g", "gn_b", "sg_ln_g", "sg_ln_b", "sg_w", "sg_b",
            "w_o", "b_o", "ln1_g", "ln1_b", "peer_wq", "peer_keys", "peer_u", "peer_v", "ple_wp", "ple_wg", "ple_bg",
            "ln2_g", "ln2_b"]


def _prep_weights(inp):
    w = {}
    for k in _W_NAMES:
        a = np.asarray(inp[k], dtype=np.float32)
        if k in ("ln0_g", "ln0_b"):
            w[k] = np.ascontiguousarray(a.reshape(1024))
        elif k == "peer_keys":
            w[k] = np.ascontiguousarray(a.reshape(16, 128, 128))
        else:
            w[k] = np.ascontiguousarray(a[0])
    return w


def kernel(**inputs):
    n = 8
    x = np.asarray(inputs["x"], dtype=np.float32)
    p = np.asarray(inputs["p"], dtype=np.float32)
    w = _prep_weights(inputs)
    nc = build_program(32)
    in_maps = []
    for c in range(n):
        m = {"x": np.ascontiguousarray(x[c]), "p": np.ascontiguousarray(p[0, c])}
        m.update(w)
        in_maps.append(m)
    res = run_bass_kernel_spmd(nc, in_maps, core_ids=list(range(n)))
    return np.stack([np.asarray(r["out"], dtype=np.float32) for r in res.results], axis=0)
```

```python
import numpy as np
from contextlib import ExitStack
import concourse.bass as bass
import concourse.mybir as mybir
from concourse.bass_utils import run_bass_kernel_spmd

F32 = mybir.dt.float32
BF16 = mybir.dt.bfloat16
U32 = mybir.dt.uint32
AF = mybir.ActivationFunctionType
ALU = mybir.AluOpType
AX = mybir.AxisListType

D = 1024
SEQ = 4096
ALPHA = float(2.0 ** 0.25)
EPS = 1e-5
NEG = -1.0e30
ENGS = ["sp", "pool", "act", "dve", "pe"]
NRING = 16
GRP = 8
NDIAG = 16


class Buf:
    __slots__ = ("name", "w", "r", "dsem", "dcnt")

    def __init__(self, name):
        self.name = name
        self.w = None
        self.r = []
        self.dsem = None
        self.dcnt = 0


class Prog:
    def __init__(self, nc, es):
        self.nc = nc
        self.es = es
        self.streams = {e: [] for e in ENGS}
        self.esem = {e: es.enter_context(nc.semaphore("es_" + e)) for e in ENGS}
        self.ecnt = {e: 0 for e in ENGS}
        self.waited = {e: {} for e in ENGS}
        self.dma_toks = []
        self.nd = 0

    def _wait(self, e, tok):
        sem, val = tok
        if self.waited[e].get(sem, 0) >= val:
            return
        self.waited[e][sem] = val
        self.streams[e].append(("w", sem, val))

    def _deps(self, e, who, reads, writes):
        for b in reads:
            if b.w is not None:
                self._wait(e, b.w[1])
        for b in writes:
            if b.w is not None and not (who == "pe" and b.w[0] == "pe"):
                self._wait(e, b.w[1])
            for (re_, tok) in b.r:
                self._wait(e, tok)

    def op(self, e, fns, reads=(), writes=()):
        if callable(fns):
            fns = [fns]
        self._deps(e, e, reads, writes)
        self.ecnt[e] += 1
        tok = (self.esem[e], self.ecnt[e])
        self.streams[e].append(("i", fns, self.esem[e], 1))
        for b in reads:
            b.r.append((e, tok))
        for b in writes:
            b.w = (e, tok)
            b.r = []
        return tok

    def dma(self, e, fn, sbuf, reads=(), writes=()):
        if sbuf.dsem is None:
            self.nd += 1
            sbuf.dsem = self.es.enter_context(self.nc.semaphore("ds%d" % self.nd))
        wr = list(writes)
        if sbuf not in wr:
            wr.append(sbuf)
        rd = [b for b in reads if b is not sbuf]
        self._deps(e, "dma", rd, wr)
        sbuf.dcnt += 16
        tok = (sbuf.dsem, sbuf.dcnt)
        self.streams[e].append(("i", [fn], sbuf.dsem, 16))
        for b in rd:
            b.r.append(("dma", tok))
        for b in writes:
            b.w = ("dma", tok)
            b.r = []
        if sbuf not in writes:
            sbuf.r.append(("dma", tok))
        self.dma_toks.append(tok)
        return tok

    def dma_nowait(self, e, fn, buf):
        if buf.dsem is None:
            self.nd += 1
            buf.dsem = self.es.enter_context(self.nc.semaphore("ds%d" % self.nd))
        buf.dcnt += 16
        tok = (buf.dsem, buf.dcnt)
        self.streams[e].append(("i", [fn], buf.dsem, 16))
        self.dma_toks.append(tok)
        return tok

    def barrier(self):
        toks = [(self.esem[e], self.ecnt[e]) for e in ENGS if self.ecnt[e] > 0] + self.dma_toks
        mx = {}
        for (sem, val) in toks:
            if mx.get(sem, 0) < val:
                mx[sem] = val
        for e in ENGS:
            for sem, val in mx.items():
                self._wait(e, (sem, val))
        self.dma_toks = []

    def emit(self, block):
        def mk(en):
            items = self.streams[en]

            def f(eng):
                for it in items:
                    if it[0] == "w":
                        eng.wait_ge(it[1], it[2])
                    else:
                        ins = None
                        for fn in it[1]:
                            ins = fn(eng)
                        ins.then_inc(it[2], it[3])
            return f
        block.sync(mk("sp"))
        block.gpsimd(mk("pool"))
        block.scalar(mk("act"))
        block.vector(mk("dve"))
        block.tensor(mk("pe"))
        self.streams = {e: [] for e in ENGS}


def build_program(nblk=32, debug_h1=False):
    nc = bass.Bass("TRN2", target_bir_lowering=False)
    ntok = nblk * 128

    def din(name, shape):
        return nc.dram_tensor(name, list(shape), F32, kind="ExternalInput").ap()

    x_d = din("x", [ntok, D])
    p_d = din("p", [ntok, 256])
    ln0_g = din("ln0_g", [D]); ln0_b = din("ln0_b", [D])
    w_in = din("w_in", [D, 2048]); b_in = din("b_in", [2048])
    conv_w = din("conv_w", [31, 512]); conv_b = din("conv_b", [512])
    gn_g = din("gn_g", [512]); gn_b = din("gn_b", [512])
    sg_ln_g = din("sg_ln_g", [512]); sg_ln_b = din("sg_ln_b", [512])
    sg_w = din("sg_w", [8, 128, 128]); sg_b = din("sg_b", [8, 128])
    w_o = din("w_o", [D, D]); b_o = din("b_o", [D])
    ln1_g = din("ln1_g", [D]); ln1_b = din("ln1_b", [D])
    peer_wq = din("peer_wq", [D, 2048])
    peer_keys = din("peer_keys", [16, 128, 128])
    peer_u = din("peer_u", [16384, D]); peer_v = din("peer_v", [16384, D])
    ple_wp = din("ple_wp", [256, D]); ple_wg = din("ple_wg", [D, D]); ple_bg = din("ple_bg", [D])
    ln2_g = din("ln2_g", [D]); ln2_b = din("ln2_b", [D])
    out_d = nc.dram_tensor("out", [ntok, D], F32, kind="ExternalOutput").ap()
    h1_d = nc.dram_tensor("h1s", [ntok, D], F32, kind="Internal").ap()
    uv_d = nc.dram_tensor("uvbf", [16384, 2 * D], BF16, kind="Internal").ap()

    with ExitStack() as outer:
        P = Prog(nc, outer)
        h1d_bufs = [Buf("h1d%d" % b) for b in range(nblk)]

        with ExitStack() as s1:
            def sbt(name, shape, dt):
                return s1.enter_context(nc.sbuf_tensor(name, list(shape), dt))

            def pst(name, shape, dt):
                return s1.enter_context(nc.psum_tensor(name, list(shape), dt))

            w_in_bf = sbt("w_in_bf", [128, 8, 2048], BF16); B_w_in = Buf("w_in")
            w_o_bf = sbt("w_o_bf", [128, 8, 1024], BF16); B_w_o = Buf("w_o")
            cdiag = sbt("cdiag", [128, 124, 128], BF16); B_cdiag = Buf("cdiag")
            g0 = sbt("g0", [128, D], F32); b0 = sbt("b0", [128, D], F32)
            g1 = sbt("g1", [128, D], F32); b1 = sbt("b1", [128, D], F32)
            sglg = sbt("sglg", [128, 512], F32); sglb = sbt("sglb", [128, 512], F32)
            B_bc = Buf("bc")
            identf = sbt("identf", [128, 128], F32); ident = sbt("ident", [128, 128], BF16)
            B_ident = Buf("ident")
            Gm = sbt("Gm", [128, 128], F32); B_G = Buf("G")
            rows = sbt("rows", [28, 128], F32); B_rows = Buf("rows")
            cwrow = sbt("cwrow", [31, 512], F32); B_cwrow = Buf("cwrow")
            cols = sbt("cols", [128, 28], F32); B_cols = Buf("cols")
            cw = sbt("cw", [128, 4, 31], F32); B_cw = Buf("cw")
            brow = sbt("brow", [1, 2048], BF16); B_brow = Buf("brow")
            ones_row = sbt("ones_row", [1, 128], BF16); B_ones = Buf("ones")
            eps_t = sbt("eps_t", [128, 1], F32); B_eps = Buf("eps")
            sgw_f = sbt("sgw_f", [128, 8, 128], F32); B_sgwf = Buf("sgwf")
            sgw_m = sbt("sgw_m", [128, 8, 128], BF16); B_sgwm = Buf("sgwm")
            wmT = sbt("wmT", [128, 8, 128], BF16); B_wmT = Buf("wmT")

            ptb = pst("ptb", [128, 8, 128], BF16); B_ptb = Buf("ptb")
            pbs = [pst("pb%d" % i, [128, 4, 128], F32) for i in range(7)]
            B_pb = [Buf("pb%d" % i) for i in range(7)]

            def flat(t):
                return t[:].rearrange("p c t -> p (c t)")

            for k2 in range(2):
                for kk in range(8):
                    P.dma("pool", (lambda g, kk=kk, k2=k2: g.dma_start(
                        out=w_in_bf[:, kk, k2 * 1024:(k2 + 1) * 1024],
                        in_=w_in[kk * 128:(kk + 1) * 128, k2 * 1024:(k2 + 1) * 1024])), B_w_in, writes=[B_w_in])
            for kk in range(8):
                P.dma("pool", (lambda g, kk=kk: g.dma_start(
                    out=w_o_bf[:, kk, :], in_=w_o[kk * 128:(kk + 1) * 128, :])), B_w_o, writes=[B_w_o])
            P.dma("pool", lambda g: g.dma_start(out=brow[:, 0:1024], in_=b_in[1024:2048].unsqueeze(0)), B_brow, writes=[B_brow])
            P.dma("pool", lambda g: g.dma_start(out=brow[:, 1024:2048], in_=b_o.unsqueeze(0)), B_brow, writes=[B_brow])
            for (t_, v_) in ((g0, ln0_g), (b0, ln0_b), (g1, ln1_g), (b1, ln1_b), (sglg, sg_ln_g), (sglb, sg_ln_b)):
                P.dma("sp", (lambda q, t_=t_, v_=v_: q.dma_start(out=t_[:], in_=v_.partition_broadcast(128))), B_bc, writes=[B_bc])
            P.dma("sp", lambda q: q.dma_start(out=rows[0:8, :], in_=b_in[0:1024].rearrange("(c p) -> c p", p=128)), B_rows, writes=[B_rows])
            P.dma("sp", lambda q: q.dma_start(out=rows[8:12, :], in_=conv_b.rearrange("(c p) -> c p", p=128)), B_rows, writes=[B_rows])
            P.dma("sp", lambda q: q.dma_start(out=rows[12:16, :], in_=gn_g.rearrange("(c p) -> c p", p=128)), B_rows, writes=[B_rows])
            P.dma("sp", lambda q: q.dma_start(out=rows[16:20, :], in_=gn_b.rearrange("(c p) -> c p", p=128)), B_rows, writes=[B_rows])
            P.dma("sp", lambda q: q.dma_start(out=rows[20:28, :], in_=sg_b), B_rows, writes=[B_rows])
            P.dma("sp", lambda q: q.dma_start(out=cwrow[:], in_=conv_w), B_cwrow, writes=[B_cwrow])
            P.dma("sp", lambda q: q.dma_start(out=sgw_f[:], in_=sg_w.rearrange("h t s -> t h s")), B_sgwf, writes=[B_sgwf])

            P.op("pool", lambda g: g.memset(identf[:], 0.0), writes=[B_ident])
            P.op("pool", lambda g: g.affine_select(out=identf[:], in_=identf[:], pattern=[[-1, 128]],
                                                   compare_op=ALU.not_equal, fill=1.0, base=0, channel_multiplier=1),
                 reads=[B_ident], writes=[B_ident])
            P.op("dve", lambda v: v.tensor_copy(out=ident[:], in_=identf[:]), reads=[B_ident], writes=[B_ident])
            P.op("dve", lambda v: v.memset(Gm[:], 0.0), writes=[B_G])
            P.op("dve", lambda v: v.memset(Gm[0:64, 0:64], 1.0 / 64.0), writes=[B_G])
            P.op("dve", lambda v: v.memset(Gm[64:128, 64:128], 1.0 / 64.0), writes=[B_G])
            P.op("dve", lambda v: v.memset(ones_row[:], 1.0), writes=[B_ones])
            P.op("dve", lambda v: v.memset(eps_t[:], EPS), writes=[B_eps])
            P.op("pe", lambda t: t.transpose(out=pbs[0][:, 0, 0:28], in_=rows[:, :], identity=identf[0:28, 0:28]),
                 reads=[B_rows, B_ident], writes=[B_pb[0]])
            P.op("dve", lambda v: v.tensor_copy(out=cols[:], in_=pbs[0][:, 0, 0:28]), reads=[B_pb[0]], writes=[B_cols])
            P.op("pe", [(lambda t, c=c: t.transpose(out=pbs[1][:, c, 0:31], in_=cwrow[:, c * 128:(c + 1) * 128],
                                                      identity=identf[0:31, 0:31])) for c in range(4)],
                 reads=[B_cwrow, B_ident], writes=[B_pb[1]])
            P.op("dve", lambda v: v.tensor_copy(out=cw[:], in_=pbs[1][:, :, 0:31]), reads=[B_pb[1]], writes=[B_cw])
            for c in range(4):
                P.op("dve", (lambda v, c=c: v.tensor_tensor(
                    out=cdiag[:, c * 31:(c + 1) * 31, :],
                    in0=identf[:].unsqueeze(1).to_broadcast([128, 31, 128]),
                    in1=cw[:, c, :].unsqueeze(2).to_broadcast([128, 31, 128]), op=ALU.mult)),
                    reads=[B_ident, B_cw], writes=[B_cdiag])
            P.op("pool", lambda g: g.affine_select(out=sgw_m[:], in_=sgw_f[:], pattern=[[0, 8], [-1, 128]],
                                                   compare_op=ALU.is_ge, fill=0.0, base=0, channel_multiplier=1),
                 reads=[B_sgwf], writes=[B_sgwm])
            P.op("pe", [(lambda t, h=h: t.transpose(out=ptb[:, h, :], in_=sgw_m[:, h, :], identity=ident[:])) for h in range(8)],
                 reads=[B_sgwm, B_ident], writes=[B_ptb])
            P.op("dve", lambda v: v.tensor_copy(out=wmT[:], in_=ptb[:]), reads=[B_ptb], writes=[B_wmT])

            def dbl(name, shape, dt):
                return [sbt("%s%d" % (name, i), shape, dt) for i in range(2)], [Buf("%s%d" % (name, i)) for i in range(2)]
            xb, B_xb = dbl("xb", [128, D], F32)
            h0bf, B_h0bf = dbl("h0bf", [128, D], BF16)
            h0T, B_h0T = dbl("h0T", [128, 8, 128], BF16)
            sig, B_sig = dbl("sig", [128, 4, 128], F32)
            cT, B_cT = dbl("cT", [128, 4, 158], BF16)
            yT, B_yT = dbl("yT", [128, 4, 128], F32)
            dd, B_dd = dbl("dd", [128, 4, 128], F32)
            sq, B_sq = dbl("sq", [128, 4, 128], F32)
            coT, B_coT = dbl("coT", [128, 4, 128], BF16)
            uu, B_uu = dbl("uu", [128, 512], F32)
            gv, B_gv = dbl("gv", [128, 512], F32)
            sq2, B_sq2 = dbl("sq2", [128, 512], F32)
            vbf, B_vbf = dbl("vbf", [128, 512], BF16)
            sgo, B_sgo = dbl("sgo", [128, 512], BF16)
            sgoT, B_sgoT = dbl("sgoT", [128, 4, 128], BF16)
            r1, B_r1 = dbl("r1", [128, D], F32)
            st, B_st = dbl("st", [128, 64], F32)

            P.op("dve", lambda v: v.memset(cT[0][:, :, 0:30], 0.0), writes=[B_cT[0]])

            def layer_norm(src, Bsrc, dst, Bdst, gt, bt, stt, Bstt, base):
                s_stats = stt[:, base:base + 12]
                s_mv = stt[:, base + 12:base + 14]
                s_sd = stt[:, base + 14:base + 15]
                s_rs = stt[:, base + 15:base + 16]
                s_nm = stt[:, base + 16:base + 17]
                P.op("dve", [lambda v: v.bn_stats(out=stt[:, base:base + 6], in_=src[:, 0:512]),
                             lambda v: v.bn_stats(out=stt[:, base + 6:base + 12], in_=src[:, 512:1024])],
                     reads=[Bsrc], writes=[Bstt])
                P.op("dve", lambda v: v.bn_aggr(out=s_mv, in_=s_stats), reads=[Bstt], writes=[Bstt])
                P.op("act", lambda a: a.activation(out=s_sd, in_=stt[:, base + 13:base + 14], func=AF.Sqrt,
                                                   bias=eps_t[:, 0:1], scale=1.0), reads=[Bstt, B_eps], writes=[Bstt])
                P.op("dve", lambda v: v.reciprocal(out=s_rs, in_=s_sd), reads=[Bstt], writes=[Bstt])
                P.op("dve", lambda v: v.tensor_scalar(out=s_nm, in0=stt[:, base + 12:base + 13], scalar1=s_rs, scalar2=-1.0,
                                                      op0=ALU.mult, op1=ALU.mult), reads=[Bstt], writes=[Bstt])
                P.op("act", lambda a: a.activation(out=dst[:], in_=src[:], func=AF.Identity, bias=s_nm, scale=s_rs),
                     reads=[Bsrc, Bstt], writes=[Bdst])

            B_uvtab = Buf("uvtab")
            conv_jobs = [(t_, c_) for c_ in range(32) for t_ in range(2)]

            def issue_conv(k):
                for (t_, c_) in conv_jobs[k::nblk] if nblk < 32 else conv_jobs[2 * k:2 * k + 2]:
                    tab = peer_u if t_ == 0 else peer_v
                    P.dma_nowait("pool", (lambda q, tab=tab, t_=t_, c_=c_: q.dma_start(
                        out=uv_d[c_ * 512:(c_ + 1) * 512, t_ * D:(t_ + 1) * D], in_=tab[c_ * 512:(c_ + 1) * 512, :])), B_uvtab)

            for b in range(nblk):
                i = b % 2
                j = (b + 1) % 2
                r0 = b * 128
                X = xb[i]; BX = B_xb[i]
                if not debug_h1:
                    issue_conv(b)
                P.dma("sp", (lambda q, X=X, r0=r0: q.dma_start(out=X[:], in_=x_d[r0:r0 + 128, :])), BX, writes=[BX])
                layer_norm(X, BX, X, BX, g0, b0, st[i], B_st[i], 0)
                P.op("pool", lambda g, X=X: g.tensor_tensor(out=X[:], in0=X[:], in1=g0[:], op=ALU.mult), reads=[BX, B_bc], writes=[BX])
                P.op("pool", lambda g, X=X: g.tensor_tensor(out=X[:], in0=X[:], in1=b0[:], op=ALU.add), reads=[BX, B_bc], writes=[BX])
                P.op("act", lambda a, X=X, i=i: a.activation(out=h0bf[i][:], in_=X[:], func=AF.Copy), reads=[BX], writes=[B_h0bf[i]])
                P.op("pe", [(lambda t, k=k, i=i: t.transpose(out=ptb[:, k, :], in_=h0bf[i][:, k * 128:(k + 1) * 128], identity=ident[:]))
                            for k in range(8)], reads=[B_h0bf[i], B_ident], writes=[B_ptb])
                P.op("act", lambda a, i=i: a.activation(out=h0T[i][:], in_=ptb[:], func=AF.Copy), reads=[B_ptb], writes=[B_h0T[i]])
                for (bank, off) in ((0, 0), (1, 512)):
                    P.op("pe", [(lambda t, c=c, k=k, bank=bank, off=off, i=i: t.matmul(
                        pbs[bank][:, c, :], lhsT=w_in_bf[:, k, off + c * 128:off + (c + 1) * 128], rhs=h0T[i][:, k, :],
                        start=(k == 0), stop=(k == 7))) for c in range(4) for k in range(8)],
                        reads=[B_w_in, B_h0T[i]], writes=[B_pb[bank]])
                for (bank, off) in ((2, 1024), (3, 1536)):
                    fl = [(lambda t, bank=bank, off=off: t.matmul(flat(pbs[bank]), lhsT=ones_row[:, :], rhs=brow[:, off - 1024:off - 512],
                                                                   start=True, stop=False))]
                    fl += [(lambda t, k=k, bank=bank, off=off, i=i: t.matmul(flat(pbs[bank]), lhsT=h0T[i][:, k, :],
                                                                              rhs=w_in_bf[:, k, off:off + 512], start=False, stop=(k == 7)))
                           for k in range(8)]
                    P.op("pe", fl, reads=[B_w_in, B_h0T[i], B_ones, B_brow], writes=[B_pb[bank]])
                P.op("act", [(lambda a, c=c, i=i: a.activation(out=sig[i][:, c, :], in_=pbs[1][:, c, :], func=AF.Sigmoid,
                                                               bias=cols[:, 4 + c:5 + c], scale=1.0)) for c in range(4)],
                     reads=[B_pb[1], B_cols], writes=[B_sig[i]])
                P.op("dve", [(lambda v, c=c, i=i: v.scalar_tensor_tensor(out=cT[i][:, c, 30:158], in0=pbs[0][:, c, :],
                                                                          scalar=cols[:, c:c + 1], in1=sig[i][:, c, :],
                                                                          op0=ALU.add, op1=ALU.mult)) for c in range(4)],
                     reads=[B_pb[0], B_sig[i], B_cols], writes=[B_cT[i]])
                if b + 1 < nblk:
                    P.op("pool", lambda g, i=i, j=j: g.tensor_copy(out=cT[j][:, :, 0:30], in_=cT[i][:, :, 128:158]),
                         reads=[B_cT[i]], writes=[B_cT[j]])
                P.op("act", lambda a, i=i: a.activation(out=uu[i][:], in_=flat(pbs[2]), func=AF.Gelu), reads=[B_pb[2]], writes=[B_uu[i]])
                P.op("act", lambda a, i=i: a.activation(out=gv[i][:], in_=flat(pbs[3]), func=AF.Gelu), reads=[B_pb[3]], writes=[B_gv[i]])
                P.op("pe", [(lambda t, c=c, k=k, i=i: t.matmul(pbs[0][:, c, :], lhsT=cdiag[:, c * 31 + k, :], rhs=cT[i][:, c, k:k + 128],
                                                               start=(k == 0), stop=(k == 30))) for c in range(4) for k in range(31)],
                     reads=[B_cdiag, B_cT[i]], writes=[B_pb[0]])
                P.op("act", [(lambda a, c=c, i=i: a.activation(out=yT[i][:, c, :], in_=pbs[0][:, c, :], func=AF.Identity,
                                                               bias=cols[:, 8 + c:9 + c], scale=1.0)) for c in range(4)],
                     reads=[B_pb[0], B_cols], writes=[B_yT[i]])
                P.op("pe", lambda t, i=i: t.matmul(flat(pbs[1]), lhsT=Gm[:], rhs=flat(yT[i]), start=True, stop=True),
                     reads=[B_G, B_yT[i]], writes=[B_pb[1]])
                P.op("dve", lambda v, i=i: v.tensor_tensor(out=flat(dd[i]), in0=flat(yT[i]), in1=flat(pbs[1]), op=ALU.subtract),
                     reads=[B_yT[i], B_pb[1]], writes=[B_dd[i]])
                P.op("act", lambda a, i=i: a.activation(out=flat(sq[i]), in_=flat(dd[i]), func=AF.Square), reads=[B_dd[i]], writes=[B_sq[i]])
                P.op("pe", lambda t, i=i: t.matmul(flat(pbs[5]), lhsT=Gm[:], rhs=flat(sq[i]), start=True, stop=True),
                     reads=[B_G, B_sq[i]], writes=[B_pb[5]])
                P.op("act", lambda a, i=i: a.activation(out=flat(sq[i]), in_=flat(pbs[5]), func=AF.Sqrt, bias=eps_t[:, 0:1], scale=1.0),
                     reads=[B_pb[5], B_eps], writes=[B_sq[i]])
                P.op("dve", lambda v, i=i: v.reciprocal(out=flat(sq[i]), in_=flat(sq[i])), reads=[B_sq[i]], writes=[B_sq[i]])
                P.op("dve", lambda v, i=i: v.tensor_tensor(out=flat(dd[i]), in0=flat(dd[i]), in1=flat(sq[i]), op=ALU.mult),
                     reads=[B_dd[i], B_sq[i]], writes=[B_dd[i]])
                P.op("act", [(lambda a, c=c, i=i: a.activation(out=coT[i][:, c, :], in_=dd[i][:, c, :], func=AF.Silu,
                                                               bias=cols[:, 16 + c:17 + c], scale=cols[:, 12 + c:13 + c])) for c in range(4)],
                     reads=[B_dd[i], B_cols], writes=[B_coT[i]])
                gv3 = gv[i][:].rearrange("p (g d) -> p g d", g=8)
                sq3 = sq2[i][:].rearrange("p (g d) -> p g d", g=8)
                S = st[i]; BS = B_st[i]
                P.op("dve", lambda v, gv3=gv3, S=S: v.tensor_reduce(out=S[:, 24:32], in_=gv3, axis=AX.X, op=ALU.add), reads=[B_gv[i]], writes=[BS])
                P.op("dve", lambda v, S=S: v.tensor_scalar(out=S[:, 24:32], in0=S[:, 24:32], scalar1=1.0 / 64.0, scalar2=None, op0=ALU.mult),
                     reads=[BS], writes=[BS])
                P.op("dve", lambda v, gv3=gv3, S=S: v.tensor_tensor(out=gv3, in0=gv3, in1=S[:, 24:32].unsqueeze(2).to_broadcast([128, 8, 64]),
                                                                     op=ALU.subtract), reads=[B_gv[i], BS], writes=[B_gv[i]])
                P.op("act", lambda a, i=i: a.activation(out=sq2[i][:], in_=gv[i][:], func=AF.Square), reads=[B_gv[i]], writes=[B_sq2[i]])
                P.op("dve", lambda v, sq3=sq3, S=S: v.tensor_reduce(out=S[:, 32:40], in_=sq3, axis=AX.X, op=ALU.add), reads=[B_sq2[i]], writes=[BS])
                P.op("act", lambda a, S=S: a.activation(out=S[:, 40:48], in_=S[:, 32:40], func=AF.Sqrt, bias=eps_t[:, 0:1], scale=1.0 / 64.0),
                     reads=[BS, B_eps], writes=[BS])
                P.op("dve", lambda v, S=S: v.reciprocal(out=S[:, 48:56], in_=S[:, 40:48]), reads=[BS], writes=[BS])
                P.op("dve", lambda v, gv3=gv3, S=S: v.tensor_tensor(out=gv3, in0=gv3, in1=S[:, 48:56].unsqueeze(2).to_broadcast([128, 8, 64]),
                                                                     op=ALU.mult), reads=[B_gv[i], BS], writes=[B_gv[i]])
                P.op("pool", lambda g, i=i: g.tensor_tensor(out=gv[i][:], in0=gv[i][:], in1=sglg[:], op=ALU.mult), reads=[B_gv[i], B_bc], writes=[B_gv[i]])
                P.op("pool", lambda g, i=i: g.tensor_tensor(out=vbf[i][:], in0=gv[i][:], in1=sglb[:], op=ALU.add), reads=[B_gv[i], B_bc], writes=[B_vbf[i]])
                P.op("pe", [(lambda t, h=h, i=i: t.matmul(flat(pbs[4])[:, h * 64:(h + 1) * 64], lhsT=wmT[:, h, :], rhs=vbf[i][:, h * 64:(h + 1) * 64],
                                                          start=True, stop=True)) for h in range(8)],
                     reads=[B_wmT, B_vbf[i]], writes=[B_pb[4]])
                f3 = flat(pbs[4]).rearrange("p (g d) -> p g d", g=8)
                P.op("dve", lambda v, f3=f3, i=i: v.tensor_tensor(out=sq2[i][:].rearrange("p (g d) -> p g d", g=8), in0=f3,
                                                                  in1=cols[:, 20:28].unsqueeze(2).to_broadcast([128, 8, 64]), op=ALU.add),
                     reads=[B_pb[4], B_cols], writes=[B_sq2[i]])
                P.op("dve", lambda v, i=i: v.tensor_tensor(out=sgo[i][:], in0=sq2[i][:], in1=uu[i][:], op=ALU.mult),
                     reads=[B_sq2[i], B_uu[i]], writes=[B_sgo[i]])
                P.op("pe", [(lambda t, c=c, i=i: t.transpose(out=ptb[:, c, :], in_=sgo[i][:, c * 128:(c + 1) * 128], identity=ident[:]))
                            for c in range(4)], reads=[B_sgo[i], B_ident], writes=[B_ptb])
                P.op("act", lambda a, i=i: a.activation(out=sgoT[i][:], in_=ptb[:, 0:4, :], func=AF.Copy), reads=[B_ptb], writes=[B_sgoT[i]])
                for n in range(2):
                    fl = [(lambda t, n=n: t.matmul(flat(pbs[2 + n]), lhsT=ones_row[:, :], rhs=brow[:, 1024 + n * 512:1024 + (n + 1) * 512],
                                                   start=True, stop=False))]
                    fl += [(lambda t, k=k, n=n, i=i: t.matmul(flat(pbs[2 + n]), lhsT=(coT[i][:, k, :] if k < 4 else sgoT[i][:, k - 4, :]),
                                                              rhs=w_o_bf[:, k, n * 512:(n + 1) * 512], start=False, stop=(k == 7)))
                           for k in range(8)]
                    P.op("pe", fl, reads=[B_w_o, B_coT[i], B_sgoT[i], B_ones, B_brow], writes=[B_pb[2 + n]])
                P.op("dve", [(lambda v, n=n, i=i, X=X: v.scalar_tensor_tensor(out=r1[i][:, n * 512:(n + 1) * 512], in0=X[:, n * 512:(n + 1) * 512],
                                                                               scalar=ALPHA, in1=flat(pbs[2 + n]), op0=ALU.mult, op1=ALU.add))
                             for n in range(2)], reads=[BX, B_pb[2], B_pb[3]], writes=[B_r1[i]])
                R = r1[i]; BR = B_r1[i]
                layer_norm(R, BR, R, BR, g1, b1, st[i], B_st[i], 0)
                P.op("pool", lambda g, R=R: g.tensor_tensor(out=R[:], in0=R[:], in1=g1[:], op=ALU.mult), reads=[BR, B_bc], writes=[BR])
                P.op("pool", lambda g, R=R: g.tensor_tensor(out=R[:], in0=R[:], in1=b1[:], op=ALU.add), reads=[BR, B_bc], writes=[BR])
                dst = out_d if debug_h1 else h1_d
                P.dma("sp", (lambda q, R=R, r0=r0, dst=dst: q.dma_start(out=dst[r0:r0 + 128, :], in_=R[:])), BR, reads=[BR], writes=[h1d_bufs[b]])

            P.barrier()
            with nc.Block() as blk:
                P.emit(blk)

        if debug_h1:
            return nc

        with ExitStack() as s2:
            def sbt(name, shape, dt):
                return s2.enter_context(nc.sbuf_tensor(name, list(shape), dt))

            def pst(name, shape, dt):
                return s2.enter_context(nc.psum_tensor(name, list(shape), dt))

            def flat(t):
                return t[:].rearrange("p c t -> p (c t)")

            wq_bf = sbt("wq_bf", [128, 8, 2048], BF16); B_wq = Buf("wq")
            wg_bf = sbt("wg_bf", [128, 8, 1024], BF16); B_wg = Buf("wg")
            wp_bf = sbt("wp_bf", [128, 2, 1024], BF16); B_wp = Buf("wp")
            keys_f = sbt("keys_f", [128, 16, 128], BF16); B_keysf = Buf("keysf")
            keysT = sbt("keysT", [128, 16, 128], BF16); B_keysT = Buf("keysT")
            g2 = sbt("g2", [128, D], F32); b2 = sbt("b2", [128, D], F32); B_bc = Buf("bc2")
            identf = sbt("identf2", [128, 128], F32); ident = sbt("ident2", [128, 128], BF16); B_ident = Buf("ident2")
            brow = sbt("brow2", [1, 1024], BF16); B_brow = Buf("brow2")
            ones_row = sbt("ones_row2", [1, 128], BF16); B_ones = Buf("ones2")
            eps_t = sbt("eps_t2", [128, 1], F32); B_eps = Buf("eps2")
            iota16 = sbt("iota16", [128, 16], F32); B_iota = Buf("iota")

            ptb = pst("ptb2", [128, 8, 128], BF16); B_ptb = Buf("ptb2")
            pq = [pst("pq%d" % i, [128, 4, 128], F32) for i in range(2)]; B_pq = [Buf("pq%d" % i) for i in range(2)]
            psc = [pst("psc%d" % i, [128, 4, 128], F32) for i in range(2)]; B_psc = [Buf("psc%d" % i) for i in range(2)]
            ppl = pst("ppl", [128, 4, 128], F32); B_ppl = Buf("ppl")
            py = [pst("py%d" % i, [128, 4, 128], F32) for i in range(2)]; B_py = Buf("py")

            for k2 in range(2):
                for kk in range(8):
                    P.dma("pool", (lambda g, kk=kk, k2=k2: g.dma_start(
                        out=wq_bf[:, kk, k2 * 1024:(k2 + 1) * 1024],
                        in_=peer_wq[kk * 128:(kk + 1) * 128, k2 * 1024:(k2 + 1) * 1024])), B_wq, writes=[B_wq])
            for kk in range(8):
                P.dma("pool", (lambda g, kk=kk: g.dma_start(out=wg_bf[:, kk, :], in_=ple_wg[kk * 128:(kk + 1) * 128, :])), B_wg, writes=[B_wg])
            for kk in range(2):
                P.dma("pool", (lambda g, kk=kk: g.dma_start(out=wp_bf[:, kk, :], in_=ple_wp[kk * 128:(kk + 1) * 128, :])), B_wp, writes=[B_wp])
            P.dma("pool", lambda g: g.dma_start(out=keys_f[:], in_=peer_keys.rearrange("h k d -> k h d")), B_keysf, writes=[B_keysf])
            P.dma("pool", lambda g: g.dma_start(out=brow[:, :], in_=ple_bg.unsqueeze(0)), B_brow, writes=[B_brow])
            for (t_, v_) in ((g2, ln2_g), (b2, ln2_b)):
                P.dma("sp", (lambda q, t_=t_, v_=v_: q.dma_start(out=t_[:], in_=v_.partition_broadcast(128))), B_bc, writes=[B_bc])
            P.op("pool", lambda g: g.memset(identf[:], 0.0), writes=[B_ident])
            P.op("pool", lambda g: g.affine_select(out=identf[:], in_=identf[:], pattern=[[-1, 128]],
                                                   compare_op=ALU.not_equal, fill=1.0, base=0, channel_multiplier=1),
                 reads=[B_ident], writes=[B_ident])
            P.op("pool", lambda g: g.iota(iota16[:], pattern=[[1, 16]], base=0, channel_multiplier=0, allow_small_or_imprecise_dtypes=True),
                 writes=[B_iota])
            P.op("dve", lambda v: v.tensor_copy(out=ident[:], in_=identf[:]), reads=[B_ident], writes=[B_ident])
            P.op("dve", lambda v: v.memset(ones_row[:], 1.0), writes=[B_ones])
            P.op("dve", lambda v: v.memset(eps_t[:], EPS), writes=[B_eps])
            for half in range(2):
                P.op("pe", [(lambda t, q=q, half=half: t.transpose(out=ptb[:, q, :], in_=keys_f[:, half * 8 + q, :], identity=ident[:]))
                            for q in range(8)], reads=[B_keysf, B_ident], writes=[B_ptb])
                P.op("dve", lambda v, half=half: v.tensor_copy(out=keysT[:, half * 8:(half + 1) * 8, :], in_=ptb[:]),
                     reads=[B_ptb], writes=[B_keysT])

            UV = [sbt("UV%d" % s, [128, 2 * D], BF16) for s in range(NRING)]; B_UV = [Buf("UV%d" % s) for s in range(NRING)]
            dring = [sbt("dg%d" % s, [128, 128], BF16) for s in range(NDIAG)]; B_dg = [Buf("dg%d" % s) for s in range(NDIAG)]

            def dbl(name, shape, dt):
                return [sbt("%s%d" % (name, i), shape, dt) for i in range(2)], [Buf("%s%d" % (name, i)) for i in range(2)]
            h1, B_h1 = dbl("h1_", [128, D], F32)
            rp, B_rp = dbl("rp", [128, D], F32)
            idx, B_idx = dbl("idx", [128, 128], U32)
            gsm, B_gsm = dbl("gsm", [128, 128], F32)
            pld, B_pld = dbl("pld", [128, 256], F32)
            h1bf = sbt("h1bf", [128, D], BF16); B_h1bf = Buf("h1bf")
            h1T = sbt("h1T", [128, 8, 128], BF16); B_h1T = Buf("h1T")
            pbf = sbt("pbf", [128, 256], BF16); B_pbf = Buf("pbf")
            pT = sbt("pT", [128, 2, 128], BF16); B_pT = Buf("pT")
            qT = sbt("qT", [128, 16, 128], BF16); B_qT = [Buf("qT%d" % i) for i in range(4)]
            scs = sbt("scs", [128, 16, 128], F32); B_scs = [Buf("scs%d" % i) for i in range(4)]
            wk = sbt("wk", [128, 8, 128], F32); B_wk = Buf("wk")
            m8 = sbt("m8", [128, 16, 16], F32); B_m8 = Buf("m8")
            i8 = sbt("i8", [128, 16, 16], U32); B_i8 = Buf("i8")
            i8f = sbt("i8f", [128, 16, 16], F32); B_i8f = Buf("i8f")
            comb = sbt("comb", [128, 4, 256], F32); B_comb = Buf("comb")
            wk2 = sbt("wk2", [128, 4, 256], F32); B_wk2 = Buf("wk2")
            c16 = sbt("c16", [128, 8, 16], F32); B_c16 = Buf("c16")
            pos = sbt("pos", [128, 8, 16], U32); B_pos = Buf("pos")
            apos = sbt("apos", [128, 8, 16], U32); bpos = sbt("bpos", [128, 8, 16], U32)
            aposf = sbt("aposf", [128, 8, 16], F32); bposf = sbt("bposf", [128, 8, 16], F32); B_ab = Buf("ab")
            oh = sbt("oh", [128, 4, 16, 16], F32); B_oh = Buf("oh")
            asel = sbt("asel", [128, 8, 16], F32); bsel = sbt("bsel", [128, 8, 16], F32); B_sel = Buf("sel")
            idxf = sbt("idxf", [128, 128], F32); B_idxf = Buf("idxf")
            sm = sbt("sm", [128, 32], F32); B_sm = Buf("sm")
            gate = sbt("gate", [128, 512], F32); B_gate = Buf("gate")
            junk = sbt("junk", [128, D], BF16); B_junk = Buf("junk")
            actt = sbt("actt", [128, 128], F32); B_actg = [Buf("actg%d" % g) for g in range(128 // GRP)]
            gel = sbt("gel", [128, 128], F32); B_gelg = [Buf("gelg%d" % g) for g in range(128 // GRP)]
            wgt = sbt("wgt", [128, 128], F32); B_wgtg = [Buf("wgtg%d" % g) for g in range(128 // GRP)]
            st2 = sbt("st2", [128, 32], F32); B_st2 = Buf("st2")

            NG = 128 // GRP
            ring_ctr = [0]

            def front(b):
                steps = []

                def op(*a_, **k_):
                    steps.append(lambda: P.op(*a_, **k_))

                def dma(*a_, **k_):
                    steps.append(lambda: P.dma(*a_, **k_))
                i = b % 2
                r0 = b * 128
                H = h1[i]; BH = B_h1[i]
                dma("sp", (lambda q, H=H, r0=r0: q.dma_start(out=H[:], in_=h1_d[r0:r0 + 128, :])), BH, reads=[h1d_bufs[b]], writes=[BH])
                dma("sp", (lambda q, i=i, r0=r0: q.dma_start(out=pld[i][:], in_=p_d[r0:r0 + 128, :])), B_pld[i], writes=[B_pld[i]])
                op("act", lambda a, H=H: a.activation(out=h1bf[:], in_=H[:], func=AF.Copy), reads=[BH], writes=[B_h1bf])
                op("act", lambda a, i=i: a.activation(out=pbf[:], in_=pld[i][:], func=AF.Copy), reads=[B_pld[i]], writes=[B_pbf])
                op("pe", [(lambda t, k=k: t.transpose(out=ptb[:, k, :], in_=h1bf[:, k * 128:(k + 1) * 128], identity=ident[:]))
                            for k in range(8)], reads=[B_h1bf, B_ident], writes=[B_ptb])
                op("act", lambda a: a.activation(out=h1T[:], in_=ptb[:], func=AF.Copy), reads=[B_ptb], writes=[B_h1T])
                op("pe", [(lambda t, k=k: t.transpose(out=ptb[:, k, :], in_=pbf[:, k * 128:(k + 1) * 128], identity=ident[:]))
                            for k in range(2)], reads=[B_pbf, B_ident], writes=[B_ptb])
                op("act", lambda a: a.activation(out=pT[:], in_=ptb[:, 0:2, :], func=AF.Copy), reads=[B_ptb], writes=[B_pT])
                for qd in range(4):
                    pb_ = pq[qd % 2]; Bp = B_pq[qd % 2]
                    op("pe", [(lambda t, c=c, k=k, qd=qd, pb_=pb_: t.matmul(
                        pb_[:, c, :], lhsT=wq_bf[:, k, (qd * 4 + c) * 128:(qd * 4 + c + 1) * 128], rhs=h1T[:, k, :],
                        start=(k == 0), stop=(k == 7))) for c in range(4) for k in range(8)],
                        reads=[B_wq, B_h1T], writes=[Bp])
                    op("act", lambda a, qd=qd, pb_=pb_: a.activation(out=qT[:, qd * 4:(qd + 1) * 4, :], in_=pb_[:], func=AF.Copy),
                         reads=[Bp], writes=[B_qT[qd]])
                    ps_ = psc[qd % 2]; Bs = B_psc[qd % 2]
                    op("pe", [(lambda t, c=c, qd=qd, ps_=ps_: t.matmul(ps_[:, c, :], lhsT=qT[:, qd * 4 + c, :], rhs=keysT[:, qd * 4 + c, :],
                                                                          start=True, stop=True)) for c in range(4)],
                         reads=[B_qT[qd], B_keysT], writes=[Bs])
                    op("act", lambda a, qd=qd, ps_=ps_: a.activation(out=scs[:, qd * 4:(qd + 1) * 4, :], in_=ps_[:], func=AF.Copy),
                         reads=[Bs], writes=[B_scs[qd]])
                for n in range(2):
                    fl = [(lambda t, n=n: t.matmul(flat(ppl), lhsT=ones_row[:, :], rhs=brow[:, n * 512:(n + 1) * 512], start=True, stop=False))]
                    fl += [(lambda t, k=k, n=n: t.matmul(flat(ppl), lhsT=h1T[:, k, :], rhs=wg_bf[:, k, n * 512:(n + 1) * 512],
                                                          start=False, stop=(k == 7))) for k in range(8)]
                    op("pe", fl, reads=[B_wg, B_h1T, B_ones, B_brow], writes=[B_ppl])
                    op("act", lambda a: a.activation(out=gate[:], in_=flat(ppl), func=AF.Sigmoid), reads=[B_ppl], writes=[B_gate])
                    op("pe", [(lambda t, k=k, n=n: t.matmul(flat(ppl), lhsT=pT[:, k, :], rhs=wp_bf[:, k, n * 512:(n + 1) * 512],
                                                               start=(k == 0), stop=(k == 1))) for k in range(2)],
                         reads=[B_wp, B_pT], writes=[B_ppl])
                    op("dve", lambda v: v.tensor_tensor(out=gate[:], in0=gate[:], in1=flat(ppl), op=ALU.mult),
                         reads=[B_gate, B_ppl], writes=[B_gate])
                    op("dve", lambda v, n=n, i=i, H=H: v.scalar_tensor_tensor(out=rp[i][:, n * 512:(n + 1) * 512], in0=H[:, n * 512:(n + 1) * 512],
                                                                               scalar=ALPHA, in1=gate[:], op0=ALU.mult, op1=ALU.add),
                         reads=[BH, B_gate], writes=[B_rp[i]])
                op("dve", [(lambda v, hh=hh: v.max(out=m8[:, hh, 0:8], in_=scs[:, hh, :])) for hh in range(16)], reads=B_scs, writes=[B_m8])
                for hv in range(2):
                    op("dve", [(lambda v, hh=hh, hv=hv: v.match_replace(out=wk[:, hh - 8 * hv, :], in_to_replace=m8[:, hh, 0:8],
                                                                          in_values=scs[:, hh, :], imm_value=NEG))
                                 for hh in range(8 * hv, 8 * hv + 8)], reads=B_scs + [B_m8], writes=[B_wk])
                    op("dve", [(lambda v, hh=hh, hv=hv: v.max(out=m8[:, hh, 8:16], in_=wk[:, hh - 8 * hv, :]))
                                 for hh in range(8 * hv, 8 * hv + 8)], reads=[B_wk], writes=[B_m8])
                op("dve", [(lambda v, hh=hh, o=o: v.max_index(out=i8[:, hh, o:o + 8], in_max=m8[:, hh, o:o + 8], in_values=scs[:, hh, :]))
                             for hh in range(16) for o in (0, 8)], reads=B_scs + [B_m8], writes=[B_i8])
                op("dve", lambda v: v.tensor_copy(out=i8f[:], in_=i8[:]), reads=[B_i8], writes=[B_i8f])
                m84 = m8[:].rearrange("p (h t) k -> p h t k", t=2)
                i84 = i8f[:].rearrange("p (h t) k -> p h t k", t=2)
                for hf in range(2):
                    hs = slice(hf * 4, hf * 4 + 4)
                    comb4 = comb[:].rearrange("p h (a c) -> p h a c", a=16)
                    op("dve", lambda v, hs=hs, comb4=comb4: v.tensor_tensor(
                        out=comb4, in0=m84[:, hs, 0, :].unsqueeze(3).to_broadcast([128, 4, 16, 16]),
                        in1=m84[:, hs, 1, :].unsqueeze(2).to_broadcast([128, 4, 16, 16]), op=ALU.add),
                        reads=[B_m8], writes=[B_comb])
                    op("dve", [(lambda v, h=h, hf=hf: v.max(out=c16[:, hf * 4 + h, 0:8], in_=comb[:, h, :])) for h in range(4)],
                         reads=[B_comb], writes=[B_c16])
                    op("dve", [(lambda v, h=h, hf=hf: v.match_replace(out=wk2[:, h, :], in_to_replace=c16[:, hf * 4 + h, 0:8],
                                                                         in_values=comb[:, h, :], imm_value=NEG)) for h in range(4)],
                         reads=[B_comb, B_c16], writes=[B_wk2])
                    op("dve", [(lambda v, h=h, hf=hf: v.max(out=c16[:, hf * 4 + h, 8:16], in_=wk2[:, h, :])) for h in range(4)],
                         reads=[B_wk2], writes=[B_c16])
                    op("dve", [(lambda v, h=h, hf=hf, o=o: v.max_index(out=pos[:, hf * 4 + h, o:o + 8], in_max=c16[:, hf * 4 + h, o:o + 8],
                                                                          in_values=comb[:, h, :])) for h in range(4) for o in (0, 8)],
                         reads=[B_comb, B_c16], writes=[B_pos])
                op("dve", [lambda v: v.tensor_single_scalar(out=apos[:], in_=pos[:], scalar=4, op=ALU.logical_shift_right),
                             lambda v: v.tensor_single_scalar(out=bpos[:], in_=pos[:], scalar=15, op=ALU.bitwise_and)],
                     reads=[B_pos], writes=[B_ab])
                op("dve", [lambda v: v.tensor_copy(out=aposf[:], in_=apos[:]),
                             lambda v: v.tensor_copy(out=bposf[:], in_=bpos[:])], reads=[B_ab], writes=[B_ab])
                io4 = iota16[:].unsqueeze(1).unsqueeze(1).to_broadcast([128, 4, 16, 16])
                for (pf, tsel, sel) in ((aposf, 0, asel), (bposf, 1, bsel)):
                    for hf in range(2):
                        hs = slice(hf * 4, hf * 4 + 4)
                        op("dve", lambda v, pf=pf, hs=hs: v.tensor_tensor(out=oh[:], in0=pf[:, hs, :].unsqueeze(3).to_broadcast([128, 4, 16, 16]),
                                                                            in1=io4, op=ALU.is_equal), reads=[B_ab, B_iota], writes=[B_oh])
                        op("dve", lambda v, tsel=tsel, hs=hs: v.tensor_tensor(out=oh[:], in0=oh[:],
                                                                                in1=i84[:, hs, tsel, :].unsqueeze(2).to_broadcast([128, 4, 16, 16]),
                                                                                op=ALU.mult), reads=[B_oh, B_i8f], writes=[B_oh])
                        op("dve", lambda v, sel=sel, hs=hs: v.tensor_reduce(out=sel[:, hs, :], in_=oh[:], axis=AX.X, op=ALU.add),
                             reads=[B_oh], writes=[B_sel])
                op("dve", lambda v: v.scalar_tensor_tensor(out=idxf[:], in0=asel[:].rearrange("p h k -> p (h k)"), scalar=128.0,
                                                             in1=bsel[:].rearrange("p h k -> p (h k)"), op0=ALU.mult, op1=ALU.add),
                     reads=[B_sel], writes=[B_idxf])
                op("dve", lambda v, i=i: v.tensor_copy(out=idx[i][:], in_=idxf[:]), reads=[B_idxf], writes=[B_idx[i]])
                op("dve", lambda v: v.tensor_scalar(out=sm[:, 0:8], in0=c16[:, :, 0], scalar1=-1.0, scalar2=None, op0=ALU.mult),
                     reads=[B_c16], writes=[B_sm])
                G3 = gsm[i][:].rearrange("p (h k) -> p h k", h=8)
                op("act", [(lambda a, h=h, G3=G3: a.activation(out=G3[:, h, :], in_=c16[:, h, :], func=AF.Exp, bias=sm[:, h:h + 1], scale=1.0,
                                                                  accum_out=sm[:, 8 + h:9 + h])) for h in range(8)],
                     reads=[B_c16, B_sm], writes=[B_gsm[i], B_sm])
                op("dve", lambda v: v.reciprocal(out=sm[:, 16:24], in_=sm[:, 8:16]), reads=[B_sm], writes=[B_sm])
                op("dve", lambda v, G3=G3: v.tensor_tensor(out=G3, in0=G3, in1=sm[:, 16:24].unsqueeze(2).to_broadcast([128, 8, 16]), op=ALU.mult),
                     reads=[B_gsm[i], B_sm], writes=[B_gsm[i]])

                def gen():
                    for st_ in steps:
                        st_()
                        yield
                return gen()

            nuse = nblk * 128

            def issue_gather(n):
                if n >= nuse:
                    return
                b_, jj = divmod(n, 128)
                i_ = b_ % 2
                s_ = n % NRING
                P.dma("pool", (lambda q, s_=s_, jj=jj, i_=i_: q.indirect_dma_start(
                    out=UV[s_][:], out_offset=None, in_=uv_d,
                    in_offset=bass.IndirectOffsetOnAxis(ap=idx[i_][:, jj:jj + 1], axis=0))),
                    B_UV[s_], reads=[B_idx[i_]], writes=[B_UV[s_]])

            def wmul(b, g):
                i = b % 2
                gs = slice(g * GRP, (g + 1) * GRP)
                P.op("dve", lambda v, gs=gs, i=i: v.tensor_tensor(out=wgt[:, gs], in0=gel[:, gs], in1=gsm[i][:, gs], op=ALU.mult),
                     reads=[B_gelg[g], B_gsm[i]], writes=[B_wgtg[g]])

            def vside(b, g):
                for e in range(GRP):
                    jj = g * GRP + e
                    s = (b * 128 + jj) % NRING
                    ds = (b * 128 + jj) % NDIAG
                    P.op("act", lambda a, ds=ds, jj=jj: a.activation(out=dring[ds][:], in_=identf[:], func=AF.Copy, scale=wgt[:, jj:jj + 1]),
                         reads=[B_wgtg[g], B_ident], writes=[B_dg[ds]])
                    P.op("pe", [(lambda t, n=n, ds=ds, s=s, jj=jj: t.matmul(flat(py[n]), lhsT=dring[ds][:], rhs=UV[s][:, D + n * 512:D + (n + 1) * 512],
                                                                             start=(jj == 0), stop=(jj == 127))) for n in range(2)],
                         reads=[B_dg[ds], B_UV[s]], writes=[B_py])
                    issue_gather(b * 128 + jj + NRING)

            def tail(b):
                i = b % 2
                r0 = b * 128
                r2 = rp[i]; B_r2 = B_rp[i]
                P.op("dve", [(lambda v, n=n, r2=r2: v.tensor_tensor(out=r2[:, n * 512:(n + 1) * 512], in0=r2[:, n * 512:(n + 1) * 512],
                                                                    in1=flat(py[n]), op=ALU.add)) for n in range(2)],
                     reads=[B_r2, B_py], writes=[B_r2])
                S = st2
                P.op("dve", [lambda v, r2=r2: v.bn_stats(out=S[:, 0:6], in_=r2[:, 0:512]),
                             lambda v, r2=r2: v.bn_stats(out=S[:, 6:12], in_=r2[:, 512:1024])], reads=[B_r2], writes=[B_st2])
                P.op("dve", lambda v: v.bn_aggr(out=S[:, 12:14], in_=S[:, 0:12]), reads=[B_st2], writes=[B_st2])
                P.op("act", lambda a: a.activation(out=S[:, 14:15], in_=S[:, 13:14], func=AF.Sqrt, bias=eps_t[:, 0:1], scale=1.0),
                     reads=[B_st2, B_eps], writes=[B_st2])
                P.op("dve", lambda v: v.reciprocal(out=S[:, 15:16], in_=S[:, 14:15]), reads=[B_st2], writes=[B_st2])
                P.op("dve", lambda v: v.tensor_scalar(out=S[:, 16:17], in0=S[:, 12:13], scalar1=S[:, 15:16], scalar2=-1.0, op0=ALU.mult, op1=ALU.mult),
                     reads=[B_st2], writes=[B_st2])
                P.op("act", lambda a, r2=r2: a.activation(out=r2[:], in_=r2[:], func=AF.Identity, bias=S[:, 16:17], scale=S[:, 15:16]),
                     reads=[B_r2, B_st2], writes=[B_r2])
                P.op("dve", lambda v, r2=r2: v.tensor_tensor(out=r2[:], in0=r2[:], in1=g2[:], op=ALU.mult), reads=[B_r2, B_bc], writes=[B_r2])
                P.op("dve", lambda v, r2=r2: v.tensor_tensor(out=r2[:], in0=r2[:], in1=b2[:], op=ALU.add), reads=[B_r2, B_bc], writes=[B_r2])
                P.dma("sp", (lambda q, r0=r0, r2=r2: q.dma_start(out=out_d[r0:r0 + 128, :], in_=r2[:])), B_r2, reads=[B_r2])

            def back(b, fg):
                i = b % 2
                H = h1[i]; BH = B_h1[i]
                for g in range(NG):
                    gs = slice(g * GRP, (g + 1) * GRP)
                    if g == NG - 1 and fg is not None:
                        for _ in fg:
                            pass
                    for e in range(GRP):
                        jj = g * GRP + e
                        s = (b * 128 + jj) % NRING
                        P.op("dve", lambda v, s=s, jj=jj, H=H: v.scalar_tensor_tensor(out=junk[:], in0=UV[s][:, 0:D], scalar=1.0, in1=H[:],
                                                                                     op0=ALU.mult, op1=ALU.mult, accum_out=actt[:, jj:jj + 1]),
                             reads=[B_UV[s], BH], writes=[B_junk, B_actg[g]])
                        if e == 0 and g >= 1:
                            wmul(b, g - 1)
                            vside(b, g - 1)
                        if e == GRP - 1 and g == 0 and b >= 1:
                            tail(b - 1)
                        if fg is not None and g < NG - 1:
                            next(fg, None)
                    P.op("act", lambda a, gs=gs: a.activation(out=gel[:, gs], in_=actt[:, gs], func=AF.Gelu), reads=[B_actg[g]], writes=[B_gelg[g]])
                wmul(b, NG - 1)
                vside(b, NG - 1)
                if b == nblk - 1:
                    tail(b)

            for _ in front(0):
                pass
            for n_ in range(NRING):
                issue_gather(n_)
            for b in range(nblk):
                back(b, front(b + 1) if b + 1 < nblk else None)

            P.barrier()
            with nc.Block() as blk:
                P.emit(blk)
    return nc


_W_NAMES = ["ln0_g", "ln0_b", "w_in", "b_in", "conv_w", "conv_b", "gn_g", "gn_b", "sg_ln_g", "sg_ln_b", "sg_w", "sg_b",
            "w_o", "b_o", "ln1_g", "ln1_b", "peer_wq", "peer_keys", "peer_u", "peer_v", "ple_wp", "ple_wg", "ple_bg",
            "ln2_g", "ln2_b"]


def _prep_weights(inp):
    w = {}
    for k in _W_NAMES:
        a = np.asarray(inp[k], dtype=np.float32)
        if k in ("ln0_g", "ln0_b"):
            w[k] = np.ascontiguousarray(a.reshape(1024))
        elif k == "peer_keys":
            w[k] = np.ascontiguousarray(a.reshape(16, 128, 128))
        else:
            w[k] = np.ascontiguousarray(a[0])
    return w


def kernel(**inputs):
    n = 8
    x = np.asarray(inputs["x"], dtype=np.float32)
    p = np.asarray(inputs["p"], dtype=np.float32)
    w = _prep_weights(inputs)
    nc = build_program(32)
    in_maps = []
    for c in range(n):
        m = {"x": np.ascontiguousarray(x[c]), "p": np.ascontiguousarray(p[0, c])}
        m.update(w)
        in_maps.append(m)
    res = run_bass_kernel_spmd(nc, in_maps, core_ids=list(range(n)))
    return np.stack([np.asarray(r["out"], dtype=np.float32) for r in res.results], axis=0)
```

```python
import numpy as np
from contextlib import ExitStack
import concourse.bass as bass
import concourse.mybir as mybir
from concourse.bass_utils import run_bass_kernel_spmd

F32 = mybir.dt.float32
BF16 = mybir.dt.bfloat16
U32 = mybir.dt.uint32
AF = mybir.ActivationFunctionType
ALU = mybir.AluOpType
AX = mybir.AxisListType

D = 1024
SEQ = 4096
ALPHA = float(2.0 ** 0.25)
EPS = 1e-5
NEG = -1.0e30
ENGS = ["sp", "pool", "act", "dve", "pe"]
NRING = 16
GRP = 8
NDIAG = 16


class Buf:
    __slots__ = ("name", "w", "r", "dsem", "dcnt")

    def __init__(self, name):
        self.name = name
        self.w = None
        self.r = []
        self.dsem = None
        self.dcnt = 0


class Prog:
    def __init__(self, nc, es):
        self.nc = nc
        self.es = es
        self.streams = {e: [] for e in ENGS}
        self.esem = {e: es.enter_context(nc.semaphore("es_" + e)) for e in ENGS}
        self.ecnt = {e: 0 for e in ENGS}
        self.waited = {e: {} for e in ENGS}
        self.dma_toks = []
        self.nd = 0

    def _wait(self, e, tok):
        sem, val = tok
        if self.waited[e].get(sem, 0) >= val:
            return
        self.waited[e][sem] = val
        self.streams[e].append(("w", sem, val))

    def _deps(self, e, who, reads, writes):
        for b in reads:
            if b.w is not None:
                self._wait(e, b.w[1])
        for b in writes:
            if b.w is not None and not (who == "pe" and b.w[0] == "pe"):
                self._wait(e, b.w[1])
            for (re_, tok) in b.r:
                self._wait(e, tok)

    def op(self, e, fns, reads=(), writes=()):
        if callable(fns):
            fns = [fns]
        self._deps(e, e, reads, writes)
        self.ecnt[e] += 1
        tok = (self.esem[e], self.ecnt[e])
        self.streams[e].append(("i", fns, self.esem[e], 1))
        for b in reads:
            b.r.append((e, tok))
        for b in writes:
            b.w = (e, tok)
            b.r = []
        return tok

    def dma(self, e, fn, sbuf, reads=(), writes=()):
        if sbuf.dsem is None:
            self.nd += 1
            sbuf.dsem = self.es.enter_context(self.nc.semaphore("ds%d" % self.nd))
        wr = list(writes)
        if sbuf not in wr:
            wr.append(sbuf)
        rd = [b for b in reads if b is not sbuf]
        self._deps(e, "dma", rd, wr)
        sbuf.dcnt += 16
        tok = (sbuf.dsem, sbuf.dcnt)
        self.streams[e].append(("i", [fn], sbuf.dsem, 16))
        for b in rd:
            b.r.append(("dma", tok))
        for b in writes:
            b.w = ("dma", tok)
            b.r = []
        if sbuf not in writes:
            sbuf.r.append(("dma", tok))
        self.dma_toks.append(tok)
        return tok

    def dma_nowait(self, e, fn, buf):
        if buf.dsem is None:
            self.nd += 1
            buf.dsem = self.es.enter_context(self.nc.semaphore("ds%d" % self.nd))
        buf.dcnt += 16
        tok = (buf.dsem, buf.dcnt)
        self.streams[e].append(("i", [fn], buf.dsem, 16))
        self.dma_toks.append(tok)
        return tok

    def barrier(self):
        toks = [(self.esem[e], self.ecnt[e]) for e in ENGS if self.ecnt[e] > 0] + self.dma_toks
        mx = {}
        for (sem, val) in toks:
            if mx.get(sem, 0) < val:
                mx[sem] = val
        for e in ENGS:
            for sem, val in mx.items():
                self._wait(e, (sem, val))
        self.dma_toks = []

    def emit(self, block):
        def mk(en):
            items = self.streams[en]

            def f(eng):
                for it in items:
                    if it[0] == "w":
                        eng.wait_ge(it[1], it[2])
                    else:
                        ins = None
                        for fn in it[1]:
                            ins = fn(eng)
                        ins.then_inc(it[2], it[3])
            return f
        block.sync(mk("sp"))
        block.gpsimd(mk("pool"))
        block.scalar(mk("act"))
        block.vector(mk("dve"))
        block.tensor(mk("pe"))
        self.streams = {e: [] for e in ENGS}


def build_program(nblk=32, debug_h1=False):
    nc = bass.Bass("TRN2", target_bir_lowering=False)
    ntok = nblk * 128

    def din(name, shape):
        return nc.dram_tensor(name, list(shape), F32, kind="ExternalInput").ap()

    x_d = din("x", [ntok, D])
    p_d = din("p", [ntok, 256])
    ln0_g = din("ln0_g", [D]); ln0_b = din("ln0_b", [D])
    w_in = din("w_in", [D, 2048]); b_in = din("b_in", [2048])
    conv_w = din("conv_w", [31, 512]); conv_b = din("conv_b", [512])
    gn_g = din("gn_g", [512]); gn_b = din("gn_b", [512])
    sg_ln_g = din("sg_ln_g", [512]); sg_ln_b = din("sg_ln_b", [512])
    sg_w = din("sg_w", [8, 128, 128]); sg_b = din("sg_b", [8, 128])
    w_o = din("w_o", [D, D]); b_o = din("b_o", [D])
    ln1_g = din("ln1_g", [D]); ln1_b = din("ln1_b", [D])
    peer_wq = din("peer_wq", [D, 2048])
    peer_keys = din("peer_keys", [16, 128, 128])
    peer_u = din("peer_u", [16384, D]); peer_v = din("peer_v", [16384, D])
    ple_wp = din("ple_wp", [256, D]); ple_wg = din("ple_wg", [D, D]); ple_bg = din("ple_bg", [D])
    ln2_g = din("ln2_g", [D]); ln2_b = din("ln2_b", [D])
    out_d = nc.dram_tensor("out", [ntok, D], F32, kind="ExternalOutput").ap()
    h1_d = nc.dram_tensor("h1s", [ntok, D], F32, kind="Internal").ap()
    uv_d = nc.dram_tensor("uvbf", [16384, 2 * D], BF16, kind="Internal").ap()

    with ExitStack() as outer:
        P = Prog(nc, outer)
        h1d_bufs = [Buf("h1d%d" % b) for b in range(nblk)]

        with ExitStack() as s1:
            def sbt(name, shape, dt):
                return s1.enter_context(nc.sbuf_tensor(name, list(shape), dt))

            def pst(name, shape, dt):
                return s1.enter_context(nc.psum_tensor(name, list(shape), dt))

            w_in_bf = sbt("w_in_bf", [128, 8, 2048], BF16); B_w_in = [Buf("w_in_%d" % q_) for q_ in range(4)]
            w_o_bf = sbt("w_o_bf", [128, 8, 1024], BF16); B_w_o = [Buf("w_o_%d" % q_) for q_ in range(4)]
            cdiag = sbt("cdiag", [128, 124, 128], BF16); B_cdiag = Buf("cdiag")
            g0 = sbt("g0", [128, D], F32); b0 = sbt("b0", [128, D], F32)
            g1 = sbt("g1", [128, D], F32); b1 = sbt("b1", [128, D], F32)
            sglg = sbt("sglg", [128, 512], F32); sglb = sbt("sglb", [128, 512], F32)
            B_bc = Buf("bc")
            identf = sbt("identf", [128, 128], F32); ident = sbt("ident", [128, 128], BF16)
            B_ident = Buf("ident")
            Gm = sbt("Gm", [128, 128], F32); B_G = Buf("G")
            rows = sbt("rows", [28, 128], F32); B_rows = Buf("rows")
            cwrow = sbt("cwrow", [31, 512], F32); B_cwrow = Buf("cwrow")
            cols = sbt("cols", [128, 28], F32); B_cols = Buf("cols")
            cw = sbt("cw", [128, 4, 31], F32); B_cw = Buf("cw")
            brow = sbt("brow", [1, 2048], BF16); B_brow = Buf("brow")
            ones_row = sbt("ones_row", [1, 128], BF16); B_ones = Buf("ones")
            eps_t = sbt("eps_t", [128, 1], F32); B_eps = Buf("eps")
            sgw_f = sbt("sgw_f", [128, 8, 128], F32); B_sgwf = Buf("sgwf")
            sgw_m = sbt("sgw_m", [128, 8, 128], BF16); B_sgwm = Buf("sgwm")
            wmT = sbt("wmT", [128, 8, 128], BF16); B_wmT = Buf("wmT")

            ptb = pst("ptb", [128, 8, 128], BF16); B_ptb = Buf("ptb")
            pbs = [pst("pb%d" % i, [128, 4, 128], F32) for i in range(7)]
            B_pb = [Buf("pb%d" % i) for i in range(7)]

            def flat(t):
                return t[:].rearrange("p c t -> p (c t)")

            for k2 in range(2):
                for kk in range(8):
                    P.dma("pool", (lambda g, kk=kk, k2=k2: g.dma_start(
                        out=w_in_bf[:, kk, k2 * 1024:(k2 + 1) * 1024],
                        in_=w_in[kk * 128:(kk + 1) * 128, k2 * 1024:(k2 + 1) * 1024])), B_w_in[kk % 4], writes=[B_w_in[kk % 4]])
            for kk in range(8):
                P.dma("pool", (lambda g, kk=kk: g.dma_start(
                    out=w_o_bf[:, kk, :], in_=w_o[kk * 128:(kk + 1) * 128, :])), B_w_o[kk % 4], writes=[B_w_o[kk % 4]])
            P.dma("pool", lambda g: g.dma_start(out=brow[:, 0:1024], in_=b_in[1024:2048].unsqueeze(0)), B_brow, writes=[B_brow])
            P.dma("pool", lambda g: g.dma_start(out=brow[:, 1024:2048], in_=b_o.unsqueeze(0)), B_brow, writes=[B_brow])
            for (t_, v_) in ((g0, ln0_g), (b0, ln0_b), (g1, ln1_g), (b1, ln1_b), (sglg, sg_ln_g), (sglb, sg_ln_b)):
                P.dma("sp", (lambda q, t_=t_, v_=v_: q.dma_start(out=t_[:], in_=v_.partition_broadcast(128))), B_bc, writes=[B_bc])
            P.dma("sp", lambda q: q.dma_start(out=rows[0:8, :], in_=b_in[0:1024].rearrange("(c p) -> c p", p=128)), B_rows, writes=[B_rows])
            P.dma("sp", lambda q: q.dma_start(out=rows[8:12, :], in_=conv_b.rearrange("(c p) -> c p", p=128)), B_rows, writes=[B_rows])
            P.dma("sp", lambda q: q.dma_start(out=rows[12:16, :], in_=gn_g.rearrange("(c p) -> c p", p=128)), B_rows, writes=[B_rows])
            P.dma("sp", lambda q: q.dma_start(out=rows[16:20, :], in_=gn_b.rearrange("(c p) -> c p", p=128)), B_rows, writes=[B_rows])
            P.dma("sp", lambda q: q.dma_start(out=rows[20:28, :], in_=sg_b), B_rows, writes=[B_rows])
            P.dma("sp", lambda q: q.dma_start(out=cwrow[:], in_=conv_w), B_cwrow, writes=[B_cwrow])
            P.dma("sp", lambda q: q.dma_start(out=sgw_f[:], in_=sg_w.rearrange("h t s -> t h s")), B_sgwf, writes=[B_sgwf])

            P.op("pool", lambda g: g.memset(identf[:], 0.0), writes=[B_ident])
            P.op("pool", lambda g: g.affine_select(out=identf[:], in_=identf[:], pattern=[[-1, 128]],
                                                   compare_op=ALU.not_equal, fill=1.0, base=0, channel_multiplier=1),
                 reads=[B_ident], writes=[B_ident])
            P.op("dve", lambda v: v.tensor_copy(out=ident[:], in_=identf[:]), reads=[B_ident], writes=[B_ident])
            P.op("dve", lambda v: v.memset(Gm[:], 0.0), writes=[B_G])
            P.op("dve", lambda v: v.memset(Gm[0:64, 0:64], 1.0 / 64.0), writes=[B_G])
            P.op("dve", lambda v: v.memset(Gm[64:128, 64:128], 1.0 / 64.0), writes=[B_G])
            P.op("dve", lambda v: v.memset(ones_row[:], 1.0), writes=[B_ones])
            P.op("dve", lambda v: v.memset(eps_t[:], EPS), writes=[B_eps])
            P.op("pe", lambda t: t.transpose(out=pbs[0][:, 0, 0:28], in_=rows[:, :], identity=identf[0:28, 0:28]),
                 reads=[B_rows, B_ident], writes=[B_pb[0]])
            P.op("dve", lambda v: v.tensor_copy(out=cols[:], in_=pbs[0][:, 0, 0:28]), reads=[B_pb[0]], writes=[B_cols])
            P.op("pe", [(lambda t, c=c: t.transpose(out=pbs[1][:, c, 0:31], in_=cwrow[:, c * 128:(c + 1) * 128],
                                                      identity=identf[0:31, 0:31])) for c in range(4)],
                 reads=[B_cwrow, B_ident], writes=[B_pb[1]])
            P.op("dve", lambda v: v.tensor_copy(out=cw[:], in_=pbs[1][:, :, 0:31]), reads=[B_pb[1]], writes=[B_cw])
            for c in range(4):
                P.op("dve", (lambda v, c=c: v.tensor_tensor(
                    out=cdiag[:, c * 31:(c + 1) * 31, :],
                    in0=identf[:].unsqueeze(1).to_broadcast([128, 31, 128]),
                    in1=cw[:, c, :].unsqueeze(2).to_broadcast([128, 31, 128]), op=ALU.mult)),
                    reads=[B_ident, B_cw], writes=[B_cdiag])
            P.op("pool", lambda g: g.affine_select(out=sgw_m[:], in_=sgw_f[:], pattern=[[0, 8], [-1, 128]],
                                                   compare_op=ALU.is_ge, fill=0.0, base=0, channel_multiplier=1),
                 reads=[B_sgwf], writes=[B_sgwm])
            P.op("pe", [(lambda t, h=h: t.transpose(out=ptb[:, h, :], in_=sgw_m[:, h, :], identity=ident[:])) for h in range(8)],
                 reads=[B_sgwm, B_ident], writes=[B_ptb])
            P.op("dve", lambda v: v.tensor_copy(out=wmT[:], in_=ptb[:]), reads=[B_ptb], writes=[B_wmT])

            def dbl(name, shape, dt):
                return [sbt("%s%d" % (name, i), shape, dt) for i in range(2)], [Buf("%s%d" % (name, i)) for i in range(2)]
            xb, B_xb = dbl("xb", [128, D], F32)
            h0bf, B_h0bf = dbl("h0bf", [128, D], BF16)
            h0T, B_h0T = dbl("h0T", [128, 8, 128], BF16)
            sig, B_sig = dbl("sig", [128, 4, 128], F32)
            cT, B_cT = dbl("cT", [128, 4, 158], BF16)
            yT, B_yT = dbl("yT", [128, 4, 128], F32)
            dd, B_dd = dbl("dd", [128, 4, 128], F32)
            sq, B_sq = dbl("sq", [128, 4, 128], F32)
            coT, B_coT = dbl("coT", [128, 4, 128], BF16)
            uu, B_uu = dbl("uu", [128, 512], F32)
            gv, B_gv = dbl("gv", [128, 512], F32)
            sq2, B_sq2 = dbl("sq2", [128, 512], F32)
            vbf, B_vbf = dbl("vbf", [128, 512], BF16)
            sgo, B_sgo = dbl("sgo", [128, 512], BF16)
            sgoT, B_sgoT = dbl("sgoT", [128, 4, 128], BF16)
            r1, B_r1 = dbl("r1", [128, D], F32)
            st, B_st = dbl("st", [128, 64], F32)

            P.op("dve", lambda v: v.memset(cT[0][:, :, 0:30], 0.0), writes=[B_cT[0]])

            def mk_ops(steps):
                def op(*a_, **k_):
                    steps.append(lambda: P.op(*a_, **k_))

                def dma(*a_, **k_):
                    steps.append(lambda: P.dma(*a_, **k_))
                return op, dma

            def layer_norm(op, src, Bsrc, dst, Bdst, stt, Bstt, base):
                s_stats = stt[:, base:base + 12]
                s_mv = stt[:, base + 12:base + 14]
                s_sd = stt[:, base + 14:base + 15]
                s_rs = stt[:, base + 15:base + 16]
                s_nm = stt[:, base + 16:base + 17]
                op("dve", [lambda v: v.bn_stats(out=stt[:, base:base + 6], in_=src[:, 0:512]),
                           lambda v: v.bn_stats(out=stt[:, base + 6:base + 12], in_=src[:, 512:1024])],
                   reads=[Bsrc], writes=[Bstt])
                op("dve", lambda v: v.bn_aggr(out=s_mv, in_=s_stats), reads=[Bstt], writes=[Bstt])
                op("act", lambda a: a.activation(out=s_sd, in_=stt[:, base + 13:base + 14], func=AF.Sqrt,
                                                 bias=eps_t[:, 0:1], scale=1.0), reads=[Bstt, B_eps], writes=[Bstt])
                op("dve", lambda v: v.reciprocal(out=s_rs, in_=s_sd), reads=[Bstt], writes=[Bstt])
                op("dve", lambda v: v.tensor_scalar(out=s_nm, in0=stt[:, base + 12:base + 13], scalar1=s_rs, scalar2=-1.0,
                                                    op0=ALU.mult, op1=ALU.mult), reads=[Bstt], writes=[Bstt])
                op("act", lambda a: a.activation(out=dst[:], in_=src[:], func=AF.Identity, bias=s_nm, scale=s_rs),
                   reads=[Bsrc, Bstt], writes=[Bdst])

            def stageA(b):
                steps = []
                op, dma = mk_ops(steps)
                i = b % 2
                j = (b + 1) % 2
                r0 = b * 128
                X = xb[i]; BX = B_xb[i]
                dma("sp", (lambda q, X=X, r0=r0: q.dma_start(out=X[:], in_=x_d[r0:r0 + 128, :])), BX, writes=[BX])
                layer_norm(op, X, BX, X, BX, st[i], B_st[i], 0)
                op("pool", lambda g, X=X: g.tensor_tensor(out=X[:], in0=X[:], in1=g0[:], op=ALU.mult), reads=[BX, B_bc], writes=[BX])
                op("pool", lambda g, X=X: g.tensor_tensor(out=X[:], in0=X[:], in1=b0[:], op=ALU.add), reads=[BX, B_bc], writes=[BX])
                op("act", lambda a, X=X, i=i: a.activation(out=h0bf[i][:], in_=X[:], func=AF.Copy), reads=[BX], writes=[B_h0bf[i]])
                op("pe", [(lambda t, k=k, i=i: t.transpose(out=ptb[:, k, :], in_=h0bf[i][:, k * 128:(k + 1) * 128], identity=ident[:]))
                          for k in range(8)], reads=[B_h0bf[i], B_ident], writes=[B_ptb])
                op("act", lambda a, i=i: a.activation(out=h0T[i][:], in_=ptb[:], func=AF.Copy), reads=[B_ptb], writes=[B_h0T[i]])
                for (bank, off) in ((0, 0), (1, 512)):
                    op("pe", [(lambda t, c=c, k=k, bank=bank, off=off, i=i: t.matmul(
                        pbs[bank][:, c, :], lhsT=w_in_bf[:, k, off + c * 128:off + (c + 1) * 128], rhs=h0T[i][:, k, :],
                        start=(k == 0), stop=(k == 7))) for c in range(4) for k in range(8)],
                        reads=B_w_in + [B_h0T[i]], writes=[B_pb[bank]])
                for (bank, off) in ((2, 1024), (3, 1536)):
                    fl = [(lambda t, bank=bank, off=off: t.matmul(flat(pbs[bank]), lhsT=ones_row[:, :], rhs=brow[:, off - 1024:off - 512],
                                                                   start=True, stop=False))]
                    fl += [(lambda t, k=k, bank=bank, off=off, i=i: t.matmul(flat(pbs[bank]), lhsT=h0T[i][:, k, :],
                                                                              rhs=w_in_bf[:, k, off:off + 512], start=False, stop=(k == 7)))
                           for k in range(8)]
                    op("pe", fl, reads=B_w_in + [B_h0T[i], B_ones, B_brow], writes=[B_pb[bank]])
                op("act", [(lambda a, c=c, i=i: a.activation(out=sig[i][:, c, :], in_=pbs[1][:, c, :], func=AF.Sigmoid,
                                                             bias=cols[:, 4 + c:5 + c], scale=1.0)) for c in range(4)],
                   reads=[B_pb[1], B_cols], writes=[B_sig[i]])
                op("dve", [(lambda v, c=c, i=i: v.scalar_tensor_tensor(out=cT[i][:, c, 30:158], in0=pbs[0][:, c, :],
                                                                        scalar=cols[:, c:c + 1], in1=sig[i][:, c, :],
                                                                        op0=ALU.add, op1=ALU.mult)) for c in range(4)],
                   reads=[B_pb[0], B_sig[i], B_cols], writes=[B_cT[i]])
                if b + 1 < nblk:
                    op("pool", lambda g, i=i, j=j: g.tensor_copy(out=cT[j][:, :, 0:30], in_=cT[i][:, :, 128:158]),
                       reads=[B_cT[i]], writes=[B_cT[j]])
                op("act", lambda a, i=i: a.activation(out=uu[i][:], in_=flat(pbs[2]), func=AF.Gelu), reads=[B_pb[2]], writes=[B_uu[i]])
                op("act", lambda a, i=i: a.activation(out=gv[i][:], in_=flat(pbs[3]), func=AF.Gelu), reads=[B_pb[3]], writes=[B_gv[i]])
                return steps

            def stageB(b):
                steps = []
                op, dma = mk_ops(steps)
                i = b % 2
                r0 = b * 128
                X = xb[i]; BX = B_xb[i]
                op("pe", [(lambda t, c=c, k=k, i=i: t.matmul(pbs[4][:, c, :], lhsT=cdiag[:, c * 31 + k, :], rhs=cT[i][:, c, k:k + 128],
                                                             start=(k == 0), stop=(k == 30))) for c in range(4) for k in range(31)],
                   reads=[B_cdiag, B_cT[i]], writes=[B_pb[4]])
                op("act", [(lambda a, c=c, i=i: a.activation(out=yT[i][:, c, :], in_=pbs[4][:, c, :], func=AF.Identity,
                                                             bias=cols[:, 8 + c:9 + c], scale=1.0)) for c in range(4)],
                   reads=[B_pb[4], B_cols], writes=[B_yT[i]])
                gv3 = gv[i][:].rearrange("p (g d) -> p g d", g=8)
                sq3 = sq2[i][:].rearrange("p (g d) -> p g d", g=8)
                S = st[i]; BS = B_st[i]
                op("dve", lambda v, gv3=gv3, S=S: v.tensor_reduce(out=S[:, 24:32], in_=gv3, axis=AX.X, op=ALU.add), reads=[B_gv[i]], writes=[BS])
                op("pe", lambda t, i=i: t.matmul(flat(pbs[5]), lhsT=Gm[:], rhs=flat(yT[i]), start=True, stop=True),
                   reads=[B_G, B_yT[i]], writes=[B_pb[5]])
                op("dve", lambda v, S=S: v.tensor_scalar(out=S[:, 24:32], in0=S[:, 24:32], scalar1=1.0 / 64.0, scalar2=None, op0=ALU.mult),
                   reads=[BS], writes=[BS])
                op("dve", lambda v, i=i: v.tensor_tensor(out=flat(dd[i]), in0=flat(yT[i]), in1=flat(pbs[5]), op=ALU.subtract),
                   reads=[B_yT[i], B_pb[5]], writes=[B_dd[i]])
                op("act", lambda a, i=i: a.activation(out=flat(sq[i]), in_=flat(dd[i]), func=AF.Square), reads=[B_dd[i]], writes=[B_sq[i]])
                op("dve", lambda v, gv3=gv3, S=S: v.tensor_tensor(out=gv3, in0=gv3, in1=S[:, 24:32].unsqueeze(2).to_broadcast([128, 8, 64]),
                                                                   op=ALU.subtract), reads=[B_gv[i], BS], writes=[B_gv[i]])
                op("pe", lambda t, i=i: t.matmul(flat(pbs[6]), lhsT=Gm[:], rhs=flat(sq[i]), start=True, stop=True),
                   reads=[B_G, B_sq[i]], writes=[B_pb[6]])
                op("act", lambda a, i=i: a.activation(out=sq2[i][:], in_=gv[i][:], func=AF.Square), reads=[B_gv[i]], writes=[B_sq2[i]])
                op("act", lambda a, i=i: a.activation(out=flat(sq[i]), in_=flat(pbs[6]), func=AF.Sqrt, bias=eps_t[:, 0:1], scale=1.0),
                   reads=[B_pb[6], B_eps], writes=[B_sq[i]])
                op("dve", lambda v, sq3=sq3, S=S: v.tensor_reduce(out=S[:, 32:40], in_=sq3, axis=AX.X, op=ALU.add), reads=[B_sq2[i]], writes=[BS])
                op("dve", lambda v, i=i: v.reciprocal(out=flat(sq[i]), in_=flat(sq[i])), reads=[B_sq[i]], writes=[B_sq[i]])
                op("act", lambda a, S=S: a.activation(out=S[:, 40:48], in_=S[:, 32:40], func=AF.Sqrt, bias=eps_t[:, 0:1], scale=1.0 / 64.0),
                   reads=[BS, B_eps], writes=[BS])
                op("dve", lambda v, i=i: v.tensor_tensor(out=flat(dd[i]), in0=flat(dd[i]), in1=flat(sq[i]), op=ALU.mult),
                   reads=[B_dd[i], B_sq[i]], writes=[B_dd[i]])
                op("act", [(lambda a, c=c, i=i: a.activation(out=coT[i][:, c, :], in_=dd[i][:, c, :], func=AF.Silu,
                                                             bias=cols[:, 16 + c:17 + c], scale=cols[:, 12 + c:13 + c])) for c in range(4)],
                   reads=[B_dd[i], B_cols], writes=[B_coT[i]])
                op("dve", lambda v, S=S: v.reciprocal(out=S[:, 48:56], in_=S[:, 40:48]), reads=[BS], writes=[BS])
                op("dve", lambda v, gv3=gv3, S=S: v.tensor_tensor(out=gv3, in0=gv3, in1=S[:, 48:56].unsqueeze(2).to_broadcast([128, 8, 64]),
                                                                   op=ALU.mult), reads=[B_gv[i], BS], writes=[B_gv[i]])
                op("pool", lambda g, i=i: g.tensor_tensor(out=gv[i][:], in0=gv[i][:], in1=sglg[:], op=ALU.mult), reads=[B_gv[i], B_bc], writes=[B_gv[i]])
                op("pool", lambda g, i=i: g.tensor_tensor(out=vbf[i][:], in0=gv[i][:], in1=sglb[:], op=ALU.add), reads=[B_gv[i], B_bc], writes=[B_vbf[i]])
                op("pe", [(lambda t, h=h, i=i: t.matmul(flat(pbs[5])[:, h * 64:(h + 1) * 64], lhsT=wmT[:, h, :], rhs=vbf[i][:, h * 64:(h + 1) * 64],
                                                        start=True, stop=True)) for h in range(8)],
                   reads=[B_wmT, B_vbf[i]], writes=[B_pb[5]])
                f3 = flat(pbs[5]).rearrange("p (g d) -> p g d", g=8)
                op("dve", lambda v, f3=f3, i=i: v.tensor_tensor(out=sq2[i][:].rearrange("p (g d) -> p g d", g=8), in0=f3,
                                                                in1=cols[:, 20:28].unsqueeze(2).to_broadcast([128, 8, 64]), op=ALU.add),
                   reads=[B_pb[5], B_cols], writes=[B_sq2[i]])
                op("dve", lambda v, i=i: v.tensor_tensor(out=sgo[i][:], in0=sq2[i][:], in1=uu[i][:], op=ALU.mult),
                   reads=[B_sq2[i], B_uu[i]], writes=[B_sgo[i]])
                op("pe", [(lambda t, c=c, i=i: t.transpose(out=ptb[:, c, :], in_=sgo[i][:, c * 128:(c + 1) * 128], identity=ident[:]))
                          for c in range(4)], reads=[B_sgo[i], B_ident], writes=[B_ptb])
                op("act", lambda a, i=i: a.activation(out=sgoT[i][:], in_=ptb[:, 0:4, :], func=AF.Copy), reads=[B_ptb], writes=[B_sgoT[i]])
                mb = (4, 6)
                for n in range(2):
                    fl = [(lambda t, n=n: t.matmul(flat(pbs[mb[n]]), lhsT=ones_row[:, :], rhs=brow[:, 1024 + n * 512:1024 + (n + 1) * 512],
                                                   start=True, stop=False))]
                    fl += [(lambda t, k=k, n=n, i=i: t.matmul(flat(pbs[mb[n]]), lhsT=(coT[i][:, k, :] if k < 4 else sgoT[i][:, k - 4, :]),
                                                              rhs=w_o_bf[:, k, n * 512:(n + 1) * 512], start=False, stop=(k == 7)))
                           for k in range(8)]
                    op("pe", fl, reads=B_w_o + [B_coT[i], B_sgoT[i], B_ones, B_brow], writes=[B_pb[mb[n]]])
                op("dve", [(lambda v, n=n, i=i, X=X: v.scalar_tensor_tensor(out=r1[i][:, n * 512:(n + 1) * 512], in0=X[:, n * 512:(n + 1) * 512],
                                                                             scalar=ALPHA, in1=flat(pbs[mb[n]]), op0=ALU.mult, op1=ALU.add))
                           for n in range(2)], reads=[BX, B_pb[4], B_pb[6]], writes=[B_r1[i]])
                R = r1[i]; BR = B_r1[i]
                layer_norm(op, R, BR, R, BR, st[i], B_st[i], 0)
                op("pool", lambda g, R=R: g.tensor_tensor(out=R[:], in0=R[:], in1=g1[:], op=ALU.mult), reads=[BR, B_bc], writes=[BR])
                op("pool", lambda g, R=R: g.tensor_tensor(out=R[:], in0=R[:], in1=b1[:], op=ALU.add), reads=[BR, B_bc], writes=[BR])
                dst = out_d if debug_h1 else h1_d
                dma("sp", (lambda q, R=R, r0=r0, dst=dst: q.dma_start(out=dst[r0:r0 + 128, :], in_=R[:])), BR, reads=[BR], writes=[h1d_bufs[b]])
                return steps

            B_uvtab = Buf("uvtab")
            conv_jobs = [(t_, c_) for c_ in range(32) for t_ in range(2)]

            def issue_conv(k):
                for (t_, c_) in conv_jobs[k::nblk] if nblk < 32 else conv_jobs[2 * k:2 * k + 2]:
                    tab = peer_u if t_ == 0 else peer_v
                    P.dma_nowait("pool", (lambda q, tab=tab, t_=t_, c_=c_: q.dma_start(
                        out=uv_d[c_ * 512:(c_ + 1) * 512, t_ * D:(t_ + 1) * D], in_=tab[c_ * 512:(c_ + 1) * 512, :])), B_uvtab)

            if not debug_h1:
                issue_conv(0)
            for st_ in stageA(0):
                st_()
            for b in range(nblk):
                sB = stageB(b)
                sA = stageA(b + 1) if b + 1 < nblk else []
                if not debug_h1 and b + 1 < nblk:
                    issue_conv(b + 1)
                ia = 0
                for k, st_ in enumerate(sB):
                    st_()
                    tgt = ((k + 1) * len(sA)) // len(sB)
                    while ia < tgt:
                        sA[ia]()
                        ia += 1
                while ia < len(sA):
                    sA[ia]()
                    ia += 1

            P.barrier()
            with nc.Block() as blk:
                P.emit(blk)

        if debug_h1:
            return nc

        with ExitStack() as s2:
            def sbt(name, shape, dt):
                return s2.enter_context(nc.sbuf_tensor(name, list(shape), dt))

            def pst(name, shape, dt):
                return s2.enter_context(nc.psum_tensor(name, list(shape), dt))

            def flat(t):
                return t[:].rearrange("p c t -> p (c t)")

            wq_bf = sbt("wq_bf", [128, 8, 2048], BF16); B_wq = [Buf("wq_%d" % q_) for q_ in range(4)]
            wg_bf = sbt("wg_bf", [128, 8, 1024], BF16); B_wg = [Buf("wg_%d" % q_) for q_ in range(4)]
            wp_bf = sbt("wp_bf", [128, 2, 1024], BF16); B_wp = Buf("wp")
            keys_f = sbt("keys_f", [128, 16, 128], BF16); B_keysf = Buf("keysf")
            keysT = sbt("keysT", [128, 16, 128], BF16); B_keysT = Buf("keysT")
            g2 = sbt("g2", [128, D], F32); b2 = sbt("b2", [128, D], F32); B_bc = Buf("bc2")
            identf = sbt("identf2", [128, 128], F32); ident = sbt("ident2", [128, 128], BF16); B_ident = Buf("ident2")
            brow = sbt("brow2", [1, 1024], BF16); B_brow = Buf("brow2")
            ones_row = sbt("ones_row2", [1, 128], BF16); B_ones = Buf("ones2")
            eps_t = sbt("eps_t2", [128, 1], F32); B_eps = Buf("eps2")
            iota16 = sbt("iota16", [128, 16], F32); B_iota = Buf("iota")

            ptb = pst("ptb2", [128, 8, 128], BF16); B_ptb = Buf("ptb2")
            pq = [pst("pq%d" % i, [128, 4, 128], F32) for i in range(2)]; B_pq = [Buf("pq%d" % i) for i in range(2)]
            psc = [pst("psc%d" % i, [128, 4, 128], F32) for i in range(2)]; B_psc = [Buf("psc%d" % i) for i in range(2)]
            ppl = pst("ppl", [128, 4, 128], F32); B_ppl = Buf("ppl")
            py = [pst("py%d" % i, [128, 4, 128], F32) for i in range(2)]; B_py = Buf("py")

            for k2 in range(2):
                for kk in range(8):
                    P.dma("pool", (lambda g, kk=kk, k2=k2: g.dma_start(
                        out=wq_bf[:, kk, k2 * 1024:(k2 + 1) * 1024],
                        in_=peer_wq[kk * 128:(kk + 1) * 128, k2 * 1024:(k2 + 1) * 1024])), B_wq[kk % 4], writes=[B_wq[kk % 4]])
            for kk in range(8):
                P.dma("pool", (lambda g, kk=kk: g.dma_start(out=wg_bf[:, kk, :], in_=ple_wg[kk * 128:(kk + 1) * 128, :])), B_wg[kk % 4], writes=[B_wg[kk % 4]])
            for kk in range(2):
                P.dma("pool", (lambda g, kk=kk: g.dma_start(out=wp_bf[:, kk, :], in_=ple_wp[kk * 128:(kk + 1) * 128, :])), B_wp, writes=[B_wp])
            P.dma("pool", lambda g: g.dma_start(out=keys_f[:], in_=peer_keys.rearrange("h k d -> k h d")), B_keysf, writes=[B_keysf])
            P.dma("pool", lambda g: g.dma_start(out=brow[:, :], in_=ple_bg.unsqueeze(0)), B_brow, writes=[B_brow])
            for (t_, v_) in ((g2, ln2_g), (b2, ln2_b)):
                P.dma("sp", (lambda q, t_=t_, v_=v_: q.dma_start(out=t_[:], in_=v_.partition_broadcast(128))), B_bc, writes=[B_bc])
            P.op("pool", lambda g: g.memset(identf[:], 0.0), writes=[B_ident])
            P.op("pool", lambda g: g.affine_select(out=identf[:], in_=identf[:], pattern=[[-1, 128]],
                                                   compare_op=ALU.not_equal, fill=1.0, base=0, channel_multiplier=1),
                 reads=[B_ident], writes=[B_ident])
            P.op("pool", lambda g: g.iota(iota16[:], pattern=[[1, 16]], base=0, channel_multiplier=0, allow_small_or_imprecise_dtypes=True),
                 writes=[B_iota])
            P.op("dve", lambda v: v.tensor_copy(out=ident[:], in_=identf[:]), reads=[B_ident], writes=[B_ident])
            P.op("dve", lambda v: v.memset(ones_row[:], 1.0), writes=[B_ones])
            P.op("dve", lambda v: v.memset(eps_t[:], EPS), writes=[B_eps])
            for half in range(2):
                P.op("pe", [(lambda t, q=q, half=half: t.transpose(out=ptb[:, q, :], in_=keys_f[:, half * 8 + q, :], identity=ident[:]))
                            for q in range(8)], reads=[B_keysf, B_ident], writes=[B_ptb])
                P.op("dve", lambda v, half=half: v.tensor_copy(out=keysT[:, half * 8:(half + 1) * 8, :], in_=ptb[:]),
                     reads=[B_ptb], writes=[B_keysT])

            UV = [sbt("UV%d" % s, [128, 2 * D], BF16) for s in range(NRING)]; B_UV = [Buf("UV%d" % s) for s in range(NRING)]
            dring = [sbt("dg%d" % s, [128, 128], BF16) for s in range(NDIAG)]; B_dg = [Buf("dg%d" % s) for s in range(NDIAG)]

            def dbl(name, shape, dt):
                return [sbt("%s%d" % (name, i), shape, dt) for i in range(2)], [Buf("%s%d" % (name, i)) for i in range(2)]
            h1, B_h1 = dbl("h1_", [128, D], F32)
            rp, B_rp = dbl("rp", [128, D], F32)
            idx, B_idx = dbl("idx", [128, 128], U32)
            gsm, B_gsm = dbl("gsm", [128, 128], F32)
            pld, B_pld = dbl("pld", [128, 256], F32)
            h1bf = sbt("h1bf", [128, D], BF16); B_h1bf = Buf("h1bf")
            h1T = sbt("h1T", [128, 8, 128], BF16); B_h1T = Buf("h1T")
            pbf = sbt("pbf", [128, 256], BF16); B_pbf = Buf("pbf")
            pT = sbt("pT", [128, 2, 128], BF16); B_pT = Buf("pT")
            qT = sbt("qT", [128, 16, 128], BF16); B_qT = [Buf("qT%d" % i) for i in range(4)]
            scs = sbt("scs", [128, 16, 128], F32); B_scs = [Buf("scs%d" % i) for i in range(4)]
            wk = sbt("wk", [128, 8, 128], F32); B_wk = Buf("wk")
            m8 = sbt("m8", [128, 16, 16], F32); B_m8 = Buf("m8")
            i8 = sbt("i8", [128, 16, 16], U32); B_i8 = Buf("i8")
            i8f = sbt("i8f", [128, 16, 16], F32); B_i8f = Buf("i8f")
            comb = sbt("comb", [128, 4, 256], F32); B_comb = Buf("comb")
            wk2 = sbt("wk2", [128, 4, 256], F32); B_wk2 = Buf("wk2")
            c16 = sbt("c16", [128, 8, 16], F32); B_c16 = Buf("c16")
            pos = sbt("pos", [128, 8, 16], U32); B_pos = Buf("pos")
            apos = sbt("apos", [128, 8, 16], U32); bpos = sbt("bpos", [128, 8, 16], U32)
            aposf = sbt("aposf", [128, 8, 16], F32); bposf = sbt("bposf", [128, 8, 16], F32); B_ab = Buf("ab")
            oh = sbt("oh", [128, 4, 16, 16], F32); B_oh = Buf("oh")
            asel = sbt("asel", [128, 8, 16], F32); bsel = sbt("bsel", [128, 8, 16], F32); B_sel = Buf("sel")
            idxf = sbt("idxf", [128, 128], F32); B_idxf = Buf("idxf")
            sm = sbt("sm", [128, 32], F32); B_sm = Buf("sm")
            gate = sbt("gate", [128, 512], F32); B_gate = Buf("gate")
            junk = sbt("junk", [128, D], BF16); B_junk = Buf("junk")
            actt = sbt("actt", [128, 128], F32); B_actg = [Buf("actg%d" % g) for g in range(128 // GRP)]
            gel = sbt("gel", [128, 128], F32); B_gelg = [Buf("gelg%d" % g) for g in range(128 // GRP)]
            wgt = sbt("wgt", [128, 128], F32); B_wgtg = [Buf("wgtg%d" % g) for g in range(128 // GRP)]
            st2 = sbt("st2", [128, 32], F32); B_st2 = Buf("st2")

            NG = 128 // GRP
            ring_ctr = [0]

            def front(b):
                steps = []

                def op(*a_, **k_):
                    steps.append(lambda: P.op(*a_, **k_))

                def dma(*a_, **k_):
                    steps.append(lambda: P.dma(*a_, **k_))
                i = b % 2
                r0 = b * 128
                H = h1[i]; BH = B_h1[i]
                dma("sp", (lambda q, H=H, r0=r0: q.dma_start(out=H[:], in_=h1_d[r0:r0 + 128, :])), BH, reads=[h1d_bufs[b]], writes=[BH])
                dma("sp", (lambda q, i=i, r0=r0: q.dma_start(out=pld[i][:], in_=p_d[r0:r0 + 128, :])), B_pld[i], writes=[B_pld[i]])
                op("act", lambda a, H=H: a.activation(out=h1bf[:], in_=H[:], func=AF.Copy), reads=[BH], writes=[B_h1bf])
                op("act", lambda a, i=i: a.activation(out=pbf[:], in_=pld[i][:], func=AF.Copy), reads=[B_pld[i]], writes=[B_pbf])
                op("pe", [(lambda t, k=k: t.transpose(out=ptb[:, k, :], in_=h1bf[:, k * 128:(k + 1) * 128], identity=ident[:]))
                            for k in range(8)], reads=[B_h1bf, B_ident], writes=[B_ptb])
                op("act", lambda a: a.activation(out=h1T[:], in_=ptb[:], func=AF.Copy), reads=[B_ptb], writes=[B_h1T])
                op("pe", [(lambda t, k=k: t.transpose(out=ptb[:, k, :], in_=pbf[:, k * 128:(k + 1) * 128], identity=ident[:]))
                            for k in range(2)], reads=[B_pbf, B_ident], writes=[B_ptb])
                op("act", lambda a: a.activation(out=pT[:], in_=ptb[:, 0:2, :], func=AF.Copy), reads=[B_ptb], writes=[B_pT])
                for qd in range(4):
                    pb_ = pq[qd % 2]; Bp = B_pq[qd % 2]
                    op("pe", [(lambda t, c=c, k=k, qd=qd, pb_=pb_: t.matmul(
                        pb_[:, c, :], lhsT=wq_bf[:, k, (qd * 4 + c) * 128:(qd * 4 + c + 1) * 128], rhs=h1T[:, k, :],
                        start=(k == 0), stop=(k == 7))) for c in range(4) for k in range(8)],
                        reads=B_wq + [B_h1T], writes=[Bp])
                    op("act", lambda a, qd=qd, pb_=pb_: a.activation(out=qT[:, qd * 4:(qd + 1) * 4, :], in_=pb_[:], func=AF.Copy),
                         reads=[Bp], writes=[B_qT[qd]])
                    ps_ = psc[qd % 2]; Bs = B_psc[qd % 2]
                    op("pe", [(lambda t, c=c, qd=qd, ps_=ps_: t.matmul(ps_[:, c, :], lhsT=qT[:, qd * 4 + c, :], rhs=keysT[:, qd * 4 + c, :],
                                                                          start=True, stop=True)) for c in range(4)],
                         reads=[B_qT[qd], B_keysT], writes=[Bs])
                    op("act", lambda a, qd=qd, ps_=ps_: a.activation(out=scs[:, qd * 4:(qd + 1) * 4, :], in_=ps_[:], func=AF.Copy),
                         reads=[Bs], writes=[B_scs[qd]])
                for n in range(2):
                    fl = [(lambda t, n=n: t.matmul(flat(ppl), lhsT=ones_row[:, :], rhs=brow[:, n * 512:(n + 1) * 512], start=True, stop=False))]
                    fl += [(lambda t, k=k, n=n: t.matmul(flat(ppl), lhsT=h1T[:, k, :], rhs=wg_bf[:, k, n * 512:(n + 1) * 512],
                                                          start=False, stop=(k == 7))) for k in range(8)]
                    op("pe", fl, reads=B_wg + [B_h1T, B_ones, B_brow], writes=[B_ppl])
                    op("act", lambda a: a.activation(out=gate[:], in_=flat(ppl), func=AF.Sigmoid), reads=[B_ppl], writes=[B_gate])
                    op("pe", [(lambda t, k=k, n=n: t.matmul(flat(ppl), lhsT=pT[:, k, :], rhs=wp_bf[:, k, n * 512:(n + 1) * 512],
                                                               start=(k == 0), stop=(k == 1))) for k in range(2)],
                         reads=[B_wp, B_pT], writes=[B_ppl])
                    op("dve", lambda v: v.tensor_tensor(out=gate[:], in0=gate[:], in1=flat(ppl), op=ALU.mult),
                         reads=[B_gate, B_ppl], writes=[B_gate])
                    op("dve", lambda v, n=n, i=i, H=H: v.scalar_tensor_tensor(out=rp[i][:, n * 512:(n + 1) * 512], in0=H[:, n * 512:(n + 1) * 512],
                                                                               scalar=ALPHA, in1=gate[:], op0=ALU.mult, op1=ALU.add),
                         reads=[BH, B_gate], writes=[B_rp[i]])
                op("dve", [(lambda v, hh=hh: v.max(out=m8[:, hh, 0:8], in_=scs[:, hh, :])) for hh in range(16)], reads=B_scs, writes=[B_m8])
                for hv in range(2):
                    op("dve", [(lambda v, hh=hh, hv=hv: v.match_replace(out=wk[:, hh - 8 * hv, :], in_to_replace=m8[:, hh, 0:8],
                                                                          in_values=scs[:, hh, :], imm_value=NEG))
                                 for hh in range(8 * hv, 8 * hv + 8)], reads=B_scs + [B_m8], writes=[B_wk])
                    op("dve", [(lambda v, hh=hh, hv=hv: v.max(out=m8[:, hh, 8:16], in_=wk[:, hh - 8 * hv, :]))
                                 for hh in range(8 * hv, 8 * hv + 8)], reads=[B_wk], writes=[B_m8])
                op("dve", [(lambda v, hh=hh, o=o: v.max_index(out=i8[:, hh, o:o + 8], in_max=m8[:, hh, o:o + 8], in_values=scs[:, hh, :]))
                             for hh in range(16) for o in (0, 8)], reads=B_scs + [B_m8], writes=[B_i8])
                op("dve", lambda v: v.tensor_copy(out=i8f[:], in_=i8[:]), reads=[B_i8], writes=[B_i8f])
                m84 = m8[:].rearrange("p (h t) k -> p h t k", t=2)
                i84 = i8f[:].rearrange("p (h t) k -> p h t k", t=2)
                for hf in range(2):
                    hs = slice(hf * 4, hf * 4 + 4)
                    comb4 = comb[:].rearrange("p h (a c) -> p h a c", a=16)
                    op("dve", lambda v, hs=hs, comb4=comb4: v.tensor_tensor(
                        out=comb4, in0=m84[:, hs, 0, :].unsqueeze(3).to_broadcast([128, 4, 16, 16]),
                        in1=m84[:, hs, 1, :].unsqueeze(2).to_broadcast([128, 4, 16, 16]), op=ALU.add),
                        reads=[B_m8], writes=[B_comb])
                    op("dve", [(lambda v, h=h, hf=hf: v.max(out=c16[:, hf * 4 + h, 0:8], in_=comb[:, h, :])) for h in range(4)],
                         reads=[B_comb], writes=[B_c16])
                    op("dve", [(lambda v, h=h, hf=hf: v.match_replace(out=wk2[:, h, :], in_to_replace=c16[:, hf * 4 + h, 0:8],
                                                                         in_values=comb[:, h, :], imm_value=NEG)) for h in range(4)],
                         reads=[B_comb, B_c16], writes=[B_wk2])
                    op("dve", [(lambda v, h=h, hf=hf: v.max(out=c16[:, hf * 4 + h, 8:16], in_=wk2[:, h, :])) for h in range(4)],
                         reads=[B_wk2], writes=[B_c16])
                    op("dve", [(lambda v, h=h, hf=hf, o=o: v.max_index(out=pos[:, hf * 4 + h, o:o + 8], in_max=c16[:, hf * 4 + h, o:o + 8],
                                                                          in_values=comb[:, h, :])) for h in range(4) for o in (0, 8)],
                         reads=[B_comb, B_c16], writes=[B_pos])
                op("dve", [lambda v: v.tensor_single_scalar(out=apos[:], in_=pos[:], scalar=4, op=ALU.logical_shift_right),
                             lambda v: v.tensor_single_scalar(out=bpos[:], in_=pos[:], scalar=15, op=ALU.bitwise_and)],
                     reads=[B_pos], writes=[B_ab])
                op("dve", [lambda v: v.tensor_copy(out=aposf[:], in_=apos[:]),
                             lambda v: v.tensor_copy(out=bposf[:], in_=bpos[:])], reads=[B_ab], writes=[B_ab])
                io4 = iota16[:].unsqueeze(1).unsqueeze(1).to_broadcast([128, 4, 16, 16])
                for (pf, tsel, sel) in ((aposf, 0, asel), (bposf, 1, bsel)):
                    for hf in range(2):
                        hs = slice(hf * 4, hf * 4 + 4)
                        op("dve", lambda v, pf=pf, hs=hs: v.tensor_tensor(out=oh[:], in0=pf[:, hs, :].unsqueeze(3).to_broadcast([128, 4, 16, 16]),
                                                                            in1=io4, op=ALU.is_equal), reads=[B_ab, B_iota], writes=[B_oh])
                        op("dve", lambda v, tsel=tsel, hs=hs: v.tensor_tensor(out=oh[:], in0=oh[:],
                                                                                in1=i84[:, hs, tsel, :].unsqueeze(2).to_broadcast([128, 4, 16, 16]),
                                                                                op=ALU.mult), reads=[B_oh, B_i8f], writes=[B_oh])
                        op("dve", lambda v, sel=sel, hs=hs: v.tensor_reduce(out=sel[:, hs, :], in_=oh[:], axis=AX.X, op=ALU.add),
                             reads=[B_oh], writes=[B_sel])
                op("dve", lambda v: v.scalar_tensor_tensor(out=idxf[:], in0=asel[:].rearrange("p h k -> p (h k)"), scalar=128.0,
                                                             in1=bsel[:].rearrange("p h k -> p (h k)"), op0=ALU.mult, op1=ALU.add),
                     reads=[B_sel], writes=[B_idxf])
                op("dve", lambda v, i=i: v.tensor_copy(out=idx[i][:], in_=idxf[:]), reads=[B_idxf], writes=[B_idx[i]])
                op("dve", lambda v: v.tensor_scalar(out=sm[:, 0:8], in0=c16[:, :, 0], scalar1=-1.0, scalar2=None, op0=ALU.mult),
                     reads=[B_c16], writes=[B_sm])
                G3 = gsm[i][:].rearrange("p (h k) -> p h k", h=8)
                op("act", [(lambda a, h=h, G3=G3: a.activation(out=G3[:, h, :], in_=c16[:, h, :], func=AF.Exp, bias=sm[:, h:h + 1], scale=1.0,
                                                                  accum_out=sm[:, 8 + h:9 + h])) for h in range(8)],
                     reads=[B_c16, B_sm], writes=[B_gsm[i], B_sm])
                op("dve", lambda v: v.reciprocal(out=sm[:, 16:24], in_=sm[:, 8:16]), reads=[B_sm], writes=[B_sm])
                op("dve", lambda v, G3=G3: v.tensor_tensor(out=G3, in0=G3, in1=sm[:, 16:24].unsqueeze(2).to_broadcast([128, 8, 16]), op=ALU.mult),
                     reads=[B_gsm[i], B_sm], writes=[B_gsm[i]])

                def gen():
                    for st_ in steps:
                        st_()
                        yield
                return gen()

            nuse = nblk * 128

            def issue_gather(n):
                if n >= nuse:
                    return
                b_, jj = divmod(n, 128)
                i_ = b_ % 2
                s_ = n % NRING
                P.dma("pool", (lambda q, s_=s_, jj=jj, i_=i_: q.indirect_dma_start(
                    out=UV[s_][:], out_offset=None, in_=uv_d,
                    in_offset=bass.IndirectOffsetOnAxis(ap=idx[i_][:, jj:jj + 1], axis=0))),
                    B_UV[s_], reads=[B_idx[i_]], writes=[B_UV[s_]])

            def wmul(b, g):
                i = b % 2
                gs = slice(g * GRP, (g + 1) * GRP)
                P.op("dve", lambda v, gs=gs, i=i: v.tensor_tensor(out=wgt[:, gs], in0=gel[:, gs], in1=gsm[i][:, gs], op=ALU.mult),
                     reads=[B_gelg[g], B_gsm[i]], writes=[B_wgtg[g]])

            def vside(b, g):
                for e in range(GRP):
                    jj = g * GRP + e
                    s = (b * 128 + jj) % NRING
                    ds = (b * 128 + jj) % NDIAG
                    P.op("act", lambda a, ds=ds, jj=jj: a.activation(out=dring[ds][:], in_=identf[:], func=AF.Copy, scale=wgt[:, jj:jj + 1]),
                         reads=[B_wgtg[g], B_ident], writes=[B_dg[ds]])
                    P.op("pe", [(lambda t, n=n, ds=ds, s=s, jj=jj: t.matmul(flat(py[n]), lhsT=dring[ds][:], rhs=UV[s][:, D + n * 512:D + (n + 1) * 512],
                                                                             start=(jj == 0), stop=(jj == 127))) for n in range(2)],
                         reads=[B_dg[ds], B_UV[s]], writes=[B_py])
                    issue_gather(b * 128 + jj + NRING)

            def tail(b):
                i = b % 2
                r0 = b * 128
                r2 = rp[i]; B_r2 = B_rp[i]
                P.op("dve", [(lambda v, n=n, r2=r2: v.tensor_tensor(out=r2[:, n * 512:(n + 1) * 512], in0=r2[:, n * 512:(n + 1) * 512],
                                                                    in1=flat(py[n]), op=ALU.add)) for n in range(2)],
                     reads=[B_r2, B_py], writes=[B_r2])
                S = st2
                P.op("dve", [lambda v, r2=r2: v.bn_stats(out=S[:, 0:6], in_=r2[:, 0:512]),
                             lambda v, r2=r2: v.bn_stats(out=S[:, 6:12], in_=r2[:, 512:1024])], reads=[B_r2], writes=[B_st2])
                P.op("dve", lambda v: v.bn_aggr(out=S[:, 12:14], in_=S[:, 0:12]), reads=[B_st2], writes=[B_st2])
                P.op("act", lambda a: a.activation(out=S[:, 14:15], in_=S[:, 13:14], func=AF.Sqrt, bias=eps_t[:, 0:1], scale=1.0),
                     reads=[B_st2, B_eps], writes=[B_st2])
                P.op("dve", lambda v: v.reciprocal(out=S[:, 15:16], in_=S[:, 14:15]), reads=[B_st2], writes=[B_st2])
                P.op("dve", lambda v: v.tensor_scalar(out=S[:, 16:17], in0=S[:, 12:13], scalar1=S[:, 15:16], scalar2=-1.0, op0=ALU.mult, op1=ALU.mult),
                     reads=[B_st2], writes=[B_st2])
                P.op("act", lambda a, r2=r2: a.activation(out=r2[:], in_=r2[:], func=AF.Identity, bias=S[:, 16:17], scale=S[:, 15:16]),
                     reads=[B_r2, B_st2], writes=[B_r2])
                P.op("dve", lambda v, r2=r2: v.tensor_tensor(out=r2[:], in0=r2[:], in1=g2[:], op=ALU.mult), reads=[B_r2, B_bc], writes=[B_r2])
                P.op("dve", lambda v, r2=r2: v.tensor_tensor(out=r2[:], in0=r2[:], in1=b2[:], op=ALU.add), reads=[B_r2, B_bc], writes=[B_r2])
                P.dma("sp", (lambda q, r0=r0, r2=r2: q.dma_start(out=out_d[r0:r0 + 128, :], in_=r2[:])), B_r2, reads=[B_r2])

            def back(b, fg):
                i = b % 2
                H = h1[i]; BH = B_h1[i]
                for g in range(NG):
                    gs = slice(g * GRP, (g + 1) * GRP)
                    if g == NG - 1 and fg is not None:
                        for _ in fg:
                            pass
                    for e in range(GRP):
                        jj = g * GRP + e
                        s = (b * 128 + jj) % NRING
                        P.op("dve", lambda v, s=s, jj=jj, H=H: v.scalar_tensor_tensor(out=junk[:], in0=UV[s][:, 0:D], scalar=1.0, in1=H[:],
                                                                                     op0=ALU.mult, op1=ALU.mult, accum_out=actt[:, jj:jj + 1]),
                             reads=[B_UV[s], BH], writes=[B_junk, B_actg[g]])
                        if e == 0 and g >= 1:
                            wmul(b, g - 1)
                            vside(b, g - 1)
                        if e == GRP - 1 and g == 0 and b >= 1:
                            tail(b - 1)
                        if fg is not None and g < NG - 1:
                            next(fg, None)
                    P.op("act", lambda a, gs=gs: a.activation(out=gel[:, gs], in_=actt[:, gs], func=AF.Gelu), reads=[B_actg[g]], writes=[B_gelg[g]])
                wmul(b, NG - 1)
                vside(b, NG - 1)
                if b == nblk - 1:
                    tail(b)

            for _ in front(0):
                pass
            for n_ in range(NRING):
                issue_gather(n_)
            for b in range(nblk):
                back(b, front(b + 1) if b + 1 < nblk else None)

            P.barrier()
            with nc.Block() as blk:
                P.emit(blk)
    return nc


_W_NAMES = ["ln0_g", "ln0_b", "w_in", "b_in", "conv_w", "conv_b", "gn_g", "gn_b", "sg_ln_g", "sg_ln_b", "sg_w", "sg_b",
            "w_o", "b_o", "ln1_g", "ln1_b", "peer_wq", "peer_keys", "peer_u", "peer_v", "ple_wp", "ple_wg", "ple_bg",
            "ln2_g", "ln2_b"]


def _prep_weights(inp):
    w = {}
    for k in _W_NAMES:
        a = np.asarray(inp[k], dtype=np.float32)
        if k in ("ln0_g", "ln0_b"):
            w[k] = np.ascontiguousarray(a.reshape(1024))
        elif k == "peer_keys":
            w[k] = np.ascontiguousarray(a.reshape(16, 128, 128))
        else:
            w[k] = np.ascontiguousarray(a[0])
    return w


def kernel(**inputs):
    n = 8
    x = np.asarray(inputs["x"], dtype=np.float32)
    p = np.asarray(inputs["p"], dtype=np.float32)
    w = _prep_weights(inputs)
    nc = build_program(32)
    in_maps = []
    for c in range(n):
        m = {"x": np.ascontiguousarray(x[c]), "p": np.ascontiguousarray(p[0, c])}
        m.update(w)
        in_maps.append(m)
    res = run_bass_kernel_spmd(nc, in_maps, core_ids=list(range(n)))
    return np.stack([np.asarray(r["out"], dtype=np.float32) for r in res.results], axis=0)
```

```python
import numpy as np
from contextlib import ExitStack
import concourse.bass as bass
import concourse.mybir as mybir
from concourse.bass_utils import run_bass_kernel_spmd

F32 = mybir.dt.float32
BF16 = mybir.dt.bfloat16
U32 = mybir.dt.uint32
AF = mybir.ActivationFunctionType
ALU = mybir.AluOpType
AX = mybir.AxisListType

D = 1024
SEQ = 4096
ALPHA = float(2.0 ** 0.25)
EPS = 1e-5
NEG = -1.0e30
ENGS = ["sp", "pool", "act", "dve", "pe"]
NRING = 19
GRP = 8
NDIAG = 16


class Buf:
    __slots__ = ("name", "w", "r", "dsem", "dcnt")

    def __init__(self, name):
        self.name = name
        self.w = None
        self.r = []
        self.dsem = None
        self.dcnt = 0


class Prog:
    def __init__(self, nc, es):
        self.nc = nc
        self.es = es
        self.streams = {e: [] for e in ENGS}
        self.esem = {e: es.enter_context(nc.semaphore("es_" + e)) for e in ENGS}
        self.ecnt = {e: 0 for e in ENGS}
        self.waited = {e: {} for e in ENGS}
        self.dma_toks = []
        self.nd = 0

    def _wait(self, e, tok):
        sem, val = tok
        if self.waited[e].get(sem, 0) >= val:
            return
        self.waited[e][sem] = val
        self.streams[e].append(("w", sem, val))

    def _deps(self, e, who, reads, writes):
        for b in reads:
            if b.w is not None:
                self._wait(e, b.w[1])
        for b in writes:
            if b.w is not None and not (who == "pe" and b.w[0] == "pe"):
                self._wait(e, b.w[1])
            for (re_, tok) in b.r:
                self._wait(e, tok)

    def op(self, e, fns, reads=(), writes=()):
        if callable(fns):
            fns = [fns]
        self._deps(e, e, reads, writes)
        self.ecnt[e] += 1
        tok = (self.esem[e], self.ecnt[e])
        self.streams[e].append(("i", fns, self.esem[e], 1))
        for b in reads:
            b.r.append((e, tok))
        for b in writes:
            b.w = (e, tok)
            b.r = []
        return tok

    def dma(self, e, fn, sbuf, reads=(), writes=()):
        if sbuf.dsem is None:
            self.nd += 1
            sbuf.dsem = self.es.enter_context(self.nc.semaphore("ds%d" % self.nd))
        wr = list(writes)
        if sbuf not in wr:
            wr.append(sbuf)
        rd = [b for b in reads if b is not sbuf]
        self._deps(e, "dma", rd, wr)
        sbuf.dcnt += 16
        tok = (sbuf.dsem, sbuf.dcnt)
        self.streams[e].append(("i", [fn], sbuf.dsem, 16))
        for b in rd:
            b.r.append(("dma", tok))
        for b in writes:
            b.w = ("dma", tok)
            b.r = []
        if sbuf not in writes:
            sbuf.r.append(("dma", tok))
        self.dma_toks.append(tok)
        return tok

    def dma_nowait(self, e, fn, buf):
        if buf.dsem is None:
            self.nd += 1
            buf.dsem = self.es.enter_context(self.nc.semaphore("ds%d" % self.nd))
        buf.dcnt += 16
        tok = (buf.dsem, buf.dcnt)
        self.streams[e].append(("i", [fn], buf.dsem, 16))
        self.dma_toks.append(tok)
        return tok

    def barrier(self):
        toks = [(self.esem[e], self.ecnt[e]) for e in ENGS if self.ecnt[e] > 0] + self.dma_toks
        mx = {}
        for (sem, val) in toks:
            if mx.get(sem, 0) < val:
                mx[sem] = val
        for e in ENGS:
            for sem, val in mx.items():
                self._wait(e, (sem, val))
        self.dma_toks = []

    def emit(self, block):
        def mk(en):
            items = self.streams[en]

            def f(eng):
                for it in items:
                    if it[0] == "w":
                        eng.wait_ge(it[1], it[2])
                    else:
                        ins = None
                        for fn in it[1]:
                            ins = fn(eng)
                        ins.then_inc(it[2], it[3])
            return f
        block.sync(mk("sp"))
        block.gpsimd(mk("pool"))
        block.scalar(mk("act"))
        block.vector(mk("dve"))
        block.tensor(mk("pe"))
        self.streams = {e: [] for e in ENGS}


def build_program(nblk=32, debug_h1=False):
    nc = bass.Bass("TRN2", target_bir_lowering=False)
    ntok = nblk * 128

    def din(name, shape):
        return nc.dram_tensor(name, list(shape), F32, kind="ExternalInput").ap()

    x_d = din("x", [ntok, D])
    p_d = din("p", [ntok, 256])
    ln0_g = din("ln0_g", [D]); ln0_b = din("ln0_b", [D])
    w_in = din("w_in", [D, 2048]); b_in = din("b_in", [2048])
    conv_w = din("conv_w", [31, 512]); conv_b = din("conv_b", [512])
    gn_g = din("gn_g", [512]); gn_b = din("gn_b", [512])
    sg_ln_g = din("sg_ln_g", [512]); sg_ln_b = din("sg_ln_b", [512])
    sg_w = din("sg_w", [8, 128, 128]); sg_b = din("sg_b", [8, 128])
    w_o = din("w_o", [D, D]); b_o = din("b_o", [D])
    ln1_g = din("ln1_g", [D]); ln1_b = din("ln1_b", [D])
    peer_wq = din("peer_wq", [D, 2048])
    peer_keys = din("peer_keys", [16, 128, 128])
    peer_u = din("peer_u", [16384, D]); peer_v = din("peer_v", [16384, D])
    ple_wp = din("ple_wp", [256, D]); ple_wg = din("ple_wg", [D, D]); ple_bg = din("ple_bg", [D])
    ln2_g = din("ln2_g", [D]); ln2_b = din("ln2_b", [D])
    out_d = nc.dram_tensor("out", [ntok, D], F32, kind="ExternalOutput").ap()
    h1_d = nc.dram_tensor("h1s", [ntok, D], F32, kind="Internal").ap()
    uv_d = nc.dram_tensor("uvbf", [16384, 2 * D], BF16, kind="Internal").ap()

    with ExitStack() as outer:
        P = Prog(nc, outer)
        h1d_bufs = [Buf("h1d%d" % b) for b in range(nblk)]

        with ExitStack() as s1:
            def sbt(name, shape, dt):
                return s1.enter_context(nc.sbuf_tensor(name, list(shape), dt))

            def pst(name, shape, dt):
                return s1.enter_context(nc.psum_tensor(name, list(shape), dt))

            w_in_bf = sbt("w_in_bf", [128, 8, 2048], BF16); B_w_in = Buf("w_in")
            w_o_bf = sbt("w_o_bf", [128, 8, 1024], BF16); B_w_o = Buf("w_o")
            cdiag = sbt("cdiag", [128, 124, 128], BF16); B_cdiag = Buf("cdiag")
            g0 = sbt("g0", [128, D], F32); b0 = sbt("b0", [128, D], F32)
            g1 = sbt("g1", [128, D], F32); b1 = sbt("b1", [128, D], F32)
            sglg = sbt("sglg", [128, 512], F32); sglb = sbt("sglb", [128, 512], F32)
            B_bc = Buf("bc")
            identf = sbt("identf", [128, 128], F32); ident = sbt("ident", [128, 128], BF16)
            B_ident = Buf("ident")
            Gm = sbt("Gm", [128, 128], F32); B_G = Buf("G")
            rows = sbt("rows", [28, 128], F32); B_rows = Buf("rows")
            cwrow = sbt("cwrow", [31, 512], F32); B_cwrow = Buf("cwrow")
            cols = sbt("cols", [128, 28], F32); B_cols = Buf("cols")
            cw = sbt("cw", [128, 4, 31], F32); B_cw = Buf("cw")
            brow = sbt("brow", [1, 2048], BF16); B_brow = Buf("brow")
            ones_row = sbt("ones_row", [1, 128], BF16); B_ones = Buf("ones")
            eps_t = sbt("eps_t", [128, 1], F32); B_eps = Buf("eps")
            sgw_f = sbt("sgw_f", [128, 8, 128], F32); B_sgwf = Buf("sgwf")
            sgw_m = sbt("sgw_m", [128, 8, 128], BF16); B_sgwm = Buf("sgwm")
            wmT = sbt("wmT", [128, 8, 128], BF16); B_wmT = Buf("wmT")

            ptb = pst("ptb", [128, 8, 128], BF16); B_ptb = Buf("ptb")
            pbs = [pst("pb%d" % i, [128, 4, 128], F32) for i in range(7)]
            B_pb = [Buf("pb%d" % i) for i in range(7)]

            def flat(t):
                return t[:].rearrange("p c t -> p (c t)")

            for k2 in range(2):
                for kk in range(8):
                    P.dma("pool", (lambda g, kk=kk, k2=k2: g.dma_start(
                        out=w_in_bf[:, kk, k2 * 1024:(k2 + 1) * 1024],
                        in_=w_in[kk * 128:(kk + 1) * 128, k2 * 1024:(k2 + 1) * 1024])), B_w_in, writes=[B_w_in])
            for kk in range(8):
                P.dma("pool", (lambda g, kk=kk: g.dma_start(
                    out=w_o_bf[:, kk, :], in_=w_o[kk * 128:(kk + 1) * 128, :])), B_w_o, writes=[B_w_o])
            P.dma("pool", lambda g: g.dma_start(out=brow[:, 0:1024], in_=b_in[1024:2048].unsqueeze(0)), B_brow, writes=[B_brow])
            P.dma("pool", lambda g: g.dma_start(out=brow[:, 1024:2048], in_=b_o.unsqueeze(0)), B_brow, writes=[B_brow])
            for (t_, v_) in ((g0, ln0_g), (b0, ln0_b), (g1, ln1_g), (b1, ln1_b), (sglg, sg_ln_g), (sglb, sg_ln_b)):
                P.dma("sp", (lambda q, t_=t_, v_=v_: q.dma_start(out=t_[:], in_=v_.partition_broadcast(128))), B_bc, writes=[B_bc])
            P.dma("sp", lambda q: q.dma_start(out=rows[0:8, :], in_=b_in[0:1024].rearrange("(c p) -> c p", p=128)), B_rows, writes=[B_rows])
            P.dma("sp", lambda q: q.dma_start(out=rows[8:12, :], in_=conv_b.rearrange("(c p) -> c p", p=128)), B_rows, writes=[B_rows])
            P.dma("sp", lambda q: q.dma_start(out=rows[12:16, :], in_=gn_g.rearrange("(c p) -> c p", p=128)), B_rows, writes=[B_rows])
            P.dma("sp", lambda q: q.dma_start(out=rows[16:20, :], in_=gn_b.rearrange("(c p) -> c p", p=128)), B_rows, writes=[B_rows])
            P.dma("sp", lambda q: q.dma_start(out=rows[20:28, :], in_=sg_b), B_rows, writes=[B_rows])
            P.dma("sp", lambda q: q.dma_start(out=cwrow[:], in_=conv_w), B_cwrow, writes=[B_cwrow])
            P.dma("sp", lambda q: q.dma_start(out=sgw_f[:], in_=sg_w.rearrange("h t s -> t h s")), B_sgwf, writes=[B_sgwf])

            P.op("pool", lambda g: g.memset(identf[:], 0.0), writes=[B_ident])
            P.op("pool", lambda g: g.affine_select(out=identf[:], in_=identf[:], pattern=[[-1, 128]],
                                                   compare_op=ALU.not_equal, fill=1.0, base=0, channel_multiplier=1),
                 reads=[B_ident], writes=[B_ident])
            P.op("dve", lambda v: v.tensor_copy(out=ident[:], in_=identf[:]), reads=[B_ident], writes=[B_ident])
            P.op("dve", lambda v: v.memset(Gm[:], 0.0), writes=[B_G])
            P.op("dve", lambda v: v.memset(Gm[0:64, 0:64], 1.0 / 64.0), writes=[B_G])
            P.op("dve", lambda v: v.memset(Gm[64:128, 64:128], 1.0 / 64.0), writes=[B_G])
            P.op("dve", lambda v: v.memset(ones_row[:], 1.0), writes=[B_ones])
            P.op("dve", lambda v: v.memset(eps_t[:], EPS), writes=[B_eps])
            P.op("pe", lambda t: t.transpose(out=pbs[0][:, 0, 0:28], in_=rows[:, :], identity=identf[0:28, 0:28]),
                 reads=[B_rows, B_ident], writes=[B_pb[0]])
            P.op("dve", lambda v: v.tensor_copy(out=cols[:], in_=pbs[0][:, 0, 0:28]), reads=[B_pb[0]], writes=[B_cols])
            P.op("pe", [(lambda t, c=c: t.transpose(out=pbs[1][:, c, 0:31], in_=cwrow[:, c * 128:(c + 1) * 128],
                                                      identity=identf[0:31, 0:31])) for c in range(4)],
                 reads=[B_cwrow, B_ident], writes=[B_pb[1]])
            P.op("dve", lambda v: v.tensor_copy(out=cw[:], in_=pbs[1][:, :, 0:31]), reads=[B_pb[1]], writes=[B_cw])
            for c in range(4):
                P.op("dve", (lambda v, c=c: v.tensor_tensor(
                    out=cdiag[:, c * 31:(c + 1) * 31, :],
                    in0=identf[:].unsqueeze(1).to_broadcast([128, 31, 128]),
                    in1=cw[:, c, :].unsqueeze(2).to_broadcast([128, 31, 128]), op=ALU.mult)),
                    reads=[B_ident, B_cw], writes=[B_cdiag])
            P.op("pool", lambda g: g.affine_select(out=sgw_m[:], in_=sgw_f[:], pattern=[[0, 8], [-1, 128]],
                                                   compare_op=ALU.is_ge, fill=0.0, base=0, channel_multiplier=1),
                 reads=[B_sgwf], writes=[B_sgwm])
            P.op("pe", [(lambda t, h=h: t.transpose(out=ptb[:, h, :], in_=sgw_m[:, h, :], identity=ident[:])) for h in range(8)],
                 reads=[B_sgwm, B_ident], writes=[B_ptb])
            P.op("dve", lambda v: v.tensor_copy(out=wmT[:], in_=ptb[:]), reads=[B_ptb], writes=[B_wmT])

            def dbl(name, shape, dt):
                return [sbt("%s%d" % (name, i), shape, dt) for i in range(2)], [Buf("%s%d" % (name, i)) for i in range(2)]
            xb, B_xb = dbl("xb", [128, D], F32)
            h0bf, B_h0bf = dbl("h0bf", [128, D], BF16)
            h0T, B_h0T = dbl("h0T", [128, 8, 128], BF16)
            sig, B_sig = dbl("sig", [128, 4, 128], F32)
            cT, B_cT = dbl("cT", [128, 4, 158], BF16)
            yT, B_yT = dbl("yT", [128, 4, 128], F32)
            dd, B_dd = dbl("dd", [128, 4, 128], F32)
            sq, B_sq = dbl("sq", [128, 4, 128], F32)
            coT, B_coT = dbl("coT", [128, 4, 128], BF16)
            uu, B_uu = dbl("uu", [128, 512], F32)
            gv, B_gv = dbl("gv", [128, 512], F32)
            sq2, B_sq2 = dbl("sq2", [128, 512], F32)
            vbf, B_vbf = dbl("vbf", [128, 512], BF16)
            sgo, B_sgo = dbl("sgo", [128, 512], BF16)
            sgoT, B_sgoT = dbl("sgoT", [128, 4, 128], BF16)
            r1, B_r1 = dbl("r1", [128, D], F32)
            st, B_st = dbl("st", [128, 64], F32)

            P.op("dve", lambda v: v.memset(cT[0][:, :, 0:30], 0.0), writes=[B_cT[0]])

            def mk_ops(steps):
                def op(*a_, **k_):
                    steps.append(lambda: P.op(*a_, **k_))

                def dma(*a_, **k_):
                    steps.append(lambda: P.dma(*a_, **k_))
                return op, dma

            def layer_norm(op, src, Bsrc, dst, Bdst, stt, Bstt, base):
                s_stats = stt[:, base:base + 12]
                s_mv = stt[:, base + 12:base + 14]
                s_sd = stt[:, base + 14:base + 15]
                s_rs = stt[:, base + 15:base + 16]
                s_nm = stt[:, base + 16:base + 17]
                op("dve", [lambda v: v.bn_stats(out=stt[:, base:base + 6], in_=src[:, 0:512]),
                           lambda v: v.bn_stats(out=stt[:, base + 6:base + 12], in_=src[:, 512:1024])],
                   reads=[Bsrc], writes=[Bstt])
                op("dve", lambda v: v.bn_aggr(out=s_mv, in_=s_stats), reads=[Bstt], writes=[Bstt])
                op("act", lambda a: a.activation(out=s_sd, in_=stt[:, base + 13:base + 14], func=AF.Sqrt,
                                                 bias=eps_t[:, 0:1], scale=1.0), reads=[Bstt, B_eps], writes=[Bstt])
                op("dve", lambda v: v.reciprocal(out=s_rs, in_=s_sd), reads=[Bstt], writes=[Bstt])
                op("dve", lambda v: v.tensor_scalar(out=s_nm, in0=stt[:, base + 12:base + 13], scalar1=s_rs, scalar2=-1.0,
                                                    op0=ALU.mult, op1=ALU.mult), reads=[Bstt], writes=[Bstt])
                op("act", lambda a: a.activation(out=dst[:], in_=src[:], func=AF.Identity, bias=s_nm, scale=s_rs),
                   reads=[Bsrc, Bstt], writes=[Bdst])

            def stageA(b):
                steps = []
                op, dma = mk_ops(steps)
                i = b % 2
                j = (b + 1) % 2
                r0 = b * 128
                X = xb[i]; BX = B_xb[i]
                dma("sp", (lambda q, X=X, r0=r0: q.dma_start(out=X[:], in_=x_d[r0:r0 + 128, :])), BX, writes=[BX])
                layer_norm(op, X, BX, X, BX, st[i], B_st[i], 0)
                op("pool", lambda g, X=X: g.tensor_tensor(out=X[:], in0=X[:], in1=g0[:], op=ALU.mult), reads=[BX, B_bc], writes=[BX])
                op("pool", lambda g, X=X: g.tensor_tensor(out=X[:], in0=X[:], in1=b0[:], op=ALU.add), reads=[BX, B_bc], writes=[BX])
                op("act", lambda a, X=X, i=i: a.activation(out=h0bf[i][:], in_=X[:], func=AF.Copy), reads=[BX], writes=[B_h0bf[i]])
                op("pe", [(lambda t, k=k, i=i: t.transpose(out=ptb[:, k, :], in_=h0bf[i][:, k * 128:(k + 1) * 128], identity=ident[:]))
                          for k in range(8)], reads=[B_h0bf[i], B_ident], writes=[B_ptb])
                op("act", lambda a, i=i: a.activation(out=h0T[i][:], in_=ptb[:], func=AF.Copy), reads=[B_ptb], writes=[B_h0T[i]])
                for (bank, off) in ((0, 0), (1, 512)):
                    op("pe", [(lambda t, c=c, k=k, bank=bank, off=off, i=i: t.matmul(
                        pbs[bank][:, c, :], lhsT=w_in_bf[:, k, off + c * 128:off + (c + 1) * 128], rhs=h0T[i][:, k, :],
                        start=(k == 0), stop=(k == 7))) for c in range(4) for k in range(8)],
                        reads=[B_w_in, B_h0T[i]], writes=[B_pb[bank]])
                for (bank, off) in ((2, 1024), (3, 1536)):
                    fl = [(lambda t, bank=bank, off=off: t.matmul(flat(pbs[bank]), lhsT=ones_row[:, :], rhs=brow[:, off - 1024:off - 512],
                                                                   start=True, stop=False))]
                    fl += [(lambda t, k=k, bank=bank, off=off, i=i: t.matmul(flat(pbs[bank]), lhsT=h0T[i][:, k, :],
                                                                              rhs=w_in_bf[:, k, off:off + 512], start=False, stop=(k == 7)))
                           for k in range(8)]
                    op("pe", fl, reads=[B_w_in, B_h0T[i], B_ones, B_brow], writes=[B_pb[bank]])
                op("act", [(lambda a, c=c, i=i: a.activation(out=sig[i][:, c, :], in_=pbs[1][:, c, :], func=AF.Sigmoid,
                                                             bias=cols[:, 4 + c:5 + c], scale=1.0)) for c in range(4)],
                   reads=[B_pb[1], B_cols], writes=[B_sig[i]])
                op("dve", [(lambda v, c=c, i=i: v.scalar_tensor_tensor(out=cT[i][:, c, 30:158], in0=pbs[0][:, c, :],
                                                                        scalar=cols[:, c:c + 1], in1=sig[i][:, c, :],
                                                                        op0=ALU.add, op1=ALU.mult)) for c in range(4)],
                   reads=[B_pb[0], B_sig[i], B_cols], writes=[B_cT[i]])
                if b + 1 < nblk:
                    op("pool", lambda g, i=i, j=j: g.tensor_copy(out=cT[j][:, :, 0:30], in_=cT[i][:, :, 128:158]),
                       reads=[B_cT[i]], writes=[B_cT[j]])
                op("act", lambda a, i=i: a.activation(out=uu[i][:], in_=flat(pbs[2]), func=AF.Gelu), reads=[B_pb[2]], writes=[B_uu[i]])
                op("act", lambda a, i=i: a.activation(out=gv[i][:], in_=flat(pbs[3]), func=AF.Gelu), reads=[B_pb[3]], writes=[B_gv[i]])
                return steps

            def stageB(b):
                steps = []
                op, dma = mk_ops(steps)
                i = b % 2
                r0 = b * 128
                X = xb[i]; BX = B_xb[i]
                op("pe", [(lambda t, c=c, k=k, i=i: t.matmul(pbs[4][:, c, :], lhsT=cdiag[:, c * 31 + k, :], rhs=cT[i][:, c, k:k + 128],
                                                             start=(k == 0), stop=(k == 30))) for c in range(4) for k in range(31)],
                   reads=[B_cdiag, B_cT[i]], writes=[B_pb[4]])
                op("act", [(lambda a, c=c, i=i: a.activation(out=yT[i][:, c, :], in_=pbs[4][:, c, :], func=AF.Identity,
                                                             bias=cols[:, 8 + c:9 + c], scale=1.0)) for c in range(4)],
                   reads=[B_pb[4], B_cols], writes=[B_yT[i]])
                gv3 = gv[i][:].rearrange("p (g d) -> p g d", g=8)
                sq3 = sq2[i][:].rearrange("p (g d) -> p g d", g=8)
                S = st[i]; BS = B_st[i]
                op("dve", lambda v, gv3=gv3, S=S: v.tensor_reduce(out=S[:, 24:32], in_=gv3, axis=AX.X, op=ALU.add), reads=[B_gv[i]], writes=[BS])
                op("pe", lambda t, i=i: t.matmul(flat(pbs[5]), lhsT=Gm[:], rhs=flat(yT[i]), start=True, stop=True),
                   reads=[B_G, B_yT[i]], writes=[B_pb[5]])
                op("dve", lambda v, S=S: v.tensor_scalar(out=S[:, 24:32], in0=S[:, 24:32], scalar1=1.0 / 64.0, scalar2=None, op0=ALU.mult),
                   reads=[BS], writes=[BS])
                op("dve", lambda v, i=i: v.tensor_tensor(out=flat(dd[i]), in0=flat(yT[i]), in1=flat(pbs[5]), op=ALU.subtract),
                   reads=[B_yT[i], B_pb[5]], writes=[B_dd[i]])
                op("act", lambda a, i=i: a.activation(out=flat(sq[i]), in_=flat(dd[i]), func=AF.Square), reads=[B_dd[i]], writes=[B_sq[i]])
                op("dve", lambda v, gv3=gv3, S=S: v.tensor_tensor(out=gv3, in0=gv3, in1=S[:, 24:32].unsqueeze(2).to_broadcast([128, 8, 64]),
                                                                   op=ALU.subtract), reads=[B_gv[i], BS], writes=[B_gv[i]])
                op("pe", lambda t, i=i: t.matmul(flat(pbs[6]), lhsT=Gm[:], rhs=flat(sq[i]), start=True, stop=True),
                   reads=[B_G, B_sq[i]], writes=[B_pb[6]])
                op("act", lambda a, i=i: a.activation(out=sq2[i][:], in_=gv[i][:], func=AF.Square), reads=[B_gv[i]], writes=[B_sq2[i]])
                op("act", lambda a, i=i: a.activation(out=flat(sq[i]), in_=flat(pbs[6]), func=AF.Sqrt, bias=eps_t[:, 0:1], scale=1.0),
                   reads=[B_pb[6], B_eps], writes=[B_sq[i]])
                op("dve", lambda v, sq3=sq3, S=S: v.tensor_reduce(out=S[:, 32:40], in_=sq3, axis=AX.X, op=ALU.add), reads=[B_sq2[i]], writes=[BS])
                op("dve", lambda v, i=i: v.reciprocal(out=flat(sq[i]), in_=flat(sq[i])), reads=[B_sq[i]], writes=[B_sq[i]])
                op("act", lambda a, S=S: a.activation(out=S[:, 40:48], in_=S[:, 32:40], func=AF.Sqrt, bias=eps_t[:, 0:1], scale=1.0 / 64.0),
                   reads=[BS, B_eps], writes=[BS])
                op("dve", lambda v, i=i: v.tensor_tensor(out=flat(dd[i]), in0=flat(dd[i]), in1=flat(sq[i]), op=ALU.mult),
                   reads=[B_dd[i], B_sq[i]], writes=[B_dd[i]])
                op("act", [(lambda a, c=c, i=i: a.activation(out=coT[i][:, c, :], in_=dd[i][:, c, :], func=AF.Silu,
                                                             bias=cols[:, 16 + c:17 + c], scale=cols[:, 12 + c:13 + c])) for c in range(4)],
                   reads=[B_dd[i], B_cols], writes=[B_coT[i]])
                op("dve", lambda v, S=S: v.reciprocal(out=S[:, 48:56], in_=S[:, 40:48]), reads=[BS], writes=[BS])
                op("dve", lambda v, gv3=gv3, S=S: v.tensor_tensor(out=gv3, in0=gv3, in1=S[:, 48:56].unsqueeze(2).to_broadcast([128, 8, 64]),
                                                                   op=ALU.mult), reads=[B_gv[i], BS], writes=[B_gv[i]])
                op("pool", lambda g, i=i: g.tensor_tensor(out=gv[i][:], in0=gv[i][:], in1=sglg[:], op=ALU.mult), reads=[B_gv[i], B_bc], writes=[B_gv[i]])
                op("pool", lambda g, i=i: g.tensor_tensor(out=vbf[i][:], in0=gv[i][:], in1=sglb[:], op=ALU.add), reads=[B_gv[i], B_bc], writes=[B_vbf[i]])
                op("pe", [(lambda t, h=h, i=i: t.matmul(flat(pbs[5])[:, h * 64:(h + 1) * 64], lhsT=wmT[:, h, :], rhs=vbf[i][:, h * 64:(h + 1) * 64],
                                                        start=True, stop=True)) for h in range(8)],
                   reads=[B_wmT, B_vbf[i]], writes=[B_pb[5]])
                f3 = flat(pbs[5]).rearrange("p (g d) -> p g d", g=8)
                op("dve", lambda v, f3=f3, i=i: v.tensor_tensor(out=sq2[i][:].rearrange("p (g d) -> p g d", g=8), in0=f3,
                                                                in1=cols[:, 20:28].unsqueeze(2).to_broadcast([128, 8, 64]), op=ALU.add),
                   reads=[B_pb[5], B_cols], writes=[B_sq2[i]])
                op("dve", lambda v, i=i: v.tensor_tensor(out=sgo[i][:], in0=sq2[i][:], in1=uu[i][:], op=ALU.mult),
                   reads=[B_sq2[i], B_uu[i]], writes=[B_sgo[i]])
                op("pe", [(lambda t, c=c, i=i: t.transpose(out=ptb[:, c, :], in_=sgo[i][:, c * 128:(c + 1) * 128], identity=ident[:]))
                          for c in range(4)], reads=[B_sgo[i], B_ident], writes=[B_ptb])
                op("act", lambda a, i=i: a.activation(out=sgoT[i][:], in_=ptb[:, 0:4, :], func=AF.Copy), reads=[B_ptb], writes=[B_sgoT[i]])
                mb = (4, 6)
                for n in range(2):
                    fl = [(lambda t, n=n: t.matmul(flat(pbs[mb[n]]), lhsT=ones_row[:, :], rhs=brow[:, 1024 + n * 512:1024 + (n + 1) * 512],
                                                   start=True, stop=False))]
                    fl += [(lambda t, k=k, n=n, i=i: t.matmul(flat(pbs[mb[n]]), lhsT=(coT[i][:, k, :] if k < 4 else sgoT[i][:, k - 4, :]),
                                                              rhs=w_o_bf[:, k, n * 512:(n + 1) * 512], start=False, stop=(k == 7)))
                           for k in range(8)]
                    op("pe", fl, reads=[B_w_o, B_coT[i], B_sgoT[i], B_ones, B_brow], writes=[B_pb[mb[n]]])
                op("dve", [(lambda v, n=n, i=i, X=X: v.scalar_tensor_tensor(out=r1[i][:, n * 512:(n + 1) * 512], in0=X[:, n * 512:(n + 1) * 512],
                                                                             scalar=ALPHA, in1=flat(pbs[mb[n]]), op0=ALU.mult, op1=ALU.add))
                           for n in range(2)], reads=[BX, B_pb[4], B_pb[6]], writes=[B_r1[i]])
                R = r1[i]; BR = B_r1[i]
                layer_norm(op, R, BR, R, BR, st[i], B_st[i], 0)
                op("pool", lambda g, R=R: g.tensor_tensor(out=R[:], in0=R[:], in1=g1[:], op=ALU.mult), reads=[BR, B_bc], writes=[BR])
                op("pool", lambda g, R=R: g.tensor_tensor(out=R[:], in0=R[:], in1=b1[:], op=ALU.add), reads=[BR, B_bc], writes=[BR])
                dst = out_d if debug_h1 else h1_d
                dma("sp", (lambda q, R=R, r0=r0, dst=dst: q.dma_start(out=dst[r0:r0 + 128, :], in_=R[:])), BR, reads=[BR], writes=[h1d_bufs[b]])
                return steps

            B_uvtab = Buf("uvtab")
            conv_jobs = [(t_, c_) for c_ in range(32) for t_ in range(2)]

            def issue_conv(k):
                for (t_, c_) in conv_jobs[k::nblk] if nblk < 32 else conv_jobs[2 * k:2 * k + 2]:
                    tab = peer_u if t_ == 0 else peer_v
                    P.dma_nowait("pool", (lambda q, tab=tab, t_=t_, c_=c_: q.dma_start(
                        out=uv_d[c_ * 512:(c_ + 1) * 512, t_ * D:(t_ + 1) * D], in_=tab[c_ * 512:(c_ + 1) * 512, :])), B_uvtab)

            if not debug_h1:
                issue_conv(0)
            for st_ in stageA(0):
                st_()
            for b in range(nblk):
                sB = stageB(b)
                sA = stageA(b + 1) if b + 1 < nblk else []
                if not debug_h1 and b + 1 < nblk:
                    issue_conv(b + 1)
                ia = 0
                for k, st_ in enumerate(sB):
                    st_()
                    tgt = ((k + 1) * len(sA)) // len(sB)
                    while ia < tgt:
                        sA[ia]()
                        ia += 1
                while ia < len(sA):
                    sA[ia]()
                    ia += 1

            P.barrier()
            with nc.Block() as blk:
                P.emit(blk)

        if debug_h1:
            return nc

        with ExitStack() as s2:
            def sbt(name, shape, dt):
                return s2.enter_context(nc.sbuf_tensor(name, list(shape), dt))

            def pst(name, shape, dt):
                return s2.enter_context(nc.psum_tensor(name, list(shape), dt))

            def flat(t):
                return t[:].rearrange("p c t -> p (c t)")

            wq_bf = sbt("wq_bf", [128, 8, 2048], BF16); B_wq = Buf("wq")
            wg_bf = sbt("wg_bf", [128, 8, 1024], BF16); B_wg = Buf("wg")
            wp_bf = sbt("wp_bf", [128, 2, 1024], BF16); B_wp = Buf("wp")
            keys_f = sbt("keys_f", [128, 16, 128], BF16); B_keysf = Buf("keysf")
            keysT = sbt("keysT", [128, 16, 128], BF16); B_keysT = Buf("keysT")
            g2 = sbt("g2", [128, D], F32); b2 = sbt("b2", [128, D], F32); B_bc = Buf("bc2")
            identf = sbt("identf2", [128, 128], F32); ident = sbt("ident2", [128, 128], BF16); B_ident = Buf("ident2")
            brow = sbt("brow2", [1, 1024], BF16); B_brow = Buf("brow2")
            ones_row = sbt("ones_row2", [1, 128], BF16); B_ones = Buf("ones2")
            eps_t = sbt("eps_t2", [128, 1], F32); B_eps = Buf("eps2")
            iota16 = sbt("iota16", [128, 16], F32); B_iota = Buf("iota")

            ptb = pst("ptb2", [128, 8, 128], BF16); B_ptb = Buf("ptb2")
            pq = [pst("pq%d" % i, [128, 4, 128], F32) for i in range(2)]; B_pq = [Buf("pq%d" % i) for i in range(2)]
            psc = [pst("psc%d" % i, [128, 4, 128], F32) for i in range(2)]; B_psc = [Buf("psc%d" % i) for i in range(2)]
            ppl = pst("ppl", [128, 4, 128], F32); B_ppl = Buf("ppl")
            py = [pst("py%d" % i, [128, 4, 128], F32) for i in range(2)]; B_py = Buf("py")

            for k2 in range(2):
                for kk in range(8):
                    P.dma("pool", (lambda g, kk=kk, k2=k2: g.dma_start(
                        out=wq_bf[:, kk, k2 * 1024:(k2 + 1) * 1024],
                        in_=peer_wq[kk * 128:(kk + 1) * 128, k2 * 1024:(k2 + 1) * 1024])), B_wq, writes=[B_wq])
            for kk in range(8):
                P.dma("pool", (lambda g, kk=kk: g.dma_start(out=wg_bf[:, kk, :], in_=ple_wg[kk * 128:(kk + 1) * 128, :])), B_wg, writes=[B_wg])
            for kk in range(2):
                P.dma("pool", (lambda g, kk=kk: g.dma_start(out=wp_bf[:, kk, :], in_=ple_wp[kk * 128:(kk + 1) * 128, :])), B_wp, writes=[B_wp])
            P.dma("pool", lambda g: g.dma_start(out=keys_f[:], in_=peer_keys.rearrange("h k d -> k h d")), B_keysf, writes=[B_keysf])
            P.dma("pool", lambda g: g.dma_start(out=brow[:, :], in_=ple_bg.unsqueeze(0)), B_brow, writes=[B_brow])
            for (t_, v_) in ((g2, ln2_g), (b2, ln2_b)):
                P.dma("sp", (lambda q, t_=t_, v_=v_: q.dma_start(out=t_[:], in_=v_.partition_broadcast(128))), B_bc, writes=[B_bc])
            P.op("pool", lambda g: g.memset(identf[:], 0.0), writes=[B_ident])
            P.op("pool", lambda g: g.affine_select(out=identf[:], in_=identf[:], pattern=[[-1, 128]],
                                                   compare_op=ALU.not_equal, fill=1.0, base=0, channel_multiplier=1),
                 reads=[B_ident], writes=[B_ident])
            P.op("pool", lambda g: g.iota(iota16[:], pattern=[[1, 16]], base=0, channel_multiplier=0, allow_small_or_imprecise_dtypes=True),
                 writes=[B_iota])
            P.op("dve", lambda v: v.tensor_copy(out=ident[:], in_=identf[:]), reads=[B_ident], writes=[B_ident])
            P.op("dve", lambda v: v.memset(ones_row[:], 1.0), writes=[B_ones])
            P.op("dve", lambda v: v.memset(eps_t[:], EPS), writes=[B_eps])
            for half in range(2):
                P.op("pe", [(lambda t, q=q, half=half: t.transpose(out=ptb[:, q, :], in_=keys_f[:, half * 8 + q, :], identity=ident[:]))
                            for q in range(8)], reads=[B_keysf, B_ident], writes=[B_ptb])
                P.op("dve", lambda v, half=half: v.tensor_copy(out=keysT[:, half * 8:(half + 1) * 8, :], in_=ptb[:]),
                     reads=[B_ptb], writes=[B_keysT])

            UV = [sbt("UV%d" % s, [128, 2 * D], BF16) for s in range(NRING)]; B_UV = [Buf("UV%d" % s) for s in range(NRING)]
            dring = [sbt("dg%d" % s, [128, 128], BF16) for s in range(NDIAG)]; B_dg = [Buf("dg%d" % s) for s in range(NDIAG)]

            def dbl(name, shape, dt):
                return [sbt("%s%d" % (name, i), shape, dt) for i in range(2)], [Buf("%s%d" % (name, i)) for i in range(2)]
            h1, B_h1 = dbl("h1_", [128, D], F32)
            rp, B_rp = dbl("rp", [128, D], F32)
            idx, B_idx = dbl("idx", [128, 128], U32)
            gsm, B_gsm = dbl("gsm", [128, 128], F32)
            pld, B_pld = dbl("pld", [128, 256], F32)
            h1bf = sbt("h1bf", [128, D], BF16); B_h1bf = Buf("h1bf")
            h1T = sbt("h1T", [128, 8, 128], BF16); B_h1T = Buf("h1T")
            pbf = sbt("pbf", [128, 256], BF16); B_pbf = Buf("pbf")
            pT = sbt("pT", [128, 2, 128], BF16); B_pT = Buf("pT")
            qT = sbt("qT", [128, 16, 128], BF16); B_qT = [Buf("qT%d" % i) for i in range(4)]
            scs = sbt("scs", [128, 16, 128], F32); B_scs = [Buf("scs%d" % i) for i in range(4)]
            wk = sbt("wk", [128, 8, 128], F32); B_wk = Buf("wk")
            m8 = sbt("m8", [128, 16, 16], F32); B_m8 = Buf("m8")
            i8 = sbt("i8", [128, 16, 16], U32); B_i8 = Buf("i8")
            i8f = sbt("i8f", [128, 16, 16], F32); B_i8f = Buf("i8f")
            comb = scs[:, 0:8, :].rearrange("p (h a) k -> p h (a k)", a=2)
            wk2 = scs[:, 8:16, :].rearrange("p (h a) k -> p h (a k)", a=2)
            B_combL = [B_scs[0], B_scs[1]]
            B_wk2L = [B_scs[2], B_scs[3]]
            c16 = sbt("c16", [128, 8, 16], F32); B_c16 = Buf("c16")
            pos = sbt("pos", [128, 8, 16], U32); B_pos = Buf("pos")
            apos = sbt("apos", [128, 8, 16], U32); bpos = sbt("bpos", [128, 8, 16], U32)
            aposf = sbt("aposf", [128, 8, 16], F32); bposf = sbt("bposf", [128, 8, 16], F32); B_ab = Buf("ab")
            oh = wk[:].rearrange("p (h a) k -> p h (a k)", a=2).rearrange("p h (a c) -> p h a c", a=16)
            B_oh = B_wk
            asel = sbt("asel", [128, 8, 16], F32); bsel = sbt("bsel", [128, 8, 16], F32); B_sel = Buf("sel")
            idxf = sbt("idxf", [128, 128], F32); B_idxf = Buf("idxf")
            sm = sbt("sm", [128, 32], F32); B_sm = Buf("sm")
            gate = sbt("gate", [128, 512], F32); B_gate = Buf("gate")
            junk = sbt("junk", [128, D], BF16); B_junk = Buf("junk")
            actt = sbt("actt", [128, 128], F32); B_actg = [Buf("actg%d" % g) for g in range(128 // GRP)]
            gel = sbt("gel", [128, 128], F32); B_gelg = [Buf("gelg%d" % g) for g in range(128 // GRP)]
            wgt = sbt("wgt", [128, 128], F32); B_wgtg = [Buf("wgtg%d" % g) for g in range(128 // GRP)]
            st2 = sbt("st2", [128, 32], F32); B_st2 = Buf("st2")

            NG = 128 // GRP
            ring_ctr = [0]

            def front(b):
                steps = []

                def op(*a_, **k_):
                    steps.append(lambda: P.op(*a_, **k_))

                def dma(*a_, **k_):
                    steps.append(lambda: P.dma(*a_, **k_))
                i = b % 2
                r0 = b * 128
                H = h1[i]; BH = B_h1[i]
                dma("sp", (lambda q, H=H, r0=r0: q.dma_start(out=H[:], in_=h1_d[r0:r0 + 128, :])), BH, reads=[h1d_bufs[b]], writes=[BH])
                dma("sp", (lambda q, i=i, r0=r0: q.dma_start(out=pld[i][:], in_=p_d[r0:r0 + 128, :])), B_pld[i], writes=[B_pld[i]])
                op("act", lambda a, H=H: a.activation(out=h1bf[:], in_=H[:], func=AF.Copy), reads=[BH], writes=[B_h1bf])
                op("act", lambda a, i=i: a.activation(out=pbf[:], in_=pld[i][:], func=AF.Copy), reads=[B_pld[i]], writes=[B_pbf])
                op("pe", [(lambda t, k=k: t.transpose(out=ptb[:, k, :], in_=h1bf[:, k * 128:(k + 1) * 128], identity=ident[:]))
                            for k in range(8)], reads=[B_h1bf, B_ident], writes=[B_ptb])
                op("act", lambda a: a.activation(out=h1T[:], in_=ptb[:], func=AF.Copy), reads=[B_ptb], writes=[B_h1T])
                op("pe", [(lambda t, k=k: t.transpose(out=ptb[:, k, :], in_=pbf[:, k * 128:(k + 1) * 128], identity=ident[:]))
                            for k in range(2)], reads=[B_pbf, B_ident], writes=[B_ptb])
                op("act", lambda a: a.activation(out=pT[:], in_=ptb[:, 0:2, :], func=AF.Copy), reads=[B_ptb], writes=[B_pT])
                for qd in range(4):
                    pb_ = pq[qd % 2]; Bp = B_pq[qd % 2]
                    op("pe", [(lambda t, c=c, k=k, qd=qd, pb_=pb_: t.matmul(
                        pb_[:, c, :], lhsT=wq_bf[:, k, (qd * 4 + c) * 128:(qd * 4 + c + 1) * 128], rhs=h1T[:, k, :],
                        start=(k == 0), stop=(k == 7))) for c in range(4) for k in range(8)],
                        reads=[B_wq, B_h1T], writes=[Bp])
                    op("act", lambda a, qd=qd, pb_=pb_: a.activation(out=qT[:, qd * 4:(qd + 1) * 4, :], in_=pb_[:], func=AF.Copy),
                         reads=[Bp], writes=[B_qT[qd]])
                    ps_ = psc[qd % 2]; Bs = B_psc[qd % 2]
                    op("pe", [(lambda t, c=c, qd=qd, ps_=ps_: t.matmul(ps_[:, c, :], lhsT=qT[:, qd * 4 + c, :], rhs=keysT[:, qd * 4 + c, :],
                                                                          start=True, stop=True)) for c in range(4)],
                         reads=[B_qT[qd], B_keysT], writes=[Bs])
                    op("act", lambda a, qd=qd, ps_=ps_: a.activation(out=scs[:, qd * 4:(qd + 1) * 4, :], in_=ps_[:], func=AF.Copy),
                         reads=[Bs], writes=[B_scs[qd]])
                for n in range(2):
                    fl = [(lambda t, n=n: t.matmul(flat(ppl), lhsT=ones_row[:, :], rhs=brow[:, n * 512:(n + 1) * 512], start=True, stop=False))]
                    fl += [(lambda t, k=k, n=n: t.matmul(flat(ppl), lhsT=h1T[:, k, :], rhs=wg_bf[:, k, n * 512:(n + 1) * 512],
                                                          start=False, stop=(k == 7))) for k in range(8)]
                    op("pe", fl, reads=[B_wg, B_h1T, B_ones, B_brow], writes=[B_ppl])
                    op("act", lambda a: a.activation(out=gate[:], in_=flat(ppl), func=AF.Sigmoid), reads=[B_ppl], writes=[B_gate])
                    op("pe", [(lambda t, k=k, n=n: t.matmul(flat(ppl), lhsT=pT[:, k, :], rhs=wp_bf[:, k, n * 512:(n + 1) * 512],
                                                               start=(k == 0), stop=(k == 1))) for k in range(2)],
                         reads=[B_wp, B_pT], writes=[B_ppl])
                    op("dve", lambda v: v.tensor_tensor(out=gate[:], in0=gate[:], in1=flat(ppl), op=ALU.mult),
                         reads=[B_gate, B_ppl], writes=[B_gate])
                    op("dve", lambda v, n=n, i=i, H=H: v.scalar_tensor_tensor(out=rp[i][:, n * 512:(n + 1) * 512], in0=H[:, n * 512:(n + 1) * 512],
                                                                               scalar=ALPHA, in1=gate[:], op0=ALU.mult, op1=ALU.add),
                         reads=[BH, B_gate], writes=[B_rp[i]])
                op("dve", [(lambda v, hh=hh: v.max(out=m8[:, hh, 0:8], in_=scs[:, hh, :])) for hh in range(16)], reads=B_scs, writes=[B_m8])
                for hv in range(2):
                    op("dve", [(lambda v, hh=hh, hv=hv: v.match_replace(out=wk[:, hh - 8 * hv, :], in_to_replace=m8[:, hh, 0:8],
                                                                          in_values=scs[:, hh, :], imm_value=NEG))
                                 for hh in range(8 * hv, 8 * hv + 8)], reads=B_scs + [B_m8], writes=[B_wk])
                    op("dve", [(lambda v, hh=hh, hv=hv: v.max(out=m8[:, hh, 8:16], in_=wk[:, hh - 8 * hv, :]))
                                 for hh in range(8 * hv, 8 * hv + 8)], reads=[B_wk], writes=[B_m8])
                op("dve", [(lambda v, hh=hh, o=o: v.max_index(out=i8[:, hh, o:o + 8], in_max=m8[:, hh, o:o + 8], in_values=scs[:, hh, :]))
                             for hh in range(16) for o in (0, 8)], reads=B_scs + [B_m8], writes=[B_i8])
                op("dve", lambda v: v.tensor_copy(out=i8f[:], in_=i8[:]), reads=[B_i8], writes=[B_i8f])
                m84 = m8[:].rearrange("p (h t) k -> p h t k", t=2)
                i84 = i8f[:].rearrange("p (h t) k -> p h t k", t=2)
                for hf in range(2):
                    hs = slice(hf * 4, hf * 4 + 4)
                    comb4 = comb[:].rearrange("p h (a c) -> p h a c", a=16)
                    op("dve", lambda v, hs=hs, comb4=comb4: v.tensor_tensor(
                        out=comb4, in0=m84[:, hs, 0, :].unsqueeze(3).to_broadcast([128, 4, 16, 16]),
                        in1=m84[:, hs, 1, :].unsqueeze(2).to_broadcast([128, 4, 16, 16]), op=ALU.add),
                        reads=[B_m8], writes=B_combL)
                    op("dve", [(lambda v, h=h, hf=hf: v.max(out=c16[:, hf * 4 + h, 0:8], in_=comb[:, h, :])) for h in range(4)],
                         reads=B_combL, writes=[B_c16])
                    op("dve", [(lambda v, h=h, hf=hf: v.match_replace(out=wk2[:, h, :], in_to_replace=c16[:, hf * 4 + h, 0:8],
                                                                         in_values=comb[:, h, :], imm_value=NEG)) for h in range(4)],
                         reads=B_combL + [B_c16], writes=B_wk2L)
                    op("dve", [(lambda v, h=h, hf=hf: v.max(out=c16[:, hf * 4 + h, 8:16], in_=wk2[:, h, :])) for h in range(4)],
                         reads=B_wk2L, writes=[B_c16])
                    op("dve", [(lambda v, h=h, hf=hf, o=o: v.max_index(out=pos[:, hf * 4 + h, o:o + 8], in_max=c16[:, hf * 4 + h, o:o + 8],
                                                                          in_values=comb[:, h, :])) for h in range(4) for o in (0, 8)],
                         reads=B_combL + [B_c16], writes=[B_pos])
                op("dve", [lambda v: v.tensor_single_scalar(out=apos[:], in_=pos[:], scalar=4, op=ALU.logical_shift_right),
                             lambda v: v.tensor_single_scalar(out=bpos[:], in_=pos[:], scalar=15, op=ALU.bitwise_and)],
                     reads=[B_pos], writes=[B_ab])
                op("dve", [lambda v: v.tensor_copy(out=aposf[:], in_=apos[:]),
                             lambda v: v.tensor_copy(out=bposf[:], in_=bpos[:])], reads=[B_ab], writes=[B_ab])
                io4 = iota16[:].unsqueeze(1).unsqueeze(1).to_broadcast([128, 4, 16, 16])
                for (pf, tsel, sel) in ((aposf, 0, asel), (bposf, 1, bsel)):
                    for hf in range(2):
                        hs = slice(hf * 4, hf * 4 + 4)
                        op("dve", lambda v, pf=pf, hs=hs: v.tensor_tensor(out=oh[:], in0=pf[:, hs, :].unsqueeze(3).to_broadcast([128, 4, 16, 16]),
                                                                            in1=io4, op=ALU.is_equal), reads=[B_ab, B_iota], writes=[B_oh])
                        op("dve", lambda v, tsel=tsel, hs=hs: v.tensor_tensor(out=oh[:], in0=oh[:],
                                                                                in1=i84[:, hs, tsel, :].unsqueeze(2).to_broadcast([128, 4, 16, 16]),
                                                                                op=ALU.mult), reads=[B_oh, B_i8f], writes=[B_oh])
                        op("dve", lambda v, sel=sel, hs=hs: v.tensor_reduce(out=sel[:, hs, :], in_=oh[:], axis=AX.X, op=ALU.add),
                             reads=[B_oh], writes=[B_sel])
                op("dve", lambda v: v.scalar_tensor_tensor(out=idxf[:], in0=asel[:].rearrange("p h k -> p (h k)"), scalar=128.0,
                                                             in1=bsel[:].rearrange("p h k -> p (h k)"), op0=ALU.mult, op1=ALU.add),
                     reads=[B_sel], writes=[B_idxf])
                op("dve", lambda v, i=i: v.tensor_copy(out=idx[i][:], in_=idxf[:]), reads=[B_idxf], writes=[B_idx[i]])
                op("dve", lambda v: v.tensor_scalar(out=sm[:, 0:8], in0=c16[:, :, 0], scalar1=-1.0, scalar2=None, op0=ALU.mult),
                     reads=[B_c16], writes=[B_sm])
                G3 = gsm[i][:].rearrange("p (h k) -> p h k", h=8)
                op("act", [(lambda a, h=h, G3=G3: a.activation(out=G3[:, h, :], in_=c16[:, h, :], func=AF.Exp, bias=sm[:, h:h + 1], scale=1.0,
                                                                  accum_out=sm[:, 8 + h:9 + h])) for h in range(8)],
                     reads=[B_c16, B_sm], writes=[B_gsm[i], B_sm])
                op("dve", lambda v: v.reciprocal(out=sm[:, 16:24], in_=sm[:, 8:16]), reads=[B_sm], writes=[B_sm])
                op("dve", lambda v, G3=G3: v.tensor_tensor(out=G3, in0=G3, in1=sm[:, 16:24].unsqueeze(2).to_broadcast([128, 8, 16]), op=ALU.mult),
                     reads=[B_gsm[i], B_sm], writes=[B_gsm[i]])

                def gen():
                    for st_ in steps:
                        st_()
                        yield
                return gen()

            nuse = nblk * 128

            def issue_gather(n):
                if n >= nuse:
                    return
                b_, jj = divmod(n, 128)
                i_ = b_ % 2
                s_ = n % NRING
                P.dma("pool", (lambda q, s_=s_, jj=jj, i_=i_: q.indirect_dma_start(
                    out=UV[s_][:], out_offset=None, in_=uv_d,
                    in_offset=bass.IndirectOffsetOnAxis(ap=idx[i_][:, jj:jj + 1], axis=0))),
                    B_UV[s_], reads=[B_idx[i_]], writes=[B_UV[s_]])

            def wmul(b, g):
                i = b % 2
                gs = slice(g * GRP, (g + 1) * GRP)
                P.op("dve", lambda v, gs=gs, i=i: v.tensor_tensor(out=wgt[:, gs], in0=gel[:, gs], in1=gsm[i][:, gs], op=ALU.mult),
                     reads=[B_gelg[g], B_gsm[i]], writes=[B_wgtg[g]])

            def vside(b, g):
                for e in range(GRP):
                    jj = g * GRP + e
                    s = (b * 128 + jj) % NRING
                    ds = (b * 128 + jj) % NDIAG
                    P.op("act", lambda a, ds=ds, jj=jj: a.activation(out=dring[ds][:], in_=identf[:], func=AF.Copy, scale=wgt[:, jj:jj + 1]),
                         reads=[B_wgtg[g], B_ident], writes=[B_dg[ds]])
                    P.op("pe", [(lambda t, n=n, ds=ds, s=s, jj=jj: t.matmul(flat(py[n]), lhsT=dring[ds][:], rhs=UV[s][:, D + n * 512:D + (n + 1) * 512],
                                                                             start=(jj == 0), stop=(jj == 127))) for n in range(2)],
                         reads=[B_dg[ds], B_UV[s]], writes=[B_py])
                    issue_gather(b * 128 + jj + NRING)

            def tail(b):
                i = b % 2
                r0 = b * 128
                r2 = rp[i]; B_r2 = B_rp[i]
                P.op("dve", [(lambda v, n=n, r2=r2: v.tensor_tensor(out=r2[:, n * 512:(n + 1) * 512], in0=r2[:, n * 512:(n + 1) * 512],
                                                                    in1=flat(py[n]), op=ALU.add)) for n in range(2)],
                     reads=[B_r2, B_py], writes=[B_r2])
                S = st2
                P.op("dve", [lambda v, r2=r2: v.bn_stats(out=S[:, 0:6], in_=r2[:, 0:512]),
                             lambda v, r2=r2: v.bn_stats(out=S[:, 6:12], in_=r2[:, 512:1024])], reads=[B_r2], writes=[B_st2])
                P.op("dve", lambda v: v.bn_aggr(out=S[:, 12:14], in_=S[:, 0:12]), reads=[B_st2], writes=[B_st2])
                P.op("act", lambda a: a.activation(out=S[:, 14:15], in_=S[:, 13:14], func=AF.Sqrt, bias=eps_t[:, 0:1], scale=1.0),
                     reads=[B_st2, B_eps], writes=[B_st2])
                P.op("dve", lambda v: v.reciprocal(out=S[:, 15:16], in_=S[:, 14:15]), reads=[B_st2], writes=[B_st2])
                P.op("dve", lambda v: v.tensor_scalar(out=S[:, 16:17], in0=S[:, 12:13], scalar1=S[:, 15:16], scalar2=-1.0, op0=ALU.mult, op1=ALU.mult),
                     reads=[B_st2], writes=[B_st2])
                P.op("act", lambda a, r2=r2: a.activation(out=r2[:], in_=r2[:], func=AF.Identity, bias=S[:, 16:17], scale=S[:, 15:16]),
                     reads=[B_r2, B_st2], writes=[B_r2])
                P.op("dve", lambda v, r2=r2: v.tensor_tensor(out=r2[:], in0=r2[:], in1=g2[:], op=ALU.mult), reads=[B_r2, B_bc], writes=[B_r2])
                P.op("dve", lambda v, r2=r2: v.tensor_tensor(out=r2[:], in0=r2[:], in1=b2[:], op=ALU.add), reads=[B_r2, B_bc], writes=[B_r2])
                P.dma("sp", (lambda q, r0=r0, r2=r2: q.dma_start(out=out_d[r0:r0 + 128, :], in_=r2[:])), B_r2, reads=[B_r2])

            def back(b, fg):
                i = b % 2
                H = h1[i]; BH = B_h1[i]
                for g in range(NG):
                    gs = slice(g * GRP, (g + 1) * GRP)
                    if g == NG - 1 and fg is not None:
                        for _ in fg:
                            pass
                    for e in range(GRP):
                        jj = g * GRP + e
                        s = (b * 128 + jj) % NRING
                        P.op("dve", lambda v, s=s, jj=jj, H=H: v.scalar_tensor_tensor(out=junk[:], in0=UV[s][:, 0:D], scalar=1.0, in1=H[:],
                                                                                     op0=ALU.mult, op1=ALU.mult, accum_out=actt[:, jj:jj + 1]),
                             reads=[B_UV[s], BH], writes=[B_junk, B_actg[g]])
                        if e == 0 and g >= 1:
                            wmul(b, g - 1)
                            vside(b, g - 1)
                        if e == GRP - 1 and g == 0 and b >= 1:
                            tail(b - 1)
                        if fg is not None and g < NG - 1:
                            next(fg, None)
                    P.op("act", lambda a, gs=gs: a.activation(out=gel[:, gs], in_=actt[:, gs], func=AF.Gelu), reads=[B_actg[g]], writes=[B_gelg[g]])
                wmul(b, NG - 1)
                vside(b, NG - 1)
                if b == nblk - 1:
                    tail(b)

            for _ in front(0):
                pass
            for n_ in range(NRING):
                issue_gather(n_)
            for b in range(nblk):
                back(b, front(b + 1) if b + 1 < nblk else None)

            P.barrier()
            with nc.Block() as blk:
                P.emit(blk)
    return nc


_W_NAMES = ["ln0_g", "ln0_b", "w_in", "b_in", "conv_w", "conv_b", "gn_g", "gn_b", "sg_ln_g", "sg_ln_b", "sg_w", "sg_b",
            "w_o", "b_o", "ln1_g", "ln1_b", "peer_wq", "peer_keys", "peer_u", "peer_v", "ple_wp", "ple_wg", "ple_bg",
            "ln2_g", "ln2_b"]


def _prep_weights(inp):
    w = {}
    for k in _W_NAMES:
        a = np.asarray(inp[k], dtype=np.float32)
        if k in ("ln0_g", "ln0_b"):
            w[k] = np.ascontiguousarray(a.reshape(1024))
        elif k == "peer_keys":
            w[k] = np.ascontiguousarray(a.reshape(16, 128, 128))
        else:
            w[k] = np.ascontiguousarray(a[0])
    return w


def kernel(**inputs):
    n = 8
    x = np.asarray(inputs["x"], dtype=np.float32)
    p = np.asarray(inputs["p"], dtype=np.float32)
    w = _prep_weights(inputs)
    nc = build_program(32)
    in_maps = []
    for c in range(n):
        m = {"x": np.ascontiguousarray(x[c]), "p": np.ascontiguousarray(p[0, c])}
        m.update(w)
        in_maps.append(m)
    res = run_bass_kernel_spmd(nc, in_maps, core_ids=list(range(n)))
    return np.stack([np.asarray(r["out"], dtype=np.float32) for r in res.results], axis=0)
```

```python
import numpy as np
from contextlib import ExitStack
import concourse.bass as bass
import concourse.mybir as mybir
from concourse.bass_utils import run_bass_kernel_spmd

F32 = mybir.dt.float32
BF16 = mybir.dt.bfloat16
U32 = mybir.dt.uint32
AF = mybir.ActivationFunctionType
ALU = mybir.AluOpType
AX = mybir.AxisListType

D = 1024
SEQ = 4096
ALPHA = float(2.0 ** 0.25)
EPS = 1e-5
NEG = -1.0e30
ENGS = ["sp", "pool", "act", "dve", "pe"]
NRING = 16
GRP = 8
NDIAG = 16


class Buf:
    __slots__ = ("name", "w", "r", "dsem", "dcnt")

    def __init__(self, name):
        self.name = name
        self.w = None
        self.r = []
        self.dsem = None
        self.dcnt = 0


class Prog:
    def __init__(self, nc, es):
        self.nc = nc
        self.es = es
        self.streams = {e: [] for e in ENGS}
        self.esem = {e: es.enter_context(nc.semaphore("es_" + e)) for e in ENGS}
        self.ecnt = {e: 0 for e in ENGS}
        self.waited = {e: {} for e in ENGS}
        self.dma_toks = []
        self.nd = 0

    def _wait(self, e, tok):
        sem, val = tok
        if self.waited[e].get(sem, 0) >= val:
            return
        self.waited[e][sem] = val
        self.streams[e].append(("w", sem, val))

    def _deps(self, e, who, reads, writes):
        for b in reads:
            if b.w is not None:
                self._wait(e, b.w[1])
        for b in writes:
            if b.w is not None and not (who == "pe" and b.w[0] == "pe"):
                self._wait(e, b.w[1])
            for (re_, tok) in b.r:
                self._wait(e, tok)

    def op(self, e, fns, reads=(), writes=()):
        if callable(fns):
            fns = [fns]
        self._deps(e, e, reads, writes)
        self.ecnt[e] += 1
        tok = (self.esem[e], self.ecnt[e])
        self.streams[e].append(("i", fns, self.esem[e], 1))
        for b in reads:
            b.r.append((e, tok))
        for b in writes:
            b.w = (e, tok)
            b.r = []
        return tok

    def dma(self, e, fn, sbuf, reads=(), writes=()):
        if sbuf.dsem is None:
            self.nd += 1
            sbuf.dsem = self.es.enter_context(self.nc.semaphore("ds%d" % self.nd))
        wr = list(writes)
        if sbuf not in wr:
            wr.append(sbuf)
        rd = [b for b in reads if b is not sbuf]
        self._deps(e, "dma", rd, wr)
        sbuf.dcnt += 16
        tok = (sbuf.dsem, sbuf.dcnt)
        self.streams[e].append(("i", [fn], sbuf.dsem, 16))
        for b in rd:
            b.r.append(("dma", tok))
        for b in writes:
            b.w = ("dma", tok)
            b.r = []
        if sbuf not in writes:
            sbuf.r.append(("dma", tok))
        self.dma_toks.append(tok)
        return tok

    def dma_nowait(self, e, fn, buf):
        if buf.dsem is None:
            self.nd += 1
            buf.dsem = self.es.enter_context(self.nc.semaphore("ds%d" % self.nd))
        buf.dcnt += 16
        tok = (buf.dsem, buf.dcnt)
        self.streams[e].append(("i", [fn], buf.dsem, 16))
        self.dma_toks.append(tok)
        return tok

    def barrier(self):
        toks = [(self.esem[e], self.ecnt[e]) for e in ENGS if self.ecnt[e] > 0] + self.dma_toks
        mx = {}
        for (sem, val) in toks:
            if mx.get(sem, 0) < val:
                mx[sem] = val
        for e in ENGS:
            for sem, val in mx.items():
                self._wait(e, (sem, val))
        self.dma_toks = []

    def emit(self, block):
        def mk(en):
            items = self.streams[en]

            def f(eng):
                for it in items:
                    if it[0] == "w":
                        eng.wait_ge(it[1], it[2])
                    else:
                        ins = None
                        for fn in it[1]:
                            ins = fn(eng)
                        ins.then_inc(it[2], it[3])
            return f
        block.sync(mk("sp"))
        block.gpsimd(mk("pool"))
        block.scalar(mk("act"))
        block.vector(mk("dve"))
        block.tensor(mk("pe"))
        self.streams = {e: [] for e in ENGS}


def build_program(nblk=32, debug_h1=False):
    nc = bass.Bass("TRN2", target_bir_lowering=False)
    ntok = nblk * 128

    def din(name, shape):
        return nc.dram_tensor(name, list(shape), F32, kind="ExternalInput").ap()

    x_d = din("x", [ntok, D])
    p_d = din("p", [ntok, 256])
    ln0_g = din("ln0_g", [D]); ln0_b = din("ln0_b", [D])
    w_in = din("w_in", [D, 2048]); b_in = din("b_in", [2048])
    conv_w = din("conv_w", [31, 512]); conv_b = din("conv_b", [512])
    gn_g = din("gn_g", [512]); gn_b = din("gn_b", [512])
    sg_ln_g = din("sg_ln_g", [512]); sg_ln_b = din("sg_ln_b", [512])
    sg_w = din("sg_w", [8, 128, 128]); sg_b = din("sg_b", [8, 128])
    w_o = din("w_o", [D, D]); b_o = din("b_o", [D])
    ln1_g = din("ln1_g", [D]); ln1_b = din("ln1_b", [D])
    peer_wq = din("peer_wq", [D, 2048])
    peer_keys = din("peer_keys", [16, 128, 128])
    peer_u = din("peer_u", [16384, D]); peer_v = din("peer_v", [16384, D])
    ple_wp = din("ple_wp", [256, D]); ple_wg = din("ple_wg", [D, D]); ple_bg = din("ple_bg", [D])
    ln2_g = din("ln2_g", [D]); ln2_b = din("ln2_b", [D])
    out_d = nc.dram_tensor("out", [ntok, D], F32, kind="ExternalOutput").ap()
    h1_d = nc.dram_tensor("h1s", [ntok, D], F32, kind="Internal").ap()
    uv_d = nc.dram_tensor("uvbf", [16384, 2 * D], BF16, kind="Internal").ap()

    with ExitStack() as outer:
        P = Prog(nc, outer)
        h1d_bufs = [Buf("h1d%d" % b) for b in range(nblk)]

        with ExitStack() as s1:
            def sbt(name, shape, dt):
                return s1.enter_context(nc.sbuf_tensor(name, list(shape), dt))

            def pst(name, shape, dt):
                return s1.enter_context(nc.psum_tensor(name, list(shape), dt))

            w_in_bf = sbt("w_in_bf", [128, 8, 2048], BF16); B_w_in = Buf("w_in")
            w_o_bf = sbt("w_o_bf", [128, 8, 1024], BF16); B_w_o = Buf("w_o")
            cdiag = sbt("cdiag", [128, 124, 128], BF16); B_cdiag = Buf("cdiag")
            g0 = sbt("g0", [128, D], F32); b0 = sbt("b0", [128, D], F32)
            g1 = sbt("g1", [128, D], F32); b1 = sbt("b1", [128, D], F32)
            sglg = sbt("sglg", [128, 512], F32); sglb = sbt("sglb", [128, 512], F32)
            B_bc = Buf("bc")
            identf = sbt("identf", [128, 128], F32); ident = sbt("ident", [128, 128], BF16)
            B_ident = Buf("ident")
            Gm = sbt("Gm", [128, 128], F32); B_G = Buf("G")
            rows = sbt("rows", [28, 128], F32); B_rows = Buf("rows")
            cwrow = sbt("cwrow", [31, 512], F32); B_cwrow = Buf("cwrow")
            cols = sbt("cols", [128, 28], F32); B_cols = Buf("cols")
            cw = sbt("cw", [128, 4, 31], F32); B_cw = Buf("cw")
            brow = sbt("brow", [1, 2048], BF16); B_brow = Buf("brow")
            ones_row = sbt("ones_row", [1, 128], BF16); B_ones = Buf("ones")
            eps_t = sbt("eps_t", [128, 1], F32); B_eps = Buf("eps")
            sgw_f = sbt("sgw_f", [128, 8, 128], F32); B_sgwf = Buf("sgwf")
            sgw_m = sbt("sgw_m", [128, 8, 128], BF16); B_sgwm = Buf("sgwm")
            wmT = sbt("wmT", [128, 8, 128], BF16); B_wmT = Buf("wmT")

            ptb = pst("ptb", [128, 8, 128], BF16); B_ptb = Buf("ptb")
            pbs = [pst("pb%d" % i, [128, 4, 128], F32) for i in range(7)]
            B_pb = [Buf("pb%d" % i) for i in range(7)]

            def flat(t):
                return t[:].rearrange("p c t -> p (c t)")

            for k2 in range(2):
                for kk in range(8):
                    P.dma("pool", (lambda g, kk=kk, k2=k2: g.dma_start(
                        out=w_in_bf[:, kk, k2 * 1024:(k2 + 1) * 1024],
                        in_=w_in[kk * 128:(kk + 1) * 128, k2 * 1024:(k2 + 1) * 1024])), B_w_in, writes=[B_w_in])
            for kk in range(8):
                P.dma("pool", (lambda g, kk=kk: g.dma_start(
                    out=w_o_bf[:, kk, :], in_=w_o[kk * 128:(kk + 1) * 128, :])), B_w_o, writes=[B_w_o])
            P.dma("pool", lambda g: g.dma_start(out=brow[:, 0:1024], in_=b_in[1024:2048].unsqueeze(0)), B_brow, writes=[B_brow])
            P.dma("pool", lambda g: g.dma_start(out=brow[:, 1024:2048], in_=b_o.unsqueeze(0)), B_brow, writes=[B_brow])
            for (t_, v_) in ((g0, ln0_g), (b0, ln0_b), (g1, ln1_g), (b1, ln1_b), (sglg, sg_ln_g), (sglb, sg_ln_b)):
                P.dma("sp", (lambda q, t_=t_, v_=v_: q.dma_start(out=t_[:], in_=v_.partition_broadcast(128))), B_bc, writes=[B_bc])
            P.dma("sp", lambda q: q.dma_start(out=rows[0:8, :], in_=b_in[0:1024].rearrange("(c p) -> c p", p=128)), B_rows, writes=[B_rows])
            P.dma("sp", lambda q: q.dma_start(out=rows[8:12, :], in_=conv_b.rearrange("(c p) -> c p", p=128)), B_rows, writes=[B_rows])
            P.dma("sp", lambda q: q.dma_start(out=rows[12:16, :], in_=gn_g.rearrange("(c p) -> c p", p=128)), B_rows, writes=[B_rows])
            P.dma("sp", lambda q: q.dma_start(out=rows[16:20, :], in_=gn_b.rearrange("(c p) -> c p", p=128)), B_rows, writes=[B_rows])
            P.dma("sp", lambda q: q.dma_start(out=rows[20:28, :], in_=sg_b), B_rows, writes=[B_rows])
            P.dma("sp", lambda q: q.dma_start(out=cwrow[:], in_=conv_w), B_cwrow, writes=[B_cwrow])
            P.dma("sp", lambda q: q.dma_start(out=sgw_f[:], in_=sg_w.rearrange("h t s -> t h s")), B_sgwf, writes=[B_sgwf])

            P.op("pool", lambda g: g.memset(identf[:], 0.0), writes=[B_ident])
            P.op("pool", lambda g: g.affine_select(out=identf[:], in_=identf[:], pattern=[[-1, 128]],
                                                   compare_op=ALU.not_equal, fill=1.0, base=0, channel_multiplier=1),
                 reads=[B_ident], writes=[B_ident])
            P.op("dve", lambda v: v.tensor_copy(out=ident[:], in_=identf[:]), reads=[B_ident], writes=[B_ident])
            P.op("dve", lambda v: v.memset(Gm[:], 0.0), writes=[B_G])
            P.op("dve", lambda v: v.memset(Gm[0:64, 0:64], 1.0 / 64.0), writes=[B_G])
            P.op("dve", lambda v: v.memset(Gm[64:128, 64:128], 1.0 / 64.0), writes=[B_G])
            P.op("dve", lambda v: v.memset(ones_row[:], 1.0), writes=[B_ones])
            P.op("dve", lambda v: v.memset(eps_t[:], EPS), writes=[B_eps])
            P.op("pe", lambda t: t.transpose(out=pbs[0][:, 0, 0:28], in_=rows[:, :], identity=identf[0:28, 0:28]),
                 reads=[B_rows, B_ident], writes=[B_pb[0]])
            P.op("dve", lambda v: v.tensor_copy(out=cols[:], in_=pbs[0][:, 0, 0:28]), reads=[B_pb[0]], writes=[B_cols])
            P.op("pe", [(lambda t, c=c: t.transpose(out=pbs[1][:, c, 0:31], in_=cwrow[:, c * 128:(c + 1) * 128],
                                                      identity=identf[0:31, 0:31])) for c in range(4)],
                 reads=[B_cwrow, B_ident], writes=[B_pb[1]])
            P.op("dve", lambda v: v.tensor_copy(out=cw[:], in_=pbs[1][:, :, 0:31]), reads=[B_pb[1]], writes=[B_cw])
            for c in range(4):
                P.op("dve", (lambda v, c=c: v.tensor_tensor(
                    out=cdiag[:, c * 31:(c + 1) * 31, :],
                    in0=identf[:].unsqueeze(1).to_broadcast([128, 31, 128]),
                    in1=cw[:, c, :].unsqueeze(2).to_broadcast([128, 31, 128]), op=ALU.mult)),
                    reads=[B_ident, B_cw], writes=[B_cdiag])
            P.op("pool", lambda g: g.affine_select(out=sgw_m[:], in_=sgw_f[:], pattern=[[0, 8], [-1, 128]],
                                                   compare_op=ALU.is_ge, fill=0.0, base=0, channel_multiplier=1),
                 reads=[B_sgwf], writes=[B_sgwm])
            P.op("pe", [(lambda t, h=h: t.transpose(out=ptb[:, h, :], in_=sgw_m[:, h, :], identity=ident[:])) for h in range(8)],
                 reads=[B_sgwm, B_ident], writes=[B_ptb])
            P.op("dve", lambda v: v.tensor_copy(out=wmT[:], in_=ptb[:]), reads=[B_ptb], writes=[B_wmT])

            def dbl(name, shape, dt):
                return [sbt("%s%d" % (name, i), shape, dt) for i in range(2)], [Buf("%s%d" % (name, i)) for i in range(2)]
            xb = [sbt("xb%d" % q_, [128, D], F32) for q_ in range(3)]; B_xb = [Buf("xb%d" % q_) for q_ in range(3)]
            stB, B_stB = dbl("stB", [128, 32], F32)
            h0bf, B_h0bf = dbl("h0bf", [128, D], BF16)
            h0T, B_h0T = dbl("h0T", [128, 8, 128], BF16)
            sig, B_sig = dbl("sig", [128, 4, 128], F32)
            cT, B_cT = dbl("cT", [128, 4, 158], BF16)
            yT, B_yT = dbl("yT", [128, 4, 128], F32)
            dd, B_dd = dbl("dd", [128, 4, 128], F32)
            sq, B_sq = dbl("sq", [128, 4, 128], F32)
            coT, B_coT = dbl("coT", [128, 4, 128], BF16)
            uu, B_uu = dbl("uu", [128, 512], F32)
            gv, B_gv = dbl("gv", [128, 512], F32)
            sq2, B_sq2 = dbl("sq2", [128, 512], F32)
            vbf, B_vbf = dbl("vbf", [128, 512], BF16)
            sgo, B_sgo = dbl("sgo", [128, 512], BF16)
            sgoT, B_sgoT = dbl("sgoT", [128, 4, 128], BF16)
            r1, B_r1 = dbl("r1", [128, D], F32)
            st, B_st = dbl("st", [128, 64], F32)

            P.op("dve", lambda v: v.memset(cT[0][:, :, 0:30], 0.0), writes=[B_cT[0]])

            def mk_ops(steps):
                def op(*a_, **k_):
                    steps.append(lambda: P.op(*a_, **k_))

                def dma(*a_, **k_):
                    steps.append(lambda: P.dma(*a_, **k_))
                return op, dma

            def layer_norm(op, src, Bsrc, dst, Bdst, stt, Bstt, base):
                s_stats = stt[:, base:base + 12]
                s_mv = stt[:, base + 12:base + 14]
                s_sd = stt[:, base + 14:base + 15]
                s_rs = stt[:, base + 15:base + 16]
                s_nm = stt[:, base + 16:base + 17]
                op("dve", [lambda v: v.bn_stats(out=stt[:, base:base + 6], in_=src[:, 0:512]),
                           lambda v: v.bn_stats(out=stt[:, base + 6:base + 12], in_=src[:, 512:1024])],
                   reads=[Bsrc], writes=[Bstt])
                op("dve", lambda v: v.bn_aggr(out=s_mv, in_=s_stats), reads=[Bstt], writes=[Bstt])
                op("act", lambda a: a.activation(out=s_sd, in_=stt[:, base + 13:base + 14], func=AF.Sqrt,
                                                 bias=eps_t[:, 0:1], scale=1.0), reads=[Bstt, B_eps], writes=[Bstt])
                op("dve", lambda v: v.reciprocal(out=s_rs, in_=s_sd), reads=[Bstt], writes=[Bstt])
                op("dve", lambda v: v.tensor_scalar(out=s_nm, in0=stt[:, base + 12:base + 13], scalar1=s_rs, scalar2=-1.0,
                                                    op0=ALU.mult, op1=ALU.mult), reads=[Bstt], writes=[Bstt])
                op("act", lambda a: a.activation(out=dst[:], in_=src[:], func=AF.Identity, bias=s_nm, scale=s_rs),
                   reads=[Bsrc, Bstt], writes=[Bdst])

            def stageA(b):
                steps = []
                op, dma = mk_ops(steps)
                i = b % 2
                j = (b + 1) % 2
                r0 = b * 128
                X = xb[b % 3]; BX = B_xb[b % 3]
                dma("sp", (lambda q, X=X, r0=r0: q.dma_start(out=X[:], in_=x_d[r0:r0 + 128, :])), BX, writes=[BX])
                layer_norm(op, X, BX, X, BX, st[i], B_st[i], 0)
                op("pool", lambda g, X=X: g.tensor_tensor(out=X[:], in0=X[:], in1=g0[:], op=ALU.mult), reads=[BX, B_bc], writes=[BX])
                op("pool", lambda g, X=X: g.tensor_tensor(out=X[:], in0=X[:], in1=b0[:], op=ALU.add), reads=[BX, B_bc], writes=[BX])
                op("act", lambda a, X=X, i=i: a.activation(out=h0bf[i][:], in_=X[:], func=AF.Copy), reads=[BX], writes=[B_h0bf[i]])
                op("pe", [(lambda t, k=k, i=i: t.transpose(out=ptb[:, k, :], in_=h0bf[i][:, k * 128:(k + 1) * 128], identity=ident[:]))
                          for k in range(8)], reads=[B_h0bf[i], B_ident], writes=[B_ptb])
                op("act", lambda a, i=i: a.activation(out=h0T[i][:], in_=ptb[:], func=AF.Copy), reads=[B_ptb], writes=[B_h0T[i]])
                for (bank, off) in ((0, 0), (1, 512)):
                    op("pe", [(lambda t, c=c, k=k, bank=bank, off=off, i=i: t.matmul(
                        pbs[bank][:, c, :], lhsT=w_in_bf[:, k, off + c * 128:off + (c + 1) * 128], rhs=h0T[i][:, k, :],
                        start=(k == 0), stop=(k == 7))) for c in range(4) for k in range(8)],
                        reads=[B_w_in, B_h0T[i]], writes=[B_pb[bank]])
                def susv(off):
                    fl = [(lambda t, off=off: t.matmul(flat(pbs[2]), lhsT=ones_row[:, :], rhs=brow[:, off - 1024:off - 512],
                                                       start=True, stop=False))]
                    fl += [(lambda t, k=k, off=off, i=i: t.matmul(flat(pbs[2]), lhsT=h0T[i][:, k, :],
                                                                   rhs=w_in_bf[:, k, off:off + 512], start=False, stop=(k == 7)))
                           for k in range(8)]
                    op("pe", fl, reads=[B_w_in, B_h0T[i], B_ones, B_brow], writes=[B_pb[2]])
                susv(1024)
                op("act", [(lambda a, c=c, i=i: a.activation(out=sig[i][:, c, :], in_=pbs[1][:, c, :], func=AF.Sigmoid,
                                                             bias=cols[:, 4 + c:5 + c], scale=1.0)) for c in range(4)],
                   reads=[B_pb[1], B_cols], writes=[B_sig[i]])
                op("act", lambda a, i=i: a.activation(out=uu[i][:], in_=flat(pbs[2]), func=AF.Gelu), reads=[B_pb[2]], writes=[B_uu[i]])
                op("dve", [(lambda v, c=c, i=i: v.scalar_tensor_tensor(out=cT[i][:, c, 30:158], in0=pbs[0][:, c, :],
                                                                        scalar=cols[:, c:c + 1], in1=sig[i][:, c, :],
                                                                        op0=ALU.add, op1=ALU.mult)) for c in range(4)],
                   reads=[B_pb[0], B_sig[i], B_cols], writes=[B_cT[i]])
                susv(1536)
                if b + 1 < nblk:
                    op("pool", lambda g, i=i, j=j: g.tensor_copy(out=cT[j][:, :, 0:30], in_=cT[i][:, :, 128:158]),
                       reads=[B_cT[i]], writes=[B_cT[j]])
                op("act", lambda a, i=i: a.activation(out=gv[i][:], in_=flat(pbs[2]), func=AF.Gelu), reads=[B_pb[2]], writes=[B_gv[i]])
                return steps

            def stageB1(b):
                steps = []
                op, dma = mk_ops(steps)
                i = b % 2
                r0 = b * 128
                op("pe", [(lambda t, c=c, k=k, i=i: t.matmul(pbs[4][:, c, :], lhsT=cdiag[:, c * 31 + k, :], rhs=cT[i][:, c, k:k + 128],
                                                             start=(k == 0), stop=(k == 30))) for c in range(4) for k in range(31)],
                   reads=[B_cdiag, B_cT[i]], writes=[B_pb[4]])
                op("act", [(lambda a, c=c, i=i: a.activation(out=yT[i][:, c, :], in_=pbs[4][:, c, :], func=AF.Identity,
                                                             bias=cols[:, 8 + c:9 + c], scale=1.0)) for c in range(4)],
                   reads=[B_pb[4], B_cols], writes=[B_yT[i]])
                gv3 = gv[i][:].rearrange("p (g d) -> p g d", g=8)
                sq3 = sq2[i][:].rearrange("p (g d) -> p g d", g=8)
                S = st[i]; BS = B_st[i]
                op("dve", lambda v, gv3=gv3, S=S: v.tensor_reduce(out=S[:, 24:32], in_=gv3, axis=AX.X, op=ALU.add), reads=[B_gv[i]], writes=[BS])
                op("pe", lambda t, i=i: t.matmul(flat(pbs[5]), lhsT=Gm[:], rhs=flat(yT[i]), start=True, stop=True),
                   reads=[B_G, B_yT[i]], writes=[B_pb[5]])
                op("dve", lambda v, S=S: v.tensor_scalar(out=S[:, 24:32], in0=S[:, 24:32], scalar1=1.0 / 64.0, scalar2=None, op0=ALU.mult),
                   reads=[BS], writes=[BS])
                op("dve", lambda v, i=i: v.tensor_tensor(out=flat(dd[i]), in0=flat(yT[i]), in1=flat(pbs[5]), op=ALU.subtract),
                   reads=[B_yT[i], B_pb[5]], writes=[B_dd[i]])
                op("act", lambda a, i=i: a.activation(out=flat(sq[i]), in_=flat(dd[i]), func=AF.Square), reads=[B_dd[i]], writes=[B_sq[i]])
                op("dve", lambda v, gv3=gv3, S=S: v.tensor_tensor(out=gv3, in0=gv3, in1=S[:, 24:32].unsqueeze(2).to_broadcast([128, 8, 64]),
                                                                   op=ALU.subtract), reads=[B_gv[i], BS], writes=[B_gv[i]])
                op("pe", lambda t, i=i: t.matmul(flat(pbs[6]), lhsT=Gm[:], rhs=flat(sq[i]), start=True, stop=True),
                   reads=[B_G, B_sq[i]], writes=[B_pb[6]])
                op("act", lambda a, i=i: a.activation(out=sq2[i][:], in_=gv[i][:], func=AF.Square), reads=[B_gv[i]], writes=[B_sq2[i]])
                op("act", lambda a, i=i: a.activation(out=flat(sq[i]), in_=flat(pbs[6]), func=AF.Sqrt, bias=eps_t[:, 0:1], scale=1.0),
                   reads=[B_pb[6], B_eps], writes=[B_sq[i]])
                op("dve", lambda v, sq3=sq3, S=S: v.tensor_reduce(out=S[:, 32:40], in_=sq3, axis=AX.X, op=ALU.add), reads=[B_sq2[i]], writes=[BS])
                op("dve", lambda v, i=i: v.reciprocal(out=flat(sq[i]), in_=flat(sq[i])), reads=[B_sq[i]], writes=[B_sq[i]])
                op("act", lambda a, S=S: a.activation(out=S[:, 40:48], in_=S[:, 32:40], func=AF.Sqrt, bias=eps_t[:, 0:1], scale=1.0 / 64.0),
                   reads=[BS, B_eps], writes=[BS])
                op("dve", lambda v, i=i: v.tensor_tensor(out=flat(dd[i]), in0=flat(dd[i]), in1=flat(sq[i]), op=ALU.mult),
                   reads=[B_dd[i], B_sq[i]], writes=[B_dd[i]])
                op("act", [(lambda a, c=c, i=i: a.activation(out=coT[i][:, c, :], in_=dd[i][:, c, :], func=AF.Silu,
                                                             bias=cols[:, 16 + c:17 + c], scale=cols[:, 12 + c:13 + c])) for c in range(4)],
                   reads=[B_dd[i], B_cols], writes=[B_coT[i]])
                op("dve", lambda v, S=S: v.reciprocal(out=S[:, 48:56], in_=S[:, 40:48]), reads=[BS], writes=[BS])
                op("dve", lambda v, gv3=gv3, S=S: v.tensor_tensor(out=gv3, in0=gv3, in1=S[:, 48:56].unsqueeze(2).to_broadcast([128, 8, 64]),
                                                                   op=ALU.mult), reads=[B_gv[i], BS], writes=[B_gv[i]])
                op("pool", lambda g, i=i: g.tensor_tensor(out=gv[i][:], in0=gv[i][:], in1=sglg[:], op=ALU.mult), reads=[B_gv[i], B_bc], writes=[B_gv[i]])
                op("pool", lambda g, i=i: g.tensor_tensor(out=vbf[i][:], in0=gv[i][:], in1=sglb[:], op=ALU.add), reads=[B_gv[i], B_bc], writes=[B_vbf[i]])
                op("pe", [(lambda t, h=h, i=i: t.matmul(flat(pbs[5])[:, h * 64:(h + 1) * 64], lhsT=wmT[:, h, :], rhs=vbf[i][:, h * 64:(h + 1) * 64],
                                                        start=True, stop=True)) for h in range(8)],
                   reads=[B_wmT, B_vbf[i]], writes=[B_pb[5]])
                f3 = flat(pbs[5]).rearrange("p (g d) -> p g d", g=8)
                op("dve", lambda v, f3=f3, i=i: v.tensor_tensor(out=sq2[i][:].rearrange("p (g d) -> p g d", g=8), in0=f3,
                                                                in1=cols[:, 20:28].unsqueeze(2).to_broadcast([128, 8, 64]), op=ALU.add),
                   reads=[B_pb[5], B_cols], writes=[B_sq2[i]])
                op("dve", lambda v, i=i: v.tensor_tensor(out=sgo[i][:], in0=sq2[i][:], in1=uu[i][:], op=ALU.mult),
                   reads=[B_sq2[i], B_uu[i]], writes=[B_sgo[i]])
                op("pe", [(lambda t, c=c, i=i: t.transpose(out=ptb[:, c, :], in_=sgo[i][:, c * 128:(c + 1) * 128], identity=ident[:]))
                          for c in range(4)], reads=[B_sgo[i], B_ident], writes=[B_ptb])
                op("act", lambda a, i=i: a.activation(out=sgoT[i][:], in_=ptb[:, 0:4, :], func=AF.Copy), reads=[B_ptb], writes=[B_sgoT[i]])
                return steps

            def stageB2(b):
                steps = []
                op, dma = mk_ops(steps)
                i = b % 2
                r0 = b * 128
                X = xb[b % 3]; BX = B_xb[b % 3]
                for n in range(2):
                    fl = [(lambda t, n=n: t.matmul(flat(pbs[3]), lhsT=ones_row[:, :], rhs=brow[:, 1024 + n * 512:1024 + (n + 1) * 512],
                                                   start=True, stop=False))]
                    fl += [(lambda t, k=k, n=n, i=i: t.matmul(flat(pbs[3]), lhsT=(coT[i][:, k, :] if k < 4 else sgoT[i][:, k - 4, :]),
                                                              rhs=w_o_bf[:, k, n * 512:(n + 1) * 512], start=False, stop=(k == 7)))
                           for k in range(8)]
                    op("pe", fl, reads=[B_w_o, B_coT[i], B_sgoT[i], B_ones, B_brow], writes=[B_pb[3]])
                    op("dve", lambda v, n=n, i=i, X=X: v.scalar_tensor_tensor(out=r1[i][:, n * 512:(n + 1) * 512], in0=X[:, n * 512:(n + 1) * 512],
                                                                               scalar=ALPHA, in1=flat(pbs[3]), op0=ALU.mult, op1=ALU.add),
                       reads=[BX, B_pb[3]], writes=[B_r1[i]])
                R = r1[i]; BR = B_r1[i]
                layer_norm(op, R, BR, R, BR, stB[i], B_stB[i], 0)
                op("pool", lambda g, R=R: g.tensor_tensor(out=R[:], in0=R[:], in1=g1[:], op=ALU.mult), reads=[BR, B_bc], writes=[BR])
                op("pool", lambda g, R=R: g.tensor_tensor(out=R[:], in0=R[:], in1=b1[:], op=ALU.add), reads=[BR, B_bc], writes=[BR])
                dst = out_d if debug_h1 else h1_d
                dma("sp", (lambda q, R=R, r0=r0, dst=dst: q.dma_start(out=dst[r0:r0 + 128, :], in_=R[:])), BR, reads=[BR], writes=[h1d_bufs[b]])
                return steps

            B_uvtab = Buf("uvtab")
            conv_jobs = [(t_, c_) for c_ in range(32) for t_ in range(2)]

            def issue_conv(k):
                for (t_, c_) in conv_jobs[k::nblk] if nblk < 32 else conv_jobs[2 * k:2 * k + 2]:
                    tab = peer_u if t_ == 0 else peer_v
                    P.dma_nowait("pool", (lambda q, tab=tab, t_=t_, c_=c_: q.dma_start(
                        out=uv_d[c_ * 512:(c_ + 1) * 512, t_ * D:(t_ + 1) * D], in_=tab[c_ * 512:(c_ + 1) * 512, :])), B_uvtab)

            for t in range(nblk + 2):
                lists = []
                if 0 <= t - 2 < nblk:
                    lists.append(stageB2(t - 2))
                if 0 <= t - 1 < nblk:
                    lists.append(stageB1(t - 1))
                if t < nblk:
                    if not debug_h1:
                        issue_conv(t)
                    lists.append(stageA(t))
                mlen = max(len(l_) for l_ in lists)
                pos_ = [0] * len(lists)
                for k in range(mlen):
                    for q_, l_ in enumerate(lists):
                        tgt = ((k + 1) * len(l_)) // mlen
                        while pos_[q_] < tgt:
                            l_[pos_[q_]]()
                            pos_[q_] += 1

            P.barrier()
            with nc.Block() as blk:
                P.emit(blk)

        if debug_h1:
            return nc

        with ExitStack() as s2:
            def sbt(name, shape, dt):
                return s2.enter_context(nc.sbuf_tensor(name, list(shape), dt))

            def pst(name, shape, dt):
                return s2.enter_context(nc.psum_tensor(name, list(shape), dt))

            def flat(t):
                return t[:].rearrange("p c t -> p (c t)")

            wq_bf = sbt("wq_bf", [128, 8, 2048], BF16); B_wq = Buf("wq")
            wg_bf = sbt("wg_bf", [128, 8, 1024], BF16); B_wg = Buf("wg")
            wp_bf = sbt("wp_bf", [128, 2, 1024], BF16); B_wp = Buf("wp")
            keys_f = sbt("keys_f", [128, 16, 128], BF16); B_keysf = Buf("keysf")
            keysT = sbt("keysT", [128, 16, 128], BF16); B_keysT = Buf("keysT")
            g2 = sbt("g2", [128, D], F32); b2 = sbt("b2", [128, D], F32); B_bc = Buf("bc2")
            identf = sbt("identf2", [128, 128], F32); ident = sbt("ident2", [128, 128], BF16); B_ident = Buf("ident2")
            brow = sbt("brow2", [1, 1024], BF16); B_brow = Buf("brow2")
            ones_row = sbt("ones_row2", [1, 128], BF16); B_ones = Buf("ones2")
            eps_t = sbt("eps_t2", [128, 1], F32); B_eps = Buf("eps2")
            iota16 = sbt("iota16", [128, 16], F32); B_iota = Buf("iota")

            ptb = pst("ptb2", [128, 8, 128], BF16); B_ptb = Buf("ptb2")
            pq = [pst("pq%d" % i, [128, 4, 128], F32) for i in range(2)]; B_pq = [Buf("pq%d" % i) for i in range(2)]
            psc = [pst("psc%d" % i, [128, 4, 128], F32) for i in range(2)]; B_psc = [Buf("psc%d" % i) for i in range(2)]
            ppl = pst("ppl", [128, 4, 128], F32); B_ppl = Buf("ppl")
            py = [pst("py%d" % i, [128, 4, 128], F32) for i in range(2)]; B_py = Buf("py")

            for k2 in range(2):
                for kk in range(8):
                    P.dma("pool", (lambda g, kk=kk, k2=k2: g.dma_start(
                        out=wq_bf[:, kk, k2 * 1024:(k2 + 1) * 1024],
                        in_=peer_wq[kk * 128:(kk + 1) * 128, k2 * 1024:(k2 + 1) * 1024])), B_wq, writes=[B_wq])
            for kk in range(8):
                P.dma("pool", (lambda g, kk=kk: g.dma_start(out=wg_bf[:, kk, :], in_=ple_wg[kk * 128:(kk + 1) * 128, :])), B_wg, writes=[B_wg])
            for kk in range(2):
                P.dma("pool", (lambda g, kk=kk: g.dma_start(out=wp_bf[:, kk, :], in_=ple_wp[kk * 128:(kk + 1) * 128, :])), B_wp, writes=[B_wp])
            P.dma("pool", lambda g: g.dma_start(out=keys_f[:], in_=peer_keys.rearrange("h k d -> k h d")), B_keysf, writes=[B_keysf])
            P.dma("pool", lambda g: g.dma_start(out=brow[:, :], in_=ple_bg.unsqueeze(0)), B_brow, writes=[B_brow])
            for (t_, v_) in ((g2, ln2_g), (b2, ln2_b)):
                P.dma("sp", (lambda q, t_=t_, v_=v_: q.dma_start(out=t_[:], in_=v_.partition_broadcast(128))), B_bc, writes=[B_bc])
            P.op("pool", lambda g: g.memset(identf[:], 0.0), writes=[B_ident])
            P.op("pool", lambda g: g.affine_select(out=identf[:], in_=identf[:], pattern=[[-1, 128]],
                                                   compare_op=ALU.not_equal, fill=1.0, base=0, channel_multiplier=1),
                 reads=[B_ident], writes=[B_ident])
            P.op("pool", lambda g: g.iota(iota16[:], pattern=[[1, 16]], base=0, channel_multiplier=0, allow_small_or_imprecise_dtypes=True),
                 writes=[B_iota])
            P.op("dve", lambda v: v.tensor_copy(out=ident[:], in_=identf[:]), reads=[B_ident], writes=[B_ident])
            P.op("dve", lambda v: v.memset(ones_row[:], 1.0), writes=[B_ones])
            P.op("dve", lambda v: v.memset(eps_t[:], EPS), writes=[B_eps])
            for half in range(2):
                P.op("pe", [(lambda t, q=q, half=half: t.transpose(out=ptb[:, q, :], in_=keys_f[:, half * 8 + q, :], identity=ident[:]))
                            for q in range(8)], reads=[B_keysf, B_ident], writes=[B_ptb])
                P.op("dve", lambda v, half=half: v.tensor_copy(out=keysT[:, half * 8:(half + 1) * 8, :], in_=ptb[:]),
                     reads=[B_ptb], writes=[B_keysT])

            UV = [sbt("UV%d" % s, [128, 2 * D], BF16) for s in range(NRING)]; B_UV = [Buf("UV%d" % s) for s in range(NRING)]
            dring = [sbt("dg%d" % s, [128, 128], BF16) for s in range(NDIAG)]; B_dg = [Buf("dg%d" % s) for s in range(NDIAG)]

            def dbl(name, shape, dt):
                return [sbt("%s%d" % (name, i), shape, dt) for i in range(2)], [Buf("%s%d" % (name, i)) for i in range(2)]
            h1, B_h1 = dbl("h1_", [128, D], F32)
            rp, B_rp = dbl("rp", [128, D], F32)
            idx, B_idx = dbl("idx", [128, 128], U32)
            gsm, B_gsm = dbl("gsm", [128, 128], F32)
            pld, B_pld = dbl("pld", [128, 256], F32)
            h1bf = sbt("h1bf", [128, D], BF16); B_h1bf = Buf("h1bf")
            h1T = sbt("h1T", [128, 8, 128], BF16); B_h1T = Buf("h1T")
            pbf = sbt("pbf", [128, 256], BF16); B_pbf = Buf("pbf")
            pT = sbt("pT", [128, 2, 128], BF16); B_pT = Buf("pT")
            qT = sbt("qT", [128, 16, 128], BF16); B_qT = [Buf("qT%d" % i) for i in range(4)]
            scs = sbt("scs", [128, 16, 128], F32); B_scs = [Buf("scs%d" % i) for i in range(4)]
            wk = sbt("wk", [128, 8, 128], F32); B_wk = Buf("wk")
            m8 = sbt("m8", [128, 16, 16], F32); B_m8 = Buf("m8")
            i8 = sbt("i8", [128, 16, 16], U32); B_i8 = Buf("i8")
            i8f = sbt("i8f", [128, 16, 16], F32); B_i8f = Buf("i8f")
            comb = sbt("comb", [128, 4, 256], F32); B_comb = Buf("comb")
            wk2 = sbt("wk2", [128, 4, 256], F32); B_wk2 = Buf("wk2")
            c16 = sbt("c16", [128, 8, 16], F32); B_c16 = Buf("c16")
            pos = sbt("pos", [128, 8, 16], U32); B_pos = Buf("pos")
            apos = sbt("apos", [128, 8, 16], U32); bpos = sbt("bpos", [128, 8, 16], U32)
            aposf = sbt("aposf", [128, 8, 16], F32); bposf = sbt("bposf", [128, 8, 16], F32); B_ab = Buf("ab")
            oh = sbt("oh", [128, 4, 16, 16], F32); B_oh = Buf("oh")
            asel = sbt("asel", [128, 8, 16], F32); bsel = sbt("bsel", [128, 8, 16], F32); B_sel = Buf("sel")
            idxf = sbt("idxf", [128, 128], F32); B_idxf = Buf("idxf")
            sm = sbt("sm", [128, 32], F32); B_sm = Buf("sm")
            gate = sbt("gate", [128, 512], F32); B_gate = Buf("gate")
            junk = sbt("junk", [128, D], BF16); B_junk = Buf("junk")
            actt = sbt("actt", [128, 128], F32); B_actg = [Buf("actg%d" % g) for g in range(128 // GRP)]
            gel = sbt("gel", [128, 128], F32); B_gelg = [Buf("gelg%d" % g) for g in range(128 // GRP)]
            wgt = sbt("wgt", [128, 128], F32); B_wgtg = [Buf("wgtg%d" % g) for g in range(128 // GRP)]
            st2 = sbt("st2", [128, 32], F32); B_st2 = Buf("st2")

            NG = 128 // GRP
            ring_ctr = [0]

            def front(b):
                steps = []

                def op(*a_, **k_):
                    steps.append(lambda: P.op(*a_, **k_))

                def dma(*a_, **k_):
                    steps.append(lambda: P.dma(*a_, **k_))
                i = b % 2
                r0 = b * 128
                H = h1[i]; BH = B_h1[i]
                dma("sp", (lambda q, H=H, r0=r0: q.dma_start(out=H[:], in_=h1_d[r0:r0 + 128, :])), BH, reads=[h1d_bufs[b]], writes=[BH])
                dma("sp", (lambda q, i=i, r0=r0: q.dma_start(out=pld[i][:], in_=p_d[r0:r0 + 128, :])), B_pld[i], writes=[B_pld[i]])
                op("act", lambda a, H=H: a.activation(out=h1bf[:], in_=H[:], func=AF.Copy), reads=[BH], writes=[B_h1bf])
                op("act", lambda a, i=i: a.activation(out=pbf[:], in_=pld[i][:], func=AF.Copy), reads=[B_pld[i]], writes=[B_pbf])
                op("pe", [(lambda t, k=k: t.transpose(out=ptb[:, k, :], in_=h1bf[:, k * 128:(k + 1) * 128], identity=ident[:]))
                            for k in range(8)], reads=[B_h1bf, B_ident], writes=[B_ptb])
                op("act", lambda a: a.activation(out=h1T[:], in_=ptb[:], func=AF.Copy), reads=[B_ptb], writes=[B_h1T])
                op("pe", [(lambda t, k=k: t.transpose(out=ptb[:, k, :], in_=pbf[:, k * 128:(k + 1) * 128], identity=ident[:]))
                            for k in range(2)], reads=[B_pbf, B_ident], writes=[B_ptb])
                op("act", lambda a: a.activation(out=pT[:], in_=ptb[:, 0:2, :], func=AF.Copy), reads=[B_ptb], writes=[B_pT])
                for qd in range(4):
                    pb_ = pq[qd % 2]; Bp = B_pq[qd % 2]
                    op("pe", [(lambda t, c=c, k=k, qd=qd, pb_=pb_: t.matmul(
                        pb_[:, c, :], lhsT=wq_bf[:, k, (qd * 4 + c) * 128:(qd * 4 + c + 1) * 128], rhs=h1T[:, k, :],
                        start=(k == 0), stop=(k == 7))) for c in range(4) for k in range(8)],
                        reads=[B_wq, B_h1T], writes=[Bp])
                    op("act", lambda a, qd=qd, pb_=pb_: a.activation(out=qT[:, qd * 4:(qd + 1) * 4, :], in_=pb_[:], func=AF.Copy),
                         reads=[Bp], writes=[B_qT[qd]])
                    ps_ = psc[qd % 2]; Bs = B_psc[qd % 2]
                    op("pe", [(lambda t, c=c, qd=qd, ps_=ps_: t.matmul(ps_[:, c, :], lhsT=qT[:, qd * 4 + c, :], rhs=keysT[:, qd * 4 + c, :],
                                                                          start=True, stop=True)) for c in range(4)],
                         reads=[B_qT[qd], B_keysT], writes=[Bs])
                    op("act", lambda a, qd=qd, ps_=ps_: a.activation(out=scs[:, qd * 4:(qd + 1) * 4, :], in_=ps_[:], func=AF.Copy),
                         reads=[Bs], writes=[B_scs[qd]])
                for n in range(2):
                    fl = [(lambda t, n=n: t.matmul(flat(ppl), lhsT=ones_row[:, :], rhs=brow[:, n * 512:(n + 1) * 512], start=True, stop=False))]
                    fl += [(lambda t, k=k, n=n: t.matmul(flat(ppl), lhsT=h1T[:, k, :], rhs=wg_bf[:, k, n * 512:(n + 1) * 512],
                                                          start=False, stop=(k == 7))) for k in range(8)]
                    op("pe", fl, reads=[B_wg, B_h1T, B_ones, B_brow], writes=[B_ppl])
                    op("act", lambda a: a.activation(out=gate[:], in_=flat(ppl), func=AF.Sigmoid), reads=[B_ppl], writes=[B_gate])
                    op("pe", [(lambda t, k=k, n=n: t.matmul(flat(ppl), lhsT=pT[:, k, :], rhs=wp_bf[:, k, n * 512:(n + 1) * 512],
                                                               start=(k == 0), stop=(k == 1))) for k in range(2)],
                         reads=[B_wp, B_pT], writes=[B_ppl])
                    op("dve", lambda v: v.tensor_tensor(out=gate[:], in0=gate[:], in1=flat(ppl), op=ALU.mult),
                         reads=[B_gate, B_ppl], writes=[B_gate])
                    op("dve", lambda v, n=n, i=i, H=H: v.scalar_tensor_tensor(out=rp[i][:, n * 512:(n + 1) * 512], in0=H[:, n * 512:(n + 1) * 512],
                                                                               scalar=ALPHA, in1=gate[:], op0=ALU.mult, op1=ALU.add),
                         reads=[BH, B_gate], writes=[B_rp[i]])
                op("dve", [(lambda v, hh=hh: v.max(out=m8[:, hh, 0:8], in_=scs[:, hh, :])) for hh in range(16)], reads=B_scs, writes=[B_m8])
                for hv in range(2):
                    op("dve", [(lambda v, hh=hh, hv=hv: v.match_replace(out=wk[:, hh - 8 * hv, :], in_to_replace=m8[:, hh, 0:8],
                                                                          in_values=scs[:, hh, :], imm_value=NEG))
                                 for hh in range(8 * hv, 8 * hv + 8)], reads=B_scs + [B_m8], writes=[B_wk])
                    op("dve", [(lambda v, hh=hh, hv=hv: v.max(out=m8[:, hh, 8:16], in_=wk[:, hh - 8 * hv, :]))
                                 for hh in range(8 * hv, 8 * hv + 8)], reads=[B_wk], writes=[B_m8])
                op("dve", [(lambda v, hh=hh, o=o: v.max_index(out=i8[:, hh, o:o + 8], in_max=m8[:, hh, o:o + 8], in_values=scs[:, hh, :]))
                             for hh in range(16) for o in (0, 8)], reads=B_scs + [B_m8], writes=[B_i8])
                op("dve", lambda v: v.tensor_copy(out=i8f[:], in_=i8[:]), reads=[B_i8], writes=[B_i8f])
                m84 = m8[:].rearrange("p (h t) k -> p h t k", t=2)
                i84 = i8f[:].rearrange("p (h t) k -> p h t k", t=2)
                for hf in range(2):
                    hs = slice(hf * 4, hf * 4 + 4)
                    comb4 = comb[:].rearrange("p h (a c) -> p h a c", a=16)
                    op("dve", lambda v, hs=hs, comb4=comb4: v.tensor_tensor(
                        out=comb4, in0=m84[:, hs, 0, :].unsqueeze(3).to_broadcast([128, 4, 16, 16]),
                        in1=m84[:, hs, 1, :].unsqueeze(2).to_broadcast([128, 4, 16, 16]), op=ALU.add),
                        reads=[B_m8], writes=[B_comb])
                    op("dve", [(lambda v, h=h, hf=hf: v.max(out=c16[:, hf * 4 + h, 0:8], in_=comb[:, h, :])) for h in range(4)],
                         reads=[B_comb], writes=[B_c16])
                    op("dve", [(lambda v, h=h, hf=hf: v.match_replace(out=wk2[:, h, :], in_to_replace=c16[:, hf * 4 + h, 0:8],
                                                                         in_values=comb[:, h, :], imm_value=NEG)) for h in range(4)],
                         reads=[B_comb, B_c16], writes=[B_wk2])
                    op("dve", [(lambda v, h=h, hf=hf: v.max(out=c16[:, hf * 4 + h, 8:16], in_=wk2[:, h, :])) for h in range(4)],
                         reads=[B_wk2], writes=[B_c16])
                    op("dve", [(lambda v, h=h, hf=hf, o=o: v.max_index(out=pos[:, hf * 4 + h, o:o + 8], in_max=c16[:, hf * 4 + h, o:o + 8],
                                                                          in_values=comb[:, h, :])) for h in range(4) for o in (0, 8)],
                         reads=[B_comb, B_c16], writes=[B_pos])
                op("dve", [lambda v: v.tensor_single_scalar(out=apos[:], in_=pos[:], scalar=4, op=ALU.logical_shift_right),
                             lambda v: v.tensor_single_scalar(out=bpos[:], in_=pos[:], scalar=15, op=ALU.bitwise_and)],
                     reads=[B_pos], writes=[B_ab])
                op("dve", [lambda v: v.tensor_copy(out=aposf[:], in_=apos[:]),
                             lambda v: v.tensor_copy(out=bposf[:], in_=bpos[:])], reads=[B_ab], writes=[B_ab])
                io4 = iota16[:].unsqueeze(1).unsqueeze(1).to_broadcast([128, 4, 16, 16])
                for (pf, tsel, sel) in ((aposf, 0, asel), (bposf, 1, bsel)):
                    for hf in range(2):
                        hs = slice(hf * 4, hf * 4 + 4)
                        op("dve", lambda v, pf=pf, hs=hs: v.tensor_tensor(out=oh[:], in0=pf[:, hs, :].unsqueeze(3).to_broadcast([128, 4, 16, 16]),
                                                                            in1=io4, op=ALU.is_equal), reads=[B_ab, B_iota], writes=[B_oh])
                        op("dve", lambda v, tsel=tsel, hs=hs: v.tensor_tensor(out=oh[:], in0=oh[:],
                                                                                in1=i84[:, hs, tsel, :].unsqueeze(2).to_broadcast([128, 4, 16, 16]),
                                                                                op=ALU.mult), reads=[B_oh, B_i8f], writes=[B_oh])
                        op("dve", lambda v, sel=sel, hs=hs: v.tensor_reduce(out=sel[:, hs, :], in_=oh[:], axis=AX.X, op=ALU.add),
                             reads=[B_oh], writes=[B_sel])
                op("dve", lambda v: v.scalar_tensor_tensor(out=idxf[:], in0=asel[:].rearrange("p h k -> p (h k)"), scalar=128.0,
                                                             in1=bsel[:].rearrange("p h k -> p (h k)"), op0=ALU.mult, op1=ALU.add),
                     reads=[B_sel], writes=[B_idxf])
                op("dve", lambda v, i=i: v.tensor_copy(out=idx[i][:], in_=idxf[:]), reads=[B_idxf], writes=[B_idx[i]])
                op("dve", lambda v: v.tensor_scalar(out=sm[:, 0:8], in0=c16[:, :, 0], scalar1=-1.0, scalar2=None, op0=ALU.mult),
                     reads=[B_c16], writes=[B_sm])
                G3 = gsm[i][:].rearrange("p (h k) -> p h k", h=8)
                op("act", [(lambda a, h=h, G3=G3: a.activation(out=G3[:, h, :], in_=c16[:, h, :], func=AF.Exp, bias=sm[:, h:h + 1], scale=1.0,
                                                                  accum_out=sm[:, 8 + h:9 + h])) for h in range(8)],
                     reads=[B_c16, B_sm], writes=[B_gsm[i], B_sm])
                op("dve", lambda v: v.reciprocal(out=sm[:, 16:24], in_=sm[:, 8:16]), reads=[B_sm], writes=[B_sm])
                op("dve", lambda v, G3=G3: v.tensor_tensor(out=G3, in0=G3, in1=sm[:, 16:24].unsqueeze(2).to_broadcast([128, 8, 16]), op=ALU.mult),
                     reads=[B_gsm[i], B_sm], writes=[B_gsm[i]])

                def gen():
                    for st_ in steps:
                        st_()
                        yield
                return gen()

            nuse = nblk * 128

            def issue_gather(n):
                if n >= nuse:
                    return
                b_, jj = divmod(n, 128)
                i_ = b_ % 2
                s_ = n % NRING
                P.dma("pool", (lambda q, s_=s_, jj=jj, i_=i_: q.indirect_dma_start(
                    out=UV[s_][:], out_offset=None, in_=uv_d,
                    in_offset=bass.IndirectOffsetOnAxis(ap=idx[i_][:, jj:jj + 1], axis=0))),
                    B_UV[s_], reads=[B_idx[i_]], writes=[B_UV[s_]])

            def wmul(b, g):
                i = b % 2
                gs = slice(g * GRP, (g + 1) * GRP)
                P.op("dve", lambda v, gs=gs, i=i: v.tensor_tensor(out=wgt[:, gs], in0=gel[:, gs], in1=gsm[i][:, gs], op=ALU.mult),
                     reads=[B_gelg[g], B_gsm[i]], writes=[B_wgtg[g]])

            def vside(b, g):
                for e in range(GRP):
                    jj = g * GRP + e
                    s = (b * 128 + jj) % NRING
                    ds = (b * 128 + jj) % NDIAG
                    P.op("act", lambda a, ds=ds, jj=jj: a.activation(out=dring[ds][:], in_=identf[:], func=AF.Copy, scale=wgt[:, jj:jj + 1]),
                         reads=[B_wgtg[g], B_ident], writes=[B_dg[ds]])
                    P.op("pe", [(lambda t, n=n, ds=ds, s=s, jj=jj: t.matmul(flat(py[n]), lhsT=dring[ds][:], rhs=UV[s][:, D + n * 512:D + (n + 1) * 512],
                                                                             start=(jj == 0), stop=(jj == 127))) for n in range(2)],
                         reads=[B_dg[ds], B_UV[s]], writes=[B_py])
                    issue_gather(b * 128 + jj + NRING)

            def tail(b):
                i = b % 2
                r0 = b * 128
                r2 = rp[i]; B_r2 = B_rp[i]
                P.op("dve", [(lambda v, n=n, r2=r2: v.tensor_tensor(out=r2[:, n * 512:(n + 1) * 512], in0=r2[:, n * 512:(n + 1) * 512],
                                                                    in1=flat(py[n]), op=ALU.add)) for n in range(2)],
                     reads=[B_r2, B_py], writes=[B_r2])
                S = st2
                P.op("dve", [lambda v, r2=r2: v.bn_stats(out=S[:, 0:6], in_=r2[:, 0:512]),
                             lambda v, r2=r2: v.bn_stats(out=S[:, 6:12], in_=r2[:, 512:1024])], reads=[B_r2], writes=[B_st2])
                P.op("dve", lambda v: v.bn_aggr(out=S[:, 12:14], in_=S[:, 0:12]), reads=[B_st2], writes=[B_st2])
                P.op("act", lambda a: a.activation(out=S[:, 14:15], in_=S[:, 13:14], func=AF.Sqrt, bias=eps_t[:, 0:1], scale=1.0),
                     reads=[B_st2, B_eps], writes=[B_st2])
                P.op("dve", lambda v: v.reciprocal(out=S[:, 15:16], in_=S[:, 14:15]), reads=[B_st2], writes=[B_st2])
                P.op("dve", lambda v: v.tensor_scalar(out=S[:, 16:17], in0=S[:, 12:13], scalar1=S[:, 15:16], scalar2=-1.0, op0=ALU.mult, op1=ALU.mult),
                     reads=[B_st2], writes=[B_st2])
                P.op("act", lambda a, r2=r2: a.activation(out=r2[:], in_=r2[:], func=AF.Identity, bias=S[:, 16:17], scale=S[:, 15:16]),
                     reads=[B_r2, B_st2], writes=[B_r2])
                P.op("dve", lambda v, r2=r2: v.tensor_tensor(out=r2[:], in0=r2[:], in1=g2[:], op=ALU.mult), reads=[B_r2, B_bc], writes=[B_r2])
                P.op("dve", lambda v, r2=r2: v.tensor_tensor(out=r2[:], in0=r2[:], in1=b2[:], op=ALU.add), reads=[B_r2, B_bc], writes=[B_r2])
                P.dma("sp", (lambda q, r0=r0, r2=r2: q.dma_start(out=out_d[r0:r0 + 128, :], in_=r2[:])), B_r2, reads=[B_r2])

            def back(b, fg):
                i = b % 2
                H = h1[i]; BH = B_h1[i]
                for g in range(NG):
                    gs = slice(g * GRP, (g + 1) * GRP)
                    if g == NG - 1 and fg is not None:
                        for _ in fg:
                            pass
                    for e in range(GRP):
                        jj = g * GRP + e
                        s = (b * 128 + jj) % NRING
                        P.op("dve", lambda v, s=s, jj=jj, H=H: v.scalar_tensor_tensor(out=junk[:], in0=UV[s][:, 0:D], scalar=1.0, in1=H[:],
                                                                                     op0=ALU.mult, op1=ALU.mult, accum_out=actt[:, jj:jj + 1]),
                             reads=[B_UV[s], BH], writes=[B_junk, B_actg[g]])
                        if e == 0 and g >= 1:
                            wmul(b, g - 1)
                            vside(b, g - 1)
                        if e == GRP - 1 and g == 0 and b >= 1:
                            tail(b - 1)
                        if fg is not None and g < NG - 1:
                            next(fg, None)
                    P.op("act", lambda a, gs=gs: a.activation(out=gel[:, gs], in_=actt[:, gs], func=AF.Gelu), reads=[B_actg[g]], writes=[B_gelg[g]])
                wmul(b, NG - 1)
                vside(b, NG - 1)
                if b == nblk - 1:
                    tail(b)

            for _ in front(0):
                pass
            for n_ in range(NRING):
                issue_gather(n_)
            for b in range(nblk):
                back(b, front(b + 1) if b + 1 < nblk else None)

            P.barrier()
            with nc.Block() as blk:
                P.emit(blk)
    return nc


_W_NAMES = ["ln0_g", "ln0_b", "w_in", "b_in", "conv_w", "conv_b", "gn_g", "gn_b", "sg_ln_g", "sg_ln_b", "sg_w", "sg_b",
            "w_o", "b_o", "ln1_g", "ln1_b", "peer_wq", "peer_keys", "peer_u", "peer_v", "ple_wp", "ple_wg", "ple_bg",
            "ln2_g", "ln2_b"]


def _prep_weights(inp):
    w = {}
    for k in _W_NAMES:
        a = np.asarray(inp[k], dtype=np.float32)
        if k in ("ln0_g", "ln0_b"):
            w[k] = np.ascontiguousarray(a.reshape(1024))
        elif k == "peer_keys":
            w[k] = np.ascontiguousarray(a.reshape(16, 128, 128))
        else:
            w[k] = np.ascontiguousarray(a[0])
    return w


def kernel(**inputs):
    n = 8
    x = np.asarray(inputs["x"], dtype=np.float32)
    p = np.asarray(inputs["p"], dtype=np.float32)
    w = _prep_weights(inputs)
    nc = build_program(32)
    in_maps = []
    for c in range(n):
        m = {"x": np.ascontiguousarray(x[c]), "p": np.ascontiguousarray(p[0, c])}
        m.update(w)
        in_maps.append(m)
    res = run_bass_kernel_spmd(nc, in_maps, core_ids=list(range(n)))
    return np.stack([np.asarray(r["out"], dtype=np.float32) for r in res.results], axis=0)
```

```python
import numpy as np
from contextlib import ExitStack
import concourse.bass as bass
import concourse.mybir as mybir
from concourse.bass_utils import run_bass_kernel_spmd

F32 = mybir.dt.float32
BF16 = mybir.dt.bfloat16
U32 = mybir.dt.uint32
AF = mybir.ActivationFunctionType
ALU = mybir.AluOpType
AX = mybir.AxisListType

D = 1024
SEQ = 4096
ALPHA = float(2.0 ** 0.25)
EPS = 1e-5
NEG = -1.0e30
ENGS = ["sp", "pool", "act", "dve", "pe"]
NRING = 16
GRP = 8
NDIAG = 16


class Buf:
    __slots__ = ("name", "w", "r", "dsem", "dcnt")

    def __init__(self, name):
        self.name = name
        self.w = None
        self.r = []
        self.dsem = None
        self.dcnt = 0


class Prog:
    def __init__(self, nc, es):
        self.nc = nc
        self.es = es
        self.streams = {e: [] for e in ENGS}
        self.esem = {e: es.enter_context(nc.semaphore("es_" + e)) for e in ENGS}
        self.ecnt = {e: 0 for e in ENGS}
        self.waited = {e: {} for e in ENGS}
        self.dma_toks = []
        self.nd = 0

    def _wait(self, e, tok):
        sem, val = tok
        if self.waited[e].get(sem, 0) >= val:
            return
        self.waited[e][sem] = val
        self.streams[e].append(("w", sem, val))

    def _deps(self, e, who, reads, writes):
        for b in reads:
            if b.w is not None:
                self._wait(e, b.w[1])
        for b in writes:
            if b.w is not None and not (who == "pe" and b.w[0] == "pe"):
                self._wait(e, b.w[1])
            for (re_, tok) in b.r:
                self._wait(e, tok)

    def op(self, e, fns, reads=(), writes=()):
        if callable(fns):
            fns = [fns]
        self._deps(e, e, reads, writes)
        self.ecnt[e] += 1
        tok = (self.esem[e], self.ecnt[e])
        self.streams[e].append(("i", fns, self.esem[e], 1))
        for b in reads:
            b.r.append((e, tok))
        for b in writes:
            b.w = (e, tok)
            b.r = []
        return tok

    def dma(self, e, fn, sbuf, reads=(), writes=()):
        if sbuf.dsem is None:
            self.nd += 1
            sbuf.dsem = self.es.enter_context(self.nc.semaphore("ds%d" % self.nd))
        wr = list(writes)
        if sbuf not in wr:
            wr.append(sbuf)
        rd = [b for b in reads if b is not sbuf]
        self._deps(e, "dma", rd, wr)
        sbuf.dcnt += 16
        tok = (sbuf.dsem, sbuf.dcnt)
        self.streams[e].append(("i", [fn], sbuf.dsem, 16))
        for b in rd:
            b.r.append(("dma", tok))
        for b in writes:
            b.w = ("dma", tok)
            b.r = []
        if sbuf not in writes:
            sbuf.r.append(("dma", tok))
        self.dma_toks.append(tok)
        return tok

    def dma_nowait(self, e, fn, buf):
        if buf.dsem is None:
            self.nd += 1
            buf.dsem = self.es.enter_context(self.nc.semaphore("ds%d" % self.nd))
        buf.dcnt += 16
        tok = (buf.dsem, buf.dcnt)
        self.streams[e].append(("i", [fn], buf.dsem, 16))
        self.dma_toks.append(tok)
        return tok

    def barrier(self):
        toks = [(self.esem[e], self.ecnt[e]) for e in ENGS if self.ecnt[e] > 0] + self.dma_toks
        mx = {}
        for (sem, val) in toks:
            if mx.get(sem, 0) < val:
                mx[sem] = val
        for e in ENGS:
            for sem, val in mx.items():
                self._wait(e, (sem, val))
        self.dma_toks = []

    def emit(self, block):
        def mk(en):
            items = self.streams[en]

            def f(eng):
                for it in items:
                    if it[0] == "w":
                        eng.wait_ge(it[1], it[2])
                    else:
                        ins = None
                        for fn in it[1]:
                            ins = fn(eng)
                        ins.then_inc(it[2], it[3])
            return f
        block.sync(mk("sp"))
        block.gpsimd(mk("pool"))
        block.scalar(mk("act"))
        block.vector(mk("dve"))
        block.tensor(mk("pe"))
        self.streams = {e: [] for e in ENGS}


def build_program(nblk=32, debug_h1=False):
    nc = bass.Bass("TRN2", target_bir_lowering=False)
    ntok = nblk * 128

    def din(name, shape):
        return nc.dram_tensor(name, list(shape), F32, kind="ExternalInput").ap()

    x_d = din("x", [ntok, D])
    p_d = din("p", [ntok, 256])
    ln0_g = din("ln0_g", [D]); ln0_b = din("ln0_b", [D])
    w_in = din("w_in", [D, 2048]); b_in = din("b_in", [2048])
    conv_w = din("conv_w", [31, 512]); conv_b = din("conv_b", [512])
    gn_g = din("gn_g", [512]); gn_b = din("gn_b", [512])
    sg_ln_g = din("sg_ln_g", [512]); sg_ln_b = din("sg_ln_b", [512])
    sg_w = din("sg_w", [8, 128, 128]); sg_b = din("sg_b", [8, 128])
    w_o = din("w_o", [D, D]); b_o = din("b_o", [D])
    ln1_g = din("ln1_g", [D]); ln1_b = din("ln1_b", [D])
    peer_wq = din("peer_wq", [D, 2048])
    peer_keys = din("peer_keys", [16, 128, 128])
    peer_u = din("peer_u", [16384, D]); peer_v = din("peer_v", [16384, D])
    ple_wp = din("ple_wp", [256, D]); ple_wg = din("ple_wg", [D, D]); ple_bg = din("ple_bg", [D])
    ln2_g = din("ln2_g", [D]); ln2_b = din("ln2_b", [D])
    out_d = nc.dram_tensor("out", [ntok, D], F32, kind="ExternalOutput").ap()
    h1_d = nc.dram_tensor("h1s", [ntok, D], F32, kind="Internal").ap()
    uv_d = nc.dram_tensor("uvbf", [16384, 2 * D], BF16, kind="Internal").ap()

    with ExitStack() as outer:
        P = Prog(nc, outer)
        h1d_bufs = [Buf("h1d%d" % b) for b in range(nblk)]

        with ExitStack() as s1:
            def sbt(name, shape, dt):
                return s1.enter_context(nc.sbuf_tensor(name, list(shape), dt))

            def pst(name, shape, dt):
                return s1.enter_context(nc.psum_tensor(name, list(shape), dt))

            w_in_bf = sbt("w_in_bf", [128, 8, 2048], BF16); B_w_in = [Buf("w_in_%d" % q_) for q_ in range(4)]
            w_o_bf = sbt("w_o_bf", [128, 8, 1024], BF16); B_w_o = [Buf("w_o_%d" % q_) for q_ in range(4)]
            cdiag = sbt("cdiag", [128, 124, 128], BF16); B_cdiag = Buf("cdiag")
            g0 = sbt("g0", [128, D], F32); b0 = sbt("b0", [128, D], F32)
            g1 = sbt("g1", [128, D], F32); b1 = sbt("b1", [128, D], F32)
            sglg = sbt("sglg", [128, 512], F32); sglb = sbt("sglb", [128, 512], F32)
            B_bc = Buf("bc")
            identf = sbt("identf", [128, 128], F32); ident = sbt("ident", [128, 128], BF16)
            B_ident = Buf("ident")
            Gm = sbt("Gm", [128, 128], F32); B_G = Buf("G")
            rows = sbt("rows", [28, 128], F32); B_rows = Buf("rows")
            cwrow = sbt("cwrow", [31, 512], F32); B_cwrow = Buf("cwrow")
            cols = sbt("cols", [128, 28], F32); B_cols = Buf("cols")
            cw = sbt("cw", [128, 4, 31], F32); B_cw = Buf("cw")
            brow = sbt("brow", [1, 2048], BF16); B_brow = Buf("brow")
            ones_row = sbt("ones_row", [1, 128], BF16); B_ones = Buf("ones")
            eps_t = sbt("eps_t", [128, 1], F32); B_eps = Buf("eps")
            sgw_f = sbt("sgw_f", [128, 8, 128], F32); B_sgwf = Buf("sgwf")
            sgw_m = sbt("sgw_m", [128, 8, 128], BF16); B_sgwm = Buf("sgwm")
            wmT = sbt("wmT", [128, 8, 128], BF16); B_wmT = Buf("wmT")

            ptb = pst("ptb", [128, 8, 128], BF16); B_ptb = Buf("ptb")
            pbs = [pst("pb%d" % i, [128, 4, 128], F32) for i in range(7)]
            B_pb = [Buf("pb%d" % i) for i in range(7)]

            def flat(t):
                return t[:].rearrange("p c t -> p (c t)")

            P.op("pool", lambda g: g.memset(identf[:], 0.0), writes=[B_ident])
            P.op("pool", lambda g: g.affine_select(out=identf[:], in_=identf[:], pattern=[[-1, 128]],
                                                   compare_op=ALU.not_equal, fill=1.0, base=0, channel_multiplier=1),
                 reads=[B_ident], writes=[B_ident])
            P.dma("sp", lambda q: q.dma_start(out=rows[0:8, :], in_=b_in[0:1024].rearrange("(c p) -> c p", p=128)), B_rows, writes=[B_rows])
            P.dma("sp", lambda q: q.dma_start(out=rows[8:12, :], in_=conv_b.rearrange("(c p) -> c p", p=128)), B_rows, writes=[B_rows])
            P.dma("sp", lambda q: q.dma_start(out=rows[12:16, :], in_=gn_g.rearrange("(c p) -> c p", p=128)), B_rows, writes=[B_rows])
            P.dma("sp", lambda q: q.dma_start(out=rows[16:20, :], in_=gn_b.rearrange("(c p) -> c p", p=128)), B_rows, writes=[B_rows])
            P.dma("sp", lambda q: q.dma_start(out=rows[20:28, :], in_=sg_b), B_rows, writes=[B_rows])
            P.dma("sp", lambda q: q.dma_start(out=cwrow[:], in_=conv_w), B_cwrow, writes=[B_cwrow])
            P.dma("pool", lambda g: g.dma_start(out=brow[:, 0:1024], in_=b_in[1024:2048].unsqueeze(0)), B_brow, writes=[B_brow])
            P.dma("pool", lambda g: g.dma_start(out=brow[:, 1024:2048], in_=b_o.unsqueeze(0)), B_brow, writes=[B_brow])
            for k2 in range(2):
                for kk in range(8):
                    P.dma("pool", (lambda g, kk=kk, k2=k2: g.dma_start(
                        out=w_in_bf[:, kk, k2 * 1024:(k2 + 1) * 1024],
                        in_=w_in[kk * 128:(kk + 1) * 128, k2 * 1024:(k2 + 1) * 1024])), B_w_in[kk % 4], writes=[B_w_in[kk % 4]])
            for kk in range(8):
                P.dma("pool", (lambda g, kk=kk: g.dma_start(
                    out=w_o_bf[:, kk, :], in_=w_o[kk * 128:(kk + 1) * 128, :])), B_w_o[kk % 4], writes=[B_w_o[kk % 4]])
            for (t_, v_) in ((g0, ln0_g), (b0, ln0_b), (g1, ln1_g), (b1, ln1_b), (sglg, sg_ln_g), (sglb, sg_ln_b)):
                P.dma("sp", (lambda q, t_=t_, v_=v_: q.dma_start(out=t_[:], in_=v_.partition_broadcast(128))), B_bc, writes=[B_bc])
            P.dma("sp", lambda q: q.dma_start(out=sgw_f[:], in_=sg_w.rearrange("h t s -> t h s")), B_sgwf, writes=[B_sgwf])

            P.op("dve", lambda v: v.tensor_copy(out=ident[:], in_=identf[:]), reads=[B_ident], writes=[B_ident])
            P.op("dve", lambda v: v.memset(Gm[:], 0.0), writes=[B_G])
            P.op("dve", lambda v: v.memset(Gm[0:64, 0:64], 1.0 / 64.0), writes=[B_G])
            P.op("dve", lambda v: v.memset(Gm[64:128, 64:128], 1.0 / 64.0), writes=[B_G])
            P.op("dve", lambda v: v.memset(ones_row[:], 1.0), writes=[B_ones])
            P.op("dve", lambda v: v.memset(eps_t[:], EPS), writes=[B_eps])
            P.op("pe", lambda t: t.transpose(out=pbs[0][:, 0, 0:28], in_=rows[:, :], identity=identf[0:28, 0:28]),
                 reads=[B_rows, B_ident], writes=[B_pb[0]])
            P.op("dve", lambda v: v.tensor_copy(out=cols[:], in_=pbs[0][:, 0, 0:28]), reads=[B_pb[0]], writes=[B_cols])
            P.op("pe", [(lambda t, c=c: t.transpose(out=pbs[1][:, c, 0:31], in_=cwrow[:, c * 128:(c + 1) * 128],
                                                      identity=identf[0:31, 0:31])) for c in range(4)],
                 reads=[B_cwrow, B_ident], writes=[B_pb[1]])
            P.op("dve", lambda v: v.tensor_copy(out=cw[:], in_=pbs[1][:, :, 0:31]), reads=[B_pb[1]], writes=[B_cw])
            for c in range(4):
                P.op("dve", (lambda v, c=c: v.tensor_tensor(
                    out=cdiag[:, c * 31:(c + 1) * 31, :],
                    in0=identf[:].unsqueeze(1).to_broadcast([128, 31, 128]),
                    in1=cw[:, c, :].unsqueeze(2).to_broadcast([128, 31, 128]), op=ALU.mult)),
                    reads=[B_ident, B_cw], writes=[B_cdiag])
            P.op("pool", lambda g: g.affine_select(out=sgw_m[:], in_=sgw_f[:], pattern=[[0, 8], [-1, 128]],
                                                   compare_op=ALU.is_ge, fill=0.0, base=0, channel_multiplier=1),
                 reads=[B_sgwf], writes=[B_sgwm])
            P.op("pe", [(lambda t, h=h: t.transpose(out=ptb[:, h, :], in_=sgw_m[:, h, :], identity=ident[:])) for h in range(8)],
                 reads=[B_sgwm, B_ident], writes=[B_ptb])
            P.op("dve", lambda v: v.tensor_copy(out=wmT[:], in_=ptb[:]), reads=[B_ptb], writes=[B_wmT])

            def dbl(name, shape, dt):
                return [sbt("%s%d" % (name, i), shape, dt) for i in range(2)], [Buf("%s%d" % (name, i)) for i in range(2)]
            xb = [sbt("xb%d" % q_, [128, D], F32) for q_ in range(3)]; B_xb = [Buf("xb%d" % q_) for q_ in range(3)]
            stB, B_stB = dbl("stB", [128, 32], F32)
            h0bf, B_h0bf = dbl("h0bf", [128, D], BF16)
            h0T, B_h0T = dbl("h0T", [128, 8, 128], BF16)
            sig, B_sig = dbl("sig", [128, 4, 128], F32)
            cT, B_cT = dbl("cT", [128, 4, 158], BF16)
            yT, B_yT = dbl("yT", [128, 4, 128], F32)
            dd, B_dd = dbl("dd", [128, 4, 128], F32)
            sq, B_sq = dbl("sq", [128, 4, 128], F32)
            coT, B_coT = dbl("coT", [128, 4, 128], BF16)
            uu, B_uu = dbl("uu", [128, 512], F32)
            gv, B_gv = dbl("gv", [128, 512], F32)
            sq2, B_sq2 = dbl("sq2", [128, 512], F32)
            vbf, B_vbf = dbl("vbf", [128, 512], BF16)
            sgo, B_sgo = dbl("sgo", [128, 512], BF16)
            sgoT, B_sgoT = dbl("sgoT", [128, 4, 128], BF16)
            r1, B_r1 = dbl("r1", [128, D], F32)
            st, B_st = dbl("st", [128, 64], F32)

            P.op("dve", lambda v: v.memset(cT[0][:, :, 0:30], 0.0), writes=[B_cT[0]])

            def mk_ops(steps):
                def op(*a_, **k_):
                    steps.append(lambda: P.op(*a_, **k_))

                def dma(*a_, **k_):
                    steps.append(lambda: P.dma(*a_, **k_))
                return op, dma

            def layer_norm(op, src, Bsrc, dst, Bdst, stt, Bstt, base):
                s_stats = stt[:, base:base + 12]
                s_mv = stt[:, base + 12:base + 14]
                s_sd = stt[:, base + 14:base + 15]
                s_rs = stt[:, base + 15:base + 16]
                s_nm = stt[:, base + 16:base + 17]
                op("dve", [lambda v: v.bn_stats(out=stt[:, base:base + 6], in_=src[:, 0:512]),
                           lambda v: v.bn_stats(out=stt[:, base + 6:base + 12], in_=src[:, 512:1024])],
                   reads=[Bsrc], writes=[Bstt])
                op("dve", lambda v: v.bn_aggr(out=s_mv, in_=s_stats), reads=[Bstt], writes=[Bstt])
                op("act", lambda a: a.activation(out=s_sd, in_=stt[:, base + 13:base + 14], func=AF.Sqrt,
                                                 bias=eps_t[:, 0:1], scale=1.0), reads=[Bstt, B_eps], writes=[Bstt])
                op("dve", lambda v: v.reciprocal(out=s_rs, in_=s_sd), reads=[Bstt], writes=[Bstt])
                op("dve", lambda v: v.tensor_scalar(out=s_nm, in0=stt[:, base + 12:base + 13], scalar1=s_rs, scalar2=-1.0,
                                                    op0=ALU.mult, op1=ALU.mult), reads=[Bstt], writes=[Bstt])
                op("act", lambda a: a.activation(out=dst[:], in_=src[:], func=AF.Identity, bias=s_nm, scale=s_rs),
                   reads=[Bsrc, Bstt], writes=[Bdst])

            def stageA(b):
                steps = []
                op, dma = mk_ops(steps)
                i = b % 2
                j = (b + 1) % 2
                r0 = b * 128
                X = xb[b % 3]; BX = B_xb[b % 3]
                layer_norm(op, X, BX, X, BX, st[i], B_st[i], 0)
                op("pool", lambda g, X=X: g.tensor_tensor(out=X[:], in0=X[:], in1=g0[:], op=ALU.mult), reads=[BX, B_bc], writes=[BX])
                op("pool", lambda g, X=X: g.tensor_tensor(out=X[:], in0=X[:], in1=b0[:], op=ALU.add), reads=[BX, B_bc], writes=[BX])
                op("act", lambda a, X=X, i=i: a.activation(out=h0bf[i][:], in_=X[:], func=AF.Copy), reads=[BX], writes=[B_h0bf[i]])
                op("pe", [(lambda t, k=k, i=i: t.transpose(out=ptb[:, k, :], in_=h0bf[i][:, k * 128:(k + 1) * 128], identity=ident[:]))
                          for k in range(8)], reads=[B_h0bf[i], B_ident], writes=[B_ptb])
                op("act", lambda a, i=i: a.activation(out=h0T[i][:], in_=ptb[:], func=AF.Copy), reads=[B_ptb], writes=[B_h0T[i]])
                for (bank, off) in ((0, 0), (1, 512)):
                    op("pe", [(lambda t, c=c, k=k, bank=bank, off=off, i=i: t.matmul(
                        pbs[bank][:, c, :], lhsT=w_in_bf[:, k, off + c * 128:off + (c + 1) * 128], rhs=h0T[i][:, k, :],
                        start=(k == 0), stop=(k == 7))) for c in range(4) for k in range(8)],
                        reads=B_w_in + [B_h0T[i]], writes=[B_pb[bank]])
                def susv(off):
                    fl = [(lambda t, off=off: t.matmul(flat(pbs[2]), lhsT=ones_row[:, :], rhs=brow[:, off - 1024:off - 512],
                                                       start=True, stop=False))]
                    fl += [(lambda t, k=k, off=off, i=i: t.matmul(flat(pbs[2]), lhsT=h0T[i][:, k, :],
                                                                   rhs=w_in_bf[:, k, off:off + 512], start=False, stop=(k == 7)))
                           for k in range(8)]
                    op("pe", fl, reads=B_w_in + [B_h0T[i], B_ones, B_brow], writes=[B_pb[2]])
                susv(1024)
                op("act", [(lambda a, c=c, i=i: a.activation(out=sig[i][:, c, :], in_=pbs[1][:, c, :], func=AF.Sigmoid,
                                                             bias=cols[:, 4 + c:5 + c], scale=1.0)) for c in range(4)],
                   reads=[B_pb[1], B_cols], writes=[B_sig[i]])
                op("act", lambda a, i=i: a.activation(out=uu[i][:], in_=flat(pbs[2]), func=AF.Gelu), reads=[B_pb[2]], writes=[B_uu[i]])
                op("dve", [(lambda v, c=c, i=i: v.scalar_tensor_tensor(out=cT[i][:, c, 30:158], in0=pbs[0][:, c, :],
                                                                        scalar=cols[:, c:c + 1], in1=sig[i][:, c, :],
                                                                        op0=ALU.add, op1=ALU.mult)) for c in range(4)],
                   reads=[B_pb[0], B_sig[i], B_cols], writes=[B_cT[i]])
                susv(1536)
                if b + 1 < nblk:
                    op("pool", lambda g, i=i, j=j: g.tensor_copy(out=cT[j][:, :, 0:30], in_=cT[i][:, :, 128:158]),
                       reads=[B_cT[i]], writes=[B_cT[j]])
                op("act", lambda a, i=i: a.activation(out=gv[i][:], in_=flat(pbs[2]), func=AF.Gelu), reads=[B_pb[2]], writes=[B_gv[i]])
                return steps

            def stageB1(b):
                steps = []
                op, dma = mk_ops(steps)
                i = b % 2
                r0 = b * 128
                op("pe", [(lambda t, c=c, k=k, i=i: t.matmul(pbs[4][:, c, :], lhsT=cdiag[:, c * 31 + k, :], rhs=cT[i][:, c, k:k + 128],
                                                             start=(k == 0), stop=(k == 30))) for c in range(4) for k in range(31)],
                   reads=[B_cdiag, B_cT[i]], writes=[B_pb[4]])
                op("act", [(lambda a, c=c, i=i: a.activation(out=yT[i][:, c, :], in_=pbs[4][:, c, :], func=AF.Identity,
                                                             bias=cols[:, 8 + c:9 + c], scale=1.0)) for c in range(4)],
                   reads=[B_pb[4], B_cols], writes=[B_yT[i]])
                gv3 = gv[i][:].rearrange("p (g d) -> p g d", g=8)
                sq3 = sq2[i][:].rearrange("p (g d) -> p g d", g=8)
                S = st[i]; BS = B_st[i]
                op("dve", lambda v, gv3=gv3, S=S: v.tensor_reduce(out=S[:, 24:32], in_=gv3, axis=AX.X, op=ALU.add), reads=[B_gv[i]], writes=[BS])
                op("pe", lambda t, i=i: t.matmul(flat(pbs[5]), lhsT=Gm[:], rhs=flat(yT[i]), start=True, stop=True),
                   reads=[B_G, B_yT[i]], writes=[B_pb[5]])
                op("dve", lambda v, S=S: v.tensor_scalar(out=S[:, 24:32], in0=S[:, 24:32], scalar1=1.0 / 64.0, scalar2=None, op0=ALU.mult),
                   reads=[BS], writes=[BS])
                op("dve", lambda v, i=i: v.tensor_tensor(out=flat(dd[i]), in0=flat(yT[i]), in1=flat(pbs[5]), op=ALU.subtract),
                   reads=[B_yT[i], B_pb[5]], writes=[B_dd[i]])
                op("act", lambda a, i=i: a.activation(out=flat(sq[i]), in_=flat(dd[i]), func=AF.Square), reads=[B_dd[i]], writes=[B_sq[i]])
                op("dve", lambda v, gv3=gv3, S=S: v.tensor_tensor(out=gv3, in0=gv3, in1=S[:, 24:32].unsqueeze(2).to_broadcast([128, 8, 64]),
                                                                   op=ALU.subtract), reads=[B_gv[i], BS], writes=[B_gv[i]])
                op("pe", lambda t, i=i: t.matmul(flat(pbs[6]), lhsT=Gm[:], rhs=flat(sq[i]), start=True, stop=True),
                   reads=[B_G, B_sq[i]], writes=[B_pb[6]])
                op("act", lambda a, i=i: a.activation(out=sq2[i][:], in_=gv[i][:], func=AF.Square), reads=[B_gv[i]], writes=[B_sq2[i]])
                op("act", lambda a, i=i: a.activation(out=flat(sq[i]), in_=flat(pbs[6]), func=AF.Sqrt, bias=eps_t[:, 0:1], scale=1.0),
                   reads=[B_pb[6], B_eps], writes=[B_sq[i]])
                op("dve", lambda v, sq3=sq3, S=S: v.tensor_reduce(out=S[:, 32:40], in_=sq3, axis=AX.X, op=ALU.add), reads=[B_sq2[i]], writes=[BS])
                op("dve", lambda v, i=i: v.reciprocal(out=flat(sq[i]), in_=flat(sq[i])), reads=[B_sq[i]], writes=[B_sq[i]])
                op("act", lambda a, S=S: a.activation(out=S[:, 40:48], in_=S[:, 32:40], func=AF.Sqrt, bias=eps_t[:, 0:1], scale=1.0 / 64.0),
                   reads=[BS, B_eps], writes=[BS])
                op("dve", lambda v, i=i: v.tensor_tensor(out=flat(dd[i]), in0=flat(dd[i]), in1=flat(sq[i]), op=ALU.mult),
                   reads=[B_dd[i], B_sq[i]], writes=[B_dd[i]])
                op("act", [(lambda a, c=c, i=i: a.activation(out=coT[i][:, c, :], in_=dd[i][:, c, :], func=AF.Silu,
                                                             bias=cols[:, 16 + c:17 + c], scale=cols[:, 12 + c:13 + c])) for c in range(4)],
                   reads=[B_dd[i], B_cols], writes=[B_coT[i]])
                op("dve", lambda v, S=S: v.reciprocal(out=S[:, 48:56], in_=S[:, 40:48]), reads=[BS], writes=[BS])
                op("dve", lambda v, gv3=gv3, S=S: v.tensor_tensor(out=gv3, in0=gv3, in1=S[:, 48:56].unsqueeze(2).to_broadcast([128, 8, 64]),
                                                                   op=ALU.mult), reads=[B_gv[i], BS], writes=[B_gv[i]])
                op("pool", lambda g, i=i: g.tensor_tensor(out=gv[i][:], in0=gv[i][:], in1=sglg[:], op=ALU.mult), reads=[B_gv[i], B_bc], writes=[B_gv[i]])
                op("pool", lambda g, i=i: g.tensor_tensor(out=vbf[i][:], in0=gv[i][:], in1=sglb[:], op=ALU.add), reads=[B_gv[i], B_bc], writes=[B_vbf[i]])
                op("pe", [(lambda t, h=h, i=i: t.matmul(flat(pbs[5])[:, h * 64:(h + 1) * 64], lhsT=wmT[:, h, :], rhs=vbf[i][:, h * 64:(h + 1) * 64],
                                                        start=True, stop=True)) for h in range(8)],
                   reads=[B_wmT, B_vbf[i]], writes=[B_pb[5]])
                f3 = flat(pbs[5]).rearrange("p (g d) -> p g d", g=8)
                op("dve", lambda v, f3=f3, i=i: v.tensor_tensor(out=sq2[i][:].rearrange("p (g d) -> p g d", g=8), in0=f3,
                                                                in1=cols[:, 20:28].unsqueeze(2).to_broadcast([128, 8, 64]), op=ALU.add),
                   reads=[B_pb[5], B_cols], writes=[B_sq2[i]])
                op("dve", lambda v, i=i: v.tensor_tensor(out=sgo[i][:], in0=sq2[i][:], in1=uu[i][:], op=ALU.mult),
                   reads=[B_sq2[i], B_uu[i]], writes=[B_sgo[i]])
                op("pe", [(lambda t, c=c, i=i: t.transpose(out=ptb[:, c, :], in_=sgo[i][:, c * 128:(c + 1) * 128], identity=ident[:]))
                          for c in range(4)], reads=[B_sgo[i], B_ident], writes=[B_ptb])
                op("act", lambda a, i=i: a.activation(out=sgoT[i][:], in_=ptb[:, 0:4, :], func=AF.Copy), reads=[B_ptb], writes=[B_sgoT[i]])
                return steps

            def stageB2(b):
                steps = []
                op, dma = mk_ops(steps)
                i = b % 2
                r0 = b * 128
                X = xb[b % 3]; BX = B_xb[b % 3]
                for n in range(2):
                    fl = [(lambda t, n=n: t.matmul(flat(pbs[3]), lhsT=ones_row[:, :], rhs=brow[:, 1024 + n * 512:1024 + (n + 1) * 512],
                                                   start=True, stop=False))]
                    fl += [(lambda t, k=k, n=n, i=i: t.matmul(flat(pbs[3]), lhsT=(coT[i][:, k, :] if k < 4 else sgoT[i][:, k - 4, :]),
                                                              rhs=w_o_bf[:, k, n * 512:(n + 1) * 512], start=False, stop=(k == 7)))
                           for k in range(8)]
                    op("pe", fl, reads=B_w_o + [B_coT[i], B_sgoT[i], B_ones, B_brow], writes=[B_pb[3]])
                    op("dve", lambda v, n=n, i=i, X=X: v.scalar_tensor_tensor(out=r1[i][:, n * 512:(n + 1) * 512], in0=X[:, n * 512:(n + 1) * 512],
                                                                               scalar=ALPHA, in1=flat(pbs[3]), op0=ALU.mult, op1=ALU.add),
                       reads=[BX, B_pb[3]], writes=[B_r1[i]])
                R = r1[i]; BR = B_r1[i]
                layer_norm(op, R, BR, R, BR, stB[i], B_stB[i], 0)
                op("pool", lambda g, R=R: g.tensor_tensor(out=R[:], in0=R[:], in1=g1[:], op=ALU.mult), reads=[BR, B_bc], writes=[BR])
                op("pool", lambda g, R=R: g.tensor_tensor(out=R[:], in0=R[:], in1=b1[:], op=ALU.add), reads=[BR, B_bc], writes=[BR])
                dst = out_d if debug_h1 else h1_d
                dma("sp", (lambda q, R=R, r0=r0, dst=dst: q.dma_start(out=dst[r0:r0 + 128, :], in_=R[:])), BR, reads=[BR], writes=[h1d_bufs[b]])
                return steps

            B_uvtab = Buf("uvtab")
            conv_jobs = [(t_, c_) for c_ in range(32) for t_ in range(2)]

            def issue_conv(k):
                for (t_, c_) in conv_jobs[k::nblk] if nblk < 32 else conv_jobs[2 * k:2 * k + 2]:
                    tab = peer_u if t_ == 0 else peer_v
                    P.dma_nowait("pool", (lambda q, tab=tab, t_=t_, c_=c_: q.dma_start(
                        out=uv_d[c_ * 512:(c_ + 1) * 512, t_ * D:(t_ + 1) * D], in_=tab[c_ * 512:(c_ + 1) * 512, :])), B_uvtab)

            def xload(b):
                P.dma("sp", (lambda q, b=b: q.dma_start(out=xb[b % 3][:], in_=x_d[b * 128:(b + 1) * 128, :])), B_xb[b % 3], writes=[B_xb[b % 3]])

            xload(0)
            for t in range(nblk + 2):
                lists = []
                pre = (lambda t=t: xload(t + 1)) if t + 1 < nblk else None
                if 0 <= t - 2 < nblk:
                    sB2 = stageB2(t - 2)
                    if pre is not None:
                        sB2.insert(4, pre)
                        pre = None
                    lists.append(sB2)
                if 0 <= t - 1 < nblk:
                    lists.append(stageB1(t - 1))
                if t < nblk:
                    if not debug_h1:
                        issue_conv(t)
                    sA = stageA(t)
                    if pre is not None:
                        sA.append(pre)
                        pre = None
                    lists.append(sA)
                mlen = max(len(l_) for l_ in lists)
                pos_ = [0] * len(lists)
                for k in range(mlen):
                    for q_, l_ in enumerate(lists):
                        tgt = ((k + 1) * len(l_)) // mlen
                        while pos_[q_] < tgt:
                            l_[pos_[q_]]()
                            pos_[q_] += 1

            P.barrier()
            with nc.Block() as blk:
                P.emit(blk)

        if debug_h1:
            return nc

        with ExitStack() as s2:
            def sbt(name, shape, dt):
                return s2.enter_context(nc.sbuf_tensor(name, list(shape), dt))

            def pst(name, shape, dt):
                return s2.enter_context(nc.psum_tensor(name, list(shape), dt))

            def flat(t):
                return t[:].rearrange("p c t -> p (c t)")

            wq_bf = sbt("wq_bf", [128, 8, 2048], BF16); B_wq = [Buf("wq_%d" % q_) for q_ in range(4)]
            wg_bf = sbt("wg_bf", [128, 8, 1024], BF16); B_wg = [Buf("wg_%d" % q_) for q_ in range(4)]
            wp_bf = sbt("wp_bf", [128, 2, 1024], BF16); B_wp = Buf("wp")
            keys_f = sbt("keys_f", [128, 16, 128], BF16); B_keysf = Buf("keysf")
            keysT = sbt("keysT", [128, 16, 128], BF16); B_keysT = Buf("keysT")
            g2 = sbt("g2", [128, D], F32); b2 = sbt("b2", [128, D], F32); B_bc = Buf("bc2")
            identf = sbt("identf2", [128, 128], F32); ident = sbt("ident2", [128, 128], BF16); B_ident = Buf("ident2")
            brow = sbt("brow2", [1, 1024], BF16); B_brow = Buf("brow2")
            ones_row = sbt("ones_row2", [1, 128], BF16); B_ones = Buf("ones2")
            eps_t = sbt("eps_t2", [128, 1], F32); B_eps = Buf("eps2")
            iota16 = sbt("iota16", [128, 16], F32); B_iota = Buf("iota")

            ptb = pst("ptb2", [128, 8, 128], BF16); B_ptb = Buf("ptb2")
            pq = [pst("pq%d" % i, [128, 4, 128], F32) for i in range(2)]; B_pq = [Buf("pq%d" % i) for i in range(2)]
            psc = [pst("psc%d" % i, [128, 4, 128], F32) for i in range(2)]; B_psc = [Buf("psc%d" % i) for i in range(2)]
            ppl = pst("ppl", [128, 4, 128], F32); B_ppl = Buf("ppl")
            py = [pst("py%d" % i, [128, 4, 128], F32) for i in range(2)]; B_py = Buf("py")

            P.op("pool", lambda g: g.memset(identf[:], 0.0), writes=[B_ident])
            P.op("pool", lambda g: g.affine_select(out=identf[:], in_=identf[:], pattern=[[-1, 128]],
                                                   compare_op=ALU.not_equal, fill=1.0, base=0, channel_multiplier=1),
                 reads=[B_ident], writes=[B_ident])
            P.op("pool", lambda g: g.iota(iota16[:], pattern=[[1, 16]], base=0, channel_multiplier=0, allow_small_or_imprecise_dtypes=True),
                 writes=[B_iota])
            P.dma("pool", lambda g: g.dma_start(out=keys_f[:], in_=peer_keys.rearrange("h k d -> k h d")), B_keysf, writes=[B_keysf])
            P.dma("pool", lambda g: g.dma_start(out=brow[:, :], in_=ple_bg.unsqueeze(0)), B_brow, writes=[B_brow])
            for k2 in range(2):
                for kk in range(8):
                    P.dma("pool", (lambda g, kk=kk, k2=k2: g.dma_start(
                        out=wq_bf[:, kk, k2 * 1024:(k2 + 1) * 1024],
                        in_=peer_wq[kk * 128:(kk + 1) * 128, k2 * 1024:(k2 + 1) * 1024])), B_wq[kk % 4], writes=[B_wq[kk % 4]])
            for kk in range(8):
                P.dma("pool", (lambda g, kk=kk: g.dma_start(out=wg_bf[:, kk, :], in_=ple_wg[kk * 128:(kk + 1) * 128, :])), B_wg[kk % 4], writes=[B_wg[kk % 4]])
            for kk in range(2):
                P.dma("pool", (lambda g, kk=kk: g.dma_start(out=wp_bf[:, kk, :], in_=ple_wp[kk * 128:(kk + 1) * 128, :])), B_wp, writes=[B_wp])
            for (t_, v_) in ((g2, ln2_g), (b2, ln2_b)):
                P.dma("sp", (lambda q, t_=t_, v_=v_: q.dma_start(out=t_[:], in_=v_.partition_broadcast(128))), B_bc, writes=[B_bc])
            P.op("dve", lambda v: v.tensor_copy(out=ident[:], in_=identf[:]), reads=[B_ident], writes=[B_ident])
            P.op("dve", lambda v: v.memset(ones_row[:], 1.0), writes=[B_ones])
            P.op("dve", lambda v: v.memset(eps_t[:], EPS), writes=[B_eps])
            for half in range(2):
                P.op("pe", [(lambda t, q=q, half=half: t.transpose(out=ptb[:, q, :], in_=keys_f[:, half * 8 + q, :], identity=ident[:]))
                            for q in range(8)], reads=[B_keysf, B_ident], writes=[B_ptb])
                P.op("dve", lambda v, half=half: v.tensor_copy(out=keysT[:, half * 8:(half + 1) * 8, :], in_=ptb[:]),
                     reads=[B_ptb], writes=[B_keysT])

            UV = [sbt("UV%d" % s, [128, 2 * D], BF16) for s in range(NRING)]; B_UV = [Buf("UV%d" % s) for s in range(NRING)]
            dring = [sbt("dg%d" % s, [128, 128], BF16) for s in range(NDIAG)]; B_dg = [Buf("dg%d" % s) for s in range(NDIAG)]

            def dbl(name, shape, dt):
                return [sbt("%s%d" % (name, i), shape, dt) for i in range(2)], [Buf("%s%d" % (name, i)) for i in range(2)]
            h1, B_h1 = dbl("h1_", [128, D], F32)
            rp, B_rp = dbl("rp", [128, D], F32)
            idx, B_idx = dbl("idx", [128, 128], U32)
            gsm, B_gsm = dbl("gsm", [128, 128], F32)
            pld, B_pld = dbl("pld", [128, 256], F32)
            h1bf = sbt("h1bf", [128, D], BF16); B_h1bf = Buf("h1bf")
            h1T = sbt("h1T", [128, 8, 128], BF16); B_h1T = Buf("h1T")
            pbf = sbt("pbf", [128, 256], BF16); B_pbf = Buf("pbf")
            pT = sbt("pT", [128, 2, 128], BF16); B_pT = Buf("pT")
            qT = sbt("qT", [128, 16, 128], BF16); B_qT = [Buf("qT%d" % i) for i in range(4)]
            scs = sbt("scs", [128, 16, 128], F32); B_scs = [Buf("scs%d" % i) for i in range(4)]
            wk = sbt("wk", [128, 8, 128], F32); B_wk = Buf("wk")
            m8 = sbt("m8", [128, 16, 16], F32); B_m8 = Buf("m8")
            i8 = sbt("i8", [128, 16, 16], U32); B_i8 = Buf("i8")
            i8f = sbt("i8f", [128, 16, 16], F32); B_i8f = Buf("i8f")
            comb = sbt("comb", [128, 4, 256], F32); B_comb = Buf("comb")
            wk2 = sbt("wk2", [128, 4, 256], F32); B_wk2 = Buf("wk2")
            c16 = sbt("c16", [128, 8, 16], F32); B_c16 = Buf("c16")
            pos = sbt("pos", [128, 8, 16], U32); B_pos = Buf("pos")
            apos = sbt("apos", [128, 8, 16], U32); bpos = sbt("bpos", [128, 8, 16], U32)
            aposf = sbt("aposf", [128, 8, 16], F32); bposf = sbt("bposf", [128, 8, 16], F32); B_ab = Buf("ab")
            oh = sbt("oh", [128, 4, 16, 16], F32); B_oh = Buf("oh")
            asel = sbt("asel", [128, 8, 16], F32); bsel = sbt("bsel", [128, 8, 16], F32); B_sel = Buf("sel")
            idxf = sbt("idxf", [128, 128], F32); B_idxf = Buf("idxf")
            sm = sbt("sm", [128, 32], F32); B_sm = Buf("sm")
            gate = sbt("gate", [128, 512], F32); B_gate = Buf("gate")
            junk = sbt("junk", [128, D], BF16); B_junk = Buf("junk")
            actt = sbt("actt", [128, 128], F32); B_actg = [Buf("actg%d" % g) for g in range(128 // GRP)]
            gel = sbt("gel", [128, 128], F32); B_gelg = [Buf("gelg%d" % g) for g in range(128 // GRP)]
            wgt = sbt("wgt", [128, 128], F32); B_wgtg = [Buf("wgtg%d" % g) for g in range(128 // GRP)]
            st2 = sbt("st2", [128, 32], F32); B_st2 = Buf("st2")

            NG = 128 // GRP
            ring_ctr = [0]

            def front(b):
                steps = []

                def op(*a_, **k_):
                    steps.append(lambda: P.op(*a_, **k_))

                def dma(*a_, **k_):
                    steps.append(lambda: P.dma(*a_, **k_))
                i = b % 2
                r0 = b * 128
                H = h1[i]; BH = B_h1[i]
                dma("sp", (lambda q, H=H, r0=r0: q.dma_start(out=H[:], in_=h1_d[r0:r0 + 128, :])), BH, reads=[h1d_bufs[b]], writes=[BH])
                dma("sp", (lambda q, i=i, r0=r0: q.dma_start(out=pld[i][:], in_=p_d[r0:r0 + 128, :])), B_pld[i], writes=[B_pld[i]])
                op("act", lambda a, H=H: a.activation(out=h1bf[:], in_=H[:], func=AF.Copy), reads=[BH], writes=[B_h1bf])
                op("act", lambda a, i=i: a.activation(out=pbf[:], in_=pld[i][:], func=AF.Copy), reads=[B_pld[i]], writes=[B_pbf])
                op("pe", [(lambda t, k=k: t.transpose(out=ptb[:, k, :], in_=h1bf[:, k * 128:(k + 1) * 128], identity=ident[:]))
                            for k in range(8)], reads=[B_h1bf, B_ident], writes=[B_ptb])
                op("act", lambda a: a.activation(out=h1T[:], in_=ptb[:], func=AF.Copy), reads=[B_ptb], writes=[B_h1T])
                op("pe", [(lambda t, k=k: t.transpose(out=ptb[:, k, :], in_=pbf[:, k * 128:(k + 1) * 128], identity=ident[:]))
                            for k in range(2)], reads=[B_pbf, B_ident], writes=[B_ptb])
                op("act", lambda a: a.activation(out=pT[:], in_=ptb[:, 0:2, :], func=AF.Copy), reads=[B_ptb], writes=[B_pT])
                for qd in range(4):
                    pb_ = pq[qd % 2]; Bp = B_pq[qd % 2]
                    op("pe", [(lambda t, c=c, k=k, qd=qd, pb_=pb_: t.matmul(
                        pb_[:, c, :], lhsT=wq_bf[:, k, (qd * 4 + c) * 128:(qd * 4 + c + 1) * 128], rhs=h1T[:, k, :],
                        start=(k == 0), stop=(k == 7))) for c in range(4) for k in range(8)],
                        reads=B_wq + [B_h1T], writes=[Bp])
                    op("act", lambda a, qd=qd, pb_=pb_: a.activation(out=qT[:, qd * 4:(qd + 1) * 4, :], in_=pb_[:], func=AF.Copy),
                         reads=[Bp], writes=[B_qT[qd]])
                    ps_ = psc[qd % 2]; Bs = B_psc[qd % 2]
                    op("pe", [(lambda t, c=c, qd=qd, ps_=ps_: t.matmul(ps_[:, c, :], lhsT=qT[:, qd * 4 + c, :], rhs=keysT[:, qd * 4 + c, :],
                                                                          start=True, stop=True)) for c in range(4)],
                         reads=[B_qT[qd], B_keysT], writes=[Bs])
                    op("act", lambda a, qd=qd, ps_=ps_: a.activation(out=scs[:, qd * 4:(qd + 1) * 4, :], in_=ps_[:], func=AF.Copy),
                         reads=[Bs], writes=[B_scs[qd]])
                for n in range(2):
                    fl = [(lambda t, n=n: t.matmul(flat(ppl), lhsT=ones_row[:, :], rhs=brow[:, n * 512:(n + 1) * 512], start=True, stop=False))]
                    fl += [(lambda t, k=k, n=n: t.matmul(flat(ppl), lhsT=h1T[:, k, :], rhs=wg_bf[:, k, n * 512:(n + 1) * 512],
                                                          start=False, stop=(k == 7))) for k in range(8)]
                    op("pe", fl, reads=B_wg + [B_h1T, B_ones, B_brow], writes=[B_ppl])
                    op("act", lambda a: a.activation(out=gate[:], in_=flat(ppl), func=AF.Sigmoid), reads=[B_ppl], writes=[B_gate])
                    op("pe", [(lambda t, k=k, n=n: t.matmul(flat(ppl), lhsT=pT[:, k, :], rhs=wp_bf[:, k, n * 512:(n + 1) * 512],
                                                               start=(k == 0), stop=(k == 1))) for k in range(2)],
                         reads=[B_wp, B_pT], writes=[B_ppl])
                    op("dve", lambda v: v.tensor_tensor(out=gate[:], in0=gate[:], in1=flat(ppl), op=ALU.mult),
                         reads=[B_gate, B_ppl], writes=[B_gate])
                    op("dve", lambda v, n=n, i=i, H=H: v.scalar_tensor_tensor(out=rp[i][:, n * 512:(n + 1) * 512], in0=H[:, n * 512:(n + 1) * 512],
                                                                               scalar=ALPHA, in1=gate[:], op0=ALU.mult, op1=ALU.add),
                         reads=[BH, B_gate], writes=[B_rp[i]])
                op("dve", [(lambda v, hh=hh: v.max(out=m8[:, hh, 0:8], in_=scs[:, hh, :])) for hh in range(16)], reads=B_scs, writes=[B_m8])
                for hv in range(2):
                    op("dve", [(lambda v, hh=hh, hv=hv: v.match_replace(out=wk[:, hh - 8 * hv, :], in_to_replace=m8[:, hh, 0:8],
                                                                          in_values=scs[:, hh, :], imm_value=NEG))
                                 for hh in range(8 * hv, 8 * hv + 8)], reads=B_scs + [B_m8], writes=[B_wk])
                    op("dve", [(lambda v, hh=hh, hv=hv: v.max(out=m8[:, hh, 8:16], in_=wk[:, hh - 8 * hv, :]))
                                 for hh in range(8 * hv, 8 * hv + 8)], reads=[B_wk], writes=[B_m8])
                op("dve", [(lambda v, hh=hh, o=o: v.max_index(out=i8[:, hh, o:o + 8], in_max=m8[:, hh, o:o + 8], in_values=scs[:, hh, :]))
                             for hh in range(16) for o in (0, 8)], reads=B_scs + [B_m8], writes=[B_i8])
                op("dve", lambda v: v.tensor_copy(out=i8f[:], in_=i8[:]), reads=[B_i8], writes=[B_i8f])
                m84 = m8[:].rearrange("p (h t) k -> p h t k", t=2)
                i84 = i8f[:].rearrange("p (h t) k -> p h t k", t=2)
                for hf in range(2):
                    hs = slice(hf * 4, hf * 4 + 4)
                    comb4 = comb[:].rearrange("p h (a c) -> p h a c", a=16)
                    op("dve", lambda v, hs=hs, comb4=comb4: v.tensor_tensor(
                        out=comb4, in0=m84[:, hs, 0, :].unsqueeze(3).to_broadcast([128, 4, 16, 16]),
                        in1=m84[:, hs, 1, :].unsqueeze(2).to_broadcast([128, 4, 16, 16]), op=ALU.add),
                        reads=[B_m8], writes=[B_comb])
                    op("dve", [(lambda v, h=h, hf=hf: v.max(out=c16[:, hf * 4 + h, 0:8], in_=comb[:, h, :])) for h in range(4)],
                         reads=[B_comb], writes=[B_c16])
                    op("dve", [(lambda v, h=h, hf=hf: v.match_replace(out=wk2[:, h, :], in_to_replace=c16[:, hf * 4 + h, 0:8],
                                                                         in_values=comb[:, h, :], imm_value=NEG)) for h in range(4)],
                         reads=[B_comb, B_c16], writes=[B_wk2])
                    op("dve", [(lambda v, h=h, hf=hf: v.max(out=c16[:, hf * 4 + h, 8:16], in_=wk2[:, h, :])) for h in range(4)],
                         reads=[B_wk2], writes=[B_c16])
                    op("dve", [(lambda v, h=h, hf=hf, o=o: v.max_index(out=pos[:, hf * 4 + h, o:o + 8], in_max=c16[:, hf * 4 + h, o:o + 8],
                                                                          in_values=comb[:, h, :])) for h in range(4) for o in (0, 8)],
                         reads=[B_comb, B_c16], writes=[B_pos])
                op("dve", [lambda v: v.tensor_single_scalar(out=apos[:], in_=pos[:], scalar=4, op=ALU.logical_shift_right),
                             lambda v: v.tensor_single_scalar(out=bpos[:], in_=pos[:], scalar=15, op=ALU.bitwise_and)],
                     reads=[B_pos], writes=[B_ab])
                op("dve", [lambda v: v.tensor_copy(out=aposf[:], in_=apos[:]),
                             lambda v: v.tensor_copy(out=bposf[:], in_=bpos[:])], reads=[B_ab], writes=[B_ab])
                io4 = iota16[:].unsqueeze(1).unsqueeze(1).to_broadcast([128, 4, 16, 16])
                for (pf, tsel, sel) in ((aposf, 0, asel), (bposf, 1, bsel)):
                    for hf in range(2):
                        hs = slice(hf * 4, hf * 4 + 4)
                        op("dve", lambda v, pf=pf, hs=hs: v.tensor_tensor(out=oh[:], in0=pf[:, hs, :].unsqueeze(3).to_broadcast([128, 4, 16, 16]),
                                                                            in1=io4, op=ALU.is_equal), reads=[B_ab, B_iota], writes=[B_oh])
                        op("dve", lambda v, tsel=tsel, hs=hs: v.tensor_tensor(out=oh[:], in0=oh[:],
                                                                                in1=i84[:, hs, tsel, :].unsqueeze(2).to_broadcast([128, 4, 16, 16]),
                                                                                op=ALU.mult), reads=[B_oh, B_i8f], writes=[B_oh])
                        op("dve", lambda v, sel=sel, hs=hs: v.tensor_reduce(out=sel[:, hs, :], in_=oh[:], axis=AX.X, op=ALU.add),
                             reads=[B_oh], writes=[B_sel])
                op("dve", lambda v: v.scalar_tensor_tensor(out=idxf[:], in0=asel[:].rearrange("p h k -> p (h k)"), scalar=128.0,
                                                             in1=bsel[:].rearrange("p h k -> p (h k)"), op0=ALU.mult, op1=ALU.add),
                     reads=[B_sel], writes=[B_idxf])
                op("dve", lambda v, i=i: v.tensor_copy(out=idx[i][:], in_=idxf[:]), reads=[B_idxf], writes=[B_idx[i]])
                op("dve", lambda v: v.tensor_scalar(out=sm[:, 0:8], in0=c16[:, :, 0], scalar1=-1.0, scalar2=None, op0=ALU.mult),
                     reads=[B_c16], writes=[B_sm])
                G3 = gsm[i][:].rearrange("p (h k) -> p h k", h=8)
                op("act", [(lambda a, h=h, G3=G3: a.activation(out=G3[:, h, :], in_=c16[:, h, :], func=AF.Exp, bias=sm[:, h:h + 1], scale=1.0,
                                                                  accum_out=sm[:, 8 + h:9 + h])) for h in range(8)],
                     reads=[B_c16, B_sm], writes=[B_gsm[i], B_sm])
                op("dve", lambda v: v.reciprocal(out=sm[:, 16:24], in_=sm[:, 8:16]), reads=[B_sm], writes=[B_sm])
                op("dve", lambda v, G3=G3: v.tensor_tensor(out=G3, in0=G3, in1=sm[:, 16:24].unsqueeze(2).to_broadcast([128, 8, 16]), op=ALU.mult),
                     reads=[B_gsm[i], B_sm], writes=[B_gsm[i]])

                def gen():
                    for st_ in steps:
                        st_()
                        yield
                return gen()

            nuse = nblk * 128

            def issue_gather(n):
                if n >= nuse:
                    return
                b_, jj = divmod(n, 128)
                i_ = b_ % 2
                s_ = n % NRING
                P.dma("pool", (lambda q, s_=s_, jj=jj, i_=i_: q.indirect_dma_start(
                    out=UV[s_][:], out_offset=None, in_=uv_d,
                    in_offset=bass.IndirectOffsetOnAxis(ap=idx[i_][:, jj:jj + 1], axis=0))),
                    B_UV[s_], reads=[B_idx[i_]], writes=[B_UV[s_]])

            def wmul(b, g):
                i = b % 2
                gs = slice(g * GRP, (g + 1) * GRP)
                P.op("dve", lambda v, gs=gs, i=i: v.tensor_tensor(out=wgt[:, gs], in0=gel[:, gs], in1=gsm[i][:, gs], op=ALU.mult),
                     reads=[B_gelg[g], B_gsm[i]], writes=[B_wgtg[g]])

            def vside(b, g):
                for e in range(GRP):
                    jj = g * GRP + e
                    s = (b * 128 + jj) % NRING
                    ds = (b * 128 + jj) % NDIAG
                    P.op("act", lambda a, ds=ds, jj=jj: a.activation(out=dring[ds][:], in_=identf[:], func=AF.Copy, scale=wgt[:, jj:jj + 1]),
                         reads=[B_wgtg[g], B_ident], writes=[B_dg[ds]])
                    P.op("pe", [(lambda t, n=n, ds=ds, s=s, jj=jj: t.matmul(flat(py[n]), lhsT=dring[ds][:], rhs=UV[s][:, D + n * 512:D + (n + 1) * 512],
                                                                             start=(jj == 0), stop=(jj == 127))) for n in range(2)],
                         reads=[B_dg[ds], B_UV[s]], writes=[B_py])
                    issue_gather(b * 128 + jj + NRING)

            def tail(b):
                i = b % 2
                r0 = b * 128
                r2 = rp[i]; B_r2 = B_rp[i]
                P.op("dve", [(lambda v, n=n, r2=r2: v.tensor_tensor(out=r2[:, n * 512:(n + 1) * 512], in0=r2[:, n * 512:(n + 1) * 512],
                                                                    in1=flat(py[n]), op=ALU.add)) for n in range(2)],
                     reads=[B_r2, B_py], writes=[B_r2])
                S = st2
                P.op("dve", [lambda v, r2=r2: v.bn_stats(out=S[:, 0:6], in_=r2[:, 0:512]),
                             lambda v, r2=r2: v.bn_stats(out=S[:, 6:12], in_=r2[:, 512:1024])], reads=[B_r2], writes=[B_st2])
                P.op("dve", lambda v: v.bn_aggr(out=S[:, 12:14], in_=S[:, 0:12]), reads=[B_st2], writes=[B_st2])
                P.op("act", lambda a: a.activation(out=S[:, 14:15], in_=S[:, 13:14], func=AF.Sqrt, bias=eps_t[:, 0:1], scale=1.0),
                     reads=[B_st2, B_eps], writes=[B_st2])
                P.op("dve", lambda v: v.reciprocal(out=S[:, 15:16], in_=S[:, 14:15]), reads=[B_st2], writes=[B_st2])
                P.op("dve", lambda v: v.tensor_scalar(out=S[:, 16:17], in0=S[:, 12:13], scalar1=S[:, 15:16], scalar2=-1.0, op0=ALU.mult, op1=ALU.mult),
                     reads=[B_st2], writes=[B_st2])
                P.op("act", lambda a, r2=r2: a.activation(out=r2[:], in_=r2[:], func=AF.Identity, bias=S[:, 16:17], scale=S[:, 15:16]),
                     reads=[B_r2, B_st2], writes=[B_r2])
                P.op("dve", lambda v, r2=r2: v.tensor_tensor(out=r2[:], in0=r2[:], in1=g2[:], op=ALU.mult), reads=[B_r2, B_bc], writes=[B_r2])
                P.op("dve", lambda v, r2=r2: v.tensor_tensor(out=r2[:], in0=r2[:], in1=b2[:], op=ALU.add), reads=[B_r2, B_bc], writes=[B_r2])
                P.dma("sp", (lambda q, r0=r0, r2=r2: q.dma_start(out=out_d[r0:r0 + 128, :], in_=r2[:])), B_r2, reads=[B_r2])

            def back(b, fg):
                i = b % 2
                H = h1[i]; BH = B_h1[i]
                for g in range(NG):
                    gs = slice(g * GRP, (g + 1) * GRP)
                    if g == NG - 1 and fg is not None:
                        for _ in fg:
                            pass
                    for e in range(GRP):
                        jj = g * GRP + e
                        s = (b * 128 + jj) % NRING
                        P.op("dve", lambda v, s=s, jj=jj, H=H: v.scalar_tensor_tensor(out=junk[:], in0=UV[s][:, 0:D], scalar=1.0, in1=H[:],
                                                                                     op0=ALU.mult, op1=ALU.mult, accum_out=actt[:, jj:jj + 1]),
                             reads=[B_UV[s], BH], writes=[B_junk, B_actg[g]])
                        if e == 0 and g >= 1:
                            wmul(b, g - 1)
                            vside(b, g - 1)
                        if e == GRP - 1 and g == 0 and b >= 1:
                            tail(b - 1)
                        if fg is not None and g < NG - 1:
                            next(fg, None)
                    P.op("act", lambda a, gs=gs: a.activation(out=gel[:, gs], in_=actt[:, gs], func=AF.Gelu), reads=[B_actg[g]], writes=[B_gelg[g]])
                wmul(b, NG - 1)
                vside(b, NG - 1)
                if b == nblk - 1:
                    tail(b)

            for _ in front(0):
                pass
            for n_ in range(NRING):
                issue_gather(n_)
            for b in range(nblk):
                back(b, front(b + 1) if b + 1 < nblk else None)

            P.barrier()
            with nc.Block() as blk:
                P.emit(blk)
    return nc


_W_NAMES = ["ln0_g", "ln0_b", "w_in", "b_in", "conv_w", "conv_b", "gn_g", "gn_b", "sg_ln_g", "sg_ln_b", "sg_w", "sg_b",
            "w_o", "b_o", "ln1_g", "ln1_b", "peer_wq", "peer_keys", "peer_u", "peer_v", "ple_wp", "ple_wg", "ple_bg",
            "ln2_g", "ln2_b"]


def _prep_weights(inp):
    w = {}
    for k in _W_NAMES:
        a = np.asarray(inp[k], dtype=np.float32)
        if k in ("ln0_g", "ln0_b"):
            w[k] = np.ascontiguousarray(a.reshape(1024))
        elif k == "peer_keys":
            w[k] = np.ascontiguousarray(a.reshape(16, 128, 128))
        else:
            w[k] = np.ascontiguousarray(a[0])
    return w


def kernel(**inputs):
    n = 8
    x = np.asarray(inputs["x"], dtype=np.float32)
    p = np.asarray(inputs["p"], dtype=np.float32)
    w = _prep_weights(inputs)
    nc = build_program(32)
    in_maps = []
    for c in range(n):
        m = {"x": np.ascontiguousarray(x[c]), "p": np.ascontiguousarray(p[0, c])}
        m.update(w)
        in_maps.append(m)
    res = run_bass_kernel_spmd(nc, in_maps, core_ids=list(range(n)))
    return np.stack([np.asarray(r["out"], dtype=np.float32) for r in res.results], axis=0)
```
